# Optimizing a Trainium2 kernel written in Bass

```python
import math
import jax, jax.numpy as jnp
from jax import lax
import numpy as np

D_MODEL = 1024
BATCH = 8
SEQ = 4096
DEPTH = 2

HEAD_DIM = 64
D_ATTN = D_MODEL // 2
N_HEADS = D_ATTN // HEAD_DIM
DILATED_GROUPS = ((128, 1), (512, 4), (2048, 16))
N_GROUPS = len(DILATED_GROUPS)
N_QKV = 3 * N_GROUPS * N_HEADS * HEAD_DIM
D_HYENA = D_MODEL // 2
HYENA_ORDER = 2
FILTER_EMB = 33
FILTER_WIDTH = 64
DECAY_TARGET = 1e-2
FAST_DECAY_PCT = 0.3
SLOW_DECAY_PCT = 1.5
N_IN = N_QKV + D_ATTN + 3 * D_HYENA + D_HYENA + 2 * D_MODEL
DEEPNORM_ALPHA = (2 * DEPTH) ** 0.25
DEEPNORM_BETA = (8 * DEPTH) ** -0.25
LN_EPS = 1e-5

kernel_name = "dilated_attn_hyena_hybrid_encoder"


def _layer_norm(x):
    xf = x.astype(jnp.float32)
    mu = jnp.mean(xf, axis=-1, keepdims=True)
    var = jnp.mean(jnp.square(xf - mu), axis=-1, keepdims=True)
    return (xf - mu) * lax.rsqrt(var + LN_EPS)


def _alibi_slopes(n_heads):
    return jnp.asarray(2.0 ** (-8.0 * (np.arange(n_heads) + 1) / n_heads), dtype=jnp.float32)


def dilated_window_attention(q, k, v, slopes, dilation, radius):
    B, S, H, E = q.shape
    L = S // dilation
    nb = -(-L // radius)
    Lp = nb * radius

    def by_residue(a):
        return a.reshape(B, L, dilation, H, E).transpose(0, 2, 1, 3, 4)

    qs, ks, vs = by_residue(q), by_residue(k), by_residue(v)
    qb = jnp.pad(qs, ((0, 0), (0, 0), (0, Lp - L), (0, 0), (0, 0))).reshape(B, dilation, nb, radius, H, E)

    def windows(a):
        ap = jnp.pad(a, ((0, 0), (0, 0), (radius, Lp - L + radius), (0, 0), (0, 0)))
        ap = ap.reshape(B, dilation, nb + 2, radius, H, E)
        return jnp.concatenate([ap[:, :, :-2], ap[:, :, 1:-1], ap[:, :, 2:]], axis=3)

    kw, vw = windows(ks), windows(vs)
    qi = jnp.arange(nb)[:, None] * radius + jnp.arange(radius)[None, :]
    ki = jnp.arange(nb)[:, None] * radius - radius + jnp.arange(3 * radius)[None, :]
    rel = ki[:, None, :] - qi[:, :, None]
    valid = (jnp.abs(rel) <= radius) & (ki[:, None, :] >= 0) & (ki[:, None, :] < L)
    dist = (jnp.abs(rel) * dilation).astype(jnp.float32)

    s = jnp.einsum('bdnqhe,bdnkhe->bdnhqk', qb, kw).astype(jnp.float32) * (E ** -0.5)
    s = s - slopes[:, None, None] * dist[:, None]
    s = jnp.where(valid[:, None], s, -jnp.inf)
    lse = jax.nn.logsumexp(s, axis=-1)
    p = jnp.exp(s - lse[..., None]).astype(v.dtype)
    o = jnp.einsum('bdnhqk,bdnkhe->bdnqhe', p, vw)

    o = o.reshape(B, dilation, Lp, H, E)[:, :, :L].transpose(0, 2, 1, 3, 4).reshape(B, S, H, E)
    lse = lse.transpose(0, 1, 2, 4, 3).reshape(B, dilation, Lp, H)[:, :, :L]
    lse = lse.transpose(0, 2, 1, 3).reshape(B, S, H)
    return o, lse


def _hyena_pos_features(L):
    t = jnp.linspace(0.0, 1.0, L, dtype=jnp.float32)[:, None]
    bands = (FILTER_EMB - 1) // 2
    w = 2.0 * math.pi * jnp.arange(L, dtype=jnp.float32)[:, None] / L
    f = jnp.linspace(1e-4, bands - 1, bands, dtype=jnp.float32)[None, :]
    return jnp.concatenate([t, jnp.cos(f * w), -jnp.sin(f * w)], axis=-1)


def implicit_filters(L, w1, b1, w2, b2, w3, b3, w4, freq):
    f32 = jnp.float32
    freq = freq.astype(f32)
    h = jnp.sin(freq * (_hyena_pos_features(L) @ w1.astype(f32) + b1.astype(f32)))
    h = jnp.sin(freq * (h @ w2.astype(f32) + b2.astype(f32)))
    h = jnp.sin(freq * (h @ w3.astype(f32) + b3.astype(f32)))
    h = h @ w4.astype(f32)
    t = jnp.linspace(0.0, 1.0, L, dtype=f32)[:, None]
    deltas = jnp.linspace(math.log(DECAY_TARGET) / SLOW_DECAY_PCT,
                          math.log(DECAY_TARGET) / FAST_DECAY_PCT, D_HYENA, dtype=f32)
    decay = jnp.exp(-t * jnp.abs(deltas)[None, :])
    return h.reshape(L, HYENA_ORDER, 2, D_HYENA) * decay[:, None, None, :]


def bidirectional_long_conv(z, h_fwd, h_bwd):
    L, C = h_fwd.shape
    circ = jnp.concatenate([h_fwd, jnp.zeros((1, C), jnp.float32), h_bwd[:0:-1]], axis=0)
    hf = jnp.fft.rfft(circ, axis=0)
    zf = jnp.fft.rfft(z.astype(jnp.float32), n=2 * L, axis=1)
    y = jnp.fft.irfft(zf * hf[None], n=2 * L, axis=1)[:, :L]
    return y.astype(z.dtype)


def centred_conv3(u, w, b):
    up = jnp.pad(u, ((0, 0), (1, 1), (0, 0)))
    return w[0] * up[:, :-2] + w[1] * up[:, 1:-1] + w[2] * up[:, 2:] + b


def hybrid_layer(x, c, w_ada, b_ada, w_in, b_in, conv_w, conv_b,
                 filt_w1, filt_b1, filt_w2, filt_b2, filt_w3, filt_b3, filt_w4, filt_freq, filt_bias,
                 w_proj_attn, w_proj_hyena, w_out, b_out, ln_g, ln_b):
    B, S, _ = x.shape
    mod = (c @ w_ada + b_ada)[:, None, :]
    shift, scale, gate = jnp.split(mod, 3, axis=-1)
    h = (_layer_norm(x) * (1.0 + scale) + shift).astype(x.dtype)

    p = h @ w_in + b_in
    i1 = N_QKV
    i2 = i1 + D_ATTN
    i3 = i2 + 3 * D_HYENA
    i4 = i3 + D_HYENA
    p_qkv, g_attn, p_hy, g_hy, r_merge = jnp.split(p, [i1, i2, i3, i4], axis=-1)
    r_attn, r_hy = jnp.split(r_merge, 2, axis=-1)

    qkv = p_qkv.reshape(B, S, 3, N_GROUPS, N_HEADS, HEAD_DIM)
    slopes = _alibi_slopes(N_HEADS)
    outs, lses = [], []
    for g, (window, dilation) in enumerate(DILATED_GROUPS):
        radius = window // (2 * dilation)
        o_g, lse_g = dilated_window_attention(qkv[:, :, 0, g], qkv[:, :, 1, g], qkv[:, :, 2, g],
                                              slopes, dilation, radius)
        outs.append(o_g)
        lses.append(lse_g)
    wts = jax.nn.softmax(jnp.stack(lses, axis=0), axis=0)
    o_attn = jnp.einsum('gbsh,gbshe->bshe', wts.astype(x.dtype), jnp.stack(outs, axis=0))
    y_attn = o_attn.reshape(B, S, D_ATTN) * jax.nn.silu(g_attn)

    u = centred_conv3(p_hy, conv_w, conv_b)
    v, x1, x2 = jnp.split(u, 3, axis=-1)
    filters = implicit_filters(S, filt_w1, filt_b1, filt_w2, filt_b2, filt_w3, filt_b3, filt_w4, filt_freq)
    z = v
    for o, gate_o in enumerate((x1, x2)):
        z = gate_o * (bidirectional_long_conv(z, filters[:, o, 0], filters[:, o, 1]) + filt_bias[o] * z)
    y_hy = z * jax.nn.silu(g_hy)

    merged = jax.nn.sigmoid(r_attn) * (y_attn @ w_proj_attn) + jax.nn.sigmoid(r_hy) * (y_hy @ w_proj_hyena)
    out = merged @ w_out + b_out

    res = DEEPNORM_ALPHA * x.astype(jnp.float32) + (gate * out).astype(jnp.float32)
    return (_layer_norm(res) * ln_g + ln_b).astype(x.dtype)


def setup_inputs(seed: int = 0) -> dict:
    key = jax.random.key(seed)
    ks = jax.random.split(key, 24)
    f32 = jnp.float32

    def nrm(k, shape, scale):
        return jax.random.normal(k, shape, f32) * scale

    return {
        "x": nrm(ks[0], (BATCH, SEQ, D_MODEL), 1.0),
        "c": nrm(ks[1], (BATCH, D_MODEL), 1.0),
        "w_ada": nrm(ks[2], (DEPTH, D_MODEL, 3 * D_MODEL), 0.5 * D_MODEL ** -0.5),
        "b_ada": nrm(ks[3], (DEPTH, 3 * D_MODEL), 0.01),
        "w_in": nrm(ks[4], (DEPTH, D_MODEL, N_IN), D_MODEL ** -0.5),
        "b_in": nrm(ks[5], (DEPTH, N_IN), 0.01),
        "conv_w": nrm(ks[6], (DEPTH, 3, 3 * D_HYENA), 3 ** -0.5),
        "conv_b": nrm(ks[7], (DEPTH, 3 * D_HYENA), 0.01),
        "filt_w1": nrm(ks[8], (DEPTH, FILTER_EMB, FILTER_WIDTH), FILTER_EMB ** -0.5),
        "filt_b1": nrm(ks[9], (DEPTH, FILTER_WIDTH), 0.1),
        "filt_w2": nrm(ks[10], (DEPTH, FILTER_WIDTH, FILTER_WIDTH), FILTER_WIDTH ** -0.5),
        "filt_b2": nrm(ks[11], (DEPTH, FILTER_WIDTH), 0.1),
        "filt_w3": nrm(ks[12], (DEPTH, FILTER_WIDTH, FILTER_WIDTH), FILTER_WIDTH ** -0.5),
        "filt_b3": nrm(ks[13], (DEPTH, FILTER_WIDTH), 0.1),
        "filt_w4": nrm(ks[14], (DEPTH, FILTER_WIDTH, HYENA_ORDER * 2 * D_HYENA), 0.1 * FILTER_WIDTH ** -0.5),
        "filt_freq": 1.0 + nrm(ks[15], (DEPTH, FILTER_WIDTH), 0.05),
        "filt_bias": nrm(ks[16], (DEPTH, HYENA_ORDER, D_HYENA), 0.1),
        "w_proj_attn": nrm(ks[17], (DEPTH, D_ATTN, D_MODEL), DEEPNORM_BETA * D_ATTN ** -0.5),
        "w_proj_hyena": nrm(ks[18], (DEPTH, D_HYENA, D_MODEL), DEEPNORM_BETA * D_HYENA ** -0.5),
        "w_out": nrm(ks[19], (DEPTH, D_MODEL, D_MODEL), DEEPNORM_BETA * D_MODEL ** -0.5),
        "b_out": nrm(ks[20], (DEPTH, D_MODEL), 0.01),
        "ln_g": 1.0 + nrm(ks[21], (DEPTH, D_MODEL), 0.05),
        "ln_b": nrm(ks[22], (DEPTH, D_MODEL), 0.01),
    }


def reference(x, c, w_ada, b_ada, w_in, b_in, conv_w, conv_b,
              filt_w1, filt_b1, filt_w2, filt_b2, filt_w3, filt_b3, filt_w4, filt_freq, filt_bias,
              w_proj_attn, w_proj_hyena, w_out, b_out, ln_g, ln_b):
    for l in range(DEPTH):
        x = hybrid_layer(x, c, w_ada[l], b_ada[l], w_in[l], b_in[l], conv_w[l], conv_b[l],
                         filt_w1[l], filt_b1[l], filt_w2[l], filt_b2[l], filt_w3[l], filt_b3[l],
                         filt_w4[l], filt_freq[l], filt_bias[l],
                         w_proj_attn[l], w_proj_hyena[l], w_out[l], b_out[l], ln_g[l], ln_b[l])
    return x
```

```python
import math
from contextlib import ExitStack
import numpy as np
import ml_dtypes
import concourse.bass as bass
import concourse.mybir as mybir
from concourse.bass_utils import run_bass_kernel_spmd

F32 = mybir.dt.float32
BF16 = mybir.dt.bfloat16
AF = mybir.ActivationFunctionType
ALU = mybir.AluOpType
NPBF = ml_dtypes.bfloat16

S = 4096
D = 1024
NIN = 9216
DEPTH = 2
ALPHA = (2 * DEPTH) ** 0.25
EPS = 1e-5
NFFT = 8192
TWO_PI = 2.0 * math.pi


class Res:
    __slots__ = ("name", "writers", "readers", "prev", "sem", "dcount")

    def __init__(self, name, sem=None):
        self.name = name
        self.writers = []
        self.readers = []
        self.prev = []
        self.sem = sem if sem is not None else {}
        self.dcount = 0


class Prog:
    ENG = ("pe", "act", "dve", "pool", "sp")

    def __init__(self, nc, stack):
        self.nc = nc
        self.stack = stack
        self.e = {"pe": nc.tensor, "act": nc.scalar, "dve": nc.vector, "pool": nc.gpsimd, "sp": nc.sync}
        self.info = []
        self.pending = {k: [] for k in self.ENG}
        self.esem = {}
        self.ecount = {k: 0 for k in self.ENG}
        self.known = {k: {} for k in self.ENG}
        self.nsem = 0
        self.dma_since_barrier = []
        self.last_sig = {k: None for k in self.ENG}

    def new_sem(self, name):
        self.nsem += 1
        return self.stack.enter_context(self.nc.semaphore(f"{name}_{self.nsem}"))

    def _deps(self, reads, writes, partial):
        deps = set()
        for r in reads:
            deps.update(r.writers)
        for w in writes:
            if partial and not w.readers:
                deps.update(w.prev)
            else:
                w.prev = w.writers + w.readers
                deps.update(w.prev)
                w.writers = []
                w.readers = []
        return deps

    def _commit(self, oid, reads, writes):
        for r in reads:
            r.readers.append(oid)
        for w in writes:
            w.writers.append(oid)

    def _waits(self, eng, deps, is_dma):
        need = {}
        for d in deps:
            inf = self.info[d]
            assert inf is not None, "dependency on a non-signalling op"
            deng, sem, val = inf
            if deng == "pe" and eng == "pe" and not is_dma:
                continue
            k = id(sem)
            if k not in need or need[k][1] < val:
                need[k] = (sem, val)
        for k, (sem, val) in need.items():
            if self.known[eng].get(k, 0) >= val:
                continue
            self.e[eng].wait_ge(sem, val)
            self.known[eng][k] = val

    def op(self, eng, fn, reads=(), writes=(), sig=True, partial=False):
        deps = self._deps(reads, writes, partial)
        self._waits(eng, deps, False)
        ins = fn(self.e[eng])
        oid = len(self.info)
        if sig:
            if self.ecount[eng] % 30000 == 0:
                self.esem[eng] = self.new_sem("e" + eng)
                self.ecount[eng] = 0
            self.ecount[eng] += 1
            ins.then_inc(self.esem[eng], 1)
            inf = (eng, self.esem[eng], self.ecount[eng])
            self.info.append(inf)
            for p in self.pending[eng]:
                self.info[p] = inf
            self.pending[eng] = []
            self.last_sig[eng] = oid
        else:
            self.info.append(None)
            self.pending[eng].append(oid)
        self._commit(oid, reads, writes)
        return oid

    def dma(self, eng, out, in_, reads, writes, partial=True, sem_of=None, **kw):
        assert len(writes) == 1
        w = sem_of if sem_of is not None else writes[0]
        deps = self._deps(reads, writes, partial)
        self._waits(eng, deps, True)
        kind = "sw" if eng == "pool" else "hw"
        if kind not in w.sem:
            w.sem[kind] = [self.new_sem("d" + kind), 0]
        w.sem[kind][1] += 1
        self.e[eng].dma_start(out=out, in_=in_, **kw).then_inc(w.sem[kind][0], 16)
        oid = len(self.info)
        self.info.append(("dma", w.sem[kind][0], 16 * w.sem[kind][1]))
        self.dma_since_barrier.append(oid)
        self._commit(oid, reads, writes)
        return oid

    def barrier(self):
        deps = set(self.dma_since_barrier)
        for k in self.ENG:
            assert not self.pending[k], "pending non-signalled ops at barrier"
            if self.last_sig[k] is not None:
                deps.add(self.last_sig[k])
        for k in self.ENG:
            need = {}
            for d in deps:
                deng, sem, val = self.info[d]
                kk = id(sem)
                if kk not in need or need[kk][1] < val:
                    need[kk] = (sem, val)
            for kk, (sem, val) in need.items():
                if self.known[k].get(kk, 0) >= val:
                    continue
                self.e[k].wait_ge(sem, val)
                self.known[k][kk] = val
        self.dma_since_barrier = []


def _const_tables():
    c = {}
    c["ident"] = np.eye(128, dtype=np.float32).astype(NPBF)
    dil = (1, 4, 16)
    p = np.arange(128)[:, None]
    j = np.arange(128)[None, :]
    tab = np.zeros((128, 12, 3, 2, 128), np.float32)
    for g in range(3):
        for h in range(8):
            slope = 2.0 ** (-(h + 1))
            for kt in range(3):
                rel = p + (kt - 1) * 128 - j
                val = -8.0 * slope * dil[g] * np.abs(rel)
                val = np.where(np.abs(rel) <= 64, val, -32768.0)
                tab[:, g * 4 + h // 2, kt, h % 2, :] = val
    c["abias"] = tab.astype(NPBF)
    osel = np.zeros((128, 2, 128), np.float32)
    osel[:, 0, :64] = 1.0
    osel[:, 1, 64:] = 1.0
    c["onesel"] = osel.astype(NPBF)
    n2 = np.arange(128)[:, None].astype(np.float64)
    k2 = np.arange(128)[None, :].astype(np.float64)
    ang = -2.0 * np.pi * n2 * (k2 + 0.5) / 256.0
    Fr, Fi = np.cos(ang), np.sin(ang)
    c["F1"] = np.concatenate([Fr, Fi, -Fi], axis=1).astype(np.float32).astype(NPBF)
    ang2 = -2.0 * np.pi * (n2 + 128.0) * (k2 + 0.5) / 256.0
    Fr2, Fi2 = -np.cos(ang2), -np.sin(ang2)
    c["F1hi"] = np.concatenate([Fr2, Fi2, -Fi2], axis=1).astype(np.float32).astype(NPBF)
    n1 = np.arange(32).astype(np.float64)
    k1 = np.arange(32).astype(np.float64)
    eye4 = np.eye(4)
    a = -2.0 * np.pi * n1[:, None] * k1[None, :] / 32.0
    F32m = np.zeros((128, 3, 128), np.float32)
    F32m[:, 0, :] = np.kron(np.cos(a), eye4)
    F32m[:, 1, :] = np.kron(np.sin(a), eye4)
    F32m[:, 2, :] = -np.kron(np.sin(a), eye4)
    c["F32m"] = F32m.astype(NPBF)
    kk2 = np.arange(128).astype(np.float64)
    a = -2.0 * np.pi * np.repeat(n1, 4)[:, None] * (kk2[None, :] + 0.5) / NFFT
    tw = np.zeros((128, 2, 8, 128), np.float32)
    tw[:, 0, :, :] = np.cos(a)[:, None, :]
    tw[:, 1, :, :] = np.sin(a)[:, None, :]
    c["tw8"] = tw
    a = 2.0 * np.pi * k1[:, None] * n1[None, :] / 32.0
    Rr = np.kron(np.cos(a), eye4)
    Ri = np.kron(np.sin(a), eye4)
    R12 = np.zeros((128, 2, 256), np.float32)
    R12[:, 0, :128], R12[:, 0, 128:] = Rr, Ri
    R12[:, 1, :128], R12[:, 1, 128:] = -Ri, Rr
    c["R12"] = R12.astype(NPBF)
    T = np.zeros((128, 32, 2, 128), np.float32)
    kk = np.arange(128)[:, None].astype(np.float64)
    nn2 = np.arange(128)[None, :].astype(np.float64)
    for a1 in range(32):
        a = 2.0 * np.pi * (a1 + 32.0 * nn2) * (kk + 0.5) / NFFT
        T[:, a1, 0, :] = (2.0 / NFFT) * np.cos(a)
        T[:, a1, 1, :] = -(2.0 / NFFT) * np.sin(a)
    c["T"] = T.astype(NPBF)
    L = S
    t = np.linspace(0.0, 1.0, L, dtype=np.float32)[:, None]
    bands = 16
    w = (2.0 * np.pi * np.arange(L, dtype=np.float32)[:, None] / L).astype(np.float32)
    f = np.linspace(1e-4, bands - 1, bands, dtype=np.float32)[None, :]
    feat = np.concatenate([t, np.cos(f * w), -np.sin(f * w)], axis=-1).astype(np.float32)
    featT = np.ascontiguousarray(feat.T)
    rev = np.zeros_like(featT)
    rev[:, 1:] = featT[:, :0:-1]
    c["featT"] = np.stack([featT, rev], 0)
    trow = np.zeros((2, 1, L), np.float32)
    trow[0, 0] = t[:, 0]
    trow[1, 0, 1:] = t[:0:-1, 0]
    c["trow"] = trow
    deltas = np.linspace(math.log(1e-2) / 1.5, math.log(1e-2) / 0.3, 512, dtype=np.float32)
    c["ndelta"] = np.ascontiguousarray((-np.abs(deltas)).reshape(4, 128).T)
    return c


CONST_DT = {"ident": BF16, "abias": BF16, "onesel": BF16, "F1": BF16, "F1hi": BF16, "F32m": BF16, "tw8": F32,
            "R12": BF16, "T": BF16, "featT": F32, "trow": F32, "ndelta": F32}


def _layout_inputs(inp, b):
    m = {}
    m["x"] = np.ascontiguousarray(inp["x"][b])
    m["crep"] = np.ascontiguousarray(np.broadcast_to(
        inp["c"][b].reshape(8, 128).T[:, :, None], (128, 8, 128))).astype(np.float32)
    m["ccol"] = np.ascontiguousarray(inp["c"][b].reshape(8, 128).T)

    def kt(w, kc):
        Ld, K, N = w.shape
        return np.ascontiguousarray(w.reshape(Ld, kc, 128, N).transpose(0, 2, 1, 3))

    m["w_ada"] = kt(inp["w_ada"], 8)
    m["w_in"] = kt(inp["w_in"], 8)
    m["w_pa"] = kt(inp["w_proj_attn"], 4)
    m["w_ph"] = kt(inp["w_proj_hyena"], 4)
    m["w_out"] = kt(inp["w_out"], 8)

    def col(v):
        Ld, N = v.shape
        return np.ascontiguousarray(v.reshape(Ld, N // 128, 128).transpose(0, 2, 1))

    m["b_ada_c"] = col(inp["b_ada"])
    m["b_ada_r"] = np.ascontiguousarray(inp["b_ada"][:, None, :])
    m["b_in_c"] = col(inp["b_in"])
    m["b_in_r"] = np.ascontiguousarray(inp["b_in"][:, None, :])
    m["conv_w_c"] = np.ascontiguousarray(inp["conv_w"].reshape(DEPTH, 3, 12, 128).transpose(0, 3, 1, 2))
    m["conv_b_c"] = col(inp["conv_b"])
    m["f_w1"] = np.ascontiguousarray(inp["filt_w1"])
    m["f_w2"] = np.ascontiguousarray(inp["filt_w2"])
    m["f_w3"] = np.ascontiguousarray(inp["filt_w3"])
    m["f_w4"] = np.ascontiguousarray(inp["filt_w4"])
    m["f_b"] = np.ascontiguousarray(np.stack([inp["filt_b1"], inp["filt_b2"], inp["filt_b3"], inp["filt_freq"]], -1))
    m["f_bias_r"] = np.ascontiguousarray(inp["filt_bias"])
    m["b_out_r"] = np.ascontiguousarray(inp["b_out"][:, None, :])
    m["ln_g_r"] = np.ascontiguousarray(inp["ln_g"][:, None, :])
    m["ln_b_r"] = np.ascontiguousarray(inp["ln_b"][:, None, :])
    return m


IN_SHAPES = {
    "x": [S, D], "crep": [128, 8, 128], "ccol": [128, 8],
    "w_ada": [DEPTH, 128, 8, 3072], "w_in": [DEPTH, 128, 8, NIN], "w_pa": [DEPTH, 128, 4, D],
    "w_ph": [DEPTH, 128, 4, D], "w_out": [DEPTH, 128, 8, D],
    "b_ada_c": [DEPTH, 128, 24], "b_ada_r": [DEPTH, 1, 3072], "b_in_c": [DEPTH, 128, 72],
    "b_in_r": [DEPTH, 1, NIN], "conv_w_c": [DEPTH, 128, 3, 12], "conv_b_c": [DEPTH, 128, 12],
    "f_w1": [DEPTH, 33, 64], "f_w2": [DEPTH, 64, 64], "f_w3": [DEPTH, 64, 64], "f_w4": [DEPTH, 64, 2048],
    "f_b": [DEPTH, 64, 4], "f_bias_r": [DEPTH, 2, 512], "b_out_r": [DEPTH, 1, D],
    "ln_g_r": [DEPTH, 1, D], "ln_b_r": [DEPTH, 1, D],
}
CONST_SHAPES = {"ident": [128, 128], "abias": [128, 12, 3, 2, 128], "onesel": [128, 2, 128], "F1": [128, 384],
                "F1hi": [128, 384], "F32m": [128, 3, 128], "tw8": [128, 2, 8, 128], "R12": [128, 2, 256], "T": [128, 32, 2, 128],
                "featT": [2, 33, S], "trow": [2, 1, S], "ndelta": [128, 4]}


def bc(ap_t, offset, n):
    return bass.AP(ap_t.tensor, offset, [[0, 128], [1, n]])


class K:
    pass


def build(dbg=None, layers=DEPTH, stop_after=None):
    nc = bass.Bass("TRN2", target_bir_lowering=False)
    try:
        nc.allow_low_precision("bf16 matmul operands with fp32 accumulation")
    except Exception:
        pass
    stack = ExitStack()
    P = Prog(nc, stack)
    I = {k: nc.dram_tensor(k, s, F32, kind="ExternalInput").ap() for k, s in IN_SHAPES.items()}
    C = {k: nc.dram_tensor("c_" + k, s, CONST_DT[k], kind="ExternalInput").ap() for k, s in CONST_SHAPES.items()}
    out = nc.dram_tensor("out", [S, D], F32, kind="ExternalOutput").ap()
    dbg_out = {}

    def dram(name, shape, dt):
        kind = "ExternalOutput" if (dbg and name in dbg) else "Internal"
        t = nc.dram_tensor(name, shape, dt, kind=kind).ap()
        if kind == "ExternalOutput":
            dbg_out[name] = t
        return t

    proj_d = dram("proj_d", [72, 128, S], BF16)
    vtm_d = dram("vtm_d", [3, 4, 128, 32, 128], BF16)
    yattn_d = dram("yattn_d", [4, 128, S], BF16)
    yhy_d = dram("yhy_d", [4, 128, S], BF16)
    xmid_d = dram("xmid_d", [S, D], F32)
    H_d = dram("H_d", [DEPTH, 2, 4, 128, 2, 32, 128], BF16)
    fam = {}
    R_proj = [Res(f"proj{i}", fam) for i in range(72)]
    R_vtm = [Res(f"vtm{g}", fam) for g in range(3)]
    R_yattn = [Res(f"yattn{i}", fam) for i in range(4)]
    R_yhy = [Res(f"yhy{i}", fam) for i in range(4)]
    R_xmid = Res("xmid", fam)
    R_H = [[[Res(f"H{l}{o}{h}", fam) for h in range(4)] for o in range(2)] for l in range(DEPTH)]
    R_out = Res("out")
    R_in = Res("inputs")

    RES = {}
    cnt = {"n": 0}

    def sb(name, shape, dt, st=None):
        cnt["n"] += 1
        t = (st or stack).enter_context(nc.sbuf_tensor(f"s_{name}_{cnt['n']}", shape, dt))
        if name not in RES:
            RES[name] = Res(name)
        return t, RES[name]

    psf = []
    for i in range(6):
        t = stack.enter_context(nc.psum_tensor(f"psf{i}", [128, 512], F32))
        psf.append((t, Res(f"psf{i}")))
    psb = []
    for i in range(2):
        t = stack.enter_context(nc.psum_tensor(f"psb{i}", [128, 1024], BF16))
        psb.append((t, Res(f"psb{i}")))
    rr = {"f": 0, "b": 0, "q": 0}

    def next_psf():
        rr["f"] = (rr["f"] + 1) % 6
        return psf[rr["f"]]

    def next_psb():
        rr["b"] = (rr["b"] + 1) % 2
        return psb[rr["b"]]

    DQ = ("sp", "pool")

    def next_q():
        rr["q"] = (rr["q"] + 1) % len(DQ)
        return DQ[rr["q"]]

    ident, r_ident = sb("ident", [128, 128], BF16)
    onesel, r_onesel = sb("onesel", [128, 2, 128], BF16)
    F1, r_F1 = sb("F1", [128, 384], BF16)
    F1hi, r_F1hi = sb("F1hi", [128, 384], BF16)
    R12, r_R12 = sb("R12", [128, 2, 256], BF16)
    F32m, _ = sb("F32m", [128, 3, 128], BF16)
    tw8, _ = sb("tw8", [128, 2, 8, 128], F32)
    ones2, r_ones2 = sb("ones2", [2, 128], BF16)
    epsT, r_eps = sb("epsT", [128, 1], F32)
    npiT, r_npi = sb("npiT", [128, 1], F32)
    r_cst = Res("cst")
    r_ident = r_onesel = r_F1 = r_F1hi = r_R12 = r_cst
    P.dma("sp", ident[:], C["ident"][:, :], [R_in], [r_ident])
    P.dma("sp", onesel[:], C["onesel"][:, :, :], [R_in], [r_onesel])
    P.dma("sp", F1[:], C["F1"][:, :], [R_in], [r_F1])
    P.dma("sp", F1hi[:], C["F1hi"][:, :], [R_in], [r_F1hi])
    P.dma("sp", R12[:], C["R12"][:, :, :], [R_in], [r_R12])
    P.dma("sp", F32m[:], C["F32m"][:, :, :], [R_in], [r_cst])
    P.dma("sp", tw8[:], C["tw8"][:, :, :, :], [R_in], [r_cst])
    P.op("pool", lambda e: e.memset(ones2[:], 1.0), [], [r_ones2])
    P.op("pool", lambda e: e.memset(epsT[:], EPS), [], [r_eps])
    P.op("pool", lambda e: e.memset(npiT[:], -math.pi), [], [r_npi])

    modcol, r_modcol = sb("modcol", [128, DEPTH, 24], F32)
    sc1, r_sc1 = sb("sc1", [128, DEPTH, 8], F32)
    gate_b, r_gate = sb("gate_b", [128, DEPTH, D], F32)
    b_in_c, r_binc = sb("b_in_c", [128, DEPTH, 72], F32)
    convw, r_convw = sb("convw", [128, DEPTH, 3, 12], F32)
    convb, r_convb = sb("convb", [128, DEPTH, 12], F32)
    bin2, r_bin2 = sb("bin2", [1, DEPTH, 1536], BF16)
    binl, r_binl = sb("binl", [1, DEPTH, 1536], BF16)
    r_binc = r_convw = r_convb = r_cst
    for l in range(DEPTH):
        P.dma("sp", b_in_c[:, l, :], I["b_in_c"][l], [R_in], [r_binc])
        P.dma("sp", convw[:, l, :, :], I["conv_w_c"][l], [R_in], [r_convw])
        P.dma("sp", convb[:, l, :], I["conv_b_c"][l], [R_in], [r_convb])

    with ExitStack() as ph:
        ccol, r_ccol = sb("ccol", [128, 8], F32, ph)
        crep, r_crep = sb("crep", [128, 8, 128], F32, ph)
        badac, r_badac = sb("badac", [128, DEPTH, 24], F32, ph)
        badar, r_badar = sb("badar", [128, DEPTH, D], F32, ph)
        vb, r_vb = sb("vb", [1, DEPTH, 1536], F32, ph)
        vbh, r_vbh = sb("vbh", [1, DEPTH, 1536], F32, ph)
        r_ccol = r_crep = r_badac = r_badar = r_vb = r_cst
        P.dma("sp", ccol[:], I["ccol"][:, :], [R_in], [r_ccol])
        P.dma("sp", crep[:], I["crep"][:, :, :], [R_in], [r_crep])
        for l in range(DEPTH):
            P.dma("sp", badac[:, l, :], I["b_ada_c"][l], [R_in], [r_badac])
            P.dma("sp", badar[:, l, :], bc(I["b_ada_r"], l * 3072 + 2048, D), [R_in], [r_badar])
            P.dma("sp", vb[:, l, :], bass.AP(I["b_in_r"].tensor, l * NIN + 3072, [[0, 1], [1, 1536]]), [R_in], [r_vb])
        P.op("dve", lambda e: e.tensor_copy(out=bin2[:], in_=vb[:]), [r_vb], [r_bin2])
        P.op("dve", lambda e: e.tensor_copy(out=vbh[:], in_=bin2[:]), [r_bin2], [r_vbh])
        P.op("dve", lambda e: e.tensor_sub(out=vbh[:], in0=vb[:], in1=vbh[:]), [r_vb, r_vbh], [r_vbh])
        P.op("dve", lambda e: e.tensor_copy(out=binl[:], in_=vbh[:]), [r_vbh], [r_binl])
        wa = [sb(f"wa{i}", [128, 8, 512], F32, ph) for i in range(2)]
        for l in range(DEPTH):
            pc, r_pc = next_psf()
            for blk in range(6):
                wt, r_wt = wa[blk % 2]
                P.dma("sp", wt[:, 0:4, :], I["w_ada"][l, :, 0:4, blk * 512:(blk + 1) * 512], [R_in], [r_wt], partial=False)
                P.dma("pool", wt[:, 4:8, :], I["w_ada"][l, :, 4:8, blk * 512:(blk + 1) * 512], [R_in], [r_wt])
                for f in range(4):
                    fi = blk * 4 + f
                    for kc in range(8):
                        P.op("pe", lambda e, wt=wt, f=f, kc=kc, fi=fi: e.matmul(
                            pc[:, fi:fi + 1], lhsT=wt[:, kc, f * 128:(f + 1) * 128], rhs=ccol[:, kc:kc + 1],
                            start=(kc == 0), stop=(kc == 7)),
                            [r_wt, r_ccol], [r_pc], sig=(kc == 7), partial=True)
                if blk >= 4:
                    pg, r_pg = next_psf()
                    for kc in range(8):
                        P.op("pe", lambda e, wt=wt, kc=kc: e.matmul(
                            pg[:, :], lhsT=crep[:, kc, :], rhs=wt[:, kc, :], start=(kc == 0), stop=(kc == 7)),
                            [r_wt, r_crep], [r_pg], sig=(kc == 7), partial=True)
                    h0 = (blk - 4) * 512
                    P.op("dve", lambda e, l=l, h0=h0, pg=pg: e.tensor_add(
                        out=gate_b[:, l, h0:h0 + 512], in0=pg[:, :], in1=badar[:, l, h0:h0 + 512]),
                        [r_pg, r_badar], [r_gate], partial=True)
            P.op("dve", lambda e, l=l, pc=pc: e.tensor_add(out=modcol[:, l, :], in0=pc[:, 0:24], in1=badac[:, l, :]),
                 [r_pc, r_badac], [r_modcol], partial=True)
            P.op("dve", lambda e, l=l: e.tensor_scalar_add(out=sc1[:, l, :], in0=modcol[:, l, 8:16], scalar1=1.0),
                 [r_modcol], [r_sc1], partial=True)
        P.barrier()


    act_dve = {"n": 0}

    def evac(out_ap, in_ap, reads, writes, partial=True, simple=False):
        act_dve["n"] += 1
        if simple and act_dve["n"] % 2:
            P.op("act", lambda e: e.copy(out=out_ap, in_=in_ap), reads, writes, partial=partial)
        else:
            P.op("dve", lambda e: e.tensor_copy(out=out_ap, in_=in_ap), reads, writes, partial=partial)

    def fft_fwd(zl, r_zl, zh, r_zh, CEf, r_CE, X, r_X, tpb):
        Cv = CEf[:, 0:8192].rearrange("p (t c k) -> p t c k", t=2, c=32, k=128)
        for cp in range(16):
            pp, r_pp = next_psf()
            for gi in range(2):
                cg = cp * 2 + gi
                P.op("pe", lambda e: e.matmul(pp[:, gi * 256:(gi + 1) * 256], lhsT=zl[:, cg * 128:(cg + 1) * 128], rhs=F1[:, 0:256],
                                              start=True, stop=(zh is None)),
                     [r_zl, r_F1], [r_pp], sig=(zh is None and gi == 1), partial=(gi > 0))
                if zh is not None:
                    P.op("pe", lambda e: e.matmul(pp[:, gi * 256:(gi + 1) * 256], lhsT=zh[:, cg * 128:(cg + 1) * 128], rhs=F1hi[:, 0:256],
                                                  start=False, stop=True),
                         [r_zh, r_F1hi], [r_pp], sig=(gi == 1), partial=True)
            for gi in range(2):
                cg = cp * 2 + gi
                for t in range(2):
                    evac(Cv[:, t, cg, :], pp[:, gi * 256 + t * 128:gi * 256 + (t + 1) * 128], [r_pp], [r_CE],
                         partial=not (cp == 0 and gi == 0 and t == 0), simple=False)
        fmode = dbg.get("fft_mode", 9) if dbg else 9
        if fmode < 2:
            return
        (t1, r1), (t2, r2), (t3, r3), (t4, r4) = tpb
        bs = t1.shape[1]
        for blk in range(32 // bs):
            cs = slice(blk * bs, blk * bs + bs)
            cr, ci = Cv[:, 0, cs, :], Cv[:, 1, cs, :]
            P.op("dve", lambda e: e.tensor_mul(out=t1[:], in0=cr, in1=tw8[:, 0, 0:bs, :]), [r_CE, r_cst], [r1], partial=False)
            P.op("dve", lambda e: e.tensor_mul(out=t2[:], in0=ci, in1=tw8[:, 1, 0:bs, :]), [r_CE, r_cst], [r2], partial=False)
            P.op("dve", lambda e: e.tensor_mul(out=t3[:], in0=cr, in1=tw8[:, 1, 0:bs, :]), [r_CE, r_cst], [r3], partial=False)
            P.op("dve", lambda e: e.tensor_mul(out=t4[:], in0=ci, in1=tw8[:, 0, 0:bs, :]), [r_CE, r_cst], [r4], partial=False)
            P.op("dve", lambda e: e.tensor_sub(out=cr, in0=t1[:], in1=t2[:]), [r1, r2], [r_CE], partial=True)
            P.op("dve", lambda e: e.tensor_add(out=ci, in0=t3[:], in1=t4[:]), [r3, r4], [r_CE], partial=True)
            for hf_ in range(bs // 4 if fmode >= 3 else 0):
                c0 = (blk * bs + hf_ * 4) * 128
                rre = CEf[:, c0:c0 + 512]
                rim = CEf[:, 4096 + c0:4096 + c0 + 512]
                for t, (la, lb) in enumerate(((0, 2), (1, 0))):
                    px, r_px = next_psf()
                    P.op("pe", lambda e: e.matmul(px[:, :], lhsT=F32m[:, la, :], rhs=rre, start=True, stop=False),
                         [r_cst, r_CE], [r_px], sig=False, partial=False)
                    P.op("pe", lambda e: e.matmul(px[:, :], lhsT=F32m[:, lb, :], rhs=rim, start=False, stop=True),
                         [r_cst, r_CE], [r_px], sig=True, partial=True)
                    x0 = (t * 32 + blk * bs + hf_ * 4) * 128
                    evac(X[:].rearrange("p t c k -> p (t c k)")[:, x0:x0 + 512], px[:, :],
                         [r_px], [r_X], partial=not (blk == 0 and hf_ == 0 and t == 0), simple=True)

    def fft_inv(Y, r_Y, CEf, r_CE, Tt, r_T, epilogue):
        E5 = CEf[:, 0:32 * 2 * 128].rearrange("p (n t g c) -> p n t g c", n=32, t=2, g=32, c=4)
        E4 = CEf[:, 0:32 * 2 * 128].rearrange("p (n t c) -> p n t c", n=32, t=2, c=128)
        for cp in range(16):
            pe_, r_pe = next_psf()
            for gi in range(2):
                cg = cp * 2 + gi
                P.op("pe", lambda e: e.matmul(pe_[:, gi * 256:(gi + 1) * 256], lhsT=Y[:, 0, cg, :], rhs=R12[:, 0, :], start=True, stop=False),
                     [r_Y, r_R12], [r_pe], sig=False, partial=(gi > 0))
                P.op("pe", lambda e: e.matmul(pe_[:, gi * 256:(gi + 1) * 256], lhsT=Y[:, 1, cg, :], rhs=R12[:, 1, :], start=False, stop=True),
                     [r_Y, r_R12], [r_pe], sig=(gi == 1), partial=True)
            pv = pe_[:, :].rearrange("p (g t n c) -> p t n g c", g=2, t=2, n=32, c=4)
            for t in range(2):
                evac(E5[:, :, t, cp * 2:cp * 2 + 2, :], pv[:, t], [r_pe], [r_CE], partial=not (cp == 0 and t == 0))
        for nb in range(8):
            py, r_py = next_psf()
            for ni in range(4):
                n1 = nb * 4 + ni
                P.op("pe", lambda e: e.matmul(py[:, ni * 128:(ni + 1) * 128], lhsT=Tt[:, n1, 0, :], rhs=E4[:, n1, 0, :], start=True, stop=False),
                     [r_T, r_CE], [r_py], sig=False, partial=(ni > 0))
                P.op("pe", lambda e: e.matmul(py[:, ni * 128:(ni + 1) * 128], lhsT=Tt[:, n1, 1, :], rhs=E4[:, n1, 1, :], start=False, stop=True),
                     [r_T, r_CE], [r_py], sig=(ni == 3), partial=True)
            epilogue(nb, py[:, :].rearrange("p (n c) -> p n c", n=4), r_py)

    def to_token_major(srcT, r_src, dst, r_dst, fftl=True):
        sv = srcT[:, :].rearrange("p (a b) -> p b a", b=32)
        for nb in range(4):
            pt, r_pt = next_psb()
            for ni in range(8):
                n1 = nb * 8 + ni
                P.op("pe", lambda e: e.transpose(out=pt[:, ni * 128:(ni + 1) * 128], in_=sv[:, n1, :], identity=ident[:]),
                     [r_src, r_ident], [r_pt], sig=(ni == 7), partial=(ni > 0))
            if fftl:
                evac(dst[:, :].rearrange("p (g n c) -> p g n c", g=32, n=32, c=4)[:, :, nb * 8:(nb + 1) * 8, :],
                     pt[:, :].rearrange("p (n g c) -> p g n c", n=8, g=32, c=4), [r_pt], [r_dst], partial=(nb > 0))
            else:
                evac(dst[:, :].rearrange("p (n c) -> p n c", n=32)[:, nb * 8:(nb + 1) * 8, :],
                     pt[:, :].rearrange("p (n c) -> p n c", n=8), [r_pt], [r_dst], partial=(nb > 0))

    if not (dbg and dbg.get("skip_filters")):
        with ExitStack() as ph:
            h3 = [[sb(f"h3_{l}{v}", [64, S], BF16, ph) for v in range(2)] for l in range(DEPTH)]
            w4b = [sb(f"w4b{l}", [64, 2048], BF16, ph) for l in range(DEPTH)]
            with ExitStack() as ph2:
                feat = [sb(f"feat{v}", [33, S], F32, ph2) for v in range(2)]
                hA, r_hA = sb("hA", [64, S], F32, ph2)
                hB, r_hB = sb("hB", [64, S], F32, ph2)
                w4f, r_w4f = sb("w4f", [64, 2048], F32, ph2)
                fw = [sb(f"fw{i}", [64, 64], F32, ph2) for i in range(3)]
                fbt, r_fbt = sb("fbt", [64, 4], F32, ph2)
                fsc, r_fsc = sb("fsc", [64, 4], F32, ph2)
                ty = [sb(f"ty{i}", [64, 512], F32, ph2) for i in range(2)]
                tki, r_tki = sb("tki", [64, 512], mybir.dt.int32, ph2)
                tkf, r_tkf = sb("tkf", [64, 512], F32, ph2)
                for v in range(2):
                    P.dma("sp", feat[v][0][:], C["featT"][v], [R_in], [feat[v][1]], partial=False)
                for l in range(DEPTH):
                    P.dma("sp", fw[0][0][0:33, :], I["f_w1"][l], [R_in], [fw[0][1]], partial=False)
                    P.dma("sp", fw[1][0][:], I["f_w2"][l], [R_in], [fw[1][1]], partial=False)
                    P.dma("sp", fw[2][0][:], I["f_w3"][l], [R_in], [fw[2][1]], partial=False)
                    P.dma("sp", w4f[:], I["f_w4"][l], [R_in], [r_w4f], partial=False)
                    P.dma("sp", fbt[:], I["f_b"][l], [R_in], [r_fbt], partial=False)
                    P.op("pool", lambda e: e.tensor_copy(out=w4b[l][0][:], in_=w4f[:]), [r_w4f], [w4b[l][1]], partial=False)
                    P.op("dve", lambda e: e.tensor_scalar_mul(out=fsc[:, 3:4], in0=fbt[:, 3:4], scalar1=1.0 / TWO_PI), [r_fbt], [r_fsc], partial=False)
                    P.op("dve", lambda e: e.tensor_scalar(out=fsc[:, 0:3], in0=fbt[:, 0:3], scalar1=fsc[:, 3:4], scalar2=8.0,
                                                          op0=ALU.mult, op1=ALU.add), [r_fbt, r_fsc], [r_fsc], partial=False)
                    for v in range(2):
                        src, r_src = feat[v]
                        kdim = 33
                        for layer_i in range(3):
                            last = (layer_i == 2)
                            dst, r_dst = (h3[l][v] if last else ((hA, r_hA) if layer_i == 0 else (hB, r_hB)))
                            wt_, r_wt_ = fw[layer_i]
                            for tb in range(8):
                                sl = slice(tb * 512, (tb + 1) * 512)
                                pp, r_pp = next_psf()
                                P.op("pe", lambda e: e.matmul(pp[0:64, :], lhsT=wt_[0:kdim, :], rhs=src[0:kdim, sl], start=True, stop=True),
                                     [r_wt_, r_src], [r_pp], partial=False)
                                tyt, r_ty = ty[tb % 2]
                                P.op("dve", lambda e: e.tensor_scalar(out=tyt[:], in0=pp[0:64, :], scalar1=fsc[:, 3:4],
                                                                      scalar2=fsc[:, layer_i:layer_i + 1], op0=ALU.mult, op1=ALU.add),
                                     [r_pp, r_fsc], [r_ty], partial=False)
                                P.op("dve", lambda e: e.tensor_copy(out=tki[:], in_=tyt[:]), [r_ty], [r_tki], partial=False)
                                P.op("dve", lambda e: e.tensor_copy(out=tkf[:], in_=tki[:]), [r_tki], [r_tkf], partial=False)
                                P.op("dve", lambda e: e.tensor_sub(out=tyt[:], in0=tyt[:], in1=tkf[:]), [r_ty, r_tkf], [r_ty], partial=False)
                                P.op("dve", lambda e: e.tensor_single_scalar(out=tkf[:], in_=tyt[:], scalar=0.5, op=ALU.is_ge),
                                     [r_ty], [r_tkf], partial=False)
                                P.op("dve", lambda e: e.tensor_sub(out=tyt[:], in0=tyt[:], in1=tkf[:]), [r_ty, r_tkf], [r_ty], partial=False)
                                P.op("act", lambda e: e.activation(out=dst[:, sl], in_=tyt[:], func=AF.Sin, scale=TWO_PI),
                                     [r_ty], [r_dst], partial=(tb > 0))
                            src, r_src = dst, r_dst
                            kdim = 64
                P.barrier()
            trb = [sb(f"trb{v}", [128, S], F32, ph) for v in range(2)]
            dec = [sb(f"dec{v}", [128, S], F32, ph) for v in range(2)]
            ndl, r_ndl = sb("ndl", [128, 4], F32, ph)
            fT, r_fT = sb("fT", [128, S], BF16, ph)
            ftm = [sb(f"ftm{v}", [128, S], BF16, ph) for v in range(2)]
            CEf, r_CE = sb("CEf", [128, 8192], BF16, ph)
            Xb, r_X = sb("Xb", [128, 2, 32, 128], BF16, ph)
            tpf = [sb(f"tpf{i}", [128, 4, 128], F32, ph) for i in range(4)]
            P.dma("sp", ndl[:], C["ndelta"][:, :], [R_in], [r_ndl], partial=False)
            for v in range(2):
                P.dma("sp", trb[v][0][:], bc(C["trow"], v * S, S), [R_in], [trb[v][1]], partial=False)
            flist = [(c, l, o) for c in range(4) for l in range(DEPTH) for o in range(2)]
            if dbg and "filt_list" in dbg:
                flist = dbg["filt_list"]
            lastc = None
            for (c, l, o) in flist:
                if c != lastc:
                    for v in range(2):
                        P.op("act", lambda e: e.activation(out=dec[v][0][:], in_=trb[v][0][:], func=AF.Exp, scale=ndl[:, c:c + 1]),
                             [trb[v][1], r_ndl], [dec[v][1]], partial=False)
                    lastc = c
                for v in range(2):
                    col0 = (o * 2 + v) * 512 + c * 128
                    for tb in range(8):
                        sl = slice(tb * 512, (tb + 1) * 512)
                        pp, r_pp = next_psf()
                        P.op("pe", lambda e: e.matmul(pp[:, :], lhsT=w4b[l][0][:, col0:col0 + 128], rhs=h3[l][v][0][:, sl], start=True, stop=True),
                             [w4b[l][1], h3[l][v][1]], [r_pp], partial=False)
                        P.op("dve", lambda e: e.tensor_mul(out=fT[:, sl], in0=pp[:, :], in1=dec[v][0][:, sl]),
                             [r_pp, dec[v][1]], [r_fT], partial=(tb > 0))
                    if v == 1:
                        P.op("dve", lambda e: e.memset(fT[:, 0:1], 0.0), [], [r_fT], partial=True)
                    to_token_major(fT, r_fT, ftm[v][0], ftm[v][1])
                fft_fwd(ftm[0][0], ftm[0][1], ftm[1][0], ftm[1][1], CEf, r_CE, Xb, r_X, tpf)
                P.dma("sp", H_d[l, o, c], Xb[:], [r_X], [R_H[l][o][c]], partial=False, sem_of=r_X)
            P.barrier()
    if stop_after == "filt":
        layers = 0

    x_src = I["x"]
    R_xsrc = R_in
    for l in range(layers):
        x_dst, R_xdst = (xmid_d, R_xmid) if l < DEPTH - 1 else (out, R_out)
        lay = ExitStack()
        hT, r_hT = sb(f"hT", [128, 8, S], BF16, lay)
        with ExitStack() as ph:
            xt = [sb(f"xt{i}", [128, D], F32, ph) for i in range(2)]
            xn = [sb(f"xn{i}", [128, D], BF16, ph) for i in range(2)]
            st = [sb(f"st{i}", [128, 12], F32, ph) for i in range(2)]
            mv = [sb(f"mv{i}", [128, 2], F32, ph) for i in range(2)]
            for t in range(32):
                xtt, r_xt = xt[t % 2]
                xnn, r_xn = xn[t % 2]
                stt, r_st = st[t % 2]
                mvv, r_mv = mv[t % 2]
                P.dma(next_q(), xtt[:], x_src[t * 128:(t + 1) * 128, :], [R_xsrc], [r_xt], partial=False)
                P.op("dve", lambda e: e.bn_stats(out=stt[:, 0:6], in_=xtt[:, 0:512]), [r_xt], [r_st], partial=False)
                P.op("dve", lambda e: e.bn_stats(out=stt[:, 6:12], in_=xtt[:, 512:1024]), [r_xt], [r_st], partial=True)
                P.op("dve", lambda e: e.bn_aggr(out=mvv[:], in_=stt[:]), [r_st], [r_mv], partial=False)
                P.op("act", lambda e: e.activation(out=mvv[:, 1:2], in_=mvv[:, 1:2], func=AF.Sqrt, bias=epsT[:], scale=1.0),
                     [r_mv, r_eps], [r_mv], partial=False)
                P.op("dve", lambda e: e.reciprocal(out=mvv[:, 1:2], in_=mvv[:, 1:2]), [r_mv], [r_mv], partial=False)
                P.op("dve", lambda e: e.tensor_scalar(out=xnn[:], in0=xtt[:], scalar1=mvv[:, 0:1], scalar2=mvv[:, 1:2],
                                                      op0=ALU.subtract, op1=ALU.mult), [r_xt, r_mv], [r_xn], partial=False)
                pt, r_pt = next_psb()
                for kc in range(8):
                    P.op("pe", lambda e, kc=kc: e.transpose(out=pt[:, kc * 128:(kc + 1) * 128], in_=xnn[:, kc * 128:(kc + 1) * 128],
                                                            identity=ident[:]),
                         [r_xn, r_ident], [r_pt], sig=(kc == 7), partial=(kc > 0))
                for kc in range(8):
                    P.op("act", lambda e, kc=kc: e.activation(out=hT[:, kc, t * 128:(t + 1) * 128], in_=pt[:, kc * 128:(kc + 1) * 128],
                                                              func=AF.Identity, scale=sc1[:, l, kc:kc + 1], bias=modcol[:, l, kc:kc + 1]),
                         [r_pt, r_sc1, r_modcol], [r_hT], partial=True)
            P.barrier()
        if stop_after == "ln":
            lay.close()
            break

        with ExitStack() as ph:
            ws = [sb(f"ws{i}", [128, 8, 128], F32, ph) for i in range(3)]
            wb = [sb(f"wb{i}", [128, 8, 128], BF16, ph) for i in range(3)]
            ob = [sb(f"ob{i}", [128, S], BF16, ph) for i in range(2)]
            nfm = 0
            chunks = [j for j in range(72) if not (24 <= j < 36)]
            if dbg and "proj_chunks" in dbg:
                chunks = dbg["proj_chunks"]
            for j in chunks:
                wst, r_ws = ws[nfm % 3]
                wbt, r_wb = wb[nfm % 3]
                obt, r_ob = ob[nfm % 2]
                nfm += 1
                P.dma(next_q(), wst[:], I["w_in"][l, :, :, j * 128:(j + 1) * 128], [R_in], [r_ws], partial=False)
                P.op("pool", lambda e: e.tensor_copy(out=wbt[:], in_=wst[:]), [r_ws], [r_wb], partial=False)
                if 36 <= j < 40 or 52 <= j < 56:
                    fn = AF.Silu
                elif j >= 56:
                    fn = AF.Sigmoid
                else:
                    fn = AF.Identity
                for tb in range(8):
                    pp, r_pp = next_psf()
                    for kc in range(8):
                        P.op("pe", lambda e, kc=kc: e.matmul(pp[:, :], lhsT=wbt[:, kc, :], rhs=hT[:, kc, tb * 512:(tb + 1) * 512],
                                                             start=(kc == 0), stop=(kc == 7)),
                             [r_wb, r_hT], [r_pp], sig=(kc == 7), partial=(kc > 0))
                    P.op("act", lambda e: e.activation(out=obt[:, tb * 512:(tb + 1) * 512], in_=pp[:, :], func=fn,
                                                       bias=b_in_c[:, l, j:j + 1], scale=1.0),
                         [r_pp, r_binc], [r_ob], partial=(tb > 0))
                P.dma(next_q(), proj_d[j, :, :], obt[:], [r_ob], [R_proj[j]], partial=False, sem_of=r_ob)
            wvs = [sb(f"wvs{i}", [128, 8, 512], F32, ph) for i in range(1)]
            wvb = [sb(f"wvb{i}", [128, 8, 512], BF16, ph) for i in range(2)]
            vo = [sb(f"vo{i}", [128, 4, 512], BF16, ph) for i in range(2)]
            nv = 0
            groups = range(3) if not (dbg and "v_groups" in dbg) else dbg["v_groups"]
            for g in groups:
                d = (1, 4, 16)[g]
                Lg = S // d
                wst, r_ws = wvs[0]
                wbt, r_wb = wvb[g % 2]
                c0 = 3072 + g * 512
                P.dma("sp", wst[:, 0:4, :], I["w_in"][l, :, 0:4, c0:c0 + 512], [R_in], [r_ws], partial=False)
                P.dma("pool", wst[:, 4:8, :], I["w_in"][l, :, 4:8, c0:c0 + 512], [R_in], [r_ws])
                P.op("pool", lambda e: e.tensor_copy(out=wbt[:], in_=wst[:]), [r_ws], [r_wb], partial=False)
                for tq in range(8):
                    vot, r_vo = vo[nv % 2]
                    nv += 1
                    for t4 in range(4):
                        ti = tq * 4 + t4
                        r_, u = divmod(ti, Lg // 128)
                        pp, r_pp = next_psf()
                        for kc in range(8):
                            lt = hT[:, kc, :].rearrange("p (i d) -> p d i", d=d)[:, r_, u * 128:(u + 1) * 128]
                            P.op("pe", lambda e, kc=kc, lt=lt: e.matmul(pp[:, :], lhsT=lt, rhs=wbt[:, kc, :], start=(kc == 0), stop=False),
                                 [r_wb, r_hT], [r_pp], sig=False, partial=(kc > 0))
                        P.op("pe", lambda e: e.matmul(pp[:, :], lhsT=ones2[0:1, :], rhs=bin2[0:1, l, g * 512:(g + 1) * 512], start=False, stop=False),
                             [r_ones2, r_bin2], [r_pp], sig=False, partial=True)
                        P.op("pe", lambda e: e.matmul(pp[:, :], lhsT=ones2[0:1, :], rhs=binl[0:1, l, g * 512:(g + 1) * 512], start=False, stop=True),
                             [r_ones2, r_binl], [r_pp], sig=True, partial=True)
                        P.op("dve", lambda e, t4=t4: e.tensor_copy(out=vot[:, t4, :], in_=pp[:, :]), [r_pp], [r_vo], partial=(t4 > 0))
                    qn = next_q()
                    for j4 in range(4):
                        P.dma(qn, vtm_d[g, j4, :, tq * 4:(tq + 1) * 4, :], vot[:, :, j4 * 128:(j4 + 1) * 128], [r_vo], [R_vtm[g]], sem_of=r_vo)
            P.barrier()
        if stop_after == "proj":
            lay.close()
            break
        lay.close()
        with ExitStack() as ph:
            Oacc, r_O = sb(f"Oacc", [128, 2, S], F32, ph)
            qTs = [sb(f"qT{i}", [128, 2, S], BF16, ph) for i in range(2)]
            kTs = [sb(f"kT{i}", [128, S], BF16, ph) for i in range(2)]
            vts = [sb(f"vt{i}", [128, 32, 2, 128], BF16, ph) for i in range(2)]
            abs_ = [sb(f"ab{i}", [128, 3, 256], BF16, ph) for i in range(2)]
            pTs = [sb(f"pT{i}", [128, 256], BF16, ph) for i in range(8)]
            gs, r_gs = sb(f"gs", [128, S], BF16, ph)
            vs, r_vs = sb(f"vs", [128, 32, 128], BF16, ph)
            yb, r_yb = sb(f"yb", [128, S], BF16, ph)
            rz, r_rz = sb(f"rz", [128, 512], F32, ph)
            tm, r_tm = sb(f"tm", [128, 512], F32, ph)
            for i in range(2):
                P.op("pool", lambda e, i=i: e.memset(vts[i][0][:], 0.0), [], [vts[i][1]], partial=False)
                P.op("pool", lambda e, i=i: e.memset(qTs[i][0][:], 0.0), [], [qTs[i][1]], partial=False)
            npT = 0
            nbuf = 0
            jlist = range(4) if not (dbg and "att_j" in dbg) else dbg["att_j"]
            for j in jlist:
                for g in (range(3) if not (dbg and "att_g" in dbg) else dbg["att_g"]):
                    d = (1, 4, 16)[g]
                    ntl = (S // d) // 128
                    qT, r_q = qTs[nbuf % 2]
                    kT, r_k = kTs[nbuf % 2]
                    vt, r_v = vts[nbuf % 2]
                    ab, r_ab = abs_[nbuf % 2]
                    nbuf += 1
                    P.dma("sp", qT[0:64, 0, :], proj_d[g * 4 + j, 0:64, :], [R_proj[g * 4 + j]], [r_q], partial=False)
                    P.dma("sp", qT[64:128, 1, :], proj_d[g * 4 + j, 64:128, :], [R_proj[g * 4 + j]], [r_q])
                    P.dma("sp", kT[:], proj_d[12 + g * 4 + j, :, :], [R_proj[12 + g * 4 + j]], [r_k], partial=False)
                    P.dma("sp", vs[:], vtm_d[g, j, :, :, :], [R_vtm[g]], [r_vs], partial=False)
                    P.op("pool", lambda e: e.tensor_copy(out=vt[:, :, 0, 0:64], in_=vs[:, :, 0:64]), [r_vs], [r_v], partial=False)
                    P.op("pool", lambda e: e.tensor_copy(out=vt[:, :, 1, 64:128], in_=vs[:, :, 64:128]), [r_vs], [r_v], partial=True)
                    P.dma("sp", ab[:], C["abias"][:, g * 4 + j, :, :, :].rearrange("p k h q -> p k (h q)"), [R_in], [r_ab], partial=False)
                    qv = [qT[:, hh, :].rearrange("p (i d) -> p d i", d=d) for hh in range(2)]
                    kv = [kT[:, :].rearrange("p (i d) -> p d i", d=d) for hh in range(2)]
                    accv = Oacc[:].rearrange("p c (i d) -> p c d i", d=d)
                    tiles = [(r_, u) for r_ in range(d) for u in range(ntl)]

                    def stageA(r_, u):
                        kts = [kt for kt in range(3) if 0 <= u + kt - 1 < ntl]
                        outl = []
                        for kt in kts:
                            ku = u + kt - 1
                            rr["sc"] = (rr.get("sc", 0) + 1) % 4
                            ps, r_ps = psf[rr["sc"]]
                            for hh in range(2):
                                P.op("pe", lambda e, hh=hh: e.matmul(ps[:, hh * 128:(hh + 1) * 128], lhsT=ident[:],
                                                                     rhs=ab[:, kt, hh * 128:(hh + 1) * 128], start=True, stop=False),
                                     [r_ident, r_ab], [r_ps], sig=False, partial=(hh > 0))
                                P.op("pe", lambda e, hh=hh: e.matmul(
                                    ps[:, hh * 128:(hh + 1) * 128], lhsT=kv[hh][:, r_, ku * 128:(ku + 1) * 128],
                                    rhs=qv[hh][:, r_, u * 128:(u + 1) * 128], start=False, stop=True),
                                    [r_k, r_q], [r_ps], sig=(hh == 1), partial=True)
                            rr["pt"] = (rr.get("pt", 0) + 1) % 8
                            pT, r_pT = pTs[rr["pt"]]
                            P.op("act", lambda e: e.activation(out=pT[:], in_=ps[:, 0:256], func=AF.Exp, scale=0.125),
                                 [r_ps], [r_pT], partial=False)
                            outl.append((r_ * ntl + ku, pT, r_pT))
                        return outl

                    def stageB(r_, u, pl):
                        rr["po"] = (rr.get("po", 0) + 1) % 2
                        po, r_po = psf[4 + rr["po"]]
                        n = len(pl) * 2
                        for part in range(2):
                            i = 0
                            for (ti, pT, r_pT) in pl:
                                for hh in range(2):
                                    lt = vt[:, ti, hh, :] if part == 0 else onesel[:, hh, :]
                                    P.op("pe", lambda e, hh=hh, lt=lt, i=i: e.matmul(
                                        po[:, part * 128:(part + 1) * 128], lhsT=lt, rhs=pT[:, hh * 128:(hh + 1) * 128],
                                        start=(i == 0), stop=(i == n - 1)),
                                        [r_v, r_onesel, r_pT], [r_po], sig=(part == 1 and i == n - 1), partial=not (part == 0 and i == 0))
                                    i += 1
                        av = accv[:, :, r_, u * 128:(u + 1) * 128]
                        pv = po[:, 0:256].rearrange("p (c q) -> p c q", c=2)
                        if g == 0:
                            P.op("dve", lambda e: e.tensor_copy(out=av, in_=pv), [r_po], [r_O], partial=True)
                        else:
                            P.op("dve", lambda e: e.tensor_add(out=av, in0=pv, in1=av), [r_po, r_O], [r_O], partial=True)

                    amode = dbg.get("att_mode", 3) if dbg else 3
                    prev = None
                    for (r_, u) in tiles:
                        cur = (r_, u, stageA(r_, u))
                        if prev is not None and amode >= 2:
                            stageB(*prev)
                        prev = cur
                    if amode >= 2:
                        stageB(*prev)
                P.dma("sp", gs[:], proj_d[36 + j, :, :], [R_proj[36 + j]], [r_gs], partial=False)
                for tb in range(8):
                    sl = slice(tb * 512, (tb + 1) * 512)
                    P.op("dve", lambda e: e.reciprocal(out=rz[:], in_=Oacc[:, 1, sl]), [r_O], [r_rz], partial=False)
                    P.op("dve", lambda e: e.tensor_mul(out=tm[:], in0=Oacc[:, 0, sl], in1=rz[:]), [r_O, r_rz], [r_tm], partial=False)
                    P.op("pool", lambda e: e.tensor_mul(out=yb[:, sl], in0=tm[:], in1=gs[:, sl]), [r_tm, r_gs], [r_yb], partial=(tb > 0))
                P.dma("sp", yattn_d[j, :, :], yb[:], [r_yb], [R_yattn[j]], partial=False, sem_of=r_yb)
            P.barrier()
        if stop_after == "att":
            break
        with ExitStack() as ph:
            pb, r_pb = sb("pb", [128, S], BF16, ph)
            uf, r_uf = sb("uf", [128, 2048], F32, ph)
            ub, r_ub = sb("ub", [128, S], BF16, ph)
            zt, r_zt = sb("zt", [128, S], BF16, ph)
            xg, r_xg = sb("xg", [128, S], BF16, ph)
            CEf, r_CE = sb("CEf", [128, 8192], BF16, ph)
            Xb, r_X = sb("Xb", [128, 2, 32, 128], BF16, ph)
            Tt, r_T = sb("Tt", [128, 32, 2, 128], BF16, ph)
            Hb = [sb(f"Hb{i}", [128, 2, 8, 128], BF16, ph) for i in range(2)]
            tp = [sb(f"tp{i}", [128, 8, 128], F32, ph) for i in range(4)]
            fb1, r_fb1 = sb("fb1", [128, 2, 128], F32, ph)
            fb4, r_fb4 = sb("fb4", [128, 2, 32, 4, 4], F32, ph)
            e1, r_e1 = sb("e1", [128, 32, 4, 4], F32, ph)
            e2, r_e2 = sb("e2", [128, 32, 4, 4], F32, ph)
            gsT, r_gsT = sb("gsT", [128, S], BF16, ph)
            yT, r_yT = sb("yT", [128, S], BF16, ph)
            P.dma("sp", Tt[:], C["T"][:, :, :, :], [R_in], [r_T], partial=False)

            def conv3(chunk, widx, dst, r_dst, fftl):
                P.dma("sp", pb[:], proj_d[chunk, :, :], [R_proj[chunk]], [r_pb], partial=False)
                w0 = convw[:, l, 0, widx:widx + 1]
                w1 = convw[:, l, 1, widx:widx + 1]
                w2 = convw[:, l, 2, widx:widx + 1]
                cb = convb[:, l, widx:widx + 1]
                for hb_ in range(2):
                    t0 = hb_ * 2048
                    P.op("dve", lambda e: e.tensor_scalar(out=uf[:, :], in0=pb[:, t0:t0 + 2048], scalar1=w1, scalar2=cb,
                                                          op0=ALU.mult, op1=ALU.add), [r_pb, r_convw, r_convb], [r_uf], partial=False)
                    a = 1 if hb_ == 0 else 0
                    P.op("dve", lambda e: e.scalar_tensor_tensor(out=uf[:, a:2048], in0=pb[:, t0 + a - 1:t0 + 2047], scalar=w0,
                                                                 in1=uf[:, a:2048], op0=ALU.mult, op1=ALU.add),
                         [r_pb, r_convw, r_uf], [r_uf], partial=False)
                    b_ = 2047 if hb_ == 1 else 2048
                    P.op("dve", lambda e: e.scalar_tensor_tensor(out=ub[:, t0:t0 + b_], in0=pb[:, t0 + 1:t0 + b_ + 1], scalar=w2,
                                                                 in1=uf[:, 0:b_], op0=ALU.mult, op1=ALU.add),
                         [r_pb, r_convw, r_uf], [r_ub], partial=(hb_ > 0))
                    if hb_ == 1:
                        P.op("dve", lambda e: e.tensor_copy(out=ub[:, S - 1:S], in_=uf[:, 2047:2048]), [r_uf], [r_ub], partial=True)
                to_token_major(ub, r_ub, dst, r_dst, fftl)

            def pointwise(o, c):
                for blk in range(4):
                    hb, r_hb = Hb[blk % 2]
                    P.dma("sp", hb[:], H_d[l, o, c, :, :, blk * 8:(blk + 1) * 8, :], [R_H[l][o][c]], [r_hb], partial=False)
                    xr, xi = Xb[:, 0, blk * 8:(blk + 1) * 8, :], Xb[:, 1, blk * 8:(blk + 1) * 8, :]
                    hr, hi = hb[:, 0, :, :], hb[:, 1, :, :]
                    (t1, r1), (t2, r2), (t3, r3), (t4, r4) = tp
                    P.op("dve", lambda e: e.tensor_mul(out=t1[:], in0=xr, in1=hr), [r_X, r_hb], [r1], partial=False)
                    P.op("dve", lambda e: e.tensor_mul(out=t2[:], in0=xi, in1=hi), [r_X, r_hb], [r2], partial=False)
                    P.op("dve", lambda e: e.tensor_mul(out=t3[:], in0=xr, in1=hi), [r_X, r_hb], [r3], partial=False)
                    P.op("dve", lambda e: e.tensor_mul(out=t4[:], in0=xi, in1=hr), [r_X, r_hb], [r4], partial=False)
                    P.op("dve", lambda e: e.tensor_sub(out=xr, in0=t1[:], in1=t2[:]), [r1, r2], [r_X], partial=True)
                    P.op("dve", lambda e: e.tensor_add(out=xi, in0=t3[:], in1=t4[:]), [r3, r4], [r_X], partial=True)

            clist = range(4) if not (dbg and "hy_c" in dbg) else dbg["hy_c"]
            for c in clist:
                for o in range(2):
                    P.dma("sp", fb1[:, o, :], bc(I["f_bias_r"], (l * 2 + o) * 512 + c * 128, 128), [R_in], [r_fb1], partial=(o > 0))
                for o in range(2):
                    for i4 in range(4):
                        P.op("dve", lambda e: e.tensor_copy(out=fb4[:, o, :, i4, :], in_=fb1[:, o, :].rearrange("p (g c) -> p g c", c=4)),
                             [r_fb1], [r_fb4], partial=not (o == 0 and i4 == 0))
                conv3(40 + c, c, zt, r_zt, True)
                conv3(44 + c, 4 + c, xg, r_xg, False)
                hmode = dbg.get("hy_mode", 9) if dbg else 9
                for o in range(2):
                    if hmode < 2:
                        break
                    fft_fwd(zt, r_zt, None, None, CEf, r_CE, Xb, r_X, tp)
                    if hmode < 3:
                        break
                    pointwise(o, c)
                    if hmode < 4:
                        break

                    def epi(nb, pyv, r_py, o=o):
                        zs = zt[:, :].rearrange("p (g n c) -> p g n c", g=32, n=32, c=4)[:, :, nb * 4:(nb + 1) * 4, :]
                        xs = xg[:, :].rearrange("p (n g c) -> p g n c", n=32, g=32, c=4)[:, :, nb * 4:(nb + 1) * 4, :]
                        pyf = pyv.rearrange("p n (g c) -> p g n c", c=4)
                        P.op("dve", lambda e: e.tensor_mul(out=e1[:], in0=zs, in1=fb4[:, o, :, :, :]), [r_zt, r_fb4], [r_e1], partial=False)
                        P.op("dve", lambda e: e.tensor_add(out=e2[:], in0=pyf, in1=e1[:]), [r_py, r_e1], [r_e2], partial=False)
                        if o == 0:
                            P.op("dve", lambda e: e.tensor_mul(out=zs, in0=e2[:], in1=xs), [r_e2, r_xg], [r_zt], partial=True)
                        else:
                            P.op("dve", lambda e: e.tensor_mul(out=xs, in0=e2[:], in1=xs), [r_e2, r_xg], [r_xg], partial=True)

                    fft_inv(Xb, r_X, CEf, r_CE, Tt, r_T, epi)
                    if o == 0:
                        conv3(48 + c, 8 + c, xg, r_xg, False)
                P.dma("sp", gsT[:], proj_d[52 + c, :, :], [R_proj[52 + c]], [r_gsT], partial=False)
                yv = yT[:, :].rearrange("p (a b) -> p b a", b=32)
                gv = gsT[:, :].rearrange("p (a b) -> p b a", b=32)
                for nb in range(4):
                    pt, r_pt = next_psb()
                    for ni in range(8):
                        n1 = nb * 8 + ni
                        P.op("pe", lambda e: e.transpose(out=pt[:, ni * 128:(ni + 1) * 128], in_=xg[:, n1 * 128:(n1 + 1) * 128], identity=ident[:]),
                             [r_xg, r_ident], [r_pt], sig=(ni == 7), partial=(ni > 0))
                    P.op("dve", lambda e: e.tensor_mul(out=yv[:, nb * 8:(nb + 1) * 8, :], in0=pt[:, :].rearrange("p (n c) -> p n c", n=8),
                                                       in1=gv[:, nb * 8:(nb + 1) * 8, :]), [r_pt, r_gsT], [r_yT], partial=(nb > 0))
                P.dma("sp", yhy_d[c, :, :], yT[:], [r_yT], [R_yhy[c]], partial=False, sem_of=r_yT)
            P.barrier()
        if stop_after == "hy":
            break
        with ExitStack() as ph:
            wpa, r_wpa = sb("wpa", [128, 4, D], BF16, ph)
            wph, r_wph = sb("wph", [128, 4, D], BF16, ph)
            wo, r_wo = sb("wo", [128, 8, D], BF16, ph)
            rowb, r_rowb = sb("rowb", [128, 3, D], F32, ph)
            with ExitStack() as ph2:
                wst, r_wst = sb("wstg", [128, 8, D], F32, ph2)
                P.dma("sp", wst[:, 0:4, :], I["w_pa"][l], [R_in], [r_wst], partial=False)
                P.op("pool", lambda e: e.tensor_copy(out=wpa[:], in_=wst[:, 0:4, :]), [r_wst], [r_wpa], partial=False)
                P.dma("sp", wst[:, 4:8, :], I["w_ph"][l], [R_in], [r_wst], partial=False)
                P.op("pool", lambda e: e.tensor_copy(out=wph[:], in_=wst[:, 4:8, :]), [r_wst], [r_wph], partial=False)
                P.dma("sp", wst[:, :, :], I["w_out"][l], [R_in], [r_wst], partial=False)
                for kc in range(8):
                    P.op("pool" if kc % 2 else "dve", lambda e: e.tensor_mul(out=wo[:, kc, :], in0=wst[:, kc, :], in1=gate_b[:, l, :]),
                         [r_wst, r_gate], [r_wo], partial=(kc > 0))
                P.barrier()
            P.dma("sp", rowb[:, 0, :], bc(I["b_out_r"], l * D, D), [R_in], [r_rowb], partial=False)
            P.dma("sp", rowb[:, 1, :], bc(I["ln_g_r"], l * D, D), [R_in], [r_rowb])
            P.dma("sp", rowb[:, 2, :], bc(I["ln_b_r"], l * D, D), [R_in], [r_rowb])
            gb, r_gb = sb("gb", [128, D], F32, ph)
            P.op("dve", lambda e: e.tensor_mul(out=gb[:], in0=rowb[:, 0, :], in1=gate_b[:, l, :]), [r_rowb, r_gate], [r_gb], partial=False)
            nb_, r_nb = sb("nbias", [128, 1], F32, ph)
            ya = [sb(f"ya{i}", [128, 4, 512], BF16, ph) for i in range(2)]
            yh = [sb(f"yh{i}", [128, 4, 512], BF16, ph) for i in range(2)]
            ga = [sb(f"ga{i}", [128, 16, 512], BF16, ph) for i in range(2)]
            mT, r_mT = sb("mT", [128, 8, 512], BF16, ph)
            m1, r_m1 = sb("m1", [128, 512], F32, ph)
            m2, r_m2 = sb("m2", [128, 512], F32, ph)
            xr_ = [sb(f"xr{i}", [128, D], F32, ph) for i in range(2)]
            rs_ = [sb(f"rs{i}", [128, D], F32, ph) for i in range(2)]
            o1, r_o1 = sb("o1", [128, 512], F32, ph)
            stt, r_st = sb("mst", [128, 12], F32, ph)
            mvv, r_mv = sb("mmv", [128, 2], F32, ph)
            xo = [sb(f"xo{i}", [128, D], F32, ph) for i in range(2)]
            tbl = range(8) if not (dbg and "merge_tb" in dbg) else dbg["merge_tb"]
            nt = 0
            for tb in tbl:
                sl = slice(tb * 512, (tb + 1) * 512)
                yat, r_ya = ya[tb % 2]
                yht, r_yh = yh[tb % 2]
                gat, r_ga = ga[tb % 2]
                P.dma("sp", yat[:], yattn_d[:, :, sl].rearrange("c p t -> p c t"), R_yattn, [r_ya], partial=False)
                P.dma("sp", yht[:], yhy_d[:, :, sl].rearrange("c p t -> p c t"), R_yhy, [r_yh], partial=False)
                P.dma("sp", gat[:], proj_d[56:72, :, sl].rearrange("c p t -> p c t"), R_proj[56:72], [r_ga], partial=False)
                for fc in range(8):
                    pa, r_pa = next_psf()
                    pq, r_pq = next_psf()
                    for kc in range(4):
                        P.op("pe", lambda e: e.matmul(pa[:, :], lhsT=wpa[:, kc, fc * 128:(fc + 1) * 128], rhs=yat[:, kc, :],
                                                      start=(kc == 0), stop=(kc == 3)), [r_wpa, r_ya], [r_pa], sig=(kc == 3), partial=(kc > 0))
                    for kc in range(4):
                        P.op("pe", lambda e: e.matmul(pq[:, :], lhsT=wph[:, kc, fc * 128:(fc + 1) * 128], rhs=yht[:, kc, :],
                                                      start=(kc == 0), stop=(kc == 3)), [r_wph, r_yh], [r_pq], sig=(kc == 3), partial=(kc > 0))
                    P.op("dve", lambda e: e.tensor_mul(out=m1[:], in0=pa[:, :], in1=gat[:, fc, :]), [r_pa, r_ga], [r_m1], partial=False)
                    P.op("dve", lambda e: e.tensor_mul(out=m2[:], in0=pq[:, :], in1=gat[:, 8 + fc, :]), [r_pq, r_ga], [r_m2], partial=False)
                    P.op("pool", lambda e: e.tensor_add(out=mT[:, fc, :], in0=m1[:], in1=m2[:]), [r_m1, r_m2], [r_mT], partial=(fc > 0))
                for tt in range(4):
                    row0 = tb * 512 + tt * 128
                    xrt, r_xr = xr_[nt % 2]
                    rst, r_rs = rs_[nt % 2]
                    xot, r_xo = xo[nt % 2]
                    nt += 1
                    P.dma("sp", xrt[:], x_src[row0:row0 + 128, :], [R_xsrc], [r_xr], partial=False)
                    P.op("dve", lambda e: e.scalar_tensor_tensor(out=xrt[:], in0=xrt[:], scalar=ALPHA, in1=gb[:], op0=ALU.mult, op1=ALU.add),
                         [r_xr, r_gb], [r_xr], partial=False)
                    for hf_ in range(2):
                        hs = slice(hf_ * 512, (hf_ + 1) * 512)
                        po_, r_po = next_psf()
                        for kc in range(8):
                            P.op("pe", lambda e: e.matmul(po_[:, :], lhsT=mT[:, kc, tt * 128:(tt + 1) * 128], rhs=wo[:, kc, hs],
                                                          start=(kc == 0), stop=(kc == 7)), [r_mT, r_wo], [r_po], sig=(kc == 7), partial=(kc > 0))
                        P.op("dve", lambda e: e.tensor_add(out=rst[:, hs], in0=po_[:, :], in1=xrt[:, hs]), [r_po, r_xr], [r_rs], partial=(hf_ > 0))
                    P.op("dve", lambda e: e.bn_stats(out=stt[:, 0:6], in_=rst[:, 0:512]), [r_rs], [r_st], partial=False)
                    P.op("dve", lambda e: e.bn_stats(out=stt[:, 6:12], in_=rst[:, 512:1024]), [r_rs], [r_st], partial=True)
                    P.op("dve", lambda e: e.bn_aggr(out=mvv[:], in_=stt[:]), [r_st], [r_mv], partial=False)
                    P.op("act", lambda e: e.activation(out=mvv[:, 1:2], in_=mvv[:, 1:2], func=AF.Sqrt, bias=epsT[:], scale=1.0),
                         [r_mv, r_eps], [r_mv], partial=False)
                    P.op("dve", lambda e: e.reciprocal(out=mvv[:, 1:2], in_=mvv[:, 1:2]), [r_mv], [r_mv], partial=False)
                    P.op("dve", lambda e: e.tensor_scalar(out=nb_[:], in0=mvv[:, 0:1], scalar1=-1.0, scalar2=mvv[:, 1:2],
                                                          op0=ALU.mult, op1=ALU.mult), [r_mv], [r_nb], partial=False)
                    P.op("act", lambda e: e.activation(out=rst[:], in_=rst[:], func=AF.Identity, scale=mvv[:, 1:2], bias=nb_[:]),
                         [r_rs, r_mv, r_nb], [r_rs], partial=False)
                    P.op("dve", lambda e: e.tensor_mul(out=rst[:], in0=rst[:], in1=rowb[:, 1, :]), [r_rs, r_rowb], [r_rs], partial=False)
                    P.op("pool", lambda e: e.tensor_add(out=xot[:], in0=rst[:], in1=rowb[:, 2, :]), [r_rs, r_rowb], [r_xo], partial=False)
                    P.dma("sp", x_dst[row0:row0 + 128, :], xot[:], [r_xo], [R_xdst], sem_of=r_xo)
            P.barrier()
        x_src, R_xsrc = x_dst, R_xdst
    P.barrier()
    stack.close()
    return nc, dbg_out


_CACHE = {}


def kernel(**inputs):
    inp = {k: np.asarray(v, dtype=np.float32) for k, v in inputs.items()}
    consts = _const_tables()
    if "nc" not in _CACHE:
        _CACHE["nc"] = build()[0]
    nc = _CACHE["nc"]
    in_maps = []
    for b in range(8):
        m = _layout_inputs(inp, b)
        for k, v in consts.items():
            m["c_" + k] = v
        in_maps.append(m)
    res = run_bass_kernel_spmd(nc, in_maps, core_ids=list(range(8)))
    return np.stack([np.asarray(r["out"], dtype=np.float32) for r in res.results], axis=0)
```

```python
import math
from contextlib import ExitStack
import numpy as np
import ml_dtypes
import concourse.bass as bass
import concourse.mybir as mybir
from concourse.bass_utils import run_bass_kernel_spmd

F32 = mybir.dt.float32
BF16 = mybir.dt.bfloat16
AF = mybir.ActivationFunctionType
ALU = mybir.AluOpType
NPBF = ml_dtypes.bfloat16

S = 4096
D = 1024
NIN = 9216
DEPTH = 2
ALPHA = (2 * DEPTH) ** 0.25
EPS = 1e-5
NFFT = 8192
TWO_PI = 2.0 * math.pi


class Res:
    __slots__ = ("name", "writers", "readers", "prev", "sem", "dcount")

    def __init__(self, name, sem=None):
        self.name = name
        self.writers = []
        self.readers = []
        self.prev = []
        self.sem = sem if sem is not None else {}
        self.dcount = 0


class Prog:
    ENG = ("pe", "act", "dve", "pool", "sp")

    def __init__(self, nc, stack):
        self.nc = nc
        self.stack = stack
        self.e = {"pe": nc.tensor, "act": nc.scalar, "dve": nc.vector, "pool": nc.gpsimd, "sp": nc.sync}
        self.info = []
        self.pending = {k: [] for k in self.ENG}
        self.esem = {}
        self.ecount = {k: 0 for k in self.ENG}
        self.known = {k: {} for k in self.ENG}
        self.nsem = 0
        self.dma_since_barrier = []
        self.last_sig = {k: None for k in self.ENG}

    def new_sem(self, name):
        self.nsem += 1
        return self.stack.enter_context(self.nc.semaphore(f"{name}_{self.nsem}"))

    def _deps(self, reads, writes, partial):
        deps = set()
        for r in reads:
            deps.update(r.writers)
        for w in writes:
            if partial and not w.readers:
                deps.update(w.prev)
            else:
                w.prev = w.writers + w.readers
                deps.update(w.prev)
                w.writers = []
                w.readers = []
        return deps

    def _commit(self, oid, reads, writes):
        for r in reads:
            r.readers.append(oid)
        for w in writes:
            w.writers.append(oid)

    def _waits(self, eng, deps, is_dma):
        need = {}
        for d in deps:
            inf = self.info[d]
            assert inf is not None, "dependency on a non-signalling op"
            deng, sem, val = inf
            if deng == "pe" and eng == "pe" and not is_dma:
                continue
            k = id(sem)
            if k not in need or need[k][1] < val:
                need[k] = (sem, val)
        for k, (sem, val) in need.items():
            if self.known[eng].get(k, 0) >= val:
                continue
            self.e[eng].wait_ge(sem, val)
            self.known[eng][k] = val

    def op(self, eng, fn, reads=(), writes=(), sig=True, partial=False):
        deps = self._deps(reads, writes, partial)
        self._waits(eng, deps, False)
        ins = fn(self.e[eng])
        oid = len(self.info)
        if sig:
            if self.ecount[eng] % 30000 == 0:
                self.esem[eng] = self.new_sem("e" + eng)
                self.ecount[eng] = 0
            self.ecount[eng] += 1
            ins.then_inc(self.esem[eng], 1)
            inf = (eng, self.esem[eng], self.ecount[eng])
            self.info.append(inf)
            for p in self.pending[eng]:
                self.info[p] = inf
            self.pending[eng] = []
            self.last_sig[eng] = oid
        else:
            self.info.append(None)
            self.pending[eng].append(oid)
        self._commit(oid, reads, writes)
        return oid

    def dma(self, eng, out, in_, reads, writes, partial=True, sem_of=None, **kw):
        assert len(writes) == 1
        w = sem_of if sem_of is not None else writes[0]
        deps = self._deps(reads, writes, partial)
        self._waits(eng, deps, True)
        kind = "sw" if eng == "pool" else "hw"
        if kind not in w.sem:
            w.sem[kind] = [self.new_sem("d" + kind), 0]
        w.sem[kind][1] += 1
        self.e[eng].dma_start(out=out, in_=in_, **kw).then_inc(w.sem[kind][0], 16)
        oid = len(self.info)
        self.info.append(("dma", w.sem[kind][0], 16 * w.sem[kind][1]))
        self.dma_since_barrier.append(oid)
        self._commit(oid, reads, writes)
        return oid

    def barrier(self):
        deps = set(self.dma_since_barrier)
        for k in self.ENG:
            assert not self.pending[k], "pending non-signalled ops at barrier"
            if self.last_sig[k] is not None:
                deps.add(self.last_sig[k])
        for k in self.ENG:
            need = {}
            for d in deps:
                deng, sem, val = self.info[d]
                kk = id(sem)
                if kk not in need or need[kk][1] < val:
                    need[kk] = (sem, val)
            for kk, (sem, val) in need.items():
                if self.known[k].get(kk, 0) >= val:
                    continue
                self.e[k].wait_ge(sem, val)
                self.known[k][kk] = val
        self.dma_since_barrier = []


def _const_tables():
    c = {}
    c["ident"] = np.eye(128, dtype=np.float32).astype(NPBF)
    dil = (1, 4, 16)
    p = np.arange(128)[:, None]
    j = np.arange(128)[None, :]
    tab = np.zeros((128, 12, 3, 2, 128), np.float32)
    for g in range(3):
        for h in range(8):
            slope = 2.0 ** (-(h + 1))
            for kt in range(3):
                rel = p + (kt - 1) * 128 - j
                val = -8.0 * slope * dil[g] * np.abs(rel)
                val = np.where(np.abs(rel) <= 64, val, -32768.0)
                tab[:, g * 4 + h // 2, kt, h % 2, :] = val
    c["abias"] = tab.astype(NPBF)
    osel = np.zeros((128, 2, 128), np.float32)
    osel[:, 0, :64] = 1.0
    osel[:, 1, 64:] = 1.0
    c["onesel"] = osel.astype(NPBF)
    n2 = np.arange(128)[:, None].astype(np.float64)
    k2 = np.arange(128)[None, :].astype(np.float64)
    ang = -2.0 * np.pi * n2 * (k2 + 0.5) / 256.0
    Fr, Fi = np.cos(ang), np.sin(ang)
    c["F1"] = np.concatenate([Fr, Fi, -Fi], axis=1).astype(np.float32).astype(NPBF)
    ang2 = -2.0 * np.pi * (n2 + 128.0) * (k2 + 0.5) / 256.0
    Fr2, Fi2 = -np.cos(ang2), -np.sin(ang2)
    c["F1hi"] = np.concatenate([Fr2, Fi2, -Fi2], axis=1).astype(np.float32).astype(NPBF)
    n1 = np.arange(32).astype(np.float64)
    k1 = np.arange(32).astype(np.float64)
    eye4 = np.eye(4)
    a = -2.0 * np.pi * n1[:, None] * k1[None, :] / 32.0
    F32m = np.zeros((128, 3, 128), np.float32)
    F32m[:, 0, :] = np.kron(np.cos(a), eye4)
    F32m[:, 1, :] = np.kron(np.sin(a), eye4)
    F32m[:, 2, :] = -np.kron(np.sin(a), eye4)
    c["F32m"] = F32m.astype(NPBF)
    kk2 = np.arange(128).astype(np.float64)
    a = -2.0 * np.pi * np.repeat(n1, 4)[:, None] * (kk2[None, :] + 0.5) / NFFT
    tw = np.zeros((128, 2, 8, 128), np.float32)
    tw[:, 0, :, :] = np.cos(a)[:, None, :]
    tw[:, 1, :, :] = np.sin(a)[:, None, :]
    c["tw8"] = tw
    a = 2.0 * np.pi * k1[:, None] * n1[None, :] / 32.0
    Rr = np.kron(np.cos(a), eye4)
    Ri = np.kron(np.sin(a), eye4)
    R12 = np.zeros((128, 2, 256), np.float32)
    R12[:, 0, :128], R12[:, 0, 128:] = Rr, Ri
    R12[:, 1, :128], R12[:, 1, 128:] = -Ri, Rr
    c["R12"] = R12.astype(NPBF)
    T = np.zeros((128, 32, 2, 128), np.float32)
    kk = np.arange(128)[:, None].astype(np.float64)
    nn2 = np.arange(128)[None, :].astype(np.float64)
    for a1 in range(32):
        a = 2.0 * np.pi * (a1 + 32.0 * nn2) * (kk + 0.5) / NFFT
        T[:, a1, 0, :] = (2.0 / NFFT) * np.cos(a)
        T[:, a1, 1, :] = -(2.0 / NFFT) * np.sin(a)
    c["T"] = T.astype(NPBF)
    L = S
    t = np.linspace(0.0, 1.0, L, dtype=np.float32)[:, None]
    bands = 16
    w = (2.0 * np.pi * np.arange(L, dtype=np.float32)[:, None] / L).astype(np.float32)
    f = np.linspace(1e-4, bands - 1, bands, dtype=np.float32)[None, :]
    feat = np.concatenate([t, np.cos(f * w), -np.sin(f * w)], axis=-1).astype(np.float32)
    featT = np.ascontiguousarray(feat.T)
    rev = np.zeros_like(featT)
    rev[:, 1:] = featT[:, :0:-1]
    c["featT"] = np.stack([featT, rev], 0)
    trow = np.zeros((2, 1, L), np.float32)
    trow[0, 0] = t[:, 0]
    trow[1, 0, 1:] = t[:0:-1, 0]
    c["trow"] = trow
    deltas = np.linspace(math.log(1e-2) / 1.5, math.log(1e-2) / 0.3, 512, dtype=np.float32)
    c["ndelta"] = np.ascontiguousarray((-np.abs(deltas)).reshape(4, 128).T)
    return c


CONST_DT = {"ident": BF16, "abias": BF16, "onesel": BF16, "F1": BF16, "F1hi": BF16, "F32m": BF16, "tw8": F32,
            "R12": BF16, "T": BF16, "featT": F32, "trow": F32, "ndelta": F32}


def _layout_inputs(inp, b):
    m = {}
    m["x"] = np.ascontiguousarray(inp["x"][b])
    m["crep"] = np.ascontiguousarray(np.broadcast_to(
        inp["c"][b].reshape(8, 128).T[:, :, None], (128, 8, 128))).astype(np.float32)
    m["ccol"] = np.ascontiguousarray(inp["c"][b].reshape(8, 128).T)

    def kt(w, kc):
        Ld, K, N = w.shape
        return np.ascontiguousarray(w.reshape(Ld, kc, 128, N).transpose(0, 2, 1, 3))

    m["w_ada"] = kt(inp["w_ada"], 8)
    m["w_in"] = kt(inp["w_in"], 8)
    m["w_pa"] = kt(inp["w_proj_attn"], 4)
    m["w_ph"] = kt(inp["w_proj_hyena"], 4)
    m["w_out"] = kt(inp["w_out"], 8)

    def col(v):
        Ld, N = v.shape
        return np.ascontiguousarray(v.reshape(Ld, N // 128, 128).transpose(0, 2, 1))

    m["b_ada_c"] = col(inp["b_ada"])
    m["b_ada_r"] = np.ascontiguousarray(inp["b_ada"][:, None, :])
    m["b_in_c"] = col(inp["b_in"])
    m["b_in_r"] = np.ascontiguousarray(inp["b_in"][:, None, :])
    m["conv_w_c"] = np.ascontiguousarray(inp["conv_w"].reshape(DEPTH, 3, 12, 128).transpose(0, 3, 1, 2))
    m["conv_b_c"] = col(inp["conv_b"])
    m["f_w1"] = np.ascontiguousarray(inp["filt_w1"])
    m["f_w2"] = np.ascontiguousarray(inp["filt_w2"])
    m["f_w3"] = np.ascontiguousarray(inp["filt_w3"])
    m["f_w4"] = np.ascontiguousarray(inp["filt_w4"])
    m["f_b"] = np.ascontiguousarray(np.stack([inp["filt_b1"], inp["filt_b2"], inp["filt_b3"], inp["filt_freq"]], -1))
    m["f_bias_r"] = np.ascontiguousarray(inp["filt_bias"])
    m["b_out_r"] = np.ascontiguousarray(inp["b_out"][:, None, :])
    m["ln_g_r"] = np.ascontiguousarray(inp["ln_g"][:, None, :])
    m["ln_b_r"] = np.ascontiguousarray(inp["ln_b"][:, None, :])
    return m


IN_SHAPES = {
    "x": [S, D], "crep": [128, 8, 128], "ccol": [128, 8],
    "w_ada": [DEPTH, 128, 8, 3072], "w_in": [DEPTH, 128, 8, NIN], "w_pa": [DEPTH, 128, 4, D],
    "w_ph": [DEPTH, 128, 4, D], "w_out": [DEPTH, 128, 8, D],
    "b_ada_c": [DEPTH, 128, 24], "b_ada_r": [DEPTH, 1, 3072], "b_in_c": [DEPTH, 128, 72],
    "b_in_r": [DEPTH, 1, NIN], "conv_w_c": [DEPTH, 128, 3, 12], "conv_b_c": [DEPTH, 128, 12],
    "f_w1": [DEPTH, 33, 64], "f_w2": [DEPTH, 64, 64], "f_w3": [DEPTH, 64, 64], "f_w4": [DEPTH, 64, 2048],
    "f_b": [DEPTH, 64, 4], "f_bias_r": [DEPTH, 2, 512], "b_out_r": [DEPTH, 1, D],
    "ln_g_r": [DEPTH, 1, D], "ln_b_r": [DEPTH, 1, D],
}
CONST_SHAPES = {"ident": [128, 128], "abias": [128, 12, 3, 2, 128], "onesel": [128, 2, 128], "F1": [128, 384],
                "F1hi": [128, 384], "F32m": [128, 3, 128], "tw8": [128, 2, 8, 128], "R12": [128, 2, 256], "T": [128, 32, 2, 128],
                "featT": [2, 33, S], "trow": [2, 1, S], "ndelta": [128, 4]}


def bc(ap_t, offset, n):
    return bass.AP(ap_t.tensor, offset, [[0, 128], [1, n]])


class K:
    pass


def build(dbg=None, layers=DEPTH, stop_after=None):
    nc = bass.Bass("TRN2", target_bir_lowering=False)
    try:
        nc.allow_low_precision("bf16 matmul operands with fp32 accumulation")
    except Exception:
        pass
    stack = ExitStack()
    P = Prog(nc, stack)
    I = {k: nc.dram_tensor(k, s, F32, kind="ExternalInput").ap() for k, s in IN_SHAPES.items()}
    C = {k: nc.dram_tensor("c_" + k, s, CONST_DT[k], kind="ExternalInput").ap() for k, s in CONST_SHAPES.items()}
    out = nc.dram_tensor("out", [S, D], F32, kind="ExternalOutput").ap()
    dbg_out = {}

    def dram(name, shape, dt):
        kind = "ExternalOutput" if (dbg and name in dbg) else "Internal"
        t = nc.dram_tensor(name, shape, dt, kind=kind).ap()
        if kind == "ExternalOutput":
            dbg_out[name] = t
        return t

    proj_d = dram("proj_d", [72, 128, S], BF16)
    vtm_d = dram("vtm_d", [3, 4, 128, 32, 128], BF16)
    yattn_d = dram("yattn_d", [4, 128, S], BF16)
    yhy_d = dram("yhy_d", [4, 128, S], BF16)
    xmid_d = dram("xmid_d", [S, D], F32)
    H_d = dram("H_d", [DEPTH, 2, 4, 128, 2, 32, 128], BF16)
    fam = {}
    R_proj = [Res(f"proj{i}", fam) for i in range(72)]
    R_vtm = [Res(f"vtm{g}", fam) for g in range(3)]
    R_yattn = [Res(f"yattn{i}", fam) for i in range(4)]
    R_yhy = [Res(f"yhy{i}", fam) for i in range(4)]
    R_xmid = Res("xmid", fam)
    R_H = [[[Res(f"H{l}{o}{h}", fam) for h in range(4)] for o in range(2)] for l in range(DEPTH)]
    R_out = Res("out")
    R_in = Res("inputs")

    RES = {}
    cnt = {"n": 0}

    def sb(name, shape, dt, st=None):
        cnt["n"] += 1
        t = (st or stack).enter_context(nc.sbuf_tensor(f"s_{name}_{cnt['n']}", shape, dt))
        if name not in RES:
            RES[name] = Res(name)
        return t, RES[name]

    psf = []
    for i in range(6):
        t = stack.enter_context(nc.psum_tensor(f"psf{i}", [128, 512], F32))
        psf.append((t, Res(f"psf{i}")))
    psb = []
    for i in range(2):
        t = stack.enter_context(nc.psum_tensor(f"psb{i}", [128, 1024], BF16))
        psb.append((t, Res(f"psb{i}")))
    rr = {"f": 0, "b": 0, "q": 0}

    def next_psf():
        rr["f"] = (rr["f"] + 1) % 6
        return psf[rr["f"]]

    def next_psb():
        rr["b"] = (rr["b"] + 1) % 2
        return psb[rr["b"]]

    DQ = ("sp", "pool")

    def next_q():
        rr["q"] = (rr["q"] + 1) % len(DQ)
        return DQ[rr["q"]]

    ident, r_ident = sb("ident", [128, 128], BF16)
    onesel, r_onesel = sb("onesel", [128, 2, 128], BF16)
    F1, r_F1 = sb("F1", [128, 384], BF16)
    F1hi, r_F1hi = sb("F1hi", [128, 384], BF16)
    R12, r_R12 = sb("R12", [128, 2, 256], BF16)
    F32m, _ = sb("F32m", [128, 3, 128], BF16)
    tw8, _ = sb("tw8", [128, 2, 8, 128], F32)
    ones2, r_ones2 = sb("ones2", [2, 128], BF16)
    epsT, r_eps = sb("epsT", [128, 1], F32)
    npiT, r_npi = sb("npiT", [128, 1], F32)
    r_cst = Res("cst")
    r_ident = r_onesel = r_F1 = r_F1hi = r_R12 = r_cst
    P.dma("sp", ident[:], C["ident"][:, :], [R_in], [r_ident])
    P.dma("sp", onesel[:], C["onesel"][:, :, :], [R_in], [r_onesel])
    P.dma("sp", F1[:], C["F1"][:, :], [R_in], [r_F1])
    P.dma("sp", F1hi[:], C["F1hi"][:, :], [R_in], [r_F1hi])
    P.dma("sp", R12[:], C["R12"][:, :, :], [R_in], [r_R12])
    P.dma("sp", F32m[:], C["F32m"][:, :, :], [R_in], [r_cst])
    P.dma("sp", tw8[:], C["tw8"][:, :, :, :], [R_in], [r_cst])
    P.op("pool", lambda e: e.memset(ones2[:], 1.0), [], [r_ones2])
    P.op("pool", lambda e: e.memset(epsT[:], EPS), [], [r_eps])
    P.op("pool", lambda e: e.memset(npiT[:], -math.pi), [], [r_npi])

    modcol, r_modcol = sb("modcol", [128, DEPTH, 24], F32)
    sc1, r_sc1 = sb("sc1", [128, DEPTH, 8], F32)
    gate_b, r_gate = sb("gate_b", [128, DEPTH, D], F32)
    b_in_c, r_binc = sb("b_in_c", [128, DEPTH, 72], F32)
    convw, r_convw = sb("convw", [128, DEPTH, 3, 12], F32)
    convb, r_convb = sb("convb", [128, DEPTH, 12], F32)
    bin2, r_bin2 = sb("bin2", [1, DEPTH, 1536], BF16)
    binl, r_binl = sb("binl", [1, DEPTH, 1536], BF16)
    r_binc = r_convw = r_convb = r_cst
    for l in range(DEPTH):
        P.dma("sp", b_in_c[:, l, :], I["b_in_c"][l], [R_in], [r_binc])
        P.dma("sp", convw[:, l, :, :], I["conv_w_c"][l], [R_in], [r_convw])
        P.dma("sp", convb[:, l, :], I["conv_b_c"][l], [R_in], [r_convb])

    with ExitStack() as ph:
        ccol, r_ccol = sb("ccol", [128, 8], F32, ph)
        crep, r_crep = sb("crep", [128, 8, 128], F32, ph)
        badac, r_badac = sb("badac", [128, DEPTH, 24], F32, ph)
        badar, r_badar = sb("badar", [128, DEPTH, D], F32, ph)
        vb, r_vb = sb("vb", [1, DEPTH, 1536], F32, ph)
        vbh, r_vbh = sb("vbh", [1, DEPTH, 1536], F32, ph)
        r_ccol = r_crep = r_badac = r_badar = r_vb = r_cst
        P.dma("sp", ccol[:], I["ccol"][:, :], [R_in], [r_ccol])
        P.dma("sp", crep[:], I["crep"][:, :, :], [R_in], [r_crep])
        for l in range(DEPTH):
            P.dma("sp", badac[:, l, :], I["b_ada_c"][l], [R_in], [r_badac])
            P.dma("sp", badar[:, l, :], bc(I["b_ada_r"], l * 3072 + 2048, D), [R_in], [r_badar])
            P.dma("sp", vb[:, l, :], bass.AP(I["b_in_r"].tensor, l * NIN + 3072, [[0, 1], [1, 1536]]), [R_in], [r_vb])
        P.op("dve", lambda e: e.tensor_copy(out=bin2[:], in_=vb[:]), [r_vb], [r_bin2])
        P.op("dve", lambda e: e.tensor_copy(out=vbh[:], in_=bin2[:]), [r_bin2], [r_vbh])
        P.op("dve", lambda e: e.tensor_sub(out=vbh[:], in0=vb[:], in1=vbh[:]), [r_vb, r_vbh], [r_vbh])
        P.op("dve", lambda e: e.tensor_copy(out=binl[:], in_=vbh[:]), [r_vbh], [r_binl])
        wa = [sb(f"wa{i}", [128, 8, 512], F32, ph) for i in range(2)]
        for l in range(DEPTH):
            pc, r_pc = next_psf()
            for blk in range(6):
                wt, r_wt = wa[blk % 2]
                P.dma("sp", wt[:, 0:4, :], I["w_ada"][l, :, 0:4, blk * 512:(blk + 1) * 512], [R_in], [r_wt], partial=False)
                P.dma("pool", wt[:, 4:8, :], I["w_ada"][l, :, 4:8, blk * 512:(blk + 1) * 512], [R_in], [r_wt])
                for f in range(4):
                    fi = blk * 4 + f
                    for kc in range(8):
                        P.op("pe", lambda e, wt=wt, f=f, kc=kc, fi=fi: e.matmul(
                            pc[:, fi:fi + 1], lhsT=wt[:, kc, f * 128:(f + 1) * 128], rhs=ccol[:, kc:kc + 1],
                            start=(kc == 0), stop=(kc == 7)),
                            [r_wt, r_ccol], [r_pc], sig=(kc == 7), partial=True)
                if blk >= 4:
                    pg, r_pg = next_psf()
                    for kc in range(8):
                        P.op("pe", lambda e, wt=wt, kc=kc: e.matmul(
                            pg[:, :], lhsT=crep[:, kc, :], rhs=wt[:, kc, :], start=(kc == 0), stop=(kc == 7)),
                            [r_wt, r_crep], [r_pg], sig=(kc == 7), partial=True)
                    h0 = (blk - 4) * 512
                    P.op("dve", lambda e, l=l, h0=h0, pg=pg: e.tensor_add(
                        out=gate_b[:, l, h0:h0 + 512], in0=pg[:, :], in1=badar[:, l, h0:h0 + 512]),
                        [r_pg, r_badar], [r_gate], partial=True)
            P.op("dve", lambda e, l=l, pc=pc: e.tensor_add(out=modcol[:, l, :], in0=pc[:, 0:24], in1=badac[:, l, :]),
                 [r_pc, r_badac], [r_modcol], partial=True)
            P.op("dve", lambda e, l=l: e.tensor_scalar_add(out=sc1[:, l, :], in0=modcol[:, l, 8:16], scalar1=1.0),
                 [r_modcol], [r_sc1], partial=True)
        P.barrier()


    act_dve = {"n": 0}

    def evac(out_ap, in_ap, reads, writes, partial=True, simple=False):
        act_dve["n"] += 1
        if simple and act_dve["n"] % 2:
            P.op("act", lambda e: e.copy(out=out_ap, in_=in_ap), reads, writes, partial=partial)
        else:
            P.op("dve", lambda e: e.tensor_copy(out=out_ap, in_=in_ap), reads, writes, partial=partial)

    def fft_fwd(zl, r_zl, zh, r_zh, CEf, r_CE, X, r_X, tpb):
        Cv = CEf[:, 0:8192].rearrange("p (t c k) -> p t c k", t=2, c=32, k=128)
        for cp in range(16):
            pp, r_pp = next_psf()
            for gi in range(2):
                cg = cp * 2 + gi
                P.op("pe", lambda e: e.matmul(pp[:, gi * 256:(gi + 1) * 256], lhsT=zl[:, cg * 128:(cg + 1) * 128], rhs=F1[:, 0:256],
                                              start=True, stop=(zh is None)),
                     [r_zl, r_F1], [r_pp], sig=(zh is None and gi == 1), partial=(gi > 0))
                if zh is not None:
                    P.op("pe", lambda e: e.matmul(pp[:, gi * 256:(gi + 1) * 256], lhsT=zh[:, cg * 128:(cg + 1) * 128], rhs=F1hi[:, 0:256],
                                                  start=False, stop=True),
                         [r_zh, r_F1hi], [r_pp], sig=(gi == 1), partial=True)
            evac(Cv[:, :, cp * 2:cp * 2 + 2, :], pp[:, :].rearrange("p (g t k) -> p t g k", g=2, t=2, k=128), [r_pp], [r_CE],
                 partial=(cp > 0), simple=False)
        fmode = dbg.get("fft_mode", 9) if dbg else 9
        if fmode < 2:
            return
        (t1, r1), (t2, r2), (t3, r3), (t4, r4) = tpb
        bs = t1.shape[1]
        for blk in range(32 // bs):
            cs = slice(blk * bs, blk * bs + bs)
            cr, ci = Cv[:, 0, cs, :], Cv[:, 1, cs, :]
            P.op("dve", lambda e: e.tensor_mul(out=t1[:], in0=cr, in1=tw8[:, 0, 0:bs, :]), [r_CE, r_cst], [r1], partial=False)
            P.op("dve", lambda e: e.tensor_mul(out=t2[:], in0=ci, in1=tw8[:, 1, 0:bs, :]), [r_CE, r_cst], [r2], partial=False)
            P.op("dve", lambda e: e.tensor_mul(out=t3[:], in0=cr, in1=tw8[:, 1, 0:bs, :]), [r_CE, r_cst], [r3], partial=False)
            P.op("dve", lambda e: e.tensor_mul(out=t4[:], in0=ci, in1=tw8[:, 0, 0:bs, :]), [r_CE, r_cst], [r4], partial=False)
            P.op("dve", lambda e: e.tensor_sub(out=cr, in0=t1[:], in1=t2[:]), [r1, r2], [r_CE], partial=True)
            P.op("dve", lambda e: e.tensor_add(out=ci, in0=t3[:], in1=t4[:]), [r3, r4], [r_CE], partial=True)
            for hf_ in range(bs // 4 if fmode >= 3 else 0):
                c0 = (blk * bs + hf_ * 4) * 128
                rre = CEf[:, c0:c0 + 512]
                rim = CEf[:, 4096 + c0:4096 + c0 + 512]
                for t, (la, lb) in enumerate(((0, 2), (1, 0))):
                    px, r_px = next_psf()
                    P.op("pe", lambda e: e.matmul(px[:, :], lhsT=F32m[:, la, :], rhs=rre, start=True, stop=False),
                         [r_cst, r_CE], [r_px], sig=False, partial=False)
                    P.op("pe", lambda e: e.matmul(px[:, :], lhsT=F32m[:, lb, :], rhs=rim, start=False, stop=True),
                         [r_cst, r_CE], [r_px], sig=True, partial=True)
                    x0 = (t * 32 + blk * bs + hf_ * 4) * 128
                    evac(X[:].rearrange("p t c k -> p (t c k)")[:, x0:x0 + 512], px[:, :],
                         [r_px], [r_X], partial=not (blk == 0 and hf_ == 0 and t == 0), simple=True)

    def fft_inv(Y, r_Y, CEf, r_CE, Tt, r_T, epilogue):
        E5 = CEf[:, 0:32 * 2 * 128].rearrange("p (n t g c) -> p n t g c", n=32, t=2, g=32, c=4)
        E4 = CEf[:, 0:32 * 2 * 128].rearrange("p (n t c) -> p n t c", n=32, t=2, c=128)
        for cp in range(16):
            pe_, r_pe = next_psf()
            for gi in range(2):
                cg = cp * 2 + gi
                P.op("pe", lambda e: e.matmul(pe_[:, gi * 256:(gi + 1) * 256], lhsT=Y[:, 0, cg, :], rhs=R12[:, 0, :], start=True, stop=False),
                     [r_Y, r_R12], [r_pe], sig=False, partial=(gi > 0))
                P.op("pe", lambda e: e.matmul(pe_[:, gi * 256:(gi + 1) * 256], lhsT=Y[:, 1, cg, :], rhs=R12[:, 1, :], start=False, stop=True),
                     [r_Y, r_R12], [r_pe], sig=(gi == 1), partial=True)
            pv = pe_[:, :].rearrange("p (g t n c) -> p t n g c", g=2, t=2, n=32, c=4)
            for t in range(2):
                evac(E5[:, :, t, cp * 2:cp * 2 + 2, :], pv[:, t], [r_pe], [r_CE], partial=not (cp == 0 and t == 0))
        for nb in range(8):
            py, r_py = next_psf()
            for ni in range(4):
                n1 = nb * 4 + ni
                P.op("pe", lambda e: e.matmul(py[:, ni * 128:(ni + 1) * 128], lhsT=Tt[:, n1, 0, :], rhs=E4[:, n1, 0, :], start=True, stop=False),
                     [r_T, r_CE], [r_py], sig=False, partial=(ni > 0))
                P.op("pe", lambda e: e.matmul(py[:, ni * 128:(ni + 1) * 128], lhsT=Tt[:, n1, 1, :], rhs=E4[:, n1, 1, :], start=False, stop=True),
                     [r_T, r_CE], [r_py], sig=(ni == 3), partial=True)
            epilogue(nb, py[:, :].rearrange("p (n c) -> p n c", n=4), r_py)

    def to_token_major(srcT, r_src, dst, r_dst, fftl=True):
        sv = srcT[:, :].rearrange("p (a b) -> p b a", b=32)
        for nb in range(4):
            pt, r_pt = next_psb()
            for ni in range(8):
                n1 = nb * 8 + ni
                P.op("pe", lambda e: e.transpose(out=pt[:, ni * 128:(ni + 1) * 128], in_=sv[:, n1, :], identity=ident[:]),
                     [r_src, r_ident], [r_pt], sig=(ni == 7), partial=(ni > 0))
            if fftl:
                evac(dst[:, :].rearrange("p (g n c) -> p g n c", g=32, n=32, c=4)[:, :, nb * 8:(nb + 1) * 8, :],
                     pt[:, :].rearrange("p (n g c) -> p g n c", n=8, g=32, c=4), [r_pt], [r_dst], partial=(nb > 0))
            else:
                evac(dst[:, :].rearrange("p (n c) -> p n c", n=32)[:, nb * 8:(nb + 1) * 8, :],
                     pt[:, :].rearrange("p (n c) -> p n c", n=8), [r_pt], [r_dst], partial=(nb > 0))

    if not (dbg and dbg.get("skip_filters")):
        with ExitStack() as ph:
            h3 = [[sb(f"h3_{l}{v}", [64, S], BF16, ph) for v in range(2)] for l in range(DEPTH)]
            w4b = [sb(f"w4b{l}", [64, 2048], BF16, ph) for l in range(DEPTH)]
            with ExitStack() as ph2:
                feat = [sb(f"feat{v}", [33, S], F32, ph2) for v in range(2)]
                hA, r_hA = sb("hA", [64, S], F32, ph2)
                hB, r_hB = sb("hB", [64, S], F32, ph2)
                w4f, r_w4f = sb("w4f", [64, 2048], F32, ph2)
                fw = [sb(f"fw{i}", [64, 64], F32, ph2) for i in range(3)]
                fbt, r_fbt = sb("fbt", [64, 4], F32, ph2)
                fsc, r_fsc = sb("fsc", [64, 4], F32, ph2)
                ty = [sb(f"ty{i}", [64, 512], F32, ph2) for i in range(2)]
                tki, r_tki = sb("tki", [64, 512], mybir.dt.int32, ph2)
                tkf, r_tkf = sb("tkf", [64, 512], F32, ph2)
                for v in range(2):
                    P.dma("sp", feat[v][0][:], C["featT"][v], [R_in], [feat[v][1]], partial=False)
                for l in range(DEPTH):
                    P.dma("sp", fw[0][0][0:33, :], I["f_w1"][l], [R_in], [fw[0][1]], partial=False)
                    P.dma("sp", fw[1][0][:], I["f_w2"][l], [R_in], [fw[1][1]], partial=False)
                    P.dma("sp", fw[2][0][:], I["f_w3"][l], [R_in], [fw[2][1]], partial=False)
                    P.dma("sp", w4f[:], I["f_w4"][l], [R_in], [r_w4f], partial=False)
                    P.dma("sp", fbt[:], I["f_b"][l], [R_in], [r_fbt], partial=False)
                    P.op("pool", lambda e: e.tensor_copy(out=w4b[l][0][:], in_=w4f[:]), [r_w4f], [w4b[l][1]], partial=False)
                    P.op("dve", lambda e: e.tensor_scalar_mul(out=fsc[:, 3:4], in0=fbt[:, 3:4], scalar1=1.0 / TWO_PI), [r_fbt], [r_fsc], partial=False)
                    P.op("dve", lambda e: e.tensor_scalar(out=fsc[:, 0:3], in0=fbt[:, 0:3], scalar1=fsc[:, 3:4], scalar2=8.0,
                                                          op0=ALU.mult, op1=ALU.add), [r_fbt, r_fsc], [r_fsc], partial=False)
                    for v in range(2):
                        src, r_src = feat[v]
                        kdim = 33
                        for layer_i in range(3):
                            last = (layer_i == 2)
                            dst, r_dst = (h3[l][v] if last else ((hA, r_hA) if layer_i == 0 else (hB, r_hB)))
                            wt_, r_wt_ = fw[layer_i]
                            for tb in range(8):
                                sl = slice(tb * 512, (tb + 1) * 512)
                                pp, r_pp = next_psf()
                                P.op("pe", lambda e: e.matmul(pp[0:64, :], lhsT=wt_[0:kdim, :], rhs=src[0:kdim, sl], start=True, stop=True),
                                     [r_wt_, r_src], [r_pp], partial=False)
                                tyt, r_ty = ty[tb % 2]
                                P.op("dve", lambda e: e.tensor_scalar(out=tyt[:], in0=pp[0:64, :], scalar1=fsc[:, 3:4],
                                                                      scalar2=fsc[:, layer_i:layer_i + 1], op0=ALU.mult, op1=ALU.add),
                                     [r_pp, r_fsc], [r_ty], partial=False)
                                P.op("dve", lambda e: e.tensor_copy(out=tki[:], in_=tyt[:]), [r_ty], [r_tki], partial=False)
                                P.op("dve", lambda e: e.tensor_copy(out=tkf[:], in_=tki[:]), [r_tki], [r_tkf], partial=False)
                                P.op("dve", lambda e: e.tensor_sub(out=tyt[:], in0=tyt[:], in1=tkf[:]), [r_ty, r_tkf], [r_ty], partial=False)
                                P.op("dve", lambda e: e.tensor_single_scalar(out=tkf[:], in_=tyt[:], scalar=0.5, op=ALU.is_ge),
                                     [r_ty], [r_tkf], partial=False)
                                P.op("dve", lambda e: e.tensor_sub(out=tyt[:], in0=tyt[:], in1=tkf[:]), [r_ty, r_tkf], [r_ty], partial=False)
                                P.op("act", lambda e: e.activation(out=dst[:, sl], in_=tyt[:], func=AF.Sin, scale=TWO_PI),
                                     [r_ty], [r_dst], partial=(tb > 0))
                            src, r_src = dst, r_dst
                            kdim = 64
                P.barrier()
            trb = [sb(f"trb{v}", [128, S], F32, ph) for v in range(2)]
            dec = [sb(f"dec{v}", [128, S], F32, ph) for v in range(2)]
            ndl, r_ndl = sb("ndl", [128, 4], F32, ph)
            fT, r_fT = sb("fT", [128, S], BF16, ph)
            ftm = [sb(f"ftm{v}", [128, S], BF16, ph) for v in range(2)]
            CEf, r_CE = sb("CEf", [128, 8192], BF16, ph)
            Xb, r_X = sb("Xb", [128, 2, 32, 128], BF16, ph)
            tpf = [sb(f"tpf{i}", [128, 4, 128], F32, ph) for i in range(4)]
            P.dma("sp", ndl[:], C["ndelta"][:, :], [R_in], [r_ndl], partial=False)
            for v in range(2):
                P.dma("sp", trb[v][0][:], bc(C["trow"], v * S, S), [R_in], [trb[v][1]], partial=False)
            flist = [(c, l, o) for c in range(4) for l in range(DEPTH) for o in range(2)]
            if dbg and "filt_list" in dbg:
                flist = dbg["filt_list"]
            lastc = None
            for (c, l, o) in flist:
                if c != lastc:
                    for v in range(2):
                        P.op("act", lambda e: e.activation(out=dec[v][0][:], in_=trb[v][0][:], func=AF.Exp, scale=ndl[:, c:c + 1]),
                             [trb[v][1], r_ndl], [dec[v][1]], partial=False)
                    lastc = c
                for v in range(2):
                    col0 = (o * 2 + v) * 512 + c * 128
                    for tb in range(8):
                        sl = slice(tb * 512, (tb + 1) * 512)
                        pp, r_pp = next_psf()
                        P.op("pe", lambda e: e.matmul(pp[:, :], lhsT=w4b[l][0][:, col0:col0 + 128], rhs=h3[l][v][0][:, sl], start=True, stop=True),
                             [w4b[l][1], h3[l][v][1]], [r_pp], partial=False)
                        P.op("dve", lambda e: e.tensor_mul(out=fT[:, sl], in0=pp[:, :], in1=dec[v][0][:, sl]),
                             [r_pp, dec[v][1]], [r_fT], partial=(tb > 0))
                    if v == 1:
                        P.op("dve", lambda e: e.memset(fT[:, 0:1], 0.0), [], [r_fT], partial=True)
                    to_token_major(fT, r_fT, ftm[v][0], ftm[v][1])
                fft_fwd(ftm[0][0], ftm[0][1], ftm[1][0], ftm[1][1], CEf, r_CE, Xb, r_X, tpf)
                P.dma("sp", H_d[l, o, c], Xb[:], [r_X], [R_H[l][o][c]], partial=False, sem_of=r_X)
            P.barrier()
    if stop_after == "filt":
        layers = 0

    x_src = I["x"]
    R_xsrc = R_in
    for l in range(layers):
        x_dst, R_xdst = (xmid_d, R_xmid) if l < DEPTH - 1 else (out, R_out)
        lay = ExitStack()
        hT, r_hT = sb(f"hT", [128, 8, S], BF16, lay)
        with ExitStack() as ph:
            xt = [sb(f"xt{i}", [128, D], F32, ph) for i in range(2)]
            xn = [sb(f"xn{i}", [128, D], BF16, ph) for i in range(2)]
            st = [sb(f"st{i}", [128, 12], F32, ph) for i in range(2)]
            mv = [sb(f"mv{i}", [128, 2], F32, ph) for i in range(2)]
            for t in range(32):
                xtt, r_xt = xt[t % 2]
                xnn, r_xn = xn[t % 2]
                stt, r_st = st[t % 2]
                mvv, r_mv = mv[t % 2]
                P.dma(next_q(), xtt[:], x_src[t * 128:(t + 1) * 128, :], [R_xsrc], [r_xt], partial=False)
                P.op("dve", lambda e: e.bn_stats(out=stt[:, 0:6], in_=xtt[:, 0:512]), [r_xt], [r_st], partial=False)
                P.op("dve", lambda e: e.bn_stats(out=stt[:, 6:12], in_=xtt[:, 512:1024]), [r_xt], [r_st], partial=True)
                P.op("dve", lambda e: e.bn_aggr(out=mvv[:], in_=stt[:]), [r_st], [r_mv], partial=False)
                P.op("act", lambda e: e.activation(out=mvv[:, 1:2], in_=mvv[:, 1:2], func=AF.Sqrt, bias=epsT[:], scale=1.0),
                     [r_mv, r_eps], [r_mv], partial=False)
                P.op("dve", lambda e: e.reciprocal(out=mvv[:, 1:2], in_=mvv[:, 1:2]), [r_mv], [r_mv], partial=False)
                P.op("dve", lambda e: e.tensor_scalar(out=xnn[:], in0=xtt[:], scalar1=mvv[:, 0:1], scalar2=mvv[:, 1:2],
                                                      op0=ALU.subtract, op1=ALU.mult), [r_xt, r_mv], [r_xn], partial=False)
                pt, r_pt = next_psb()
                for kc in range(8):
                    P.op("pe", lambda e, kc=kc: e.transpose(out=pt[:, kc * 128:(kc + 1) * 128], in_=xnn[:, kc * 128:(kc + 1) * 128],
                                                            identity=ident[:]),
                         [r_xn, r_ident], [r_pt], sig=(kc == 7), partial=(kc > 0))
                for kc in range(8):
                    P.op("act", lambda e, kc=kc: e.activation(out=hT[:, kc, t * 128:(t + 1) * 128], in_=pt[:, kc * 128:(kc + 1) * 128],
                                                              func=AF.Identity, scale=sc1[:, l, kc:kc + 1], bias=modcol[:, l, kc:kc + 1]),
                         [r_pt, r_sc1, r_modcol], [r_hT], partial=True)
            P.barrier()
        if stop_after == "ln":
            lay.close()
            break

        with ExitStack() as ph:
            ws = [sb(f"ws{i}", [128, 8, 128], F32, ph) for i in range(3)]
            wb = [sb(f"wb{i}", [128, 8, 128], BF16, ph) for i in range(3)]
            ob = [sb(f"ob{i}", [128, S], BF16, ph) for i in range(2)]
            nfm = 0
            chunks = [j for j in range(72) if not (24 <= j < 36)]
            if dbg and "proj_chunks" in dbg:
                chunks = dbg["proj_chunks"]
            for j in chunks:
                wst, r_ws = ws[nfm % 3]
                wbt, r_wb = wb[nfm % 3]
                obt, r_ob = ob[nfm % 2]
                nfm += 1
                P.dma(next_q(), wst[:], I["w_in"][l, :, :, j * 128:(j + 1) * 128], [R_in], [r_ws], partial=False)
                P.op("pool", lambda e: e.tensor_copy(out=wbt[:], in_=wst[:]), [r_ws], [r_wb], partial=False)
                if 36 <= j < 40 or 52 <= j < 56:
                    fn = AF.Silu
                elif j >= 56:
                    fn = AF.Sigmoid
                else:
                    fn = AF.Identity
                for tb in range(8):
                    pp, r_pp = next_psf()
                    for kc in range(8):
                        P.op("pe", lambda e, kc=kc: e.matmul(pp[:, :], lhsT=wbt[:, kc, :], rhs=hT[:, kc, tb * 512:(tb + 1) * 512],
                                                             start=(kc == 0), stop=(kc == 7)),
                             [r_wb, r_hT], [r_pp], sig=(kc == 7), partial=(kc > 0))
                    P.op("act", lambda e: e.activation(out=obt[:, tb * 512:(tb + 1) * 512], in_=pp[:, :], func=fn,
                                                       bias=b_in_c[:, l, j:j + 1], scale=1.0),
                         [r_pp, r_binc], [r_ob], partial=(tb > 0))
                P.dma(next_q(), proj_d[j, :, :], obt[:], [r_ob], [R_proj[j]], partial=False, sem_of=r_ob)
            wvs = [sb(f"wvs{i}", [128, 8, 512], F32, ph) for i in range(1)]
            wvb = [sb(f"wvb{i}", [128, 8, 512], BF16, ph) for i in range(2)]
            vo = [sb(f"vo{i}", [128, 4, 512], BF16, ph) for i in range(2)]
            nv = 0
            groups = range(3) if not (dbg and "v_groups" in dbg) else dbg["v_groups"]
            for g in groups:
                d = (1, 4, 16)[g]
                Lg = S // d
                wst, r_ws = wvs[0]
                wbt, r_wb = wvb[g % 2]
                c0 = 3072 + g * 512
                P.dma("sp", wst[:, 0:4, :], I["w_in"][l, :, 0:4, c0:c0 + 512], [R_in], [r_ws], partial=False)
                P.dma("pool", wst[:, 4:8, :], I["w_in"][l, :, 4:8, c0:c0 + 512], [R_in], [r_ws])
                P.op("pool", lambda e: e.tensor_copy(out=wbt[:], in_=wst[:]), [r_ws], [r_wb], partial=False)
                for tq in range(8):
                    vot, r_vo = vo[nv % 2]
                    nv += 1
                    for t4 in range(4):
                        ti = tq * 4 + t4
                        r_, u = divmod(ti, Lg // 128)
                        pp, r_pp = next_psf()
                        for kc in range(8):
                            lt = hT[:, kc, :].rearrange("p (i d) -> p d i", d=d)[:, r_, u * 128:(u + 1) * 128]
                            P.op("pe", lambda e, kc=kc, lt=lt: e.matmul(pp[:, :], lhsT=lt, rhs=wbt[:, kc, :], start=(kc == 0), stop=False),
                                 [r_wb, r_hT], [r_pp], sig=False, partial=(kc > 0))
                        P.op("pe", lambda e: e.matmul(pp[:, :], lhsT=ones2[0:1, :], rhs=bin2[0:1, l, g * 512:(g + 1) * 512], start=False, stop=False),
                             [r_ones2, r_bin2], [r_pp], sig=False, partial=True)
                        P.op("pe", lambda e: e.matmul(pp[:, :], lhsT=ones2[0:1, :], rhs=binl[0:1, l, g * 512:(g + 1) * 512], start=False, stop=True),
                             [r_ones2, r_binl], [r_pp], sig=True, partial=True)
                        P.op("dve", lambda e, t4=t4: e.tensor_copy(out=vot[:, t4, :], in_=pp[:, :]), [r_pp], [r_vo], partial=(t4 > 0))
                    qn = next_q()
                    for j4 in range(4):
                        P.dma(qn, vtm_d[g, j4, :, tq * 4:(tq + 1) * 4, :], vot[:, :, j4 * 128:(j4 + 1) * 128], [r_vo], [R_vtm[g]], sem_of=r_vo)
            P.barrier()
        if stop_after == "proj":
            lay.close()
            break
        lay.close()
        with ExitStack() as ph:
            Oacc, r_O = sb(f"Oacc", [128, 2, S], F32, ph)
            qTs = [sb(f"qT{i}", [128, 2, S], BF16, ph) for i in range(2)]
            kTs = [sb(f"kT{i}", [128, S], BF16, ph) for i in range(2)]
            vts = [sb(f"vt{i}", [128, 32, 2, 128], BF16, ph) for i in range(2)]
            abs_ = [sb(f"ab{i}", [128, 3, 256], BF16, ph) for i in range(2)]
            pTs = [sb(f"pT{i}", [128, 256], BF16, ph) for i in range(8)]
            gs, r_gs = sb(f"gs", [128, S], BF16, ph)
            vs, r_vs = sb(f"vs", [128, 32, 128], BF16, ph)
            yb, r_yb = sb(f"yb", [128, S], BF16, ph)
            rz, r_rz = sb(f"rz", [128, 512], F32, ph)
            tm, r_tm = sb(f"tm", [128, 512], F32, ph)
            for i in range(2):
                P.op("pool", lambda e, i=i: e.memset(vts[i][0][:], 0.0), [], [vts[i][1]], partial=False)
                P.op("pool", lambda e, i=i: e.memset(qTs[i][0][:], 0.0), [], [qTs[i][1]], partial=False)
            npT = 0
            nbuf = 0
            jlist = range(4) if not (dbg and "att_j" in dbg) else dbg["att_j"]
            for j in jlist:
                for g in (range(3) if not (dbg and "att_g" in dbg) else dbg["att_g"]):
                    d = (1, 4, 16)[g]
                    ntl = (S // d) // 128
                    qT, r_q = qTs[nbuf % 2]
                    kT, r_k = kTs[nbuf % 2]
                    vt, r_v = vts[nbuf % 2]
                    ab, r_ab = abs_[nbuf % 2]
                    nbuf += 1
                    P.dma("sp", qT[0:64, 0, :], proj_d[g * 4 + j, 0:64, :], [R_proj[g * 4 + j]], [r_q], partial=False)
                    P.dma("sp", qT[64:128, 1, :], proj_d[g * 4 + j, 64:128, :], [R_proj[g * 4 + j]], [r_q])
                    P.dma("sp", kT[:], proj_d[12 + g * 4 + j, :, :], [R_proj[12 + g * 4 + j]], [r_k], partial=False)
                    P.dma("sp", vs[:], vtm_d[g, j, :, :, :], [R_vtm[g]], [r_vs], partial=False)
                    P.op("pool", lambda e: e.tensor_copy(out=vt[:, :, 0, 0:64], in_=vs[:, :, 0:64]), [r_vs], [r_v], partial=False)
                    P.op("pool", lambda e: e.tensor_copy(out=vt[:, :, 1, 64:128], in_=vs[:, :, 64:128]), [r_vs], [r_v], partial=True)
                    P.dma("sp", ab[:], C["abias"][:, g * 4 + j, :, :, :].rearrange("p k h q -> p k (h q)"), [R_in], [r_ab], partial=False)
                    qv = [qT[:, hh, :].rearrange("p (i d) -> p d i", d=d) for hh in range(2)]
                    kv = [kT[:, :].rearrange("p (i d) -> p d i", d=d) for hh in range(2)]
                    accv = Oacc[:].rearrange("p c (i d) -> p c d i", d=d)
                    tiles = [(r_, u) for r_ in range(d) for u in range(ntl)]

                    def stageA(r_, u):
                        kts = [kt for kt in range(3) if 0 <= u + kt - 1 < ntl]
                        outl = []
                        for kt in kts:
                            ku = u + kt - 1
                            rr["sc"] = (rr.get("sc", 0) + 1) % 4
                            ps, r_ps = psf[rr["sc"]]
                            for hh in range(2):
                                P.op("pe", lambda e, hh=hh: e.matmul(ps[:, hh * 128:(hh + 1) * 128], lhsT=ident[:],
                                                                     rhs=ab[:, kt, hh * 128:(hh + 1) * 128], start=True, stop=False),
                                     [r_ident, r_ab], [r_ps], sig=False, partial=(hh > 0))
                                P.op("pe", lambda e, hh=hh: e.matmul(
                                    ps[:, hh * 128:(hh + 1) * 128], lhsT=kv[hh][:, r_, ku * 128:(ku + 1) * 128],
                                    rhs=qv[hh][:, r_, u * 128:(u + 1) * 128], start=False, stop=True),
                                    [r_k, r_q], [r_ps], sig=(hh == 1), partial=True)
                            rr["pt"] = (rr.get("pt", 0) + 1) % 8
                            pT, r_pT = pTs[rr["pt"]]
                            P.op("act", lambda e: e.activation(out=pT[:], in_=ps[:, 0:256], func=AF.Exp, scale=0.125),
                                 [r_ps], [r_pT], partial=False)
                            outl.append((r_ * ntl + ku, pT, r_pT))
                        return outl

                    def stageB(r_, u, pl):
                        rr["po"] = (rr.get("po", 0) + 1) % 2
                        po, r_po = psf[4 + rr["po"]]
                        n = len(pl) * 2
                        for part in range(2):
                            i = 0
                            for (ti, pT, r_pT) in pl:
                                for hh in range(2):
                                    lt = vt[:, ti, hh, :] if part == 0 else onesel[:, hh, :]
                                    P.op("pe", lambda e, hh=hh, lt=lt, i=i: e.matmul(
                                        po[:, part * 128:(part + 1) * 128], lhsT=lt, rhs=pT[:, hh * 128:(hh + 1) * 128],
                                        start=(i == 0), stop=(i == n - 1)),
                                        [r_v, r_onesel, r_pT], [r_po], sig=(part == 1 and i == n - 1), partial=not (part == 0 and i == 0))
                                    i += 1
                        av = accv[:, :, r_, u * 128:(u + 1) * 128]
                        pv = po[:, 0:256].rearrange("p (c q) -> p c q", c=2)
                        if g == 0:
                            P.op("dve", lambda e: e.tensor_copy(out=av, in_=pv), [r_po], [r_O], partial=True)
                        else:
                            P.op("dve", lambda e: e.tensor_add(out=av, in0=pv, in1=av), [r_po, r_O], [r_O], partial=True)

                    amode = dbg.get("att_mode", 3) if dbg else 3
                    prev = None
                    for (r_, u) in tiles:
                        cur = (r_, u, stageA(r_, u))
                        if prev is not None and amode >= 2:
                            stageB(*prev)
                        prev = cur
                    if amode >= 2:
                        stageB(*prev)
                P.dma("sp", gs[:], proj_d[36 + j, :, :], [R_proj[36 + j]], [r_gs], partial=False)
                for tb in range(8):
                    sl = slice(tb * 512, (tb + 1) * 512)
                    P.op("dve", lambda e: e.reciprocal(out=rz[:], in_=Oacc[:, 1, sl]), [r_O], [r_rz], partial=False)
                    P.op("dve", lambda e: e.tensor_mul(out=tm[:], in0=Oacc[:, 0, sl], in1=rz[:]), [r_O, r_rz], [r_tm], partial=False)
                    P.op("pool", lambda e: e.tensor_mul(out=yb[:, sl], in0=tm[:], in1=gs[:, sl]), [r_tm, r_gs], [r_yb], partial=(tb > 0))
                P.dma("sp", yattn_d[j, :, :], yb[:], [r_yb], [R_yattn[j]], partial=False, sem_of=r_yb)
            P.barrier()
        if stop_after == "att":
            break
        with ExitStack() as ph:
            pb, r_pb = sb("pb", [128, S], BF16, ph)
            uf, r_uf = sb("uf", [128, 2048], F32, ph)
            ub, r_ub = sb("ub", [128, S], BF16, ph)
            zt, r_zt = sb("zt", [128, S], BF16, ph)
            xg, r_xg = sb("xg", [128, S], BF16, ph)
            CEf, r_CE = sb("CEf", [128, 8192], BF16, ph)
            Xb, r_X = sb("Xb", [128, 2, 32, 128], BF16, ph)
            Tt, r_T = sb("Tt", [128, 32, 2, 128], BF16, ph)
            Hb = [sb(f"Hb{i}", [128, 2, 8, 128], BF16, ph) for i in range(2)]
            tp = [sb(f"tp{i}", [128, 8, 128], F32, ph) for i in range(4)]
            fb1, r_fb1 = sb("fb1", [128, 2, 128], F32, ph)
            fb4, r_fb4 = sb("fb4", [128, 2, 32, 4, 4], F32, ph)
            e1, r_e1 = sb("e1", [128, 32, 4, 4], F32, ph)
            e2, r_e2 = sb("e2", [128, 32, 4, 4], F32, ph)
            gsT, r_gsT = sb("gsT", [128, S], BF16, ph)
            yT, r_yT = sb("yT", [128, S], BF16, ph)
            P.dma("sp", Tt[:], C["T"][:, :, :, :], [R_in], [r_T], partial=False)

            def conv3(chunk, widx, dst, r_dst, fftl):
                P.dma("sp", pb[:], proj_d[chunk, :, :], [R_proj[chunk]], [r_pb], partial=False)
                w0 = convw[:, l, 0, widx:widx + 1]
                w1 = convw[:, l, 1, widx:widx + 1]
                w2 = convw[:, l, 2, widx:widx + 1]
                cb = convb[:, l, widx:widx + 1]
                for hb_ in range(2):
                    t0 = hb_ * 2048
                    P.op("act", lambda e: e.activation(out=uf[:, :], in_=pb[:, t0:t0 + 2048], func=AF.Identity, scale=w1, bias=cb),
                         [r_pb, r_convw, r_convb], [r_uf], partial=False)
                    a = 1 if hb_ == 0 else 0
                    P.op("dve", lambda e: e.scalar_tensor_tensor(out=uf[:, a:2048], in0=pb[:, t0 + a - 1:t0 + 2047], scalar=w0,
                                                                 in1=uf[:, a:2048], op0=ALU.mult, op1=ALU.add),
                         [r_pb, r_convw, r_uf], [r_uf], partial=False)
                    b_ = 2047 if hb_ == 1 else 2048
                    P.op("dve", lambda e: e.scalar_tensor_tensor(out=ub[:, t0:t0 + b_], in0=pb[:, t0 + 1:t0 + b_ + 1], scalar=w2,
                                                                 in1=uf[:, 0:b_], op0=ALU.mult, op1=ALU.add),
                         [r_pb, r_convw, r_uf], [r_ub], partial=(hb_ > 0))
                    if hb_ == 1:
                        P.op("dve", lambda e: e.tensor_copy(out=ub[:, S - 1:S], in_=uf[:, 2047:2048]), [r_uf], [r_ub], partial=True)
                to_token_major(ub, r_ub, dst, r_dst, fftl)

            def pointwise(o, c):
                for blk in range(4):
                    hb, r_hb = Hb[blk % 2]
                    P.dma("sp", hb[:], H_d[l, o, c, :, :, blk * 8:(blk + 1) * 8, :], [R_H[l][o][c]], [r_hb], partial=False)
                    xr, xi = Xb[:, 0, blk * 8:(blk + 1) * 8, :], Xb[:, 1, blk * 8:(blk + 1) * 8, :]
                    hr, hi = hb[:, 0, :, :], hb[:, 1, :, :]
                    (t1, r1), (t2, r2), (t3, r3), (t4, r4) = tp
                    P.op("dve", lambda e: e.tensor_mul(out=t1[:], in0=xr, in1=hr), [r_X, r_hb], [r1], partial=False)
                    P.op("dve", lambda e: e.tensor_mul(out=t2[:], in0=xi, in1=hi), [r_X, r_hb], [r2], partial=False)
                    P.op("dve", lambda e: e.tensor_mul(out=t3[:], in0=xr, in1=hi), [r_X, r_hb], [r3], partial=False)
                    P.op("dve", lambda e: e.tensor_mul(out=t4[:], in0=xi, in1=hr), [r_X, r_hb], [r4], partial=False)
                    P.op("dve", lambda e: e.tensor_sub(out=xr, in0=t1[:], in1=t2[:]), [r1, r2], [r_X], partial=True)
                    P.op("dve", lambda e: e.tensor_add(out=xi, in0=t3[:], in1=t4[:]), [r3, r4], [r_X], partial=True)

            clist = range(4) if not (dbg and "hy_c" in dbg) else dbg["hy_c"]
            for c in clist:
                for o in range(2):
                    P.dma("sp", fb1[:, o, :], bc(I["f_bias_r"], (l * 2 + o) * 512 + c * 128, 128), [R_in], [r_fb1], partial=(o > 0))
                for o in range(2):
                    for i4 in range(4):
                        P.op("dve", lambda e: e.tensor_copy(out=fb4[:, o, :, i4, :], in_=fb1[:, o, :].rearrange("p (g c) -> p g c", c=4)),
                             [r_fb1], [r_fb4], partial=not (o == 0 and i4 == 0))
                conv3(40 + c, c, zt, r_zt, True)
                conv3(44 + c, 4 + c, xg, r_xg, False)
                hmode = dbg.get("hy_mode", 9) if dbg else 9
                for o in range(2):
                    if hmode < 2:
                        break
                    fft_fwd(zt, r_zt, None, None, CEf, r_CE, Xb, r_X, tp)
                    if hmode < 3:
                        break
                    pointwise(o, c)
                    if hmode < 4:
                        break

                    def epi(nb, pyv, r_py, o=o):
                        zs = zt[:, :].rearrange("p (g n c) -> p g n c", g=32, n=32, c=4)[:, :, nb * 4:(nb + 1) * 4, :]
                        xs = xg[:, :].rearrange("p (n g c) -> p g n c", n=32, g=32, c=4)[:, :, nb * 4:(nb + 1) * 4, :]
                        pyf = pyv.rearrange("p n (g c) -> p g n c", c=4)
                        P.op("dve", lambda e: e.tensor_mul(out=e1[:], in0=zs, in1=fb4[:, o, :, :, :]), [r_zt, r_fb4], [r_e1], partial=False)
                        P.op("dve", lambda e: e.tensor_add(out=e2[:], in0=pyf, in1=e1[:]), [r_py, r_e1], [r_e2], partial=False)
                        if o == 0:
                            P.op("dve", lambda e: e.tensor_mul(out=zs, in0=e2[:], in1=xs), [r_e2, r_xg], [r_zt], partial=True)
                        else:
                            P.op("dve", lambda e: e.tensor_mul(out=xs, in0=e2[:], in1=xs), [r_e2, r_xg], [r_xg], partial=True)

                    fft_inv(Xb, r_X, CEf, r_CE, Tt, r_T, epi)
                    if o == 0:
                        conv3(48 + c, 8 + c, xg, r_xg, False)
                P.dma("sp", gsT[:], proj_d[52 + c, :, :], [R_proj[52 + c]], [r_gsT], partial=False)
                yv = yT[:, :].rearrange("p (a b) -> p b a", b=32)
                gv = gsT[:, :].rearrange("p (a b) -> p b a", b=32)
                for nb in range(4):
                    pt, r_pt = next_psb()
                    for ni in range(8):
                        n1 = nb * 8 + ni
                        P.op("pe", lambda e: e.transpose(out=pt[:, ni * 128:(ni + 1) * 128], in_=xg[:, n1 * 128:(n1 + 1) * 128], identity=ident[:]),
                             [r_xg, r_ident], [r_pt], sig=(ni == 7), partial=(ni > 0))
                    P.op("dve", lambda e: e.tensor_mul(out=yv[:, nb * 8:(nb + 1) * 8, :], in0=pt[:, :].rearrange("p (n c) -> p n c", n=8),
                                                       in1=gv[:, nb * 8:(nb + 1) * 8, :]), [r_pt, r_gsT], [r_yT], partial=(nb > 0))
                P.dma("sp", yhy_d[c, :, :], yT[:], [r_yT], [R_yhy[c]], partial=False, sem_of=r_yT)
            P.barrier()
        if stop_after == "hy":
            break
        with ExitStack() as ph:
            wpa, r_wpa = sb("wpa", [128, 4, D], BF16, ph)
            wph, r_wph = sb("wph", [128, 4, D], BF16, ph)
            wo, r_wo = sb("wo", [128, 8, D], BF16, ph)
            rowb, r_rowb = sb("rowb", [128, 3, D], F32, ph)
            with ExitStack() as ph2:
                wst, r_wst = sb("wstg", [128, 8, D], F32, ph2)
                P.dma("sp", wst[:, 0:4, :], I["w_pa"][l], [R_in], [r_wst], partial=False)
                P.op("pool", lambda e: e.tensor_copy(out=wpa[:], in_=wst[:, 0:4, :]), [r_wst], [r_wpa], partial=False)
                P.dma("sp", wst[:, 4:8, :], I["w_ph"][l], [R_in], [r_wst], partial=False)
                P.op("pool", lambda e: e.tensor_copy(out=wph[:], in_=wst[:, 4:8, :]), [r_wst], [r_wph], partial=False)
                P.dma("sp", wst[:, :, :], I["w_out"][l], [R_in], [r_wst], partial=False)
                for kc in range(8):
                    P.op("pool" if kc % 2 else "dve", lambda e: e.tensor_mul(out=wo[:, kc, :], in0=wst[:, kc, :], in1=gate_b[:, l, :]),
                         [r_wst, r_gate], [r_wo], partial=(kc > 0))
                P.barrier()
            P.dma("sp", rowb[:, 0, :], bc(I["b_out_r"], l * D, D), [R_in], [r_rowb], partial=False)
            P.dma("sp", rowb[:, 1, :], bc(I["ln_g_r"], l * D, D), [R_in], [r_rowb])
            P.dma("sp", rowb[:, 2, :], bc(I["ln_b_r"], l * D, D), [R_in], [r_rowb])
            gb, r_gb = sb("gb", [128, D], F32, ph)
            P.op("dve", lambda e: e.tensor_mul(out=gb[:], in0=rowb[:, 0, :], in1=gate_b[:, l, :]), [r_rowb, r_gate], [r_gb], partial=False)
            nb_, r_nb = sb("nbias", [128, 1], F32, ph)
            ya = [sb(f"ya{i}", [128, 4, 512], BF16, ph) for i in range(2)]
            yh = [sb(f"yh{i}", [128, 4, 512], BF16, ph) for i in range(2)]
            ga = [sb(f"ga{i}", [128, 16, 512], BF16, ph) for i in range(2)]
            mT, r_mT = sb("mT", [128, 8, 512], BF16, ph)
            m1, r_m1 = sb("m1", [128, 512], F32, ph)
            m2, r_m2 = sb("m2", [128, 512], F32, ph)
            xr_ = [sb(f"xr{i}", [128, D], F32, ph) for i in range(2)]
            rs_ = [sb(f"rs{i}", [128, D], F32, ph) for i in range(2)]
            o1, r_o1 = sb("o1", [128, 512], F32, ph)
            stt, r_st = sb("mst", [128, 12], F32, ph)
            mvv, r_mv = sb("mmv", [128, 2], F32, ph)
            xo = [sb(f"xo{i}", [128, D], F32, ph) for i in range(2)]
            tbl = range(8) if not (dbg and "merge_tb" in dbg) else dbg["merge_tb"]
            nt = 0
            for tb in tbl:
                sl = slice(tb * 512, (tb + 1) * 512)
                yat, r_ya = ya[tb % 2]
                yht, r_yh = yh[tb % 2]
                gat, r_ga = ga[tb % 2]
                P.dma("sp", yat[:], yattn_d[:, :, sl].rearrange("c p t -> p c t"), R_yattn, [r_ya], partial=False)
                P.dma("sp", yht[:], yhy_d[:, :, sl].rearrange("c p t -> p c t"), R_yhy, [r_yh], partial=False)
                P.dma("sp", gat[:], proj_d[56:72, :, sl].rearrange("c p t -> p c t"), R_proj[56:72], [r_ga], partial=False)
                for fc in range(8):
                    pa, r_pa = next_psf()
                    pq, r_pq = next_psf()
                    for kc in range(4):
                        P.op("pe", lambda e: e.matmul(pa[:, :], lhsT=wpa[:, kc, fc * 128:(fc + 1) * 128], rhs=yat[:, kc, :],
                                                      start=(kc == 0), stop=(kc == 3)), [r_wpa, r_ya], [r_pa], sig=(kc == 3), partial=(kc > 0))
                    for kc in range(4):
                        P.op("pe", lambda e: e.matmul(pq[:, :], lhsT=wph[:, kc, fc * 128:(fc + 1) * 128], rhs=yht[:, kc, :],
                                                      start=(kc == 0), stop=(kc == 3)), [r_wph, r_yh], [r_pq], sig=(kc == 3), partial=(kc > 0))
                    P.op("dve", lambda e: e.tensor_mul(out=m1[:], in0=pa[:, :], in1=gat[:, fc, :]), [r_pa, r_ga], [r_m1], partial=False)
                    P.op("dve", lambda e: e.tensor_mul(out=m2[:], in0=pq[:, :], in1=gat[:, 8 + fc, :]), [r_pq, r_ga], [r_m2], partial=False)
                    P.op("pool", lambda e: e.tensor_add(out=mT[:, fc, :], in0=m1[:], in1=m2[:]), [r_m1, r_m2], [r_mT], partial=(fc > 0))
                for tt in range(4):
                    row0 = tb * 512 + tt * 128
                    xrt, r_xr = xr_[nt % 2]
                    rst, r_rs = rs_[nt % 2]
                    xot, r_xo = xo[nt % 2]
                    nt += 1
                    P.dma("sp", xrt[:], x_src[row0:row0 + 128, :], [R_xsrc], [r_xr], partial=False)
                    P.op("dve", lambda e: e.scalar_tensor_tensor(out=xrt[:], in0=xrt[:], scalar=ALPHA, in1=gb[:], op0=ALU.mult, op1=ALU.add),
                         [r_xr, r_gb], [r_xr], partial=False)
                    for hf_ in range(2):
                        hs = slice(hf_ * 512, (hf_ + 1) * 512)
                        po_, r_po = next_psf()
                        for kc in range(8):
                            P.op("pe", lambda e: e.matmul(po_[:, :], lhsT=mT[:, kc, tt * 128:(tt + 1) * 128], rhs=wo[:, kc, hs],
                                                          start=(kc == 0), stop=(kc == 7)), [r_mT, r_wo], [r_po], sig=(kc == 7), partial=(kc > 0))
                        P.op("dve", lambda e: e.tensor_add(out=rst[:, hs], in0=po_[:, :], in1=xrt[:, hs]), [r_po, r_xr], [r_rs], partial=(hf_ > 0))
                    P.op("dve", lambda e: e.bn_stats(out=stt[:, 0:6], in_=rst[:, 0:512]), [r_rs], [r_st], partial=False)
                    P.op("dve", lambda e: e.bn_stats(out=stt[:, 6:12], in_=rst[:, 512:1024]), [r_rs], [r_st], partial=True)
                    P.op("dve", lambda e: e.bn_aggr(out=mvv[:], in_=stt[:]), [r_st], [r_mv], partial=False)
                    P.op("act", lambda e: e.activation(out=mvv[:, 1:2], in_=mvv[:, 1:2], func=AF.Sqrt, bias=epsT[:], scale=1.0),
                         [r_mv, r_eps], [r_mv], partial=False)
                    P.op("dve", lambda e: e.reciprocal(out=mvv[:, 1:2], in_=mvv[:, 1:2]), [r_mv], [r_mv], partial=False)
                    P.op("dve", lambda e: e.tensor_scalar(out=nb_[:], in0=mvv[:, 0:1], scalar1=-1.0, scalar2=mvv[:, 1:2],
                                                          op0=ALU.mult, op1=ALU.mult), [r_mv], [r_nb], partial=False)
                    P.op("act", lambda e: e.activation(out=rst[:], in_=rst[:], func=AF.Identity, scale=mvv[:, 1:2], bias=nb_[:]),
                         [r_rs, r_mv, r_nb], [r_rs], partial=False)
                    P.op("dve", lambda e: e.tensor_mul(out=rst[:], in0=rst[:], in1=rowb[:, 1, :]), [r_rs, r_rowb], [r_rs], partial=False)
                    P.op("pool", lambda e: e.tensor_add(out=xot[:], in0=rst[:], in1=rowb[:, 2, :]), [r_rs, r_rowb], [r_xo], partial=False)
                    P.dma("sp", x_dst[row0:row0 + 128, :], xot[:], [r_xo], [R_xdst], sem_of=r_xo)
            P.barrier()
        x_src, R_xsrc = x_dst, R_xdst
    P.barrier()
    stack.close()
    return nc, dbg_out


_CACHE = {}


def kernel(**inputs):
    inp = {k: np.asarray(v, dtype=np.float32) for k, v in inputs.items()}
    consts = _const_tables()
    if "nc" not in _CACHE:
        _CACHE["nc"] = build()[0]
    nc = _CACHE["nc"]
    in_maps = []
    for b in range(8):
        m = _layout_inputs(inp, b)
        for k, v in consts.items():
            m["c_" + k] = v
        in_maps.append(m)
    res = run_bass_kernel_spmd(nc, in_maps, core_ids=list(range(8)))
    return np.stack([np.asarray(r["out"], dtype=np.float32) for r in res.results], axis=0)
```

```python
import math
from contextlib import ExitStack
import numpy as np
import ml_dtypes
import concourse.bass as bass
import concourse.mybir as mybir
from concourse.bass_utils import run_bass_kernel_spmd

F32 = mybir.dt.float32
BF16 = mybir.dt.bfloat16
AF = mybir.ActivationFunctionType
ALU = mybir.AluOpType
NPBF = ml_dtypes.bfloat16

S = 4096
D = 1024
NIN = 9216
DEPTH = 2
ALPHA = (2 * DEPTH) ** 0.25
EPS = 1e-5
NFFT = 8192
TWO_PI = 2.0 * math.pi


class Res:
    __slots__ = ("name", "writers", "readers", "prev", "sem", "dcount")

    def __init__(self, name, sem=None):
        self.name = name
        self.writers = []
        self.readers = []
        self.prev = []
        self.sem = sem if sem is not None else {}
        self.dcount = 0


class Prog:
    ENG = ("pe", "act", "dve", "pool", "sp")

    def __init__(self, nc, stack):
        self.nc = nc
        self.stack = stack
        self.e = {"pe": nc.tensor, "act": nc.scalar, "dve": nc.vector, "pool": nc.gpsimd, "sp": nc.sync}
        self.info = []
        self.pending = {k: [] for k in self.ENG}
        self.esem = {}
        self.ecount = {k: 0 for k in self.ENG}
        self.known = {k: {} for k in self.ENG}
        self.nsem = 0
        self.dma_since_barrier = []
        self.last_sig = {k: None for k in self.ENG}

    def new_sem(self, name):
        self.nsem += 1
        return self.stack.enter_context(self.nc.semaphore(f"{name}_{self.nsem}"))

    def _deps(self, reads, writes, partial):
        deps = set()
        for r in reads:
            deps.update(r.writers)
        for w in writes:
            if partial and not w.readers:
                deps.update(w.prev)
            else:
                w.prev = w.writers + w.readers
                deps.update(w.prev)
                w.writers = []
                w.readers = []
        return deps

    def _commit(self, oid, reads, writes):
        for r in reads:
            r.readers.append(oid)
        for w in writes:
            w.writers.append(oid)

    def _waits(self, eng, deps, is_dma):
        need = {}
        for d in deps:
            inf = self.info[d]
            assert inf is not None, "dependency on a non-signalling op"
            deng, sem, val = inf
            if deng == "pe" and eng == "pe" and not is_dma:
                continue
            k = id(sem)
            if k not in need or need[k][1] < val:
                need[k] = (sem, val)
        for k, (sem, val) in need.items():
            if self.known[eng].get(k, 0) >= val:
                continue
            self.e[eng].wait_ge(sem, val)
            self.known[eng][k] = val

    def op(self, eng, fn, reads=(), writes=(), sig=True, partial=False):
        deps = self._deps(reads, writes, partial)
        self._waits(eng, deps, False)
        ins = fn(self.e[eng])
        oid = len(self.info)
        if sig:
            if self.ecount[eng] % 30000 == 0:
                self.esem[eng] = self.new_sem("e" + eng)
                self.ecount[eng] = 0
            self.ecount[eng] += 1
            ins.then_inc(self.esem[eng], 1)
            inf = (eng, self.esem[eng], self.ecount[eng])
            self.info.append(inf)
            for p in self.pending[eng]:
                self.info[p] = inf
            self.pending[eng] = []
            self.last_sig[eng] = oid
        else:
            self.info.append(None)
            self.pending[eng].append(oid)
        self._commit(oid, reads, writes)
        return oid

    def dma(self, eng, out, in_, reads, writes, partial=True, sem_of=None, **kw):
        assert len(writes) == 1
        w = sem_of if sem_of is not None else writes[0]
        deps = self._deps(reads, writes, partial)
        self._waits(eng, deps, True)
        kind = "sw" if eng == "pool" else "hw"
        if kind not in w.sem:
            w.sem[kind] = [self.new_sem("d" + kind), 0]
        w.sem[kind][1] += 1
        self.e[eng].dma_start(out=out, in_=in_, **kw).then_inc(w.sem[kind][0], 16)
        oid = len(self.info)
        self.info.append(("dma", w.sem[kind][0], 16 * w.sem[kind][1]))
        self.dma_since_barrier.append(oid)
        self._commit(oid, reads, writes)
        return oid

    def barrier(self):
        deps = set(self.dma_since_barrier)
        for k in self.ENG:
            assert not self.pending[k], "pending non-signalled ops at barrier"
            if self.last_sig[k] is not None:
                deps.add(self.last_sig[k])
        for k in self.ENG:
            need = {}
            for d in deps:
                deng, sem, val = self.info[d]
                kk = id(sem)
                if kk not in need or need[kk][1] < val:
                    need[kk] = (sem, val)
            for kk, (sem, val) in need.items():
                if self.known[k].get(kk, 0) >= val:
                    continue
                self.e[k].wait_ge(sem, val)
                self.known[k][kk] = val
        self.dma_since_barrier = []


def _const_tables():
    c = {}
    c["ident"] = np.eye(128, dtype=np.float32).astype(NPBF)
    dil = (1, 4, 16)
    p = np.arange(128)[:, None]
    j = np.arange(128)[None, :]
    tab = np.zeros((128, 12, 3, 2, 128), np.float32)
    for g in range(3):
        for h in range(8):
            slope = 2.0 ** (-(h + 1))
            for kt in range(3):
                rel = p + (kt - 1) * 128 - j
                val = -8.0 * slope * dil[g] * np.abs(rel)
                val = np.where(np.abs(rel) <= 64, val, -32768.0)
                tab[:, g * 4 + h // 2, kt, h % 2, :] = val
    c["abias"] = tab.astype(NPBF)
    osel = np.zeros((128, 2, 128), np.float32)
    osel[:, 0, :64] = 1.0
    osel[:, 1, 64:] = 1.0
    c["onesel"] = osel.astype(NPBF)
    n2 = np.arange(128)[:, None].astype(np.float64)
    k2 = np.arange(128)[None, :].astype(np.float64)
    ang = -2.0 * np.pi * n2 * (k2 + 0.5) / 256.0
    Fr, Fi = np.cos(ang), np.sin(ang)
    c["F1"] = np.concatenate([Fr, Fi, -Fi], axis=1).astype(np.float32).astype(NPBF)
    ang2 = -2.0 * np.pi * (n2 + 128.0) * (k2 + 0.5) / 256.0
    Fr2, Fi2 = -np.cos(ang2), -np.sin(ang2)
    c["F1hi"] = np.concatenate([Fr2, Fi2, -Fi2], axis=1).astype(np.float32).astype(NPBF)
    n1 = np.arange(32).astype(np.float64)
    k1 = np.arange(32).astype(np.float64)
    eye4 = np.eye(4)
    a = -2.0 * np.pi * n1[:, None] * k1[None, :] / 32.0
    F32m = np.zeros((128, 3, 128), np.float32)
    F32m[:, 0, :] = np.kron(np.cos(a), eye4)
    F32m[:, 1, :] = np.kron(np.sin(a), eye4)
    F32m[:, 2, :] = -np.kron(np.sin(a), eye4)
    c["F32m"] = F32m.astype(NPBF)
    kk2 = np.arange(128).astype(np.float64)
    a = -2.0 * np.pi * np.repeat(n1, 4)[:, None] * (kk2[None, :] + 0.5) / NFFT
    tw = np.zeros((128, 2, 8, 128), np.float32)
    tw[:, 0, :, :] = np.cos(a)[:, None, :]
    tw[:, 1, :, :] = np.sin(a)[:, None, :]
    c["tw8"] = tw
    a = 2.0 * np.pi * k1[:, None] * n1[None, :] / 32.0
    Rr = np.kron(np.cos(a), eye4)
    Ri = np.kron(np.sin(a), eye4)
    R12 = np.zeros((128, 2, 256), np.float32)
    R12[:, 0, :128], R12[:, 0, 128:] = Rr, Ri
    R12[:, 1, :128], R12[:, 1, 128:] = -Ri, Rr
    c["R12"] = R12.astype(NPBF)
    T = np.zeros((128, 32, 2, 128), np.float32)
    kk = np.arange(128)[:, None].astype(np.float64)
    nn2 = np.arange(128)[None, :].astype(np.float64)
    for a1 in range(32):
        a = 2.0 * np.pi * (a1 + 32.0 * nn2) * (kk + 0.5) / NFFT
        T[:, a1, 0, :] = (2.0 / NFFT) * np.cos(a)
        T[:, a1, 1, :] = -(2.0 / NFFT) * np.sin(a)
    c["T"] = T.astype(NPBF)
    L = S
    t = np.linspace(0.0, 1.0, L, dtype=np.float32)[:, None]
    bands = 16
    w = (2.0 * np.pi * np.arange(L, dtype=np.float32)[:, None] / L).astype(np.float32)
    f = np.linspace(1e-4, bands - 1, bands, dtype=np.float32)[None, :]
    feat = np.concatenate([t, np.cos(f * w), -np.sin(f * w)], axis=-1).astype(np.float32)
    featT = np.ascontiguousarray(feat.T)
    rev = np.zeros_like(featT)
    rev[:, 1:] = featT[:, :0:-1]
    c["featT"] = np.stack([featT, rev], 0)
    trow = np.zeros((2, 1, L), np.float32)
    trow[0, 0] = t[:, 0]
    trow[1, 0, 1:] = t[:0:-1, 0]
    c["trow"] = trow
    deltas = np.linspace(math.log(1e-2) / 1.5, math.log(1e-2) / 0.3, 512, dtype=np.float32)
    c["ndelta"] = np.ascontiguousarray((-np.abs(deltas)).reshape(4, 128).T)
    return c


CONST_DT = {"ident": BF16, "abias": BF16, "onesel": BF16, "F1": BF16, "F1hi": BF16, "F32m": BF16, "tw8": F32,
            "R12": BF16, "T": BF16, "featT": F32, "trow": F32, "ndelta": F32}


def _layout_inputs(inp, b):
    m = {}
    m["x"] = np.ascontiguousarray(inp["x"][b])
    m["crep"] = np.ascontiguousarray(np.broadcast_to(
        inp["c"][b].reshape(8, 128).T[:, :, None], (128, 8, 128))).astype(np.float32)
    m["ccol"] = np.ascontiguousarray(inp["c"][b].reshape(8, 128).T)

    def kt(w, kc):
        Ld, K, N = w.shape
        return np.ascontiguousarray(w.reshape(Ld, kc, 128, N).transpose(0, 2, 1, 3))

    m["w_ada"] = kt(inp["w_ada"], 8)
    m["w_in"] = kt(inp["w_in"], 8)
    m["w_pa"] = kt(inp["w_proj_attn"], 4)
    m["w_ph"] = kt(inp["w_proj_hyena"], 4)
    m["w_out"] = kt(inp["w_out"], 8)

    def col(v):
        Ld, N = v.shape
        return np.ascontiguousarray(v.reshape(Ld, N // 128, 128).transpose(0, 2, 1))

    m["b_ada_c"] = col(inp["b_ada"])
    m["b_ada_r"] = np.ascontiguousarray(inp["b_ada"][:, None, :])
    m["b_in_c"] = col(inp["b_in"])
    m["b_in_r"] = np.ascontiguousarray(inp["b_in"][:, None, :])
    m["conv_w_c"] = np.ascontiguousarray(inp["conv_w"].reshape(DEPTH, 3, 12, 128).transpose(0, 3, 1, 2))
    m["conv_b_c"] = col(inp["conv_b"])
    m["f_w1"] = np.ascontiguousarray(inp["filt_w1"])
    m["f_w2"] = np.ascontiguousarray(inp["filt_w2"])
    m["f_w3"] = np.ascontiguousarray(inp["filt_w3"])
    m["f_w4"] = np.ascontiguousarray(inp["filt_w4"])
    m["f_b"] = np.ascontiguousarray(np.stack([inp["filt_b1"], inp["filt_b2"], inp["filt_b3"], inp["filt_freq"]], -1))
    m["f_bias_r"] = np.ascontiguousarray(inp["filt_bias"])
    m["b_out_r"] = np.ascontiguousarray(inp["b_out"][:, None, :])
    m["ln_g_r"] = np.ascontiguousarray(inp["ln_g"][:, None, :])
    m["ln_b_r"] = np.ascontiguousarray(inp["ln_b"][:, None, :])
    return m


IN_SHAPES = {
    "x": [S, D], "crep": [128, 8, 128], "ccol": [128, 8],
    "w_ada": [DEPTH, 128, 8, 3072], "w_in": [DEPTH, 128, 8, NIN], "w_pa": [DEPTH, 128, 4, D],
    "w_ph": [DEPTH, 128, 4, D], "w_out": [DEPTH, 128, 8, D],
    "b_ada_c": [DEPTH, 128, 24], "b_ada_r": [DEPTH, 1, 3072], "b_in_c": [DEPTH, 128, 72],
    "b_in_r": [DEPTH, 1, NIN], "conv_w_c": [DEPTH, 128, 3, 12], "conv_b_c": [DEPTH, 128, 12],
    "f_w1": [DEPTH, 33, 64], "f_w2": [DEPTH, 64, 64], "f_w3": [DEPTH, 64, 64], "f_w4": [DEPTH, 64, 2048],
    "f_b": [DEPTH, 64, 4], "f_bias_r": [DEPTH, 2, 512], "b_out_r": [DEPTH, 1, D],
    "ln_g_r": [DEPTH, 1, D], "ln_b_r": [DEPTH, 1, D],
}
CONST_SHAPES = {"ident": [128, 128], "abias": [128, 12, 3, 2, 128], "onesel": [128, 2, 128], "F1": [128, 384],
                "F1hi": [128, 384], "F32m": [128, 3, 128], "tw8": [128, 2, 8, 128], "R12": [128, 2, 256], "T": [128, 32, 2, 128],
                "featT": [2, 33, S], "trow": [2, 1, S], "ndelta": [128, 4]}


def bc(ap_t, offset, n):
    return bass.AP(ap_t.tensor, offset, [[0, 128], [1, n]])


class K:
    pass


def build(dbg=None, layers=DEPTH, stop_after=None):
    nc = bass.Bass("TRN2", target_bir_lowering=False)
    try:
        nc.allow_low_precision("bf16 matmul operands with fp32 accumulation")
    except Exception:
        pass
    stack = ExitStack()
    P = Prog(nc, stack)
    I = {k: nc.dram_tensor(k, s, F32, kind="ExternalInput").ap() for k, s in IN_SHAPES.items()}
    C = {k: nc.dram_tensor("c_" + k, s, CONST_DT[k], kind="ExternalInput").ap() for k, s in CONST_SHAPES.items()}
    out = nc.dram_tensor("out", [S, D], F32, kind="ExternalOutput").ap()
    dbg_out = {}

    def dram(name, shape, dt):
        kind = "ExternalOutput" if (dbg and name in dbg) else "Internal"
        t = nc.dram_tensor(name, shape, dt, kind=kind).ap()
        if kind == "ExternalOutput":
            dbg_out[name] = t
        return t

    proj_d = dram("proj_d", [72, 128, S], BF16)
    vtm_d = dram("vtm_d", [3, 4, 128, 32, 128], BF16)
    yattn_d = dram("yattn_d", [4, 128, S], BF16)
    yhy_d = dram("yhy_d", [4, 128, S], BF16)
    xmid_d = dram("xmid_d", [S, D], F32)
    H_d = dram("H_d", [DEPTH, 2, 4, 128, 2, 32, 128], BF16)
    fam = {}
    R_proj = [Res(f"proj{i}", fam) for i in range(72)]
    R_vtm = [Res(f"vtm{g}", fam) for g in range(3)]
    R_yattn = [Res(f"yattn{i}", fam) for i in range(4)]
    R_yhy = [Res(f"yhy{i}", fam) for i in range(4)]
    R_xmid = Res("xmid", fam)
    R_H = [[[Res(f"H{l}{o}{h}", fam) for h in range(4)] for o in range(2)] for l in range(DEPTH)]
    R_out = Res("out")
    R_in = Res("inputs")

    RES = {}
    cnt = {"n": 0}

    def sb(name, shape, dt, st=None):
        cnt["n"] += 1
        t = (st or stack).enter_context(nc.sbuf_tensor(f"s_{name}_{cnt['n']}", shape, dt))
        if name not in RES:
            RES[name] = Res(name)
        return t, RES[name]

    psf = []
    for i in range(6):
        t = stack.enter_context(nc.psum_tensor(f"psf{i}", [128, 512], F32))
        psf.append((t, Res(f"psf{i}")))
    psb = []
    for i in range(2):
        t = stack.enter_context(nc.psum_tensor(f"psb{i}", [128, 1024], BF16))
        psb.append((t, Res(f"psb{i}")))
    rr = {"f": 0, "b": 0, "q": 0}

    def next_psf():
        rr["f"] = (rr["f"] + 1) % 6
        return psf[rr["f"]]

    def next_psb():
        rr["b"] = (rr["b"] + 1) % 2
        return psb[rr["b"]]

    DQ = ("sp", "pool")

    def next_q():
        rr["q"] = (rr["q"] + 1) % len(DQ)
        return DQ[rr["q"]]

    ident, r_ident = sb("ident", [128, 128], BF16)
    onesel, r_onesel = sb("onesel", [128, 2, 128], BF16)
    F1, r_F1 = sb("F1", [128, 384], BF16)
    F1hi, r_F1hi = sb("F1hi", [128, 384], BF16)
    R12, r_R12 = sb("R12", [128, 2, 256], BF16)
    F32m, _ = sb("F32m", [128, 3, 128], BF16)
    tw8, _ = sb("tw8", [128, 2, 8, 128], F32)
    ones2, r_ones2 = sb("ones2", [2, 128], BF16)
    epsT, r_eps = sb("epsT", [128, 1], F32)
    npiT, r_npi = sb("npiT", [128, 1], F32)
    r_cst = Res("cst")
    r_ident = r_onesel = r_F1 = r_F1hi = r_R12 = r_cst
    P.dma("sp", ident[:], C["ident"][:, :], [R_in], [r_ident])
    P.dma("sp", onesel[:], C["onesel"][:, :, :], [R_in], [r_onesel])
    P.dma("sp", F1[:], C["F1"][:, :], [R_in], [r_F1])
    P.dma("sp", F1hi[:], C["F1hi"][:, :], [R_in], [r_F1hi])
    P.dma("sp", R12[:], C["R12"][:, :, :], [R_in], [r_R12])
    P.dma("sp", F32m[:], C["F32m"][:, :, :], [R_in], [r_cst])
    P.dma("sp", tw8[:], C["tw8"][:, :, :, :], [R_in], [r_cst])
    P.op("pool", lambda e: e.memset(ones2[:], 1.0), [], [r_ones2])
    P.op("pool", lambda e: e.memset(epsT[:], EPS), [], [r_eps])
    P.op("pool", lambda e: e.memset(npiT[:], -math.pi), [], [r_npi])

    modcol, r_modcol = sb("modcol", [128, DEPTH, 24], F32)
    sc1, r_sc1 = sb("sc1", [128, DEPTH, 8], F32)
    gate_b, r_gate = sb("gate_b", [128, DEPTH, D], F32)
    b_in_c, r_binc = sb("b_in_c", [128, DEPTH, 72], F32)
    convw, r_convw = sb("convw", [128, DEPTH, 3, 12], F32)
    convb, r_convb = sb("convb", [128, DEPTH, 12], F32)
    bin2, r_bin2 = sb("bin2", [1, DEPTH, 1536], BF16)
    binl, r_binl = sb("binl", [1, DEPTH, 1536], BF16)
    r_binc = r_convw = r_convb = r_cst
    for l in range(DEPTH):
        P.dma("sp", b_in_c[:, l, :], I["b_in_c"][l], [R_in], [r_binc])
        P.dma("sp", convw[:, l, :, :], I["conv_w_c"][l], [R_in], [r_convw])
        P.dma("sp", convb[:, l, :], I["conv_b_c"][l], [R_in], [r_convb])

    with ExitStack() as ph:
        ccol, r_ccol = sb("ccol", [128, 8], F32, ph)
        crep, r_crep = sb("crep", [128, 8, 128], F32, ph)
        badac, r_badac = sb("badac", [128, DEPTH, 24], F32, ph)
        badar, r_badar = sb("badar", [128, DEPTH, D], F32, ph)
        vb, r_vb = sb("vb", [1, DEPTH, 1536], F32, ph)
        vbh, r_vbh = sb("vbh", [1, DEPTH, 1536], F32, ph)
        r_ccol = r_crep = r_badac = r_badar = r_vb = r_cst
        P.dma("sp", ccol[:], I["ccol"][:, :], [R_in], [r_ccol])
        P.dma("sp", crep[:], I["crep"][:, :, :], [R_in], [r_crep])
        for l in range(DEPTH):
            P.dma("sp", badac[:, l, :], I["b_ada_c"][l], [R_in], [r_badac])
            P.dma("sp", badar[:, l, :], bc(I["b_ada_r"], l * 3072 + 2048, D), [R_in], [r_badar])
            P.dma("sp", vb[:, l, :], bass.AP(I["b_in_r"].tensor, l * NIN + 3072, [[0, 1], [1, 1536]]), [R_in], [r_vb])
        P.op("dve", lambda e: e.tensor_copy(out=bin2[:], in_=vb[:]), [r_vb], [r_bin2])
        P.op("dve", lambda e: e.tensor_copy(out=vbh[:], in_=bin2[:]), [r_bin2], [r_vbh])
        P.op("dve", lambda e: e.tensor_sub(out=vbh[:], in0=vb[:], in1=vbh[:]), [r_vb, r_vbh], [r_vbh])
        P.op("dve", lambda e: e.tensor_copy(out=binl[:], in_=vbh[:]), [r_vbh], [r_binl])
        wa = [sb(f"wa{i}", [128, 8, 512], F32, ph) for i in range(2)]
        for l in range(DEPTH):
            pc, r_pc = next_psf()
            for blk in range(6):
                wt, r_wt = wa[blk % 2]
                P.dma("sp", wt[:, 0:4, :], I["w_ada"][l, :, 0:4, blk * 512:(blk + 1) * 512], [R_in], [r_wt], partial=False)
                P.dma("pool", wt[:, 4:8, :], I["w_ada"][l, :, 4:8, blk * 512:(blk + 1) * 512], [R_in], [r_wt])
                for f in range(4):
                    fi = blk * 4 + f
                    for kc in range(8):
                        P.op("pe", lambda e, wt=wt, f=f, kc=kc, fi=fi: e.matmul(
                            pc[:, fi:fi + 1], lhsT=wt[:, kc, f * 128:(f + 1) * 128], rhs=ccol[:, kc:kc + 1],
                            start=(kc == 0), stop=(kc == 7)),
                            [r_wt, r_ccol], [r_pc], sig=(kc == 7), partial=True)
                if blk >= 4:
                    pg, r_pg = next_psf()
                    for kc in range(8):
                        P.op("pe", lambda e, wt=wt, kc=kc: e.matmul(
                            pg[:, :], lhsT=crep[:, kc, :], rhs=wt[:, kc, :], start=(kc == 0), stop=(kc == 7)),
                            [r_wt, r_crep], [r_pg], sig=(kc == 7), partial=True)
                    h0 = (blk - 4) * 512
                    P.op("dve", lambda e, l=l, h0=h0, pg=pg: e.tensor_add(
                        out=gate_b[:, l, h0:h0 + 512], in0=pg[:, :], in1=badar[:, l, h0:h0 + 512]),
                        [r_pg, r_badar], [r_gate], partial=True)
            P.op("dve", lambda e, l=l, pc=pc: e.tensor_add(out=modcol[:, l, :], in0=pc[:, 0:24], in1=badac[:, l, :]),
                 [r_pc, r_badac], [r_modcol], partial=True)
            P.op("dve", lambda e, l=l: e.tensor_scalar_add(out=sc1[:, l, :], in0=modcol[:, l, 8:16], scalar1=1.0),
                 [r_modcol], [r_sc1], partial=True)
        P.barrier()


    act_dve = {"n": 0}

    def evac(out_ap, in_ap, reads, writes, partial=True, simple=False):
        act_dve["n"] += 1
        if simple and act_dve["n"] % 2:
            P.op("act", lambda e: e.copy(out=out_ap, in_=in_ap), reads, writes, partial=partial)
        else:
            P.op("dve", lambda e: e.tensor_copy(out=out_ap, in_=in_ap), reads, writes, partial=partial)

    def fft_fwd(zl, r_zl, zh, r_zh, CEf, r_CE, X, r_X, tpb):
        Cv = CEf[:, 0:8192].rearrange("p (t c k) -> p t c k", t=2, c=32, k=128)
        for cp in range(16):
            pp, r_pp = next_psf()
            for gi in range(2):
                cg = cp * 2 + gi
                P.op("pe", lambda e: e.matmul(pp[:, gi * 256:(gi + 1) * 256], lhsT=zl[:, cg * 128:(cg + 1) * 128], rhs=F1[:, 0:256],
                                              start=True, stop=(zh is None)),
                     [r_zl, r_F1], [r_pp], sig=(zh is None and gi == 1), partial=(gi > 0))
                if zh is not None:
                    P.op("pe", lambda e: e.matmul(pp[:, gi * 256:(gi + 1) * 256], lhsT=zh[:, cg * 128:(cg + 1) * 128], rhs=F1hi[:, 0:256],
                                                  start=False, stop=True),
                         [r_zh, r_F1hi], [r_pp], sig=(gi == 1), partial=True)
            evac(Cv[:, :, cp * 2:cp * 2 + 2, :], pp[:, :].rearrange("p (g t k) -> p t g k", g=2, t=2, k=128), [r_pp], [r_CE],
                 partial=(cp > 0), simple=False)
        fmode = dbg.get("fft_mode", 9) if dbg else 9
        if fmode < 2:
            return
        (t1, r1), (t2, r2), (t3, r3), (t4, r4) = tpb
        bs = t1.shape[1]
        for blk in range(32 // bs):
            cs = slice(blk * bs, blk * bs + bs)
            cr, ci = Cv[:, 0, cs, :], Cv[:, 1, cs, :]
            P.op("dve", lambda e: e.tensor_mul(out=t1[:], in0=cr, in1=tw8[:, 0, 0:bs, :]), [r_CE, r_cst], [r1], partial=False)
            P.op("pool", lambda e: e.tensor_mul(out=t2[:], in0=ci, in1=tw8[:, 1, 0:bs, :]), [r_CE, r_cst], [r2], partial=False)
            P.op("pool", lambda e: e.tensor_mul(out=t3[:], in0=cr, in1=tw8[:, 1, 0:bs, :]), [r_CE, r_cst], [r3], partial=False)
            P.op("dve", lambda e: e.tensor_mul(out=t4[:], in0=ci, in1=tw8[:, 0, 0:bs, :]), [r_CE, r_cst], [r4], partial=False)
            P.op("dve", lambda e: e.tensor_sub(out=cr, in0=t1[:], in1=t2[:]), [r1, r2], [r_CE], partial=True)
            P.op("dve", lambda e: e.tensor_add(out=ci, in0=t3[:], in1=t4[:]), [r3, r4], [r_CE], partial=True)
            for hf_ in range(bs // 4 if fmode >= 3 else 0):
                c0 = (blk * bs + hf_ * 4) * 128
                rre = CEf[:, c0:c0 + 512]
                rim = CEf[:, 4096 + c0:4096 + c0 + 512]
                for t, (la, lb) in enumerate(((0, 2), (1, 0))):
                    px, r_px = next_psf()
                    P.op("pe", lambda e: e.matmul(px[:, :], lhsT=F32m[:, la, :], rhs=rre, start=True, stop=False),
                         [r_cst, r_CE], [r_px], sig=False, partial=False)
                    P.op("pe", lambda e: e.matmul(px[:, :], lhsT=F32m[:, lb, :], rhs=rim, start=False, stop=True),
                         [r_cst, r_CE], [r_px], sig=True, partial=True)
                    x0 = (t * 32 + blk * bs + hf_ * 4) * 128
                    evac(X[:].rearrange("p t c k -> p (t c k)")[:, x0:x0 + 512], px[:, :],
                         [r_px], [r_X], partial=not (blk == 0 and hf_ == 0 and t == 0), simple=True)

    def fft_inv(Y, r_Y, CEf, r_CE, Tt, r_T, epilogue):
        E5 = CEf[:, 0:32 * 2 * 128].rearrange("p (n t g c) -> p n t g c", n=32, t=2, g=32, c=4)
        E4 = CEf[:, 0:32 * 2 * 128].rearrange("p (n t c) -> p n t c", n=32, t=2, c=128)
        for cp in range(16):
            pe_, r_pe = next_psf()
            for gi in range(2):
                cg = cp * 2 + gi
                P.op("pe", lambda e: e.matmul(pe_[:, gi * 256:(gi + 1) * 256], lhsT=Y[:, 0, cg, :], rhs=R12[:, 0, :], start=True, stop=False),
                     [r_Y, r_R12], [r_pe], sig=False, partial=(gi > 0))
                P.op("pe", lambda e: e.matmul(pe_[:, gi * 256:(gi + 1) * 256], lhsT=Y[:, 1, cg, :], rhs=R12[:, 1, :], start=False, stop=True),
                     [r_Y, r_R12], [r_pe], sig=(gi == 1), partial=True)
            pv = pe_[:, :].rearrange("p (g t n c) -> p t n g c", g=2, t=2, n=32, c=4)
            for t in range(2):
                evac(E5[:, :, t, cp * 2:cp * 2 + 2, :], pv[:, t], [r_pe], [r_CE], partial=not (cp == 0 and t == 0))
        for nb in range(8):
            py, r_py = next_psf()
            for ni in range(4):
                n1 = nb * 4 + ni
                P.op("pe", lambda e: e.matmul(py[:, ni * 128:(ni + 1) * 128], lhsT=Tt[:, n1, 0, :], rhs=E4[:, n1, 0, :], start=True, stop=False),
                     [r_T, r_CE], [r_py], sig=False, partial=(ni > 0))
                P.op("pe", lambda e: e.matmul(py[:, ni * 128:(ni + 1) * 128], lhsT=Tt[:, n1, 1, :], rhs=E4[:, n1, 1, :], start=False, stop=True),
                     [r_T, r_CE], [r_py], sig=(ni == 3), partial=True)
            epilogue(nb, py[:, :].rearrange("p (n c) -> p n c", n=4), r_py)

    def to_token_major(srcT, r_src, dst, r_dst, fftl=True):
        sv = srcT[:, :].rearrange("p (a b) -> p b a", b=32)
        for nb in range(4):
            pt, r_pt = next_psb()
            for ni in range(8):
                n1 = nb * 8 + ni
                P.op("pe", lambda e: e.transpose(out=pt[:, ni * 128:(ni + 1) * 128], in_=sv[:, n1, :], identity=ident[:]),
                     [r_src, r_ident], [r_pt], sig=(ni == 7), partial=(ni > 0))
            if fftl:
                evac(dst[:, :].rearrange("p (g n c) -> p g n c", g=32, n=32, c=4)[:, :, nb * 8:(nb + 1) * 8, :],
                     pt[:, :].rearrange("p (n g c) -> p g n c", n=8, g=32, c=4), [r_pt], [r_dst], partial=(nb > 0))
            else:
                evac(dst[:, :].rearrange("p (n c) -> p n c", n=32)[:, nb * 8:(nb + 1) * 8, :],
                     pt[:, :].rearrange("p (n c) -> p n c", n=8), [r_pt], [r_dst], partial=(nb > 0))

    if not (dbg and dbg.get("skip_filters")):
        with ExitStack() as ph:
            h3 = [[sb(f"h3_{l}{v}", [64, S], BF16, ph) for v in range(2)] for l in range(DEPTH)]
            w4b = [sb(f"w4b{l}", [64, 2048], BF16, ph) for l in range(DEPTH)]
            with ExitStack() as ph2:
                feat = [sb(f"feat{v}", [33, S], F32, ph2) for v in range(2)]
                hA, r_hA = sb("hA", [64, S], F32, ph2)
                hB, r_hB = sb("hB", [64, S], F32, ph2)
                w4f, r_w4f = sb("w4f", [64, 2048], F32, ph2)
                fw = [sb(f"fw{i}", [64, 64], F32, ph2) for i in range(3)]
                fbt, r_fbt = sb("fbt", [64, 4], F32, ph2)
                fsc, r_fsc = sb("fsc", [64, 4], F32, ph2)
                ty = [sb(f"ty{i}", [64, 512], F32, ph2) for i in range(2)]
                tki, r_tki = sb("tki", [64, 512], mybir.dt.int32, ph2)
                tkf, r_tkf = sb("tkf", [64, 512], F32, ph2)
                for v in range(2):
                    P.dma("sp", feat[v][0][:], C["featT"][v], [R_in], [feat[v][1]], partial=False)
                for l in range(DEPTH):
                    P.dma("sp", fw[0][0][0:33, :], I["f_w1"][l], [R_in], [fw[0][1]], partial=False)
                    P.dma("sp", fw[1][0][:], I["f_w2"][l], [R_in], [fw[1][1]], partial=False)
                    P.dma("sp", fw[2][0][:], I["f_w3"][l], [R_in], [fw[2][1]], partial=False)
                    P.dma("sp", w4f[:], I["f_w4"][l], [R_in], [r_w4f], partial=False)
                    P.dma("sp", fbt[:], I["f_b"][l], [R_in], [r_fbt], partial=False)
                    P.op("pool", lambda e: e.tensor_copy(out=w4b[l][0][:], in_=w4f[:]), [r_w4f], [w4b[l][1]], partial=False)
                    P.op("dve", lambda e: e.tensor_scalar_mul(out=fsc[:, 3:4], in0=fbt[:, 3:4], scalar1=1.0 / TWO_PI), [r_fbt], [r_fsc], partial=False)
                    P.op("dve", lambda e: e.tensor_scalar(out=fsc[:, 0:3], in0=fbt[:, 0:3], scalar1=fsc[:, 3:4], scalar2=8.0,
                                                          op0=ALU.mult, op1=ALU.add), [r_fbt, r_fsc], [r_fsc], partial=False)
                    for v in range(2):
                        src, r_src = feat[v]
                        kdim = 33
                        for layer_i in range(3):
                            last = (layer_i == 2)
                            dst, r_dst = (h3[l][v] if last else ((hA, r_hA) if layer_i == 0 else (hB, r_hB)))
                            wt_, r_wt_ = fw[layer_i]
                            for tb in range(8):
                                sl = slice(tb * 512, (tb + 1) * 512)
                                pp, r_pp = next_psf()
                                P.op("pe", lambda e: e.matmul(pp[0:64, :], lhsT=wt_[0:kdim, :], rhs=src[0:kdim, sl], start=True, stop=True),
                                     [r_wt_, r_src], [r_pp], partial=False)
                                tyt, r_ty = ty[tb % 2]
                                P.op("dve", lambda e: e.tensor_scalar(out=tyt[:], in0=pp[0:64, :], scalar1=fsc[:, 3:4],
                                                                      scalar2=fsc[:, layer_i:layer_i + 1], op0=ALU.mult, op1=ALU.add),
                                     [r_pp, r_fsc], [r_ty], partial=False)
                                P.op("dve", lambda e: e.tensor_copy(out=tki[:], in_=tyt[:]), [r_ty], [r_tki], partial=False)
                                P.op("dve", lambda e: e.tensor_copy(out=tkf[:], in_=tki[:]), [r_tki], [r_tkf], partial=False)
                                P.op("dve", lambda e: e.tensor_sub(out=tyt[:], in0=tyt[:], in1=tkf[:]), [r_ty, r_tkf], [r_ty], partial=False)
                                P.op("dve", lambda e: e.tensor_single_scalar(out=tkf[:], in_=tyt[:], scalar=0.5, op=ALU.is_ge),
                                     [r_ty], [r_tkf], partial=False)
                                P.op("dve", lambda e: e.tensor_sub(out=tyt[:], in0=tyt[:], in1=tkf[:]), [r_ty, r_tkf], [r_ty], partial=False)
                                P.op("act", lambda e: e.activation(out=dst[:, sl], in_=tyt[:], func=AF.Sin, scale=TWO_PI),
                                     [r_ty], [r_dst], partial=(tb > 0))
                            src, r_src = dst, r_dst
                            kdim = 64
                P.barrier()
            trb = [sb(f"trb{v}", [128, S], F32, ph) for v in range(2)]
            dec = [sb(f"dec{v}", [128, S], F32, ph) for v in range(2)]
            ndl, r_ndl = sb("ndl", [128, 4], F32, ph)
            fT, r_fT = sb("fT", [128, S], BF16, ph)
            ftm = [sb(f"ftm{v}", [128, S], BF16, ph) for v in range(2)]
            CEf, r_CE = sb("CEf", [128, 8192], BF16, ph)
            Xb, r_X = sb("Xb", [128, 2, 32, 128], BF16, ph)
            tpf = [sb(f"tpf{i}", [128, 4, 128], F32, ph) for i in range(4)]
            P.dma("sp", ndl[:], C["ndelta"][:, :], [R_in], [r_ndl], partial=False)
            for v in range(2):
                P.dma("sp", trb[v][0][:], bc(C["trow"], v * S, S), [R_in], [trb[v][1]], partial=False)
            flist = [(c, l, o) for c in range(4) for l in range(DEPTH) for o in range(2)]
            if dbg and "filt_list" in dbg:
                flist = dbg["filt_list"]
            lastc = None
            for (c, l, o) in flist:
                if c != lastc:
                    for v in range(2):
                        P.op("act", lambda e: e.activation(out=dec[v][0][:], in_=trb[v][0][:], func=AF.Exp, scale=ndl[:, c:c + 1]),
                             [trb[v][1], r_ndl], [dec[v][1]], partial=False)
                    lastc = c
                for v in range(2):
                    col0 = (o * 2 + v) * 512 + c * 128
                    for tb in range(8):
                        sl = slice(tb * 512, (tb + 1) * 512)
                        pp, r_pp = next_psf()
                        P.op("pe", lambda e: e.matmul(pp[:, :], lhsT=w4b[l][0][:, col0:col0 + 128], rhs=h3[l][v][0][:, sl], start=True, stop=True),
                             [w4b[l][1], h3[l][v][1]], [r_pp], partial=False)
                        P.op("dve", lambda e: e.tensor_mul(out=fT[:, sl], in0=pp[:, :], in1=dec[v][0][:, sl]),
                             [r_pp, dec[v][1]], [r_fT], partial=(tb > 0))
                    if v == 1:
                        P.op("dve", lambda e: e.memset(fT[:, 0:1], 0.0), [], [r_fT], partial=True)
                    to_token_major(fT, r_fT, ftm[v][0], ftm[v][1])
                fft_fwd(ftm[0][0], ftm[0][1], ftm[1][0], ftm[1][1], CEf, r_CE, Xb, r_X, tpf)
                P.dma("sp", H_d[l, o, c], Xb[:], [r_X], [R_H[l][o][c]], partial=False, sem_of=r_X)
            P.barrier()
    if stop_after == "filt":
        layers = 0

    x_src = I["x"]
    R_xsrc = R_in
    for l in range(layers):
        x_dst, R_xdst = (xmid_d, R_xmid) if l < DEPTH - 1 else (out, R_out)
        lay = ExitStack()
        hT, r_hT = sb(f"hT", [128, 8, S], BF16, lay)
        with ExitStack() as ph:
            xt = [sb(f"xt{i}", [128, D], F32, ph) for i in range(2)]
            xn = [sb(f"xn{i}", [128, D], BF16, ph) for i in range(2)]
            st = [sb(f"st{i}", [128, 12], F32, ph) for i in range(2)]
            mv = [sb(f"mv{i}", [128, 2], F32, ph) for i in range(2)]
            for t in range(32):
                xtt, r_xt = xt[t % 2]
                xnn, r_xn = xn[t % 2]
                stt, r_st = st[t % 2]
                mvv, r_mv = mv[t % 2]
                P.dma(next_q(), xtt[:], x_src[t * 128:(t + 1) * 128, :], [R_xsrc], [r_xt], partial=False)
                P.op("dve", lambda e: e.bn_stats(out=stt[:, 0:6], in_=xtt[:, 0:512]), [r_xt], [r_st], partial=False)
                P.op("dve", lambda e: e.bn_stats(out=stt[:, 6:12], in_=xtt[:, 512:1024]), [r_xt], [r_st], partial=True)
                P.op("dve", lambda e: e.bn_aggr(out=mvv[:], in_=stt[:]), [r_st], [r_mv], partial=False)
                P.op("act", lambda e: e.activation(out=mvv[:, 1:2], in_=mvv[:, 1:2], func=AF.Sqrt, bias=epsT[:], scale=1.0),
                     [r_mv, r_eps], [r_mv], partial=False)
                P.op("dve", lambda e: e.reciprocal(out=mvv[:, 1:2], in_=mvv[:, 1:2]), [r_mv], [r_mv], partial=False)
                P.op("dve", lambda e: e.tensor_scalar(out=xnn[:], in0=xtt[:], scalar1=mvv[:, 0:1], scalar2=mvv[:, 1:2],
                                                      op0=ALU.subtract, op1=ALU.mult), [r_xt, r_mv], [r_xn], partial=False)
                pt, r_pt = next_psb()
                for kc in range(8):
                    P.op("pe", lambda e, kc=kc: e.transpose(out=pt[:, kc * 128:(kc + 1) * 128], in_=xnn[:, kc * 128:(kc + 1) * 128],
                                                            identity=ident[:]),
                         [r_xn, r_ident], [r_pt], sig=(kc == 7), partial=(kc > 0))
                for kc in range(8):
                    P.op("act", lambda e, kc=kc: e.activation(out=hT[:, kc, t * 128:(t + 1) * 128], in_=pt[:, kc * 128:(kc + 1) * 128],
                                                              func=AF.Identity, scale=sc1[:, l, kc:kc + 1], bias=modcol[:, l, kc:kc + 1]),
                         [r_pt, r_sc1, r_modcol], [r_hT], partial=True)
            P.barrier()
        if stop_after == "ln":
            lay.close()
            break

        with ExitStack() as ph:
            ws = [sb(f"ws{i}", [128, 8, 128], F32, ph) for i in range(3)]
            wb = [sb(f"wb{i}", [128, 8, 128], BF16, ph) for i in range(3)]
            ob = [sb(f"ob{i}", [128, S], BF16, ph) for i in range(2)]
            nfm = 0
            chunks = [j for j in range(72) if not (24 <= j < 36)]
            if dbg and "proj_chunks" in dbg:
                chunks = dbg["proj_chunks"]
            for j in chunks:
                wst, r_ws = ws[nfm % 3]
                wbt, r_wb = wb[nfm % 3]
                obt, r_ob = ob[nfm % 2]
                nfm += 1
                P.dma(next_q(), wst[:], I["w_in"][l, :, :, j * 128:(j + 1) * 128], [R_in], [r_ws], partial=False)
                P.op("pool", lambda e: e.tensor_copy(out=wbt[:], in_=wst[:]), [r_ws], [r_wb], partial=False)
                if 36 <= j < 40 or 52 <= j < 56:
                    fn = AF.Silu
                elif j >= 56:
                    fn = AF.Sigmoid
                else:
                    fn = AF.Identity
                for tb in range(8):
                    pp, r_pp = next_psf()
                    for kc in range(8):
                        P.op("pe", lambda e, kc=kc: e.matmul(pp[:, :], lhsT=wbt[:, kc, :], rhs=hT[:, kc, tb * 512:(tb + 1) * 512],
                                                             start=(kc == 0), stop=(kc == 7)),
                             [r_wb, r_hT], [r_pp], sig=(kc == 7), partial=(kc > 0))
                    P.op("act", lambda e: e.activation(out=obt[:, tb * 512:(tb + 1) * 512], in_=pp[:, :], func=fn,
                                                       bias=b_in_c[:, l, j:j + 1], scale=1.0),
                         [r_pp, r_binc], [r_ob], partial=(tb > 0))
                P.dma(next_q(), proj_d[j, :, :], obt[:], [r_ob], [R_proj[j]], partial=False, sem_of=r_ob)
            wvs = [sb(f"wvs{i}", [128, 8, 512], F32, ph) for i in range(1)]
            wvb = [sb(f"wvb{i}", [128, 8, 512], BF16, ph) for i in range(2)]
            vo = [sb(f"vo{i}", [128, 4, 512], BF16, ph) for i in range(2)]
            nv = 0
            groups = range(3) if not (dbg and "v_groups" in dbg) else dbg["v_groups"]
            for g in groups:
                d = (1, 4, 16)[g]
                Lg = S // d
                wst, r_ws = wvs[0]
                wbt, r_wb = wvb[g % 2]
                c0 = 3072 + g * 512
                P.dma("sp", wst[:, 0:4, :], I["w_in"][l, :, 0:4, c0:c0 + 512], [R_in], [r_ws], partial=False)
                P.dma("pool", wst[:, 4:8, :], I["w_in"][l, :, 4:8, c0:c0 + 512], [R_in], [r_ws])
                P.op("pool", lambda e: e.tensor_copy(out=wbt[:], in_=wst[:]), [r_ws], [r_wb], partial=False)
                for tq in range(8):
                    vot, r_vo = vo[nv % 2]
                    nv += 1
                    for t4 in range(4):
                        ti = tq * 4 + t4
                        r_, u = divmod(ti, Lg // 128)
                        pp, r_pp = next_psf()
                        for kc in range(8):
                            lt = hT[:, kc, :].rearrange("p (i d) -> p d i", d=d)[:, r_, u * 128:(u + 1) * 128]
                            P.op("pe", lambda e, kc=kc, lt=lt: e.matmul(pp[:, :], lhsT=lt, rhs=wbt[:, kc, :], start=(kc == 0), stop=False),
                                 [r_wb, r_hT], [r_pp], sig=False, partial=(kc > 0))
                        P.op("pe", lambda e: e.matmul(pp[:, :], lhsT=ones2[0:1, :], rhs=bin2[0:1, l, g * 512:(g + 1) * 512], start=False, stop=False),
                             [r_ones2, r_bin2], [r_pp], sig=False, partial=True)
                        P.op("pe", lambda e: e.matmul(pp[:, :], lhsT=ones2[0:1, :], rhs=binl[0:1, l, g * 512:(g + 1) * 512], start=False, stop=True),
                             [r_ones2, r_binl], [r_pp], sig=True, partial=True)
                        P.op("dve", lambda e, t4=t4: e.tensor_copy(out=vot[:, t4, :], in_=pp[:, :]), [r_pp], [r_vo], partial=(t4 > 0))
                    qn = next_q()
                    for j4 in range(4):
                        P.dma(qn, vtm_d[g, j4, :, tq * 4:(tq + 1) * 4, :], vot[:, :, j4 * 128:(j4 + 1) * 128], [r_vo], [R_vtm[g]], sem_of=r_vo)
            P.barrier()
        if stop_after == "proj":
            lay.close()
            break
        lay.close()
        with ExitStack() as ph:
            Oacc, r_O = sb(f"Oacc", [128, 2, S], F32, ph)
            qTs = [sb(f"qT{i}", [128, 2, S], BF16, ph) for i in range(2)]
            kTs = [sb(f"kT{i}", [128, S], BF16, ph) for i in range(2)]
            vts = [sb(f"vt{i}", [128, 32, 2, 128], BF16, ph) for i in range(2)]
            abs_ = [sb(f"ab{i}", [128, 3, 256], BF16, ph) for i in range(2)]
            pTs = [sb(f"pT{i}", [128, 256], BF16, ph) for i in range(8)]
            gs, r_gs = sb(f"gs", [128, S], BF16, ph)
            vs, r_vs = sb(f"vs", [128, 32, 128], BF16, ph)
            yb, r_yb = sb(f"yb", [128, S], BF16, ph)
            rz, r_rz = sb(f"rz", [128, 512], F32, ph)
            tm, r_tm = sb(f"tm", [128, 512], F32, ph)
            for i in range(2):
                P.op("pool", lambda e, i=i: e.memset(vts[i][0][:], 0.0), [], [vts[i][1]], partial=False)
                P.op("pool", lambda e, i=i: e.memset(qTs[i][0][:], 0.0), [], [qTs[i][1]], partial=False)
            npT = 0
            nbuf = 0
            jlist = range(4) if not (dbg and "att_j" in dbg) else dbg["att_j"]
            for j in jlist:
                for g in (range(3) if not (dbg and "att_g" in dbg) else dbg["att_g"]):
                    d = (1, 4, 16)[g]
                    ntl = (S // d) // 128
                    qT, r_q = qTs[nbuf % 2]
                    kT, r_k = kTs[nbuf % 2]
                    vt, r_v = vts[nbuf % 2]
                    ab, r_ab = abs_[nbuf % 2]
                    nbuf += 1
                    P.dma("sp", qT[0:64, 0, :], proj_d[g * 4 + j, 0:64, :], [R_proj[g * 4 + j]], [r_q], partial=False)
                    P.dma("sp", qT[64:128, 1, :], proj_d[g * 4 + j, 64:128, :], [R_proj[g * 4 + j]], [r_q])
                    P.dma("sp", kT[:], proj_d[12 + g * 4 + j, :, :], [R_proj[12 + g * 4 + j]], [r_k], partial=False)
                    P.dma("sp", vs[:], vtm_d[g, j, :, :, :], [R_vtm[g]], [r_vs], partial=False)
                    P.op("pool", lambda e: e.tensor_copy(out=vt[:, :, 0, 0:64], in_=vs[:, :, 0:64]), [r_vs], [r_v], partial=False)
                    P.op("pool", lambda e: e.tensor_copy(out=vt[:, :, 1, 64:128], in_=vs[:, :, 64:128]), [r_vs], [r_v], partial=True)
                    P.dma("sp", ab[:], C["abias"][:, g * 4 + j, :, :, :].rearrange("p k h q -> p k (h q)"), [R_in], [r_ab], partial=False)
                    qv = [qT[:, hh, :].rearrange("p (i d) -> p d i", d=d) for hh in range(2)]
                    kv = [kT[:, :].rearrange("p (i d) -> p d i", d=d) for hh in range(2)]
                    accv = Oacc[:].rearrange("p c (i d) -> p c d i", d=d)
                    tiles = [(r_, u) for r_ in range(d) for u in range(ntl)]

                    def stageA(r_, u):
                        kts = [kt for kt in range(3) if 0 <= u + kt - 1 < ntl]
                        outl = []
                        for kt in kts:
                            ku = u + kt - 1
                            rr["sc"] = (rr.get("sc", 0) + 1) % 4
                            ps, r_ps = psf[rr["sc"]]
                            for hh in range(2):
                                P.op("pe", lambda e, hh=hh: e.matmul(ps[:, hh * 128:(hh + 1) * 128], lhsT=ident[:],
                                                                     rhs=ab[:, kt, hh * 128:(hh + 1) * 128], start=True, stop=False),
                                     [r_ident, r_ab], [r_ps], sig=False, partial=(hh > 0))
                                P.op("pe", lambda e, hh=hh: e.matmul(
                                    ps[:, hh * 128:(hh + 1) * 128], lhsT=kv[hh][:, r_, ku * 128:(ku + 1) * 128],
                                    rhs=qv[hh][:, r_, u * 128:(u + 1) * 128], start=False, stop=True),
                                    [r_k, r_q], [r_ps], sig=(hh == 1), partial=True)
                            rr["pt"] = (rr.get("pt", 0) + 1) % 8
                            pT, r_pT = pTs[rr["pt"]]
                            P.op("act", lambda e: e.activation(out=pT[:], in_=ps[:, 0:256], func=AF.Exp, scale=0.125),
                                 [r_ps], [r_pT], partial=False)
                            outl.append((r_ * ntl + ku, pT, r_pT))
                        return outl

                    def stageB(r_, u, pl):
                        rr["po"] = (rr.get("po", 0) + 1) % 2
                        po, r_po = psf[4 + rr["po"]]
                        n = len(pl) * 2
                        for part in range(2):
                            i = 0
                            for (ti, pT, r_pT) in pl:
                                for hh in range(2):
                                    lt = vt[:, ti, hh, :] if part == 0 else onesel[:, hh, :]
                                    P.op("pe", lambda e, hh=hh, lt=lt, i=i: e.matmul(
                                        po[:, part * 128:(part + 1) * 128], lhsT=lt, rhs=pT[:, hh * 128:(hh + 1) * 128],
                                        start=(i == 0), stop=(i == n - 1)),
                                        [r_v, r_onesel, r_pT], [r_po], sig=(part == 1 and i == n - 1), partial=not (part == 0 and i == 0))
                                    i += 1
                        av = accv[:, :, r_, u * 128:(u + 1) * 128]
                        pv = po[:, 0:256].rearrange("p (c q) -> p c q", c=2)
                        if g == 0:
                            P.op("dve", lambda e: e.tensor_copy(out=av, in_=pv), [r_po], [r_O], partial=True)
                        else:
                            P.op("dve", lambda e: e.tensor_add(out=av, in0=pv, in1=av), [r_po, r_O], [r_O], partial=True)

                    amode = dbg.get("att_mode", 3) if dbg else 3
                    prev = None
                    for (r_, u) in tiles:
                        cur = (r_, u, stageA(r_, u))
                        if prev is not None and amode >= 2:
                            stageB(*prev)
                        prev = cur
                    if amode >= 2:
                        stageB(*prev)
                P.dma("sp", gs[:], proj_d[36 + j, :, :], [R_proj[36 + j]], [r_gs], partial=False)
                for tb in range(8):
                    sl = slice(tb * 512, (tb + 1) * 512)
                    P.op("dve", lambda e: e.reciprocal(out=rz[:], in_=Oacc[:, 1, sl]), [r_O], [r_rz], partial=False)
                    P.op("dve", lambda e: e.tensor_mul(out=tm[:], in0=Oacc[:, 0, sl], in1=rz[:]), [r_O, r_rz], [r_tm], partial=False)
                    P.op("pool", lambda e: e.tensor_mul(out=yb[:, sl], in0=tm[:], in1=gs[:, sl]), [r_tm, r_gs], [r_yb], partial=(tb > 0))
                P.dma("sp", yattn_d[j, :, :], yb[:], [r_yb], [R_yattn[j]], partial=False, sem_of=r_yb)
            P.barrier()
        if stop_after == "att":
            break
        with ExitStack() as ph:
            pb, r_pb = sb("pb", [128, S], BF16, ph)
            uf, r_uf = sb("uf", [128, 2048], F32, ph)
            ub, r_ub = sb("ub", [128, S], BF16, ph)
            zt, r_zt = sb("zt", [128, S], BF16, ph)
            xg, r_xg = sb("xg", [128, S], BF16, ph)
            CEf, r_CE = sb("CEf", [128, 8192], BF16, ph)
            Xb, r_X = sb("Xb", [128, 2, 32, 128], BF16, ph)
            Tt, r_T = sb("Tt", [128, 32, 2, 128], BF16, ph)
            Hb = [sb(f"Hb{i}", [128, 2, 8, 128], BF16, ph) for i in range(2)]
            tp = [sb(f"tp{i}", [128, 8, 128], F32, ph) for i in range(4)]
            fb1, r_fb1 = sb("fb1", [128, 2, 128], F32, ph)
            fb4, r_fb4 = sb("fb4", [128, 2, 32, 4, 4], F32, ph)
            e1, r_e1 = sb("e1", [128, 32, 4, 4], F32, ph)
            e2, r_e2 = sb("e2", [128, 32, 4, 4], F32, ph)
            gsT, r_gsT = sb("gsT", [128, S], BF16, ph)
            yT, r_yT = sb("yT", [128, S], BF16, ph)
            P.dma("sp", Tt[:], C["T"][:, :, :, :], [R_in], [r_T], partial=False)

            def conv3(chunk, widx, dst, r_dst, fftl):
                P.dma("sp", pb[:], proj_d[chunk, :, :], [R_proj[chunk]], [r_pb], partial=False)
                w0 = convw[:, l, 0, widx:widx + 1]
                w1 = convw[:, l, 1, widx:widx + 1]
                w2 = convw[:, l, 2, widx:widx + 1]
                cb = convb[:, l, widx:widx + 1]
                for hb_ in range(2):
                    t0 = hb_ * 2048
                    P.op("act", lambda e: e.activation(out=uf[:, :], in_=pb[:, t0:t0 + 2048], func=AF.Identity, scale=w1, bias=cb),
                         [r_pb, r_convw, r_convb], [r_uf], partial=False)
                    a = 1 if hb_ == 0 else 0
                    P.op("dve", lambda e: e.scalar_tensor_tensor(out=uf[:, a:2048], in0=pb[:, t0 + a - 1:t0 + 2047], scalar=w0,
                                                                 in1=uf[:, a:2048], op0=ALU.mult, op1=ALU.add),
                         [r_pb, r_convw, r_uf], [r_uf], partial=False)
                    b_ = 2047 if hb_ == 1 else 2048
                    P.op("dve", lambda e: e.scalar_tensor_tensor(out=ub[:, t0:t0 + b_], in0=pb[:, t0 + 1:t0 + b_ + 1], scalar=w2,
                                                                 in1=uf[:, 0:b_], op0=ALU.mult, op1=ALU.add),
                         [r_pb, r_convw, r_uf], [r_ub], partial=(hb_ > 0))
                    if hb_ == 1:
                        P.op("dve", lambda e: e.tensor_copy(out=ub[:, S - 1:S], in_=uf[:, 2047:2048]), [r_uf], [r_ub], partial=True)
                to_token_major(ub, r_ub, dst, r_dst, fftl)

            def pointwise(o, c):
                for blk in range(4):
                    hb, r_hb = Hb[blk % 2]
                    P.dma("sp", hb[:], H_d[l, o, c, :, :, blk * 8:(blk + 1) * 8, :], [R_H[l][o][c]], [r_hb], partial=False)
                    xr, xi = Xb[:, 0, blk * 8:(blk + 1) * 8, :], Xb[:, 1, blk * 8:(blk + 1) * 8, :]
                    hr, hi = hb[:, 0, :, :], hb[:, 1, :, :]
                    (t1, r1), (t2, r2), (t3, r3), (t4, r4) = tp
                    P.op("dve", lambda e: e.tensor_mul(out=t1[:], in0=xr, in1=hr), [r_X, r_hb], [r1], partial=False)
                    P.op("dve", lambda e: e.tensor_mul(out=t2[:], in0=xi, in1=hi), [r_X, r_hb], [r2], partial=False)
                    P.op("pool", lambda e: e.tensor_mul(out=t3[:], in0=xr, in1=hi), [r_X, r_hb], [r3], partial=False)
                    P.op("pool", lambda e: e.tensor_mul(out=t4[:], in0=xi, in1=hr), [r_X, r_hb], [r4], partial=False)
                    P.op("dve", lambda e: e.tensor_sub(out=xr, in0=t1[:], in1=t2[:]), [r1, r2], [r_X], partial=True)
                    P.op("dve", lambda e: e.tensor_add(out=xi, in0=t3[:], in1=t4[:]), [r3, r4], [r_X], partial=True)

            clist = range(4) if not (dbg and "hy_c" in dbg) else dbg["hy_c"]
            for c in clist:
                for o in range(2):
                    P.dma("sp", fb1[:, o, :], bc(I["f_bias_r"], (l * 2 + o) * 512 + c * 128, 128), [R_in], [r_fb1], partial=(o > 0))
                for o in range(2):
                    for i4 in range(4):
                        P.op("dve", lambda e: e.tensor_copy(out=fb4[:, o, :, i4, :], in_=fb1[:, o, :].rearrange("p (g c) -> p g c", c=4)),
                             [r_fb1], [r_fb4], partial=not (o == 0 and i4 == 0))
                conv3(40 + c, c, zt, r_zt, True)
                conv3(44 + c, 4 + c, xg, r_xg, False)
                hmode = dbg.get("hy_mode", 9) if dbg else 9
                for o in range(2):
                    if hmode < 2:
                        break
                    fft_fwd(zt, r_zt, None, None, CEf, r_CE, Xb, r_X, tp)
                    if hmode < 3:
                        break
                    pointwise(o, c)
                    if hmode < 4:
                        break

                    def epi(nb, pyv, r_py, o=o):
                        zs = zt[:, :].rearrange("p (g n c) -> p g n c", g=32, n=32, c=4)[:, :, nb * 4:(nb + 1) * 4, :]
                        xs = xg[:, :].rearrange("p (n g c) -> p g n c", n=32, g=32, c=4)[:, :, nb * 4:(nb + 1) * 4, :]
                        pyf = pyv.rearrange("p n (g c) -> p g n c", c=4)
                        P.op("dve", lambda e: e.tensor_mul(out=e1[:], in0=zs, in1=fb4[:, o, :, :, :]), [r_zt, r_fb4], [r_e1], partial=False)
                        P.op("dve", lambda e: e.tensor_add(out=e2[:], in0=pyf, in1=e1[:]), [r_py, r_e1], [r_e2], partial=False)
                        if o == 0:
                            P.op("dve", lambda e: e.tensor_mul(out=zs, in0=e2[:], in1=xs), [r_e2, r_xg], [r_zt], partial=True)
                        else:
                            P.op("dve", lambda e: e.tensor_mul(out=xs, in0=e2[:], in1=xs), [r_e2, r_xg], [r_xg], partial=True)

                    fft_inv(Xb, r_X, CEf, r_CE, Tt, r_T, epi)
                    if o == 0:
                        conv3(48 + c, 8 + c, xg, r_xg, False)
                P.dma("sp", gsT[:], proj_d[52 + c, :, :], [R_proj[52 + c]], [r_gsT], partial=False)
                yv = yT[:, :].rearrange("p (a b) -> p b a", b=32)
                gv = gsT[:, :].rearrange("p (a b) -> p b a", b=32)
                for nb in range(4):
                    pt, r_pt = next_psb()
                    for ni in range(8):
                        n1 = nb * 8 + ni
                        P.op("pe", lambda e: e.transpose(out=pt[:, ni * 128:(ni + 1) * 128], in_=xg[:, n1 * 128:(n1 + 1) * 128], identity=ident[:]),
                             [r_xg, r_ident], [r_pt], sig=(ni == 7), partial=(ni > 0))
                    P.op("dve", lambda e: e.tensor_mul(out=yv[:, nb * 8:(nb + 1) * 8, :], in0=pt[:, :].rearrange("p (n c) -> p n c", n=8),
                                                       in1=gv[:, nb * 8:(nb + 1) * 8, :]), [r_pt, r_gsT], [r_yT], partial=(nb > 0))
                P.dma("sp", yhy_d[c, :, :], yT[:], [r_yT], [R_yhy[c]], partial=False, sem_of=r_yT)
            P.barrier()
        if stop_after == "hy":
            break
        with ExitStack() as ph:
            wpa, r_wpa = sb("wpa", [128, 4, D], BF16, ph)
            wph, r_wph = sb("wph", [128, 4, D], BF16, ph)
            wo, r_wo = sb("wo", [128, 8, D], BF16, ph)
            rowb, r_rowb = sb("rowb", [128, 3, D], F32, ph)
            with ExitStack() as ph2:
                wst, r_wst = sb("wstg", [128, 8, D], F32, ph2)
                P.dma("sp", wst[:, 0:4, :], I["w_pa"][l], [R_in], [r_wst], partial=False)
                P.op("pool", lambda e: e.tensor_copy(out=wpa[:], in_=wst[:, 0:4, :]), [r_wst], [r_wpa], partial=False)
                P.dma("sp", wst[:, 4:8, :], I["w_ph"][l], [R_in], [r_wst], partial=False)
                P.op("pool", lambda e: e.tensor_copy(out=wph[:], in_=wst[:, 4:8, :]), [r_wst], [r_wph], partial=False)
                P.dma("sp", wst[:, :, :], I["w_out"][l], [R_in], [r_wst], partial=False)
                for kc in range(8):
                    P.op("pool" if kc % 2 else "dve", lambda e: e.tensor_mul(out=wo[:, kc, :], in0=wst[:, kc, :], in1=gate_b[:, l, :]),
                         [r_wst, r_gate], [r_wo], partial=(kc > 0))
                P.barrier()
            P.dma("sp", rowb[:, 0, :], bc(I["b_out_r"], l * D, D), [R_in], [r_rowb], partial=False)
            P.dma("sp", rowb[:, 1, :], bc(I["ln_g_r"], l * D, D), [R_in], [r_rowb])
            P.dma("sp", rowb[:, 2, :], bc(I["ln_b_r"], l * D, D), [R_in], [r_rowb])
            gb, r_gb = sb("gb", [128, D], F32, ph)
            P.op("dve", lambda e: e.tensor_mul(out=gb[:], in0=rowb[:, 0, :], in1=gate_b[:, l, :]), [r_rowb, r_gate], [r_gb], partial=False)
            nb_, r_nb = sb("nbias", [128, 1], F32, ph)
            ya = [sb(f"ya{i}", [128, 4, 512], BF16, ph) for i in range(2)]
            yh = [sb(f"yh{i}", [128, 4, 512], BF16, ph) for i in range(2)]
            ga = [sb(f"ga{i}", [128, 16, 512], BF16, ph) for i in range(2)]
            mT, r_mT = sb("mT", [128, 8, 512], BF16, ph)
            m1, r_m1 = sb("m1", [128, 512], F32, ph)
            m2, r_m2 = sb("m2", [128, 512], F32, ph)
            xr_ = [sb(f"xr{i}", [128, D], F32, ph) for i in range(2)]
            rs_ = [sb(f"rs{i}", [128, D], F32, ph) for i in range(2)]
            o1, r_o1 = sb("o1", [128, 512], F32, ph)
            stt, r_st = sb("mst", [128, 12], F32, ph)
            mvv, r_mv = sb("mmv", [128, 2], F32, ph)
            xo = [sb(f"xo{i}", [128, D], F32, ph) for i in range(2)]
            tbl = range(8) if not (dbg and "merge_tb" in dbg) else dbg["merge_tb"]
            nt = 0
            for tb in tbl:
                sl = slice(tb * 512, (tb + 1) * 512)
                yat, r_ya = ya[tb % 2]
                yht, r_yh = yh[tb % 2]
                gat, r_ga = ga[tb % 2]
                P.dma("sp", yat[:], yattn_d[:, :, sl].rearrange("c p t -> p c t"), R_yattn, [r_ya], partial=False)
                P.dma("sp", yht[:], yhy_d[:, :, sl].rearrange("c p t -> p c t"), R_yhy, [r_yh], partial=False)
                P.dma("sp", gat[:], proj_d[56:72, :, sl].rearrange("c p t -> p c t"), R_proj[56:72], [r_ga], partial=False)
                for fc in range(8):
                    pa, r_pa = next_psf()
                    pq, r_pq = next_psf()
                    for kc in range(4):
                        P.op("pe", lambda e: e.matmul(pa[:, :], lhsT=wpa[:, kc, fc * 128:(fc + 1) * 128], rhs=yat[:, kc, :],
                                                      start=(kc == 0), stop=(kc == 3)), [r_wpa, r_ya], [r_pa], sig=(kc == 3), partial=(kc > 0))
                    for kc in range(4):
                        P.op("pe", lambda e: e.matmul(pq[:, :], lhsT=wph[:, kc, fc * 128:(fc + 1) * 128], rhs=yht[:, kc, :],
                                                      start=(kc == 0), stop=(kc == 3)), [r_wph, r_yh], [r_pq], sig=(kc == 3), partial=(kc > 0))
                    P.op("dve", lambda e: e.tensor_mul(out=m1[:], in0=pa[:, :], in1=gat[:, fc, :]), [r_pa, r_ga], [r_m1], partial=False)
                    P.op("dve", lambda e: e.tensor_mul(out=m2[:], in0=pq[:, :], in1=gat[:, 8 + fc, :]), [r_pq, r_ga], [r_m2], partial=False)
                    P.op("pool", lambda e: e.tensor_add(out=mT[:, fc, :], in0=m1[:], in1=m2[:]), [r_m1, r_m2], [r_mT], partial=(fc > 0))
                for tt in range(4):
                    row0 = tb * 512 + tt * 128
                    xrt, r_xr = xr_[nt % 2]
                    rst, r_rs = rs_[nt % 2]
                    xot, r_xo = xo[nt % 2]
                    nt += 1
                    P.dma("sp", xrt[:], x_src[row0:row0 + 128, :], [R_xsrc], [r_xr], partial=False)
                    P.op("dve", lambda e: e.scalar_tensor_tensor(out=xrt[:], in0=xrt[:], scalar=ALPHA, in1=gb[:], op0=ALU.mult, op1=ALU.add),
                         [r_xr, r_gb], [r_xr], partial=False)
                    for hf_ in range(2):
                        hs = slice(hf_ * 512, (hf_ + 1) * 512)
                        po_, r_po = next_psf()
                        for kc in range(8):
                            P.op("pe", lambda e: e.matmul(po_[:, :], lhsT=mT[:, kc, tt * 128:(tt + 1) * 128], rhs=wo[:, kc, hs],
                                                          start=(kc == 0), stop=(kc == 7)), [r_mT, r_wo], [r_po], sig=(kc == 7), partial=(kc > 0))
                        P.op("dve", lambda e: e.tensor_add(out=rst[:, hs], in0=po_[:, :], in1=xrt[:, hs]), [r_po, r_xr], [r_rs], partial=(hf_ > 0))
                    P.op("dve", lambda e: e.bn_stats(out=stt[:, 0:6], in_=rst[:, 0:512]), [r_rs], [r_st], partial=False)
                    P.op("dve", lambda e: e.bn_stats(out=stt[:, 6:12], in_=rst[:, 512:1024]), [r_rs], [r_st], partial=True)
                    P.op("dve", lambda e: e.bn_aggr(out=mvv[:], in_=stt[:]), [r_st], [r_mv], partial=False)
                    P.op("act", lambda e: e.activation(out=mvv[:, 1:2], in_=mvv[:, 1:2], func=AF.Sqrt, bias=epsT[:], scale=1.0),
                         [r_mv, r_eps], [r_mv], partial=False)
                    P.op("dve", lambda e: e.reciprocal(out=mvv[:, 1:2], in_=mvv[:, 1:2]), [r_mv], [r_mv], partial=False)
                    P.op("dve", lambda e: e.tensor_scalar(out=nb_[:], in0=mvv[:, 0:1], scalar1=-1.0, scalar2=mvv[:, 1:2],
                                                          op0=ALU.mult, op1=ALU.mult), [r_mv], [r_nb], partial=False)
                    P.op("act", lambda e: e.activation(out=rst[:], in_=rst[:], func=AF.Identity, scale=mvv[:, 1:2], bias=nb_[:]),
                         [r_rs, r_mv, r_nb], [r_rs], partial=False)
                    P.op("dve", lambda e: e.tensor_mul(out=rst[:], in0=rst[:], in1=rowb[:, 1, :]), [r_rs, r_rowb], [r_rs], partial=False)
                    P.op("pool", lambda e: e.tensor_add(out=xot[:], in0=rst[:], in1=rowb[:, 2, :]), [r_rs, r_rowb], [r_xo], partial=False)
                    P.dma("sp", x_dst[row0:row0 + 128, :], xot[:], [r_xo], [R_xdst], sem_of=r_xo)
            P.barrier()
        x_src, R_xsrc = x_dst, R_xdst
    P.barrier()
    stack.close()
    return nc, dbg_out


_CACHE = {}


def kernel(**inputs):
    inp = {k: np.asarray(v, dtype=np.float32) for k, v in inputs.items()}
    consts = _const_tables()
    if "nc" not in _CACHE:
        _CACHE["nc"] = build()[0]
    nc = _CACHE["nc"]
    in_maps = []
    for b in range(8):
        m = _layout_inputs(inp, b)
        for k, v in consts.items():
            m["c_" + k] = v
        in_maps.append(m)
    res = run_bass_kernel_spmd(nc, in_maps, core_ids=list(range(8)))
    return np.stack([np.asarray(r["out"], dtype=np.float32) for r in res.results], axis=0)
```

```python
import math
from contextlib import ExitStack
import numpy as np
import ml_dtypes
import concourse.bass as bass
import concourse.mybir as mybir
from concourse.bass_utils import run_bass_kernel_spmd

F32 = mybir.dt.float32
BF16 = mybir.dt.bfloat16
AF = mybir.ActivationFunctionType
ALU = mybir.AluOpType
NPBF = ml_dtypes.bfloat16

S = 4096
D = 1024
NIN = 9216
DEPTH = 2
ALPHA = (2 * DEPTH) ** 0.25
EPS = 1e-5
NFFT = 8192
TWO_PI = 2.0 * math.pi


class Res:
    __slots__ = ("name", "writers", "readers", "prev", "sem", "dcount")

    def __init__(self, name, sem=None):
        self.name = name
        self.writers = []
        self.readers = []
        self.prev = []
        self.sem = sem if sem is not None else {}
        self.dcount = 0


class Prog:
    ENG = ("pe", "act", "dve", "pool", "sp")

    def __init__(self, nc, stack):
        self.nc = nc
        self.stack = stack
        self.e = {"pe": nc.tensor, "act": nc.scalar, "dve": nc.vector, "pool": nc.gpsimd, "sp": nc.sync}
        self.info = []
        self.pending = {k: [] for k in self.ENG}
        self.esem = {}
        self.ecount = {k: 0 for k in self.ENG}
        self.known = {k: {} for k in self.ENG}
        self.nsem = 0
        self.dma_since_barrier = []
        self.last_sig = {k: None for k in self.ENG}

    def new_sem(self, name):
        self.nsem += 1
        return self.stack.enter_context(self.nc.semaphore(f"{name}_{self.nsem}"))

    def _deps(self, reads, writes, partial):
        deps = set()
        for r in reads:
            deps.update(r.writers)
        for w in writes:
            if partial and not w.readers:
                deps.update(w.prev)
            else:
                w.prev = w.writers + w.readers
                deps.update(w.prev)
                w.writers = []
                w.readers = []
        return deps

    def _commit(self, oid, reads, writes):
        for r in reads:
            r.readers.append(oid)
        for w in writes:
            w.writers.append(oid)

    def _waits(self, eng, deps, is_dma):
        need = {}
        for d in deps:
            inf = self.info[d]
            assert inf is not None, "dependency on a non-signalling op"
            deng, sem, val = inf
            if deng == "pe" and eng == "pe" and not is_dma:
                continue
            k = id(sem)
            if k not in need or need[k][1] < val:
                need[k] = (sem, val)
        for k, (sem, val) in need.items():
            if self.known[eng].get(k, 0) >= val:
                continue
            self.e[eng].wait_ge(sem, val)
            self.known[eng][k] = val

    def op(self, eng, fn, reads=(), writes=(), sig=True, partial=False):
        deps = self._deps(reads, writes, partial)
        self._waits(eng, deps, False)
        ins = fn(self.e[eng])
        oid = len(self.info)
        if sig:
            if self.ecount[eng] % 30000 == 0:
                self.esem[eng] = self.new_sem("e" + eng)
                self.ecount[eng] = 0
            self.ecount[eng] += 1
            ins.then_inc(self.esem[eng], 1)
            inf = (eng, self.esem[eng], self.ecount[eng])
            self.info.append(inf)
            for p in self.pending[eng]:
                self.info[p] = inf
            self.pending[eng] = []
            self.last_sig[eng] = oid
        else:
            self.info.append(None)
            self.pending[eng].append(oid)
        self._commit(oid, reads, writes)
        return oid

    def dma(self, eng, out, in_, reads, writes, partial=True, sem_of=None, **kw):
        assert len(writes) == 1
        w = sem_of if sem_of is not None else writes[0]
        deps = self._deps(reads, writes, partial)
        self._waits(eng, deps, True)
        kind = "sw" if eng == "pool" else "hw"
        if kind not in w.sem:
            w.sem[kind] = [self.new_sem("d" + kind), 0]
        w.sem[kind][1] += 1
        self.e[eng].dma_start(out=out, in_=in_, **kw).then_inc(w.sem[kind][0], 16)
        oid = len(self.info)
        self.info.append(("dma", w.sem[kind][0], 16 * w.sem[kind][1]))
        self.dma_since_barrier.append(oid)
        self._commit(oid, reads, writes)
        return oid

    def barrier(self):
        deps = set(self.dma_since_barrier)
        for k in self.ENG:
            assert not self.pending[k], "pending non-signalled ops at barrier"
            if self.last_sig[k] is not None:
                deps.add(self.last_sig[k])
        for k in self.ENG:
            need = {}
            for d in deps:
                deng, sem, val = self.info[d]
                kk = id(sem)
                if kk not in need or need[kk][1] < val:
                    need[kk] = (sem, val)
            for kk, (sem, val) in need.items():
                if self.known[k].get(kk, 0) >= val:
                    continue
                self.e[k].wait_ge(sem, val)
                self.known[k][kk] = val
        self.dma_since_barrier = []


def _const_tables():
    c = {}
    c["ident"] = np.eye(128, dtype=np.float32).astype(NPBF)
    dil = (1, 4, 16)
    p = np.arange(128)[:, None]
    j = np.arange(128)[None, :]
    tab = np.zeros((128, 12, 3, 2, 128), np.float32)
    for g in range(3):
        for h in range(8):
            slope = 2.0 ** (-(h + 1))
            for kt in range(3):
                rel = p + (kt - 1) * 128 - j
                val = -8.0 * slope * dil[g] * np.abs(rel)
                val = np.where(np.abs(rel) <= 64, val, -32768.0)
                tab[:, g * 4 + h // 2, kt, h % 2, :] = val
    c["abias"] = tab.astype(NPBF)
    osel = np.zeros((128, 2, 128), np.float32)
    osel[:, 0, :64] = 1.0
    osel[:, 1, 64:] = 1.0
    c["onesel"] = osel.astype(NPBF)
    n2 = np.arange(128)[:, None].astype(np.float64)
    k2 = np.arange(128)[None, :].astype(np.float64)
    ang = -2.0 * np.pi * n2 * (k2 + 0.5) / 256.0
    Fr, Fi = np.cos(ang), np.sin(ang)
    c["F1"] = np.concatenate([Fr, Fi, -Fi], axis=1).astype(np.float32).astype(NPBF)
    ang2 = -2.0 * np.pi * (n2 + 128.0) * (k2 + 0.5) / 256.0
    Fr2, Fi2 = -np.cos(ang2), -np.sin(ang2)
    c["F1hi"] = np.concatenate([Fr2, Fi2, -Fi2], axis=1).astype(np.float32).astype(NPBF)
    n1 = np.arange(32).astype(np.float64)
    k1 = np.arange(32).astype(np.float64)
    eye4 = np.eye(4)
    a = -2.0 * np.pi * n1[:, None] * k1[None, :] / 32.0
    F32m = np.zeros((128, 3, 128), np.float32)
    F32m[:, 0, :] = np.kron(np.cos(a), eye4)
    F32m[:, 1, :] = np.kron(np.sin(a), eye4)
    F32m[:, 2, :] = -np.kron(np.sin(a), eye4)
    c["F32m"] = F32m.astype(NPBF)
    kk2 = np.arange(128).astype(np.float64)
    a = -2.0 * np.pi * np.repeat(n1, 4)[:, None] * (kk2[None, :] + 0.5) / NFFT
    tw = np.zeros((128, 2, 8, 128), np.float32)
    tw[:, 0, :, :] = np.cos(a)[:, None, :]
    tw[:, 1, :, :] = np.sin(a)[:, None, :]
    c["tw8"] = tw
    a = 2.0 * np.pi * k1[:, None] * n1[None, :] / 32.0
    Rr = np.kron(np.cos(a), eye4)
    Ri = np.kron(np.sin(a), eye4)
    R12 = np.zeros((128, 2, 256), np.float32)
    R12[:, 0, :128], R12[:, 0, 128:] = Rr, Ri
    R12[:, 1, :128], R12[:, 1, 128:] = -Ri, Rr
    c["R12"] = R12.astype(NPBF)
    T = np.zeros((128, 32, 2, 128), np.float32)
    kk = np.arange(128)[:, None].astype(np.float64)
    nn2 = np.arange(128)[None, :].astype(np.float64)
    for a1 in range(32):
        a = 2.0 * np.pi * (a1 + 32.0 * nn2) * (kk + 0.5) / NFFT
        T[:, a1, 0, :] = (2.0 / NFFT) * np.cos(a)
        T[:, a1, 1, :] = -(2.0 / NFFT) * np.sin(a)
    c["T"] = T.astype(NPBF)
    L = S
    t = np.linspace(0.0, 1.0, L, dtype=np.float32)[:, None]
    bands = 16
    w = (2.0 * np.pi * np.arange(L, dtype=np.float32)[:, None] / L).astype(np.float32)
    f = np.linspace(1e-4, bands - 1, bands, dtype=np.float32)[None, :]
    feat = np.concatenate([t, np.cos(f * w), -np.sin(f * w)], axis=-1).astype(np.float32)
    featT = np.ascontiguousarray(feat.T)
    rev = np.zeros_like(featT)
    rev[:, 1:] = featT[:, :0:-1]
    c["featT"] = np.stack([featT, rev], 0)
    trow = np.zeros((2, 1, L), np.float32)
    trow[0, 0] = t[:, 0]
    trow[1, 0, 1:] = t[:0:-1, 0]
    c["trow"] = trow
    deltas = np.linspace(math.log(1e-2) / 1.5, math.log(1e-2) / 0.3, 512, dtype=np.float32)
    c["ndelta"] = np.ascontiguousarray((-np.abs(deltas)).reshape(4, 128).T)
    return c


CONST_DT = {"ident": BF16, "abias": BF16, "onesel": BF16, "F1": BF16, "F1hi": BF16, "F32m": BF16, "tw8": F32,
            "R12": BF16, "T": BF16, "featT": F32, "trow": F32, "ndelta": F32}


def _layout_inputs(inp, b):
    m = {}
    m["x"] = np.ascontiguousarray(inp["x"][b])
    m["crep"] = np.ascontiguousarray(np.broadcast_to(
        inp["c"][b].reshape(8, 128).T[:, :, None], (128, 8, 128))).astype(np.float32)
    m["ccol"] = np.ascontiguousarray(inp["c"][b].reshape(8, 128).T)

    def kt(w, kc):
        Ld, K, N = w.shape
        return np.ascontiguousarray(w.reshape(Ld, kc, 128, N).transpose(0, 2, 1, 3))

    m["w_ada"] = kt(inp["w_ada"], 8)
    m["w_in"] = kt(inp["w_in"], 8)
    m["w_pa"] = kt(inp["w_proj_attn"], 4)
    m["w_ph"] = kt(inp["w_proj_hyena"], 4)
    m["w_out"] = kt(inp["w_out"], 8)

    def col(v):
        Ld, N = v.shape
        return np.ascontiguousarray(v.reshape(Ld, N // 128, 128).transpose(0, 2, 1))

    m["b_ada_c"] = col(inp["b_ada"])
    m["b_ada_r"] = np.ascontiguousarray(inp["b_ada"][:, None, :])
    m["b_in_c"] = col(inp["b_in"])
    m["b_in_r"] = np.ascontiguousarray(inp["b_in"][:, None, :])
    m["conv_w_c"] = np.ascontiguousarray(inp["conv_w"].reshape(DEPTH, 3, 12, 128).transpose(0, 3, 1, 2))
    m["conv_b_c"] = col(inp["conv_b"])
    m["f_w1"] = np.ascontiguousarray(inp["filt_w1"])
    m["f_w2"] = np.ascontiguousarray(inp["filt_w2"])
    m["f_w3"] = np.ascontiguousarray(inp["filt_w3"])
    m["f_w4"] = np.ascontiguousarray(inp["filt_w4"])
    m["f_b"] = np.ascontiguousarray(np.stack([inp["filt_b1"], inp["filt_b2"], inp["filt_b3"], inp["filt_freq"]], -1))
    m["f_bias_r"] = np.ascontiguousarray(inp["filt_bias"])
    m["b_out_r"] = np.ascontiguousarray(inp["b_out"][:, None, :])
    m["ln_g_r"] = np.ascontiguousarray(inp["ln_g"][:, None, :])
    m["ln_b_r"] = np.ascontiguousarray(inp["ln_b"][:, None, :])
    return m


IN_SHAPES = {
    "x": [S, D], "crep": [128, 8, 128], "ccol": [128, 8],
    "w_ada": [DEPTH, 128, 8, 3072], "w_in": [DEPTH, 128, 8, NIN], "w_pa": [DEPTH, 128, 4, D],
    "w_ph": [DEPTH, 128, 4, D], "w_out": [DEPTH, 128, 8, D],
    "b_ada_c": [DEPTH, 128, 24], "b_ada_r": [DEPTH, 1, 3072], "b_in_c": [DEPTH, 128, 72],
    "b_in_r": [DEPTH, 1, NIN], "conv_w_c": [DEPTH, 128, 3, 12], "conv_b_c": [DEPTH, 128, 12],
    "f_w1": [DEPTH, 33, 64], "f_w2": [DEPTH, 64, 64], "f_w3": [DEPTH, 64, 64], "f_w4": [DEPTH, 64, 2048],
    "f_b": [DEPTH, 64, 4], "f_bias_r": [DEPTH, 2, 512], "b_out_r": [DEPTH, 1, D],
    "ln_g_r": [DEPTH, 1, D], "ln_b_r": [DEPTH, 1, D],
}
CONST_SHAPES = {"ident": [128, 128], "abias": [128, 12, 3, 2, 128], "onesel": [128, 2, 128], "F1": [128, 384],
                "F1hi": [128, 384], "F32m": [128, 3, 128], "tw8": [128, 2, 8, 128], "R12": [128, 2, 256], "T": [128, 32, 2, 128],
                "featT": [2, 33, S], "trow": [2, 1, S], "ndelta": [128, 4]}


def bc(ap_t, offset, n):
    return bass.AP(ap_t.tensor, offset, [[0, 128], [1, n]])


class K:
    pass


def build(dbg=None, layers=DEPTH, stop_after=None):
    nc = bass.Bass("TRN2", target_bir_lowering=False)
    try:
        nc.allow_low_precision("bf16 matmul operands with fp32 accumulation")
    except Exception:
        pass
    stack = ExitStack()
    P = Prog(nc, stack)
    I = {k: nc.dram_tensor(k, s, F32, kind="ExternalInput").ap() for k, s in IN_SHAPES.items()}
    C = {k: nc.dram_tensor("c_" + k, s, CONST_DT[k], kind="ExternalInput").ap() for k, s in CONST_SHAPES.items()}
    out = nc.dram_tensor("out", [S, D], F32, kind="ExternalOutput").ap()
    dbg_out = {}

    def dram(name, shape, dt):
        kind = "ExternalOutput" if (dbg and name in dbg) else "Internal"
        t = nc.dram_tensor(name, shape, dt, kind=kind).ap()
        if kind == "ExternalOutput":
            dbg_out[name] = t
        return t

    proj_d = dram("proj_d", [72, 128, S], BF16)
    vtm_d = dram("vtm_d", [3, 4, 128, 32, 128], BF16)
    yattn_d = dram("yattn_d", [4, 128, S], BF16)
    yhy_d = dram("yhy_d", [4, 128, S], BF16)
    xmid_d = dram("xmid_d", [S, D], F32)
    H_d = dram("H_d", [DEPTH, 2, 4, 128, 2, 32, 128], BF16)
    fam = {}
    R_proj = [Res(f"proj{i}", fam) for i in range(72)]
    R_vtm = [Res(f"vtm{g}", fam) for g in range(3)]
    R_yattn = [Res(f"yattn{i}", fam) for i in range(4)]
    R_yhy = [Res(f"yhy{i}", fam) for i in range(4)]
    R_xmid = Res("xmid", fam)
    R_H = [[[Res(f"H{l}{o}{h}", fam) for h in range(4)] for o in range(2)] for l in range(DEPTH)]
    R_out = Res("out")
    R_in = Res("inputs")

    RES = {}
    cnt = {"n": 0}

    def sb(name, shape, dt, st=None):
        cnt["n"] += 1
        t = (st or stack).enter_context(nc.sbuf_tensor(f"s_{name}_{cnt['n']}", shape, dt))
        if name not in RES:
            RES[name] = Res(name)
        return t, RES[name]

    psf = []
    for i in range(6):
        t = stack.enter_context(nc.psum_tensor(f"psf{i}", [128, 512], F32))
        psf.append((t, Res(f"psf{i}")))
    psb = []
    for i in range(2):
        t = stack.enter_context(nc.psum_tensor(f"psb{i}", [128, 1024], BF16))
        psb.append((t, Res(f"psb{i}")))
    rr = {"f": 0, "b": 0, "q": 0}

    def next_psf():
        rr["f"] = (rr["f"] + 1) % 6
        return psf[rr["f"]]

    def next_psb():
        rr["b"] = (rr["b"] + 1) % 2
        return psb[rr["b"]]

    DQ = ("sp", "pool")

    def next_q():
        rr["q"] = (rr["q"] + 1) % len(DQ)
        return DQ[rr["q"]]

    ident, r_ident = sb("ident", [128, 128], BF16)
    onesel, r_onesel = sb("onesel", [128, 2, 128], BF16)
    F1, r_F1 = sb("F1", [128, 384], BF16)
    F1hi, r_F1hi = sb("F1hi", [128, 384], BF16)
    R12, r_R12 = sb("R12", [128, 2, 256], BF16)
    F32m, _ = sb("F32m", [128, 3, 128], BF16)
    tw8, _ = sb("tw8", [128, 2, 8, 128], F32)
    ones2, r_ones2 = sb("ones2", [2, 128], BF16)
    epsT, r_eps = sb("epsT", [128, 1], F32)
    npiT, r_npi = sb("npiT", [128, 1], F32)
    r_cst = Res("cst")
    r_ident = r_onesel = r_F1 = r_F1hi = r_R12 = r_cst
    P.dma("sp", ident[:], C["ident"][:, :], [R_in], [r_ident])
    P.dma("sp", onesel[:], C["onesel"][:, :, :], [R_in], [r_onesel])
    P.dma("sp", F1[:], C["F1"][:, :], [R_in], [r_F1])
    P.dma("sp", F1hi[:], C["F1hi"][:, :], [R_in], [r_F1hi])
    P.dma("sp", R12[:], C["R12"][:, :, :], [R_in], [r_R12])
    P.dma("sp", F32m[:], C["F32m"][:, :, :], [R_in], [r_cst])
    P.dma("sp", tw8[:], C["tw8"][:, :, :, :], [R_in], [r_cst])
    P.op("pool", lambda e: e.memset(ones2[:], 1.0), [], [r_ones2])
    P.op("pool", lambda e: e.memset(epsT[:], EPS), [], [r_eps])
    P.op("pool", lambda e: e.memset(npiT[:], -math.pi), [], [r_npi])

    modcol, r_modcol = sb("modcol", [128, DEPTH, 24], F32)
    sc1, r_sc1 = sb("sc1", [128, DEPTH, 8], F32)
    gate_b, r_gate = sb("gate_b", [128, DEPTH, D], F32)
    b_in_c, r_binc = sb("b_in_c", [128, DEPTH, 72], F32)
    convw, r_convw = sb("convw", [128, DEPTH, 3, 12], F32)
    convb, r_convb = sb("convb", [128, DEPTH, 12], F32)
    bin2, r_bin2 = sb("bin2", [1, DEPTH, 1536], BF16)
    binl, r_binl = sb("binl", [1, DEPTH, 1536], BF16)
    r_binc = r_convw = r_convb = r_cst
    for l in range(DEPTH):
        P.dma("sp", b_in_c[:, l, :], I["b_in_c"][l], [R_in], [r_binc])
        P.dma("sp", convw[:, l, :, :], I["conv_w_c"][l], [R_in], [r_convw])
        P.dma("sp", convb[:, l, :], I["conv_b_c"][l], [R_in], [r_convb])

    with ExitStack() as ph:
        ccol, r_ccol = sb("ccol", [128, 8], F32, ph)
        crep, r_crep = sb("crep", [128, 8, 128], F32, ph)
        badac, r_badac = sb("badac", [128, DEPTH, 24], F32, ph)
        badar, r_badar = sb("badar", [128, DEPTH, D], F32, ph)
        vb, r_vb = sb("vb", [1, DEPTH, 1536], F32, ph)
        vbh, r_vbh = sb("vbh", [1, DEPTH, 1536], F32, ph)
        r_ccol = r_crep = r_badac = r_badar = r_vb = r_cst
        P.dma("sp", ccol[:], I["ccol"][:, :], [R_in], [r_ccol])
        P.dma("sp", crep[:], I["crep"][:, :, :], [R_in], [r_crep])
        for l in range(DEPTH):
            P.dma("sp", badac[:, l, :], I["b_ada_c"][l], [R_in], [r_badac])
            P.dma("sp", badar[:, l, :], bc(I["b_ada_r"], l * 3072 + 2048, D), [R_in], [r_badar])
            P.dma("sp", vb[:, l, :], bass.AP(I["b_in_r"].tensor, l * NIN + 3072, [[0, 1], [1, 1536]]), [R_in], [r_vb])
        P.op("dve", lambda e: e.tensor_copy(out=bin2[:], in_=vb[:]), [r_vb], [r_bin2])
        P.op("dve", lambda e: e.tensor_copy(out=vbh[:], in_=bin2[:]), [r_bin2], [r_vbh])
        P.op("dve", lambda e: e.tensor_sub(out=vbh[:], in0=vb[:], in1=vbh[:]), [r_vb, r_vbh], [r_vbh])
        P.op("dve", lambda e: e.tensor_copy(out=binl[:], in_=vbh[:]), [r_vbh], [r_binl])
        wa = [sb(f"wa{i}", [128, 8, 512], F32, ph) for i in range(2)]
        for l in range(DEPTH):
            pc, r_pc = next_psf()
            for blk in range(6):
                wt, r_wt = wa[blk % 2]
                P.dma("sp", wt[:, 0:4, :], I["w_ada"][l, :, 0:4, blk * 512:(blk + 1) * 512], [R_in], [r_wt], partial=False)
                P.dma("pool", wt[:, 4:8, :], I["w_ada"][l, :, 4:8, blk * 512:(blk + 1) * 512], [R_in], [r_wt])
                for f in range(4):
                    fi = blk * 4 + f
                    for kc in range(8):
                        P.op("pe", lambda e, wt=wt, f=f, kc=kc, fi=fi: e.matmul(
                            pc[:, fi:fi + 1], lhsT=wt[:, kc, f * 128:(f + 1) * 128], rhs=ccol[:, kc:kc + 1],
                            start=(kc == 0), stop=(kc == 7)),
                            [r_wt, r_ccol], [r_pc], sig=(kc == 7), partial=True)
                if blk >= 4:
                    pg, r_pg = next_psf()
                    for kc in range(8):
                        P.op("pe", lambda e, wt=wt, kc=kc: e.matmul(
                            pg[:, :], lhsT=crep[:, kc, :], rhs=wt[:, kc, :], start=(kc == 0), stop=(kc == 7)),
                            [r_wt, r_crep], [r_pg], sig=(kc == 7), partial=True)
                    h0 = (blk - 4) * 512
                    P.op("dve", lambda e, l=l, h0=h0, pg=pg: e.tensor_add(
                        out=gate_b[:, l, h0:h0 + 512], in0=pg[:, :], in1=badar[:, l, h0:h0 + 512]),
                        [r_pg, r_badar], [r_gate], partial=True)
            P.op("dve", lambda e, l=l, pc=pc: e.tensor_add(out=modcol[:, l, :], in0=pc[:, 0:24], in1=badac[:, l, :]),
                 [r_pc, r_badac], [r_modcol], partial=True)
            P.op("dve", lambda e, l=l: e.tensor_scalar_add(out=sc1[:, l, :], in0=modcol[:, l, 8:16], scalar1=1.0),
                 [r_modcol], [r_sc1], partial=True)
        P.barrier()


    act_dve = {"n": 0}

    def evac(out_ap, in_ap, reads, writes, partial=True, simple=False):
        act_dve["n"] += 1
        if simple and act_dve["n"] % 2:
            P.op("act", lambda e: e.copy(out=out_ap, in_=in_ap), reads, writes, partial=partial)
        else:
            P.op("dve", lambda e: e.tensor_copy(out=out_ap, in_=in_ap), reads, writes, partial=partial)

    def fft_fwd(zl, r_zl, zh, r_zh, CEf, r_CE, X, r_X, tpb):
        Cv = CEf[:, 0:8192].rearrange("p (t c k) -> p t c k", t=2, c=32, k=128)
        for cp in range(16):
            pp, r_pp = next_psf()
            for gi in range(2):
                cg = cp * 2 + gi
                P.op("pe", lambda e: e.matmul(pp[:, gi * 256:(gi + 1) * 256], lhsT=zl[:, cg * 128:(cg + 1) * 128], rhs=F1[:, 0:256],
                                              start=True, stop=(zh is None)),
                     [r_zl, r_F1], [r_pp], sig=(zh is None and gi == 1), partial=(gi > 0))
                if zh is not None:
                    P.op("pe", lambda e: e.matmul(pp[:, gi * 256:(gi + 1) * 256], lhsT=zh[:, cg * 128:(cg + 1) * 128], rhs=F1hi[:, 0:256],
                                                  start=False, stop=True),
                         [r_zh, r_F1hi], [r_pp], sig=(gi == 1), partial=True)
            evac(Cv[:, :, cp * 2:cp * 2 + 2, :], pp[:, :].rearrange("p (g t k) -> p t g k", g=2, t=2, k=128), [r_pp], [r_CE],
                 partial=(cp > 0), simple=False)
        fmode = dbg.get("fft_mode", 9) if dbg else 9
        if fmode < 2:
            return
        (t1, r1), (t2, r2), (t3, r3), (t4, r4) = tpb
        bs = t1.shape[1]
        for blk in range(32 // bs):
            cs = slice(blk * bs, blk * bs + bs)
            cr, ci = Cv[:, 0, cs, :], Cv[:, 1, cs, :]
            P.op("dve", lambda e: e.tensor_mul(out=t1[:], in0=cr, in1=tw8[:, 0, 0:bs, :]), [r_CE, r_cst], [r1], partial=False)
            P.op("dve", lambda e: e.tensor_mul(out=t2[:], in0=ci, in1=tw8[:, 1, 0:bs, :]), [r_CE, r_cst], [r2], partial=False)
            P.op("dve", lambda e: e.tensor_mul(out=t3[:], in0=cr, in1=tw8[:, 1, 0:bs, :]), [r_CE, r_cst], [r3], partial=False)
            P.op("dve", lambda e: e.tensor_mul(out=t4[:], in0=ci, in1=tw8[:, 0, 0:bs, :]), [r_CE, r_cst], [r4], partial=False)
            P.op("dve", lambda e: e.tensor_sub(out=cr, in0=t1[:], in1=t2[:]), [r1, r2], [r_CE], partial=True)
            P.op("dve", lambda e: e.tensor_add(out=ci, in0=t3[:], in1=t4[:]), [r3, r4], [r_CE], partial=True)
            for hf_ in range(bs // 4 if fmode >= 3 else 0):
                c0 = (blk * bs + hf_ * 4) * 128
                rre = CEf[:, c0:c0 + 512]
                rim = CEf[:, 4096 + c0:4096 + c0 + 512]
                for t, (la, lb) in enumerate(((0, 2), (1, 0))):
                    px, r_px = next_psf()
                    P.op("pe", lambda e: e.matmul(px[:, :], lhsT=F32m[:, la, :], rhs=rre, start=True, stop=False),
                         [r_cst, r_CE], [r_px], sig=False, partial=False)
                    P.op("pe", lambda e: e.matmul(px[:, :], lhsT=F32m[:, lb, :], rhs=rim, start=False, stop=True),
                         [r_cst, r_CE], [r_px], sig=True, partial=True)
                    x0 = (t * 32 + blk * bs + hf_ * 4) * 128
                    evac(X[:].rearrange("p t c k -> p (t c k)")[:, x0:x0 + 512], px[:, :],
                         [r_px], [r_X], partial=not (blk == 0 and hf_ == 0 and t == 0), simple=True)

    def fft_inv(Y, r_Y, CEf, r_CE, Tt, r_T, epilogue):
        E5 = CEf[:, 0:32 * 2 * 128].rearrange("p (n t g c) -> p n t g c", n=32, t=2, g=32, c=4)
        E4 = CEf[:, 0:32 * 2 * 128].rearrange("p (n t c) -> p n t c", n=32, t=2, c=128)
        for cp in range(16):
            pe_, r_pe = next_psf()
            for gi in range(2):
                cg = cp * 2 + gi
                P.op("pe", lambda e: e.matmul(pe_[:, gi * 256:(gi + 1) * 256], lhsT=Y[:, 0, cg, :], rhs=R12[:, 0, :], start=True, stop=False),
                     [r_Y, r_R12], [r_pe], sig=False, partial=(gi > 0))
                P.op("pe", lambda e: e.matmul(pe_[:, gi * 256:(gi + 1) * 256], lhsT=Y[:, 1, cg, :], rhs=R12[:, 1, :], start=False, stop=True),
                     [r_Y, r_R12], [r_pe], sig=(gi == 1), partial=True)
            pv = pe_[:, :].rearrange("p (g t n c) -> p t n g c", g=2, t=2, n=32, c=4)
            for t in range(2):
                evac(E5[:, :, t, cp * 2:cp * 2 + 2, :], pv[:, t], [r_pe], [r_CE], partial=not (cp == 0 and t == 0))
        for nb in range(8):
            py, r_py = next_psf()
            for ni in range(4):
                n1 = nb * 4 + ni
                P.op("pe", lambda e: e.matmul(py[:, ni * 128:(ni + 1) * 128], lhsT=Tt[:, n1, 0, :], rhs=E4[:, n1, 0, :], start=True, stop=False),
                     [r_T, r_CE], [r_py], sig=False, partial=(ni > 0))
                P.op("pe", lambda e: e.matmul(py[:, ni * 128:(ni + 1) * 128], lhsT=Tt[:, n1, 1, :], rhs=E4[:, n1, 1, :], start=False, stop=True),
                     [r_T, r_CE], [r_py], sig=(ni == 3), partial=True)
            epilogue(nb, py[:, :].rearrange("p (n c) -> p n c", n=4), r_py)

    def to_token_major(srcT, r_src, dst, r_dst, fftl=True):
        sv = srcT[:, :].rearrange("p (a b) -> p b a", b=32)
        for nb in range(4):
            pt, r_pt = next_psb()
            for ni in range(8):
                n1 = nb * 8 + ni
                P.op("pe", lambda e: e.transpose(out=pt[:, ni * 128:(ni + 1) * 128], in_=sv[:, n1, :], identity=ident[:]),
                     [r_src, r_ident], [r_pt], sig=(ni == 7), partial=(ni > 0))
            if fftl:
                evac(dst[:, :].rearrange("p (g n c) -> p g n c", g=32, n=32, c=4)[:, :, nb * 8:(nb + 1) * 8, :],
                     pt[:, :].rearrange("p (n g c) -> p g n c", n=8, g=32, c=4), [r_pt], [r_dst], partial=(nb > 0))
            else:
                evac(dst[:, :].rearrange("p (n c) -> p n c", n=32)[:, nb * 8:(nb + 1) * 8, :],
                     pt[:, :].rearrange("p (n c) -> p n c", n=8), [r_pt], [r_dst], partial=(nb > 0))

    if not (dbg and dbg.get("skip_filters")):
        with ExitStack() as ph:
            h3 = [[sb(f"h3_{l}{v}", [64, S], BF16, ph) for v in range(2)] for l in range(DEPTH)]
            w4b = [sb(f"w4b{l}", [64, 2048], BF16, ph) for l in range(DEPTH)]
            with ExitStack() as ph2:
                feat = [sb(f"feat{v}", [33, S], F32, ph2) for v in range(2)]
                hA, r_hA = sb("hA", [64, S], F32, ph2)
                hB, r_hB = sb("hB", [64, S], F32, ph2)
                w4f, r_w4f = sb("w4f", [64, 2048], F32, ph2)
                fw = [sb(f"fw{i}", [64, 64], F32, ph2) for i in range(3)]
                fbt, r_fbt = sb("fbt", [64, 4], F32, ph2)
                fsc, r_fsc = sb("fsc", [64, 4], F32, ph2)
                ty = [sb(f"ty{i}", [64, 512], F32, ph2) for i in range(2)]
                tki, r_tki = sb("tki", [64, 512], mybir.dt.int32, ph2)
                tkf, r_tkf = sb("tkf", [64, 512], F32, ph2)
                for v in range(2):
                    P.dma("sp", feat[v][0][:], C["featT"][v], [R_in], [feat[v][1]], partial=False)
                for l in range(DEPTH):
                    P.dma("sp", fw[0][0][0:33, :], I["f_w1"][l], [R_in], [fw[0][1]], partial=False)
                    P.dma("sp", fw[1][0][:], I["f_w2"][l], [R_in], [fw[1][1]], partial=False)
                    P.dma("sp", fw[2][0][:], I["f_w3"][l], [R_in], [fw[2][1]], partial=False)
                    P.dma("sp", w4f[:], I["f_w4"][l], [R_in], [r_w4f], partial=False)
                    P.dma("sp", fbt[:], I["f_b"][l], [R_in], [r_fbt], partial=False)
                    P.op("pool", lambda e: e.tensor_copy(out=w4b[l][0][:], in_=w4f[:]), [r_w4f], [w4b[l][1]], partial=False)
                    P.op("dve", lambda e: e.tensor_scalar_mul(out=fsc[:, 3:4], in0=fbt[:, 3:4], scalar1=1.0 / TWO_PI), [r_fbt], [r_fsc], partial=False)
                    P.op("dve", lambda e: e.tensor_scalar(out=fsc[:, 0:3], in0=fbt[:, 0:3], scalar1=fsc[:, 3:4], scalar2=8.0,
                                                          op0=ALU.mult, op1=ALU.add), [r_fbt, r_fsc], [r_fsc], partial=False)
                    for v in range(2):
                        src, r_src = feat[v]
                        kdim = 33
                        for layer_i in range(3):
                            last = (layer_i == 2)
                            dst, r_dst = (h3[l][v] if last else ((hA, r_hA) if layer_i == 0 else (hB, r_hB)))
                            wt_, r_wt_ = fw[layer_i]
                            for tb in range(8):
                                sl = slice(tb * 512, (tb + 1) * 512)
                                pp, r_pp = next_psf()
                                P.op("pe", lambda e: e.matmul(pp[0:64, :], lhsT=wt_[0:kdim, :], rhs=src[0:kdim, sl], start=True, stop=True),
                                     [r_wt_, r_src], [r_pp], partial=False)
                                tyt, r_ty = ty[tb % 2]
                                P.op("dve", lambda e: e.tensor_scalar(out=tyt[:], in0=pp[0:64, :], scalar1=fsc[:, 3:4],
                                                                      scalar2=fsc[:, layer_i:layer_i + 1], op0=ALU.mult, op1=ALU.add),
                                     [r_pp, r_fsc], [r_ty], partial=False)
                                P.op("dve", lambda e: e.tensor_copy(out=tki[:], in_=tyt[:]), [r_ty], [r_tki], partial=False)
                                P.op("dve", lambda e: e.tensor_copy(out=tkf[:], in_=tki[:]), [r_tki], [r_tkf], partial=False)
                                P.op("dve", lambda e: e.tensor_sub(out=tyt[:], in0=tyt[:], in1=tkf[:]), [r_ty, r_tkf], [r_ty], partial=False)
                                P.op("dve", lambda e: e.tensor_single_scalar(out=tkf[:], in_=tyt[:], scalar=0.5, op=ALU.is_ge),
                                     [r_ty], [r_tkf], partial=False)
                                P.op("dve", lambda e: e.tensor_sub(out=tyt[:], in0=tyt[:], in1=tkf[:]), [r_ty, r_tkf], [r_ty], partial=False)
                                P.op("act", lambda e: e.activation(out=dst[:, sl], in_=tyt[:], func=AF.Sin, scale=TWO_PI),
                                     [r_ty], [r_dst], partial=(tb > 0))
                            src, r_src = dst, r_dst
                            kdim = 64
                P.barrier()
            trb = [sb(f"trb{v}", [128, S], F32, ph) for v in range(2)]
            dec = [sb(f"dec{v}", [128, S], F32, ph) for v in range(2)]
            ndl, r_ndl = sb("ndl", [128, 4], F32, ph)
            fT, r_fT = sb("fT", [128, S], BF16, ph)
            ftm = [sb(f"ftm{v}", [128, S], BF16, ph) for v in range(2)]
            CEf, r_CE = sb("CEf", [128, 8192], BF16, ph)
            Xb, r_X = sb("Xb", [128, 2, 32, 128], BF16, ph)
            tpf = [sb(f"tpf{i}", [128, 4, 128], F32, ph) for i in range(4)]
            P.dma("sp", ndl[:], C["ndelta"][:, :], [R_in], [r_ndl], partial=False)
            for v in range(2):
                P.dma("sp", trb[v][0][:], bc(C["trow"], v * S, S), [R_in], [trb[v][1]], partial=False)
            flist = [(c, l, o) for c in range(4) for l in range(DEPTH) for o in range(2)]
            if dbg and "filt_list" in dbg:
                flist = dbg["filt_list"]
            lastc = None
            for (c, l, o) in flist:
                if c != lastc:
                    for v in range(2):
                        P.op("act", lambda e: e.activation(out=dec[v][0][:], in_=trb[v][0][:], func=AF.Exp, scale=ndl[:, c:c + 1]),
                             [trb[v][1], r_ndl], [dec[v][1]], partial=False)
                    lastc = c
                for v in range(2):
                    col0 = (o * 2 + v) * 512 + c * 128
                    for tb in range(8):
                        sl = slice(tb * 512, (tb + 1) * 512)
                        pp, r_pp = next_psf()
                        P.op("pe", lambda e: e.matmul(pp[:, :], lhsT=w4b[l][0][:, col0:col0 + 128], rhs=h3[l][v][0][:, sl], start=True, stop=True),
                             [w4b[l][1], h3[l][v][1]], [r_pp], partial=False)
                        P.op("dve", lambda e: e.tensor_mul(out=fT[:, sl], in0=pp[:, :], in1=dec[v][0][:, sl]),
                             [r_pp, dec[v][1]], [r_fT], partial=(tb > 0))
                    if v == 1:
                        P.op("dve", lambda e: e.memset(fT[:, 0:1], 0.0), [], [r_fT], partial=True)
                    to_token_major(fT, r_fT, ftm[v][0], ftm[v][1])
                fft_fwd(ftm[0][0], ftm[0][1], ftm[1][0], ftm[1][1], CEf, r_CE, Xb, r_X, tpf)
                P.dma("sp", H_d[l, o, c], Xb[:], [r_X], [R_H[l][o][c]], partial=False, sem_of=r_X)
            P.barrier()
    if stop_after == "filt":
        layers = 0

    x_src = I["x"]
    R_xsrc = R_in
    for l in range(layers):
        x_dst, R_xdst = (xmid_d, R_xmid) if l < DEPTH - 1 else (out, R_out)
        lay = ExitStack()
        hT, r_hT = sb(f"hT", [128, 8, S], BF16, lay)
        with ExitStack() as ph:
            xt = [sb(f"xt{i}", [128, D], F32, ph) for i in range(2)]
            xn = [sb(f"xn{i}", [128, D], BF16, ph) for i in range(2)]
            st = [sb(f"st{i}", [128, 12], F32, ph) for i in range(2)]
            mv = [sb(f"mv{i}", [128, 2], F32, ph) for i in range(2)]
            for t in range(32):
                xtt, r_xt = xt[t % 2]
                xnn, r_xn = xn[t % 2]
                stt, r_st = st[t % 2]
                mvv, r_mv = mv[t % 2]
                P.dma(next_q(), xtt[:], x_src[t * 128:(t + 1) * 128, :], [R_xsrc], [r_xt], partial=False)
                P.op("dve", lambda e: e.bn_stats(out=stt[:, 0:6], in_=xtt[:, 0:512]), [r_xt], [r_st], partial=False)
                P.op("dve", lambda e: e.bn_stats(out=stt[:, 6:12], in_=xtt[:, 512:1024]), [r_xt], [r_st], partial=True)
                P.op("dve", lambda e: e.bn_aggr(out=mvv[:], in_=stt[:]), [r_st], [r_mv], partial=False)
                P.op("act", lambda e: e.activation(out=mvv[:, 1:2], in_=mvv[:, 1:2], func=AF.Sqrt, bias=epsT[:], scale=1.0),
                     [r_mv, r_eps], [r_mv], partial=False)
                P.op("dve", lambda e: e.reciprocal(out=mvv[:, 1:2], in_=mvv[:, 1:2]), [r_mv], [r_mv], partial=False)
                P.op("dve", lambda e: e.tensor_scalar(out=xnn[:], in0=xtt[:], scalar1=mvv[:, 0:1], scalar2=mvv[:, 1:2],
                                                      op0=ALU.subtract, op1=ALU.mult), [r_xt, r_mv], [r_xn], partial=False)
                pt, r_pt = next_psb()
                for kc in range(8):
                    P.op("pe", lambda e, kc=kc: e.transpose(out=pt[:, kc * 128:(kc + 1) * 128], in_=xnn[:, kc * 128:(kc + 1) * 128],
                                                            identity=ident[:]),
                         [r_xn, r_ident], [r_pt], sig=(kc == 7), partial=(kc > 0))
                for kc in range(8):
                    P.op("act", lambda e, kc=kc: e.activation(out=hT[:, kc, t * 128:(t + 1) * 128], in_=pt[:, kc * 128:(kc + 1) * 128],
                                                              func=AF.Identity, scale=sc1[:, l, kc:kc + 1], bias=modcol[:, l, kc:kc + 1]),
                         [r_pt, r_sc1, r_modcol], [r_hT], partial=True)
            P.barrier()
        if stop_after == "ln":
            lay.close()
            break

        with ExitStack() as ph:
            ws = [sb(f"ws{i}", [128, 8, 128], F32, ph) for i in range(3)]
            wb = [sb(f"wb{i}", [128, 8, 128], BF16, ph) for i in range(3)]
            ob = [sb(f"ob{i}", [128, S], BF16, ph) for i in range(2)]
            nfm = 0
            chunks = [j for j in range(72) if not (24 <= j < 36)]
            if dbg and "proj_chunks" in dbg:
                chunks = dbg["proj_chunks"]
            for j in chunks:
                wst, r_ws = ws[nfm % 3]
                wbt, r_wb = wb[nfm % 3]
                obt, r_ob = ob[nfm % 2]
                nfm += 1
                P.dma(next_q(), wst[:], I["w_in"][l, :, :, j * 128:(j + 1) * 128], [R_in], [r_ws], partial=False)
                P.op("pool", lambda e: e.tensor_copy(out=wbt[:], in_=wst[:]), [r_ws], [r_wb], partial=False)
                if 36 <= j < 40 or 52 <= j < 56:
                    fn = AF.Silu
                elif j >= 56:
                    fn = AF.Sigmoid
                else:
                    fn = AF.Identity
                for tb in range(8):
                    pp, r_pp = next_psf()
                    for kc in range(8):
                        P.op("pe", lambda e, kc=kc: e.matmul(pp[:, :], lhsT=wbt[:, kc, :], rhs=hT[:, kc, tb * 512:(tb + 1) * 512],
                                                             start=(kc == 0), stop=(kc == 7)),
                             [r_wb, r_hT], [r_pp], sig=(kc == 7), partial=(kc > 0))
                    P.op("act", lambda e: e.activation(out=obt[:, tb * 512:(tb + 1) * 512], in_=pp[:, :], func=fn,
                                                       bias=b_in_c[:, l, j:j + 1], scale=1.0),
                         [r_pp, r_binc], [r_ob], partial=(tb > 0))
                P.dma(next_q(), proj_d[j, :, :], obt[:], [r_ob], [R_proj[j]], partial=False, sem_of=r_ob)
            wvs = [sb(f"wvs{i}", [128, 8, 512], F32, ph) for i in range(1)]
            wvb = [sb(f"wvb{i}", [128, 8, 512], BF16, ph) for i in range(2)]
            vo = [sb(f"vo{i}", [128, 4, 512], BF16, ph) for i in range(2)]
            nv = 0
            groups = range(3) if not (dbg and "v_groups" in dbg) else dbg["v_groups"]
            for g in groups:
                d = (1, 4, 16)[g]
                Lg = S // d
                wst, r_ws = wvs[0]
                wbt, r_wb = wvb[g % 2]
                c0 = 3072 + g * 512
                P.dma("sp", wst[:, 0:4, :], I["w_in"][l, :, 0:4, c0:c0 + 512], [R_in], [r_ws], partial=False)
                P.dma("pool", wst[:, 4:8, :], I["w_in"][l, :, 4:8, c0:c0 + 512], [R_in], [r_ws])
                P.op("pool", lambda e: e.tensor_copy(out=wbt[:], in_=wst[:]), [r_ws], [r_wb], partial=False)
                for tq in range(8):
                    vot, r_vo = vo[nv % 2]
                    nv += 1
                    for t4 in range(4):
                        ti = tq * 4 + t4
                        r_, u = divmod(ti, Lg // 128)
                        pp, r_pp = next_psf()
                        for kc in range(8):
                            lt = hT[:, kc, :].rearrange("p (i d) -> p d i", d=d)[:, r_, u * 128:(u + 1) * 128]
                            P.op("pe", lambda e, kc=kc, lt=lt: e.matmul(pp[:, :], lhsT=lt, rhs=wbt[:, kc, :], start=(kc == 0), stop=False),
                                 [r_wb, r_hT], [r_pp], sig=False, partial=(kc > 0))
                        P.op("pe", lambda e: e.matmul(pp[:, :], lhsT=ones2[0:1, :], rhs=bin2[0:1, l, g * 512:(g + 1) * 512], start=False, stop=False),
                             [r_ones2, r_bin2], [r_pp], sig=False, partial=True)
                        P.op("pe", lambda e: e.matmul(pp[:, :], lhsT=ones2[0:1, :], rhs=binl[0:1, l, g * 512:(g + 1) * 512], start=False, stop=True),
                             [r_ones2, r_binl], [r_pp], sig=True, partial=True)
                        P.op("dve", lambda e, t4=t4: e.tensor_copy(out=vot[:, t4, :], in_=pp[:, :]), [r_pp], [r_vo], partial=(t4 > 0))
                    qn = next_q()
                    for j4 in range(4):
                        P.dma(qn, vtm_d[g, j4, :, tq * 4:(tq + 1) * 4, :], vot[:, :, j4 * 128:(j4 + 1) * 128], [r_vo], [R_vtm[g]], sem_of=r_vo)
            P.barrier()
        if stop_after == "proj":
            lay.close()
            break
        lay.close()
        with ExitStack() as ph:
            Oacc, r_O = sb(f"Oacc", [128, 2, S], F32, ph)
            qTs = [sb(f"qT{i}", [128, 2, S], BF16, ph) for i in range(2)]
            kTs = [sb(f"kT{i}", [128, S], BF16, ph) for i in range(2)]
            vts = [sb(f"vt{i}", [128, 32, 2, 128], BF16, ph) for i in range(2)]
            abs_ = [sb(f"ab{i}", [128, 3, 256], BF16, ph) for i in range(2)]
            pTs = [sb(f"pT{i}", [128, 256], BF16, ph) for i in range(8)]
            gs, r_gs = sb(f"gs", [128, S], BF16, ph)
            vs, r_vs = sb(f"vs", [128, 32, 128], BF16, ph)
            yb, r_yb = sb(f"yb", [128, S], BF16, ph)
            rz, r_rz = sb(f"rz", [128, 512], F32, ph)
            tm, r_tm = sb(f"tm", [128, 512], F32, ph)
            for i in range(2):
                P.op("pool", lambda e, i=i: e.memset(vts[i][0][:], 0.0), [], [vts[i][1]], partial=False)
                P.op("pool", lambda e, i=i: e.memset(qTs[i][0][:], 0.0), [], [qTs[i][1]], partial=False)
            npT = 0
            nbuf = 0
            jlist = range(4) if not (dbg and "att_j" in dbg) else dbg["att_j"]
            for j in jlist:
                for g in (range(3) if not (dbg and "att_g" in dbg) else dbg["att_g"]):
                    d = (1, 4, 16)[g]
                    ntl = (S // d) // 128
                    qT, r_q = qTs[nbuf % 2]
                    kT, r_k = kTs[nbuf % 2]
                    vt, r_v = vts[nbuf % 2]
                    ab, r_ab = abs_[nbuf % 2]
                    nbuf += 1
                    P.dma("sp", qT[0:64, 0, :], proj_d[g * 4 + j, 0:64, :], [R_proj[g * 4 + j]], [r_q], partial=False)
                    P.dma("sp", qT[64:128, 1, :], proj_d[g * 4 + j, 64:128, :], [R_proj[g * 4 + j]], [r_q])
                    P.dma("sp", kT[:], proj_d[12 + g * 4 + j, :, :], [R_proj[12 + g * 4 + j]], [r_k], partial=False)
                    P.dma("sp", vs[:], vtm_d[g, j, :, :, :], [R_vtm[g]], [r_vs], partial=False)
                    P.op("pool", lambda e: e.tensor_copy(out=vt[:, :, 0, 0:64], in_=vs[:, :, 0:64]), [r_vs], [r_v], partial=False)
                    P.op("pool", lambda e: e.tensor_copy(out=vt[:, :, 1, 64:128], in_=vs[:, :, 64:128]), [r_vs], [r_v], partial=True)
                    P.dma("sp", ab[:], C["abias"][:, g * 4 + j, :, :, :].rearrange("p k h q -> p k (h q)"), [R_in], [r_ab], partial=False)
                    qv = [qT[:, hh, :].rearrange("p (i d) -> p d i", d=d) for hh in range(2)]
                    kv = [kT[:, :].rearrange("p (i d) -> p d i", d=d) for hh in range(2)]
                    accv = Oacc[:].rearrange("p c (i d) -> p c d i", d=d)
                    tiles = [(r_, u) for r_ in range(d) for u in range(ntl)]

                    def stageA(r_, u):
                        kts = [kt for kt in range(3) if 0 <= u + kt - 1 < ntl]
                        outl = []
                        for kt in kts:
                            ku = u + kt - 1
                            rr["sc"] = (rr.get("sc", 0) + 1) % 4
                            ps, r_ps = psf[rr["sc"]]
                            for hh in range(2):
                                P.op("pe", lambda e, hh=hh: e.matmul(ps[:, hh * 128:(hh + 1) * 128], lhsT=ident[:],
                                                                     rhs=ab[:, kt, hh * 128:(hh + 1) * 128], start=True, stop=False),
                                     [r_ident, r_ab], [r_ps], sig=False, partial=(hh > 0))
                                P.op("pe", lambda e, hh=hh: e.matmul(
                                    ps[:, hh * 128:(hh + 1) * 128], lhsT=kv[hh][:, r_, ku * 128:(ku + 1) * 128],
                                    rhs=qv[hh][:, r_, u * 128:(u + 1) * 128], start=False, stop=True),
                                    [r_k, r_q], [r_ps], sig=(hh == 1), partial=True)
                            rr["pt"] = (rr.get("pt", 0) + 1) % 8
                            pT, r_pT = pTs[rr["pt"]]
                            P.op("act", lambda e: e.activation(out=pT[:], in_=ps[:, 0:256], func=AF.Exp, scale=0.125),
                                 [r_ps], [r_pT], partial=False)
                            outl.append((r_ * ntl + ku, pT, r_pT))
                        return outl

                    def stageB(r_, u, pl):
                        rr["po"] = (rr.get("po", 0) + 1) % 2
                        po, r_po = psf[4 + rr["po"]]
                        n = len(pl) * 2
                        for part in range(2):
                            i = 0
                            for (ti, pT, r_pT) in pl:
                                for hh in range(2):
                                    lt = vt[:, ti, hh, :] if part == 0 else onesel[:, hh, :]
                                    P.op("pe", lambda e, hh=hh, lt=lt, i=i: e.matmul(
                                        po[:, part * 128:(part + 1) * 128], lhsT=lt, rhs=pT[:, hh * 128:(hh + 1) * 128],
                                        start=(i == 0), stop=(i == n - 1)),
                                        [r_v, r_onesel, r_pT], [r_po], sig=(part == 1 and i == n - 1), partial=not (part == 0 and i == 0))
                                    i += 1
                        av = accv[:, :, r_, u * 128:(u + 1) * 128]
                        pv = po[:, 0:256].rearrange("p (c q) -> p c q", c=2)
                        if g == 0:
                            P.op("dve", lambda e: e.tensor_copy(out=av, in_=pv), [r_po], [r_O], partial=True)
                        else:
                            P.op("dve", lambda e: e.tensor_add(out=av, in0=pv, in1=av), [r_po, r_O], [r_O], partial=True)

                    amode = dbg.get("att_mode", 3) if dbg else 3
                    prev = None
                    for (r_, u) in tiles:
                        cur = (r_, u, stageA(r_, u))
                        if prev is not None and amode >= 2:
                            stageB(*prev)
                        prev = cur
                    if amode >= 2:
                        stageB(*prev)
                P.dma("sp", gs[:], proj_d[36 + j, :, :], [R_proj[36 + j]], [r_gs], partial=False)
                for tb in range(8):
                    sl = slice(tb * 512, (tb + 1) * 512)
                    P.op("dve", lambda e: e.reciprocal(out=rz[:], in_=Oacc[:, 1, sl]), [r_O], [r_rz], partial=False)
                    P.op("dve", lambda e: e.tensor_mul(out=tm[:], in0=Oacc[:, 0, sl], in1=rz[:]), [r_O, r_rz], [r_tm], partial=False)
                    P.op("pool", lambda e: e.tensor_mul(out=yb[:, sl], in0=tm[:], in1=gs[:, sl]), [r_tm, r_gs], [r_yb], partial=(tb > 0))
                P.dma("sp", yattn_d[j, :, :], yb[:], [r_yb], [R_yattn[j]], partial=False, sem_of=r_yb)
            P.barrier()
        if stop_after == "att":
            break
        with ExitStack() as ph:
            pb, r_pb = sb("pb", [128, S], BF16, ph)
            uf, r_uf = sb("uf", [128, 2048], F32, ph)
            ub, r_ub = sb("ub", [128, S], BF16, ph)
            zt, r_zt = sb("zt", [128, S], BF16, ph)
            xg, r_xg = sb("xg", [128, S], BF16, ph)
            CEf, r_CE = sb("CEf", [128, 8192], BF16, ph)
            Xb, r_X = sb("Xb", [128, 2, 32, 128], BF16, ph)
            Tt, r_T = sb("Tt", [128, 32, 2, 128], BF16, ph)
            Hb = [sb(f"Hb{i}", [128, 2, 8, 128], BF16, ph) for i in range(2)]
            tp = [sb(f"tp{i}", [128, 8, 128], F32, ph) for i in range(4)]
            fb1, r_fb1 = sb("fb1", [128, 2, 128], F32, ph)
            fb4, r_fb4 = sb("fb4", [128, 2, 32, 4, 4], F32, ph)
            e1, r_e1 = sb("e1", [128, 32, 4, 4], F32, ph)
            e2, r_e2 = sb("e2", [128, 32, 4, 4], F32, ph)
            gsT, r_gsT = sb("gsT", [128, S], BF16, ph)
            yT, r_yT = sb("yT", [128, S], BF16, ph)
            P.dma("sp", Tt[:], C["T"][:, :, :, :], [R_in], [r_T], partial=False)

            def conv3(chunk, widx, dst, r_dst, fftl):
                P.dma("sp", pb[:], proj_d[chunk, :, :], [R_proj[chunk]], [r_pb], partial=False)
                w0 = convw[:, l, 0, widx:widx + 1]
                w1 = convw[:, l, 1, widx:widx + 1]
                w2 = convw[:, l, 2, widx:widx + 1]
                cb = convb[:, l, widx:widx + 1]
                for hb_ in range(2):
                    t0 = hb_ * 2048
                    P.op("act", lambda e: e.activation(out=uf[:, :], in_=pb[:, t0:t0 + 2048], func=AF.Identity, scale=w1, bias=cb),
                         [r_pb, r_convw, r_convb], [r_uf], partial=False)
                    a = 1 if hb_ == 0 else 0
                    P.op("dve", lambda e: e.scalar_tensor_tensor(out=uf[:, a:2048], in0=pb[:, t0 + a - 1:t0 + 2047], scalar=w0,
                                                                 in1=uf[:, a:2048], op0=ALU.mult, op1=ALU.add),
                         [r_pb, r_convw, r_uf], [r_uf], partial=False)
                    b_ = 2047 if hb_ == 1 else 2048
                    P.op("dve", lambda e: e.scalar_tensor_tensor(out=ub[:, t0:t0 + b_], in0=pb[:, t0 + 1:t0 + b_ + 1], scalar=w2,
                                                                 in1=uf[:, 0:b_], op0=ALU.mult, op1=ALU.add),
                         [r_pb, r_convw, r_uf], [r_ub], partial=(hb_ > 0))
                    if hb_ == 1:
                        P.op("dve", lambda e: e.tensor_copy(out=ub[:, S - 1:S], in_=uf[:, 2047:2048]), [r_uf], [r_ub], partial=True)
                to_token_major(ub, r_ub, dst, r_dst, fftl)

            def pointwise(o, c):
                for blk in range(4):
                    hb, r_hb = Hb[blk % 2]
                    P.dma("sp", hb[:], H_d[l, o, c, :, :, blk * 8:(blk + 1) * 8, :], [R_H[l][o][c]], [r_hb], partial=False)
                    xr, xi = Xb[:, 0, blk * 8:(blk + 1) * 8, :], Xb[:, 1, blk * 8:(blk + 1) * 8, :]
                    hr, hi = hb[:, 0, :, :], hb[:, 1, :, :]
                    (t1, r1), (t2, r2), (t3, r3), (t4, r4) = tp
                    P.op("dve", lambda e: e.tensor_mul(out=t1[:], in0=xr, in1=hr), [r_X, r_hb], [r1], partial=False)
                    P.op("dve", lambda e: e.tensor_mul(out=t2[:], in0=xi, in1=hi), [r_X, r_hb], [r2], partial=False)
                    P.op("dve", lambda e: e.tensor_mul(out=t3[:], in0=xr, in1=hi), [r_X, r_hb], [r3], partial=False)
                    P.op("dve", lambda e: e.tensor_mul(out=t4[:], in0=xi, in1=hr), [r_X, r_hb], [r4], partial=False)
                    P.op("dve", lambda e: e.tensor_sub(out=xr, in0=t1[:], in1=t2[:]), [r1, r2], [r_X], partial=True)
                    P.op("dve", lambda e: e.tensor_add(out=xi, in0=t3[:], in1=t4[:]), [r3, r4], [r_X], partial=True)

            clist = range(4) if not (dbg and "hy_c" in dbg) else dbg["hy_c"]
            for c in clist:
                for o in range(2):
                    P.dma("sp", fb1[:, o, :], bc(I["f_bias_r"], (l * 2 + o) * 512 + c * 128, 128), [R_in], [r_fb1], partial=(o > 0))
                for o in range(2):
                    for i4 in range(4):
                        P.op("dve", lambda e: e.tensor_copy(out=fb4[:, o, :, i4, :], in_=fb1[:, o, :].rearrange("p (g c) -> p g c", c=4)),
                             [r_fb1], [r_fb4], partial=not (o == 0 and i4 == 0))
                conv3(40 + c, c, zt, r_zt, True)
                conv3(44 + c, 4 + c, xg, r_xg, False)
                hmode = dbg.get("hy_mode", 9) if dbg else 9
                for o in range(2):
                    if hmode < 2:
                        break
                    fft_fwd(zt, r_zt, None, None, CEf, r_CE, Xb, r_X, tp)
                    if hmode < 3:
                        break
                    pointwise(o, c)
                    if hmode < 4:
                        break

                    def epi(nb, pyv, r_py, o=o):
                        zs = zt[:, :].rearrange("p (g n c) -> p g n c", g=32, n=32, c=4)[:, :, nb * 4:(nb + 1) * 4, :]
                        xs = xg[:, :].rearrange("p (n g c) -> p g n c", n=32, g=32, c=4)[:, :, nb * 4:(nb + 1) * 4, :]
                        pyf = pyv.rearrange("p n (g c) -> p g n c", c=4)
                        P.op("dve", lambda e: e.tensor_mul(out=e1[:], in0=zs, in1=fb4[:, o, :, :, :]), [r_zt, r_fb4], [r_e1], partial=False)
                        P.op("dve", lambda e: e.tensor_add(out=e2[:], in0=pyf, in1=e1[:]), [r_py, r_e1], [r_e2], partial=False)
                        if o == 0:
                            P.op("dve", lambda e: e.tensor_mul(out=zs, in0=e2[:], in1=xs), [r_e2, r_xg], [r_zt], partial=True)
                        else:
                            P.op("dve", lambda e: e.tensor_mul(out=xs, in0=e2[:], in1=xs), [r_e2, r_xg], [r_xg], partial=True)

                    fft_inv(Xb, r_X, CEf, r_CE, Tt, r_T, epi)
                    if o == 0:
                        conv3(48 + c, 8 + c, xg, r_xg, False)
                P.dma("sp", gsT[:], proj_d[52 + c, :, :], [R_proj[52 + c]], [r_gsT], partial=False)
                yv = yT[:, :].rearrange("p (a b) -> p b a", b=32)
                gv = gsT[:, :].rearrange("p (a b) -> p b a", b=32)
                for nb in range(4):
                    pt, r_pt = next_psb()
                    for ni in range(8):
                        n1 = nb * 8 + ni
                        P.op("pe", lambda e: e.transpose(out=pt[:, ni * 128:(ni + 1) * 128], in_=xg[:, n1 * 128:(n1 + 1) * 128], identity=ident[:]),
                             [r_xg, r_ident], [r_pt], sig=(ni == 7), partial=(ni > 0))
                    P.op("dve", lambda e: e.tensor_mul(out=yv[:, nb * 8:(nb + 1) * 8, :], in0=pt[:, :].rearrange("p (n c) -> p n c", n=8),
                                                       in1=gv[:, nb * 8:(nb + 1) * 8, :]), [r_pt, r_gsT], [r_yT], partial=(nb > 0))
                P.dma("sp", yhy_d[c, :, :], yT[:], [r_yT], [R_yhy[c]], partial=False, sem_of=r_yT)
            P.barrier()
        if stop_after == "hy":
            break
        with ExitStack() as ph:
            wpa, r_wpa = sb("wpa", [128, 4, D], BF16, ph)
            wph, r_wph = sb("wph", [128, 4, D], BF16, ph)
            wo, r_wo = sb("wo", [128, 8, D], BF16, ph)
            rowb, r_rowb = sb("rowb", [128, 3, D], F32, ph)
            with ExitStack() as ph2:
                wst, r_wst = sb("wstg", [128, 8, D], F32, ph2)
                P.dma("sp", wst[:, 0:4, :], I["w_pa"][l], [R_in], [r_wst], partial=False)
                P.op("pool", lambda e: e.tensor_copy(out=wpa[:], in_=wst[:, 0:4, :]), [r_wst], [r_wpa], partial=False)
                P.dma("sp", wst[:, 4:8, :], I["w_ph"][l], [R_in], [r_wst], partial=False)
                P.op("pool", lambda e: e.tensor_copy(out=wph[:], in_=wst[:, 4:8, :]), [r_wst], [r_wph], partial=False)
                P.dma("sp", wst[:, :, :], I["w_out"][l], [R_in], [r_wst], partial=False)
                for kc in range(8):
                    P.op("pool" if kc % 2 else "dve", lambda e: e.tensor_mul(out=wo[:, kc, :], in0=wst[:, kc, :], in1=gate_b[:, l, :]),
                         [r_wst, r_gate], [r_wo], partial=(kc > 0))
                P.barrier()
            P.dma("sp", rowb[:, 0, :], bc(I["b_out_r"], l * D, D), [R_in], [r_rowb], partial=False)
            P.dma("sp", rowb[:, 1, :], bc(I["ln_g_r"], l * D, D), [R_in], [r_rowb])
            P.dma("sp", rowb[:, 2, :], bc(I["ln_b_r"], l * D, D), [R_in], [r_rowb])
            gb, r_gb = sb("gb", [128, D], F32, ph)
            P.op("dve", lambda e: e.tensor_mul(out=gb[:], in0=rowb[:, 0, :], in1=gate_b[:, l, :]), [r_rowb, r_gate], [r_gb], partial=False)
            nb_, r_nb = sb("nbias", [128, 1], F32, ph)
            ya = [sb(f"ya{i}", [128, 4, 512], BF16, ph) for i in range(2)]
            yh = [sb(f"yh{i}", [128, 4, 512], BF16, ph) for i in range(2)]
            ga = [sb(f"ga{i}", [128, 16, 512], BF16, ph) for i in range(2)]
            mT, r_mT = sb("mT", [128, 8, 512], BF16, ph)
            m1, r_m1 = sb("m1", [128, 512], F32, ph)
            m2, r_m2 = sb("m2", [128, 512], F32, ph)
            xr_ = [sb(f"xr{i}", [128, D], F32, ph) for i in range(4)]
            rs_ = [sb(f"rs{i}", [128, D], F32, ph) for i in range(4)]
            sts = [sb(f"mst{i}", [128, 12], F32, ph) for i in range(4)]
            mvs = [sb(f"mmv{i}", [128, 2], F32, ph) for i in range(4)]
            nbs = [sb(f"nbias{i}", [128, 1], F32, ph) for i in range(4)]
            o1, r_o1 = sb("o1", [128, 512], F32, ph)
            stt, r_st = sb("mst", [128, 12], F32, ph)
            mvv, r_mv = sb("mmv", [128, 2], F32, ph)
            xo = [sb(f"xo{i}", [128, D], F32, ph) for i in range(4)]
            tbl = range(8) if not (dbg and "merge_tb" in dbg) else dbg["merge_tb"]
            nt = 0
            for tb in tbl:
                sl = slice(tb * 512, (tb + 1) * 512)
                yat, r_ya = ya[tb % 2]
                yht, r_yh = yh[tb % 2]
                gat, r_ga = ga[tb % 2]
                P.dma("sp", yat[:], yattn_d[:, :, sl].rearrange("c p t -> p c t"), R_yattn, [r_ya], partial=False)
                P.dma("sp", yht[:], yhy_d[:, :, sl].rearrange("c p t -> p c t"), R_yhy, [r_yh], partial=False)
                P.dma("sp", gat[:], proj_d[56:72, :, sl].rearrange("c p t -> p c t"), R_proj[56:72], [r_ga], partial=False)
                for fc in range(8):
                    pa, r_pa = next_psf()
                    pq, r_pq = next_psf()
                    for kc in range(4):
                        P.op("pe", lambda e: e.matmul(pa[:, :], lhsT=wpa[:, kc, fc * 128:(fc + 1) * 128], rhs=yat[:, kc, :],
                                                      start=(kc == 0), stop=(kc == 3)), [r_wpa, r_ya], [r_pa], sig=(kc == 3), partial=(kc > 0))
                    for kc in range(4):
                        P.op("pe", lambda e: e.matmul(pq[:, :], lhsT=wph[:, kc, fc * 128:(fc + 1) * 128], rhs=yht[:, kc, :],
                                                      start=(kc == 0), stop=(kc == 3)), [r_wph, r_yh], [r_pq], sig=(kc == 3), partial=(kc > 0))
                    P.op("dve", lambda e: e.tensor_mul(out=m1[:], in0=pa[:, :], in1=gat[:, fc, :]), [r_pa, r_ga], [r_m1], partial=False)
                    P.op("dve", lambda e: e.tensor_mul(out=m2[:], in0=pq[:, :], in1=gat[:, 8 + fc, :]), [r_pq, r_ga], [r_m2], partial=False)
                    P.op("pool", lambda e: e.tensor_add(out=mT[:, fc, :], in0=m1[:], in1=m2[:]), [r_m1, r_m2], [r_mT], partial=(fc > 0))
                tiles = []
                for tt in range(4):
                    row0 = tb * 512 + tt * 128
                    xrt, r_xr = xr_[tt]
                    rst, r_rs = rs_[tt]
                    xot, r_xo = xo[tt]
                    stt, r_st = sts[tt]
                    mvv, r_mv = mvs[tt]
                    nb_, r_nb = nbs[tt]
                    tiles.append((row0, xrt, r_xr, rst, r_rs, xot, r_xo, stt, r_st, mvv, r_mv, nb_, r_nb))
                    P.dma("sp", xrt[:], x_src[row0:row0 + 128, :], [R_xsrc], [r_xr], partial=False)
                for tt, (row0, xrt, r_xr, rst, r_rs, xot, r_xo, stt, r_st, mvv, r_mv, nb_, r_nb) in enumerate(tiles):
                    P.op("dve", lambda e: e.scalar_tensor_tensor(out=xrt[:], in0=xrt[:], scalar=ALPHA, in1=gb[:], op0=ALU.mult, op1=ALU.add),
                         [r_xr, r_gb], [r_xr], partial=False)
                    for hf_ in range(2):
                        hs = slice(hf_ * 512, (hf_ + 1) * 512)
                        po_, r_po = next_psf()
                        for kc in range(8):
                            P.op("pe", lambda e: e.matmul(po_[:, :], lhsT=mT[:, kc, tt * 128:(tt + 1) * 128], rhs=wo[:, kc, hs],
                                                          start=(kc == 0), stop=(kc == 7)), [r_mT, r_wo], [r_po], sig=(kc == 7), partial=(kc > 0))
                        P.op("dve", lambda e: e.tensor_add(out=rst[:, hs], in0=po_[:, :], in1=xrt[:, hs]), [r_po, r_xr], [r_rs], partial=(hf_ > 0))
                    P.op("dve", lambda e: e.bn_stats(out=stt[:, 0:6], in_=rst[:, 0:512]), [r_rs], [r_st], partial=False)
                    P.op("dve", lambda e: e.bn_stats(out=stt[:, 6:12], in_=rst[:, 512:1024]), [r_rs], [r_st], partial=True)
                    P.op("dve", lambda e: e.bn_aggr(out=mvv[:], in_=stt[:]), [r_st], [r_mv], partial=False)
                    P.op("act", lambda e: e.activation(out=mvv[:, 1:2], in_=mvv[:, 1:2], func=AF.Sqrt, bias=epsT[:], scale=1.0),
                         [r_mv, r_eps], [r_mv], partial=False)
                for (row0, xrt, r_xr, rst, r_rs, xot, r_xo, stt, r_st, mvv, r_mv, nb_, r_nb) in tiles:
                    P.op("dve", lambda e: e.reciprocal(out=mvv[:, 1:2], in_=mvv[:, 1:2]), [r_mv], [r_mv], partial=False)
                    P.op("dve", lambda e: e.tensor_scalar(out=nb_[:], in0=mvv[:, 0:1], scalar1=-1.0, scalar2=mvv[:, 1:2],
                                                          op0=ALU.mult, op1=ALU.mult), [r_mv], [r_nb], partial=False)
                    P.op("act", lambda e: e.activation(out=rst[:], in_=rst[:], func=AF.Identity, scale=mvv[:, 1:2], bias=nb_[:]),
                         [r_rs, r_mv, r_nb], [r_rs], partial=False)
                for (row0, xrt, r_xr, rst, r_rs, xot, r_xo, stt, r_st, mvv, r_mv, nb_, r_nb) in tiles:
                    P.op("dve", lambda e: e.tensor_mul(out=rst[:], in0=rst[:], in1=rowb[:, 1, :]), [r_rs, r_rowb], [r_rs], partial=False)
                    P.op("pool", lambda e: e.tensor_add(out=xot[:], in0=rst[:], in1=rowb[:, 2, :]), [r_rs, r_rowb], [r_xo], partial=False)
                    P.dma("sp", x_dst[row0:row0 + 128, :], xot[:], [r_xo], [R_xdst], sem_of=r_xo)
            P.barrier()
        x_src, R_xsrc = x_dst, R_xdst
    P.barrier()
    stack.close()
    return nc, dbg_out


_CACHE = {}


def kernel(**inputs):
    inp = {k: np.asarray(v, dtype=np.float32) for k, v in inputs.items()}
    consts = _const_tables()
    if "nc" not in _CACHE:
        _CACHE["nc"] = build()[0]
    nc = _CACHE["nc"]
    in_maps = []
    for b in range(8):
        m = _layout_inputs(inp, b)
        for k, v in consts.items():
            m["c_" + k] = v
        in_maps.append(m)
    res = run_bass_kernel_spmd(nc, in_maps, core_ids=list(range(8)))
    return np.stack([np.asarray(r["out"], dtype=np.float32) for r in res.results], axis=0)
```

```python
import math
from contextlib import ExitStack
import numpy as np
import ml_dtypes
import concourse.bass as bass
import concourse.mybir as mybir
from concourse.bass_utils import run_bass_kernel_spmd

F32 = mybir.dt.float32
BF16 = mybir.dt.bfloat16
AF = mybir.ActivationFunctionType
ALU = mybir.AluOpType
NPBF = ml_dtypes.bfloat16

S = 4096
D = 1024
NIN = 9216
DEPTH = 2
ALPHA = (2 * DEPTH) ** 0.25
EPS = 1e-5
NFFT = 8192
TWO_PI = 2.0 * math.pi


class Res:
    __slots__ = ("name", "writers", "readers", "prev", "sem", "dcount")

    def __init__(self, name, sem=None):
        self.name = name
        self.writers = []
        self.readers = []
        self.prev = []
        self.sem = sem if sem is not None else {}
        self.dcount = 0


class Prog:
    ENG = ("pe", "act", "dve", "pool", "sp")

    def __init__(self, nc, stack):
        self.nc = nc
        self.stack = stack
        self.e = {"pe": nc.tensor, "act": nc.scalar, "dve": nc.vector, "pool": nc.gpsimd, "sp": nc.sync}
        self.info = []
        self.pending = {k: [] for k in self.ENG}
        self.esem = {}
        self.ecount = {k: 0 for k in self.ENG}
        self.known = {k: {} for k in self.ENG}
        self.nsem = 0
        self.dma_since_barrier = []
        self.last_sig = {k: None for k in self.ENG}

    def new_sem(self, name):
        self.nsem += 1
        return self.stack.enter_context(self.nc.semaphore(f"{name}_{self.nsem}"))

    def _deps(self, reads, writes, partial):
        deps = set()
        for r in reads:
            deps.update(r.writers)
        for w in writes:
            if partial and not w.readers:
                deps.update(w.prev)
            else:
                w.prev = w.writers + w.readers
                deps.update(w.prev)
                w.writers = []
                w.readers = []
        return deps

    def _commit(self, oid, reads, writes):
        for r in reads:
            r.readers.append(oid)
        for w in writes:
            w.writers.append(oid)

    def _waits(self, eng, deps, is_dma):
        need = {}
        for d in deps:
            inf = self.info[d]
            assert inf is not None, "dependency on a non-signalling op"
            deng, sem, val = inf
            if deng == "pe" and eng == "pe" and not is_dma:
                continue
            k = id(sem)
            if k not in need or need[k][1] < val:
                need[k] = (sem, val)
        for k, (sem, val) in need.items():
            if self.known[eng].get(k, 0) >= val:
                continue
            self.e[eng].wait_ge(sem, val)
            self.known[eng][k] = val

    def op(self, eng, fn, reads=(), writes=(), sig=True, partial=False):
        deps = self._deps(reads, writes, partial)
        self._waits(eng, deps, False)
        ins = fn(self.e[eng])
        oid = len(self.info)
        if sig:
            if self.ecount[eng] % 30000 == 0:
                self.esem[eng] = self.new_sem("e" + eng)
                self.ecount[eng] = 0
            self.ecount[eng] += 1
            ins.then_inc(self.esem[eng], 1)
            inf = (eng, self.esem[eng], self.ecount[eng])
            self.info.append(inf)
            for p in self.pending[eng]:
                self.info[p] = inf
            self.pending[eng] = []
            self.last_sig[eng] = oid
        else:
            self.info.append(None)
            self.pending[eng].append(oid)
        self._commit(oid, reads, writes)
        return oid

    def dma(self, eng, out, in_, reads, writes, partial=True, sem_of=None, **kw):
        assert len(writes) == 1
        w = sem_of if sem_of is not None else writes[0]
        deps = self._deps(reads, writes, partial)
        self._waits(eng, deps, True)
        kind = "sw" if eng == "pool" else "hw"
        if kind not in w.sem:
            w.sem[kind] = [self.new_sem("d" + kind), 0]
        w.sem[kind][1] += 1
        self.e[eng].dma_start(out=out, in_=in_, **kw).then_inc(w.sem[kind][0], 16)
        oid = len(self.info)
        self.info.append(("dma", w.sem[kind][0], 16 * w.sem[kind][1]))
        self.dma_since_barrier.append(oid)
        self._commit(oid, reads, writes)
        return oid

    def barrier(self):
        deps = set(self.dma_since_barrier)
        for k in self.ENG:
            assert not self.pending[k], "pending non-signalled ops at barrier"
            if self.last_sig[k] is not None:
                deps.add(self.last_sig[k])
        for k in self.ENG:
            need = {}
            for d in deps:
                deng, sem, val = self.info[d]
                kk = id(sem)
                if kk not in need or need[kk][1] < val:
                    need[kk] = (sem, val)
            for kk, (sem, val) in need.items():
                if self.known[k].get(kk, 0) >= val:
                    continue
                self.e[k].wait_ge(sem, val)
                self.known[k][kk] = val
        self.dma_since_barrier = []


def _const_tables():
    c = {}
    c["ident"] = np.eye(128, dtype=np.float32).astype(NPBF)
    dil = (1, 4, 16)
    p = np.arange(128)[:, None]
    j = np.arange(128)[None, :]
    tab = np.zeros((128, 12, 3, 2, 128), np.float32)
    for g in range(3):
        for h in range(8):
            slope = 2.0 ** (-(h + 1))
            for kt in range(3):
                rel = p + (kt - 1) * 128 - j
                val = -8.0 * slope * dil[g] * np.abs(rel)
                val = np.where(np.abs(rel) <= 64, val, -32768.0)
                tab[:, g * 4 + h // 2, kt, h % 2, :] = val
    c["abias"] = tab.astype(NPBF)
    osel = np.zeros((128, 2, 128), np.float32)
    osel[:, 0, :64] = 1.0
    osel[:, 1, 64:] = 1.0
    c["onesel"] = osel.astype(NPBF)
    n2 = np.arange(128)[:, None].astype(np.float64)
    k2 = np.arange(128)[None, :].astype(np.float64)
    ang = -2.0 * np.pi * n2 * (k2 + 0.5) / 256.0
    Fr, Fi = np.cos(ang), np.sin(ang)
    c["F1"] = np.concatenate([Fr, Fi, -Fi], axis=1).astype(np.float32).astype(NPBF)
    ang2 = -2.0 * np.pi * (n2 + 128.0) * (k2 + 0.5) / 256.0
    Fr2, Fi2 = -np.cos(ang2), -np.sin(ang2)
    c["F1hi"] = np.concatenate([Fr2, Fi2, -Fi2], axis=1).astype(np.float32).astype(NPBF)
    n1 = np.arange(32).astype(np.float64)
    k1 = np.arange(32).astype(np.float64)
    eye4 = np.eye(4)
    a = -2.0 * np.pi * n1[:, None] * k1[None, :] / 32.0
    F32m = np.zeros((128, 3, 128), np.float32)
    F32m[:, 0, :] = np.kron(np.cos(a), eye4)
    F32m[:, 1, :] = np.kron(np.sin(a), eye4)
    F32m[:, 2, :] = -np.kron(np.sin(a), eye4)
    c["F32m"] = F32m.astype(NPBF)
    kk2 = np.arange(128).astype(np.float64)
    a = -2.0 * np.pi * np.repeat(n1, 4)[:, None] * (kk2[None, :] + 0.5) / NFFT
    tw = np.zeros((128, 2, 8, 128), np.float32)
    tw[:, 0, :, :] = np.cos(a)[:, None, :]
    tw[:, 1, :, :] = np.sin(a)[:, None, :]
    c["tw8"] = tw
    a = 2.0 * np.pi * k1[:, None] * n1[None, :] / 32.0
    Rr = np.kron(np.cos(a), eye4)
    Ri = np.kron(np.sin(a), eye4)
    R12 = np.zeros((128, 2, 256), np.float32)
    R12[:, 0, :128], R12[:, 0, 128:] = Rr, Ri
    R12[:, 1, :128], R12[:, 1, 128:] = -Ri, Rr
    c["R12"] = R12.astype(NPBF)
    T = np.zeros((128, 32, 2, 128), np.float32)
    kk = np.arange(128)[:, None].astype(np.float64)
    nn2 = np.arange(128)[None, :].astype(np.float64)
    for a1 in range(32):
        a = 2.0 * np.pi * (a1 + 32.0 * nn2) * (kk + 0.5) / NFFT
        T[:, a1, 0, :] = (2.0 / NFFT) * np.cos(a)
        T[:, a1, 1, :] = -(2.0 / NFFT) * np.sin(a)
    c["T"] = T.astype(NPBF)
    L = S
    t = np.linspace(0.0, 1.0, L, dtype=np.float32)[:, None]
    bands = 16
    w = (2.0 * np.pi * np.arange(L, dtype=np.float32)[:, None] / L).astype(np.float32)
    f = np.linspace(1e-4, bands - 1, bands, dtype=np.float32)[None, :]
    feat = np.concatenate([t, np.cos(f * w), -np.sin(f * w)], axis=-1).astype(np.float32)
    featT = np.ascontiguousarray(feat.T)
    rev = np.zeros_like(featT)
    rev[:, 1:] = featT[:, :0:-1]
    c["featT"] = np.stack([featT, rev], 0)
    trow = np.zeros((2, 1, L), np.float32)
    trow[0, 0] = t[:, 0]
    trow[1, 0, 1:] = t[:0:-1, 0]
    c["trow"] = trow
    deltas = np.linspace(math.log(1e-2) / 1.5, math.log(1e-2) / 0.3, 512, dtype=np.float32)
    c["ndelta"] = np.ascontiguousarray((-np.abs(deltas)).reshape(4, 128).T)
    return c


CONST_DT = {"ident": BF16, "abias": BF16, "onesel": BF16, "F1": BF16, "F1hi": BF16, "F32m": BF16, "tw8": F32,
            "R12": BF16, "T": BF16, "featT": F32, "trow": F32, "ndelta": F32}


def _layout_inputs(inp, b):
    m = {}
    m["x"] = np.ascontiguousarray(inp["x"][b])
    m["crep"] = np.ascontiguousarray(np.broadcast_to(
        inp["c"][b].reshape(8, 128).T[:, :, None], (128, 8, 128))).astype(np.float32)
    m["ccol"] = np.ascontiguousarray(inp["c"][b].reshape(8, 128).T)

    def kt(w, kc):
        Ld, K, N = w.shape
        return np.ascontiguousarray(w.reshape(Ld, kc, 128, N).transpose(0, 2, 1, 3))

    m["w_ada"] = kt(inp["w_ada"], 8)
    m["w_in"] = kt(inp["w_in"], 8)
    m["w_pa"] = kt(inp["w_proj_attn"], 4)
    m["w_ph"] = kt(inp["w_proj_hyena"], 4)
    m["w_out"] = kt(inp["w_out"], 8)

    def col(v):
        Ld, N = v.shape
        return np.ascontiguousarray(v.reshape(Ld, N // 128, 128).transpose(0, 2, 1))

    m["b_ada_c"] = col(inp["b_ada"])
    m["b_ada_r"] = np.ascontiguousarray(inp["b_ada"][:, None, :])
    m["b_in_c"] = col(inp["b_in"])
    m["b_in_r"] = np.ascontiguousarray(inp["b_in"][:, None, :])
    m["conv_w_c"] = np.ascontiguousarray(inp["conv_w"].reshape(DEPTH, 3, 12, 128).transpose(0, 3, 1, 2))
    m["conv_b_c"] = col(inp["conv_b"])
    m["f_w1"] = np.ascontiguousarray(inp["filt_w1"])
    m["f_w2"] = np.ascontiguousarray(inp["filt_w2"])
    m["f_w3"] = np.ascontiguousarray(inp["filt_w3"])
    m["f_w4"] = np.ascontiguousarray(inp["filt_w4"])
    m["f_b"] = np.ascontiguousarray(np.stack([inp["filt_b1"], inp["filt_b2"], inp["filt_b3"], inp["filt_freq"]], -1))
    m["f_bias_r"] = np.ascontiguousarray(inp["filt_bias"])
    m["b_out_r"] = np.ascontiguousarray(inp["b_out"][:, None, :])
    m["ln_g_r"] = np.ascontiguousarray(inp["ln_g"][:, None, :])
    m["ln_b_r"] = np.ascontiguousarray(inp["ln_b"][:, None, :])
    return m


IN_SHAPES = {
    "x": [S, D], "crep": [128, 8, 128], "ccol": [128, 8],
    "w_ada": [DEPTH, 128, 8, 3072], "w_in": [DEPTH, 128, 8, NIN], "w_pa": [DEPTH, 128, 4, D],
    "w_ph": [DEPTH, 128, 4, D], "w_out": [DEPTH, 128, 8, D],
    "b_ada_c": [DEPTH, 128, 24], "b_ada_r": [DEPTH, 1, 3072], "b_in_c": [DEPTH, 128, 72],
    "b_in_r": [DEPTH, 1, NIN], "conv_w_c": [DEPTH, 128, 3, 12], "conv_b_c": [DEPTH, 128, 12],
    "f_w1": [DEPTH, 33, 64], "f_w2": [DEPTH, 64, 64], "f_w3": [DEPTH, 64, 64], "f_w4": [DEPTH, 64, 2048],
    "f_b": [DEPTH, 64, 4], "f_bias_r": [DEPTH, 2, 512], "b_out_r": [DEPTH, 1, D],
    "ln_g_r": [DEPTH, 1, D], "ln_b_r": [DEPTH, 1, D],
}
CONST_SHAPES = {"ident": [128, 128], "abias": [128, 12, 3, 2, 128], "onesel": [128, 2, 128], "F1": [128, 384],
                "F1hi": [128, 384], "F32m": [128, 3, 128], "tw8": [128, 2, 8, 128], "R12": [128, 2, 256], "T": [128, 32, 2, 128],
                "featT": [2, 33, S], "trow": [2, 1, S], "ndelta": [128, 4]}


def bc(ap_t, offset, n):
    return bass.AP(ap_t.tensor, offset, [[0, 128], [1, n]])


class K:
    pass


def build(dbg=None, layers=DEPTH, stop_after=None):
    nc = bass.Bass("TRN2", target_bir_lowering=False)
    try:
        nc.allow_low_precision("bf16 matmul operands with fp32 accumulation")
    except Exception:
        pass
    stack = ExitStack()
    P = Prog(nc, stack)
    I = {k: nc.dram_tensor(k, s, F32, kind="ExternalInput").ap() for k, s in IN_SHAPES.items()}
    C = {k: nc.dram_tensor("c_" + k, s, CONST_DT[k], kind="ExternalInput").ap() for k, s in CONST_SHAPES.items()}
    out = nc.dram_tensor("out", [S, D], F32, kind="ExternalOutput").ap()
    dbg_out = {}

    def dram(name, shape, dt):
        kind = "ExternalOutput" if (dbg and name in dbg) else "Internal"
        t = nc.dram_tensor(name, shape, dt, kind=kind).ap()
        if kind == "ExternalOutput":
            dbg_out[name] = t
        return t

    proj_d = dram("proj_d", [72, 128, S], BF16)
    vtm_d = dram("vtm_d", [3, 4, 128, 32, 128], BF16)
    yattn_d = dram("yattn_d", [4, 128, S], BF16)
    yhy_d = dram("yhy_d", [4, 128, S], BF16)
    xmid_d = dram("xmid_d", [S, D], F32)
    H_d = dram("H_d", [DEPTH, 2, 4, 128, 2, 32, 128], BF16)
    fam = {}
    R_proj = [Res(f"proj{i}", fam) for i in range(72)]
    R_vtm = [Res(f"vtm{g}", fam) for g in range(3)]
    R_yattn = [Res(f"yattn{i}", fam) for i in range(4)]
    R_yhy = [Res(f"yhy{i}", fam) for i in range(4)]
    R_xmid = Res("xmid", fam)
    R_H = [[[Res(f"H{l}{o}{h}", fam) for h in range(4)] for o in range(2)] for l in range(DEPTH)]
    R_out = Res("out")
    R_in = Res("inputs")

    RES = {}
    cnt = {"n": 0}

    def sb(name, shape, dt, st=None):
        cnt["n"] += 1
        t = (st or stack).enter_context(nc.sbuf_tensor(f"s_{name}_{cnt['n']}", shape, dt))
        if name not in RES:
            RES[name] = Res(name)
        return t, RES[name]

    psf = []
    for i in range(6):
        t = stack.enter_context(nc.psum_tensor(f"psf{i}", [128, 512], F32))
        psf.append((t, Res(f"psf{i}")))
    psb = []
    for i in range(2):
        t = stack.enter_context(nc.psum_tensor(f"psb{i}", [128, 1024], BF16))
        psb.append((t, Res(f"psb{i}")))
    rr = {"f": 0, "b": 0, "q": 0}

    def next_psf():
        rr["f"] = (rr["f"] + 1) % 6
        return psf[rr["f"]]

    def next_psb():
        rr["b"] = (rr["b"] + 1) % 2
        return psb[rr["b"]]

    DQ = ("sp", "pool")

    def next_q():
        rr["q"] = (rr["q"] + 1) % len(DQ)
        return DQ[rr["q"]]

    ident, r_ident = sb("ident", [128, 128], BF16)
    onesel, r_onesel = sb("onesel", [128, 2, 128], BF16)
    F1, r_F1 = sb("F1", [128, 384], BF16)
    F1hi, r_F1hi = sb("F1hi", [128, 384], BF16)
    R12, r_R12 = sb("R12", [128, 2, 256], BF16)
    F32m, _ = sb("F32m", [128, 3, 128], BF16)
    tw8, _ = sb("tw8", [128, 2, 8, 128], F32)
    ones2, r_ones2 = sb("ones2", [2, 128], BF16)
    epsT, r_eps = sb("epsT", [128, 1], F32)
    npiT, r_npi = sb("npiT", [128, 1], F32)
    r_cst = Res("cst")
    r_ident = r_onesel = r_F1 = r_F1hi = r_R12 = r_cst
    P.dma("sp", ident[:], C["ident"][:, :], [R_in], [r_ident])
    P.dma("sp", onesel[:], C["onesel"][:, :, :], [R_in], [r_onesel])
    P.dma("sp", F1[:], C["F1"][:, :], [R_in], [r_F1])
    P.dma("sp", F1hi[:], C["F1hi"][:, :], [R_in], [r_F1hi])
    P.dma("sp", R12[:], C["R12"][:, :, :], [R_in], [r_R12])
    P.dma("sp", F32m[:], C["F32m"][:, :, :], [R_in], [r_cst])
    P.dma("sp", tw8[:], C["tw8"][:, :, :, :], [R_in], [r_cst])
    P.op("pool", lambda e: e.memset(ones2[:], 1.0), [], [r_ones2])
    P.op("pool", lambda e: e.memset(epsT[:], EPS), [], [r_eps])
    P.op("pool", lambda e: e.memset(npiT[:], -math.pi), [], [r_npi])

    modcol, r_modcol = sb("modcol", [128, DEPTH, 24], F32)
    sc1, r_sc1 = sb("sc1", [128, DEPTH, 8], F32)
    gate_b, r_gate = sb("gate_b", [128, DEPTH, D], F32)
    b_in_c, r_binc = sb("b_in_c", [128, DEPTH, 72], F32)
    convw, r_convw = sb("convw", [128, DEPTH, 3, 12], F32)
    convb, r_convb = sb("convb", [128, DEPTH, 12], F32)
    bin2, r_bin2 = sb("bin2", [1, DEPTH, 1536], BF16)
    binl, r_binl = sb("binl", [1, DEPTH, 1536], BF16)
    r_binc = r_convw = r_convb = r_cst
    for l in range(DEPTH):
        P.dma("sp", b_in_c[:, l, :], I["b_in_c"][l], [R_in], [r_binc])
        P.dma("sp", convw[:, l, :, :], I["conv_w_c"][l], [R_in], [r_convw])
        P.dma("sp", convb[:, l, :], I["conv_b_c"][l], [R_in], [r_convb])

    with ExitStack() as ph:
        ccol, r_ccol = sb("ccol", [128, 8], F32, ph)
        crep, r_crep = sb("crep", [128, 8, 128], F32, ph)
        badac, r_badac = sb("badac", [128, DEPTH, 24], F32, ph)
        badar, r_badar = sb("badar", [128, DEPTH, D], F32, ph)
        vb, r_vb = sb("vb", [1, DEPTH, 1536], F32, ph)
        vbh, r_vbh = sb("vbh", [1, DEPTH, 1536], F32, ph)
        r_ccol = r_crep = r_badac = r_badar = r_vb = r_cst
        P.dma("sp", ccol[:], I["ccol"][:, :], [R_in], [r_ccol])
        P.dma("sp", crep[:], I["crep"][:, :, :], [R_in], [r_crep])
        for l in range(DEPTH):
            P.dma("sp", badac[:, l, :], I["b_ada_c"][l], [R_in], [r_badac])
            P.dma("sp", badar[:, l, :], bc(I["b_ada_r"], l * 3072 + 2048, D), [R_in], [r_badar])
            P.dma("sp", vb[:, l, :], bass.AP(I["b_in_r"].tensor, l * NIN + 3072, [[0, 1], [1, 1536]]), [R_in], [r_vb])
        P.op("dve", lambda e: e.tensor_copy(out=bin2[:], in_=vb[:]), [r_vb], [r_bin2])
        P.op("dve", lambda e: e.tensor_copy(out=vbh[:], in_=bin2[:]), [r_bin2], [r_vbh])
        P.op("dve", lambda e: e.tensor_sub(out=vbh[:], in0=vb[:], in1=vbh[:]), [r_vb, r_vbh], [r_vbh])
        P.op("dve", lambda e: e.tensor_copy(out=binl[:], in_=vbh[:]), [r_vbh], [r_binl])
        wa = [sb(f"wa{i}", [128, 8, 512], F32, ph) for i in range(2)]
        for l in range(DEPTH):
            pc, r_pc = next_psf()
            for blk in range(6):
                wt, r_wt = wa[blk % 2]
                P.dma("sp", wt[:, 0:4, :], I["w_ada"][l, :, 0:4, blk * 512:(blk + 1) * 512], [R_in], [r_wt], partial=False)
                P.dma("pool", wt[:, 4:8, :], I["w_ada"][l, :, 4:8, blk * 512:(blk + 1) * 512], [R_in], [r_wt])
                for f in range(4):
                    fi = blk * 4 + f
                    for kc in range(8):
                        P.op("pe", lambda e, wt=wt, f=f, kc=kc, fi=fi: e.matmul(
                            pc[:, fi:fi + 1], lhsT=wt[:, kc, f * 128:(f + 1) * 128], rhs=ccol[:, kc:kc + 1],
                            start=(kc == 0), stop=(kc == 7)),
                            [r_wt, r_ccol], [r_pc], sig=(kc == 7), partial=True)
                if blk >= 4:
                    pg, r_pg = next_psf()
                    for kc in range(8):
                        P.op("pe", lambda e, wt=wt, kc=kc: e.matmul(
                            pg[:, :], lhsT=crep[:, kc, :], rhs=wt[:, kc, :], start=(kc == 0), stop=(kc == 7)),
                            [r_wt, r_crep], [r_pg], sig=(kc == 7), partial=True)
                    h0 = (blk - 4) * 512
                    P.op("dve", lambda e, l=l, h0=h0, pg=pg: e.tensor_add(
                        out=gate_b[:, l, h0:h0 + 512], in0=pg[:, :], in1=badar[:, l, h0:h0 + 512]),
                        [r_pg, r_badar], [r_gate], partial=True)
            P.op("dve", lambda e, l=l, pc=pc: e.tensor_add(out=modcol[:, l, :], in0=pc[:, 0:24], in1=badac[:, l, :]),
                 [r_pc, r_badac], [r_modcol], partial=True)
            P.op("dve", lambda e, l=l: e.tensor_scalar_add(out=sc1[:, l, :], in0=modcol[:, l, 8:16], scalar1=1.0),
                 [r_modcol], [r_sc1], partial=True)
        P.barrier()


    act_dve = {"n": 0}

    def evac(out_ap, in_ap, reads, writes, partial=True, simple=False):
        act_dve["n"] += 1
        if simple and act_dve["n"] % 2:
            P.op("act", lambda e: e.copy(out=out_ap, in_=in_ap), reads, writes, partial=partial)
        else:
            P.op("dve", lambda e: e.tensor_copy(out=out_ap, in_=in_ap), reads, writes, partial=partial)

    def fft_fwd(zl, r_zl, zh, r_zh, CEf, r_CE, X, r_X, tpb):
        Cv = CEf[:, 0:8192].rearrange("p (t c k) -> p t c k", t=2, c=32, k=128)
        for cp in range(16):
            pp, r_pp = next_psf()
            for gi in range(2):
                cg = cp * 2 + gi
                P.op("pe", lambda e: e.matmul(pp[:, gi * 256:(gi + 1) * 256], lhsT=zl[:, cg * 128:(cg + 1) * 128], rhs=F1[:, 0:256],
                                              start=True, stop=(zh is None)),
                     [r_zl, r_F1], [r_pp], sig=(zh is None and gi == 1), partial=(gi > 0))
                if zh is not None:
                    P.op("pe", lambda e: e.matmul(pp[:, gi * 256:(gi + 1) * 256], lhsT=zh[:, cg * 128:(cg + 1) * 128], rhs=F1hi[:, 0:256],
                                                  start=False, stop=True),
                         [r_zh, r_F1hi], [r_pp], sig=(gi == 1), partial=True)
            evac(Cv[:, :, cp * 2:cp * 2 + 2, :], pp[:, :].rearrange("p (g t k) -> p t g k", g=2, t=2, k=128), [r_pp], [r_CE],
                 partial=(cp > 0), simple=False)
        fmode = dbg.get("fft_mode", 9) if dbg else 9
        if fmode < 2:
            return
        (t1, r1), (t2, r2), (t3, r3), (t4, r4) = tpb
        bs = t1.shape[1]
        for blk in range(32 // bs):
            cs = slice(blk * bs, blk * bs + bs)
            cr, ci = Cv[:, 0, cs, :], Cv[:, 1, cs, :]
            P.op("dve", lambda e: e.tensor_mul(out=t1[:], in0=cr, in1=tw8[:, 0, 0:bs, :]), [r_CE, r_cst], [r1], partial=False)
            P.op("dve", lambda e: e.tensor_mul(out=t2[:], in0=ci, in1=tw8[:, 1, 0:bs, :]), [r_CE, r_cst], [r2], partial=False)
            P.op("dve", lambda e: e.tensor_mul(out=t3[:], in0=cr, in1=tw8[:, 1, 0:bs, :]), [r_CE, r_cst], [r3], partial=False)
            P.op("dve", lambda e: e.tensor_mul(out=t4[:], in0=ci, in1=tw8[:, 0, 0:bs, :]), [r_CE, r_cst], [r4], partial=False)
            P.op("dve", lambda e: e.tensor_sub(out=cr, in0=t1[:], in1=t2[:]), [r1, r2], [r_CE], partial=True)
            P.op("dve", lambda e: e.tensor_add(out=ci, in0=t3[:], in1=t4[:]), [r3, r4], [r_CE], partial=True)
            for hf_ in range(bs // 4 if fmode >= 3 else 0):
                c0 = (blk * bs + hf_ * 4) * 128
                rre = CEf[:, c0:c0 + 512]
                rim = CEf[:, 4096 + c0:4096 + c0 + 512]
                for t, (la, lb) in enumerate(((0, 2), (1, 0))):
                    px, r_px = next_psf()
                    P.op("pe", lambda e: e.matmul(px[:, :], lhsT=F32m[:, la, :], rhs=rre, start=True, stop=False),
                         [r_cst, r_CE], [r_px], sig=False, partial=False)
                    P.op("pe", lambda e: e.matmul(px[:, :], lhsT=F32m[:, lb, :], rhs=rim, start=False, stop=True),
                         [r_cst, r_CE], [r_px], sig=True, partial=True)
                    x0 = (t * 32 + blk * bs + hf_ * 4) * 128
                    evac(X[:].rearrange("p t c k -> p (t c k)")[:, x0:x0 + 512], px[:, :],
                         [r_px], [r_X], partial=not (blk == 0 and hf_ == 0 and t == 0), simple=True)

    def fft_inv(Y, r_Y, CEf, r_CE, Tt, r_T, epilogue):
        E5 = CEf[:, 0:32 * 2 * 128].rearrange("p (n t g c) -> p n t g c", n=32, t=2, g=32, c=4)
        E4 = CEf[:, 0:32 * 2 * 128].rearrange("p (n t c) -> p n t c", n=32, t=2, c=128)
        for cp in range(16):
            pe_, r_pe = next_psf()
            for gi in range(2):
                cg = cp * 2 + gi
                P.op("pe", lambda e: e.matmul(pe_[:, gi * 256:(gi + 1) * 256], lhsT=Y[:, 0, cg, :], rhs=R12[:, 0, :], start=True, stop=False),
                     [r_Y, r_R12], [r_pe], sig=False, partial=(gi > 0))
                P.op("pe", lambda e: e.matmul(pe_[:, gi * 256:(gi + 1) * 256], lhsT=Y[:, 1, cg, :], rhs=R12[:, 1, :], start=False, stop=True),
                     [r_Y, r_R12], [r_pe], sig=(gi == 1), partial=True)
            pv = pe_[:, :].rearrange("p (g t n c) -> p t n g c", g=2, t=2, n=32, c=4)
            for t in range(2):
                evac(E5[:, :, t, cp * 2:cp * 2 + 2, :], pv[:, t], [r_pe], [r_CE], partial=not (cp == 0 and t == 0))
        for nb in range(8):
            py, r_py = next_psf()
            for ni in range(4):
                n1 = nb * 4 + ni
                P.op("pe", lambda e: e.matmul(py[:, ni * 128:(ni + 1) * 128], lhsT=Tt[:, n1, 0, :], rhs=E4[:, n1, 0, :], start=True, stop=False),
                     [r_T, r_CE], [r_py], sig=False, partial=(ni > 0))
                P.op("pe", lambda e: e.matmul(py[:, ni * 128:(ni + 1) * 128], lhsT=Tt[:, n1, 1, :], rhs=E4[:, n1, 1, :], start=False, stop=True),
                     [r_T, r_CE], [r_py], sig=(ni == 3), partial=True)
            epilogue(nb, py[:, :].rearrange("p (n c) -> p n c", n=4), r_py)

    def to_token_major(srcT, r_src, dst, r_dst, fftl=True):
        sv = srcT[:, :].rearrange("p (a b) -> p b a", b=32)
        for nb in range(4):
            pt, r_pt = next_psb()
            for ni in range(8):
                n1 = nb * 8 + ni
                P.op("pe", lambda e: e.transpose(out=pt[:, ni * 128:(ni + 1) * 128], in_=sv[:, n1, :], identity=ident[:]),
                     [r_src, r_ident], [r_pt], sig=(ni == 7), partial=(ni > 0))
            if fftl:
                evac(dst[:, :].rearrange("p (g n c) -> p g n c", g=32, n=32, c=4)[:, :, nb * 8:(nb + 1) * 8, :],
                     pt[:, :].rearrange("p (n g c) -> p g n c", n=8, g=32, c=4), [r_pt], [r_dst], partial=(nb > 0))
            else:
                evac(dst[:, :].rearrange("p (n c) -> p n c", n=32)[:, nb * 8:(nb + 1) * 8, :],
                     pt[:, :].rearrange("p (n c) -> p n c", n=8), [r_pt], [r_dst], partial=(nb > 0))

    if not (dbg and dbg.get("skip_filters")):
        with ExitStack() as ph:
            h3 = [[sb(f"h3_{l}{v}", [64, S], BF16, ph) for v in range(2)] for l in range(DEPTH)]
            w4b = [sb(f"w4b{l}", [64, 2048], BF16, ph) for l in range(DEPTH)]
            with ExitStack() as ph2:
                feat = [sb(f"feat{v}", [33, S], F32, ph2) for v in range(2)]
                hA, r_hA = sb("hA", [64, S], F32, ph2)
                hB, r_hB = sb("hB", [64, S], F32, ph2)
                w4f, r_w4f = sb("w4f", [64, 2048], F32, ph2)
                fw = [sb(f"fw{i}", [64, 64], F32, ph2) for i in range(3)]
                fbt, r_fbt = sb("fbt", [64, 4], F32, ph2)
                fsc, r_fsc = sb("fsc", [64, 4], F32, ph2)
                ty = [sb(f"ty{i}", [64, 512], F32, ph2) for i in range(2)]
                tki, r_tki = sb("tki", [64, 512], mybir.dt.int32, ph2)
                tkf, r_tkf = sb("tkf", [64, 512], F32, ph2)
                for v in range(2):
                    P.dma("sp", feat[v][0][:], C["featT"][v], [R_in], [feat[v][1]], partial=False)
                for l in range(DEPTH):
                    P.dma("sp", fw[0][0][0:33, :], I["f_w1"][l], [R_in], [fw[0][1]], partial=False)
                    P.dma("sp", fw[1][0][:], I["f_w2"][l], [R_in], [fw[1][1]], partial=False)
                    P.dma("sp", fw[2][0][:], I["f_w3"][l], [R_in], [fw[2][1]], partial=False)
                    P.dma("sp", w4f[:], I["f_w4"][l], [R_in], [r_w4f], partial=False)
                    P.dma("sp", fbt[:], I["f_b"][l], [R_in], [r_fbt], partial=False)
                    P.op("pool", lambda e: e.tensor_copy(out=w4b[l][0][:], in_=w4f[:]), [r_w4f], [w4b[l][1]], partial=False)
                    P.op("dve", lambda e: e.tensor_scalar_mul(out=fsc[:, 3:4], in0=fbt[:, 3:4], scalar1=1.0 / TWO_PI), [r_fbt], [r_fsc], partial=False)
                    P.op("dve", lambda e: e.tensor_scalar(out=fsc[:, 0:3], in0=fbt[:, 0:3], scalar1=fsc[:, 3:4], scalar2=8.0,
                                                          op0=ALU.mult, op1=ALU.add), [r_fbt, r_fsc], [r_fsc], partial=False)
                    for v in range(2):
                        src, r_src = feat[v]
                        kdim = 33
                        for layer_i in range(3):
                            last = (layer_i == 2)
                            dst, r_dst = (h3[l][v] if last else ((hA, r_hA) if layer_i == 0 else (hB, r_hB)))
                            wt_, r_wt_ = fw[layer_i]
                            for tb in range(8):
                                sl = slice(tb * 512, (tb + 1) * 512)
                                pp, r_pp = next_psf()
                                P.op("pe", lambda e: e.matmul(pp[0:64, :], lhsT=wt_[0:kdim, :], rhs=src[0:kdim, sl], start=True, stop=True),
                                     [r_wt_, r_src], [r_pp], partial=False)
                                tyt, r_ty = ty[tb % 2]
                                P.op("dve", lambda e: e.tensor_scalar(out=tyt[:], in0=pp[0:64, :], scalar1=fsc[:, 3:4],
                                                                      scalar2=fsc[:, layer_i:layer_i + 1], op0=ALU.mult, op1=ALU.add),
                                     [r_pp, r_fsc], [r_ty], partial=False)
                                P.op("dve", lambda e: e.tensor_copy(out=tki[:], in_=tyt[:]), [r_ty], [r_tki], partial=False)
                                P.op("dve", lambda e: e.tensor_copy(out=tkf[:], in_=tki[:]), [r_tki], [r_tkf], partial=False)
                                P.op("dve", lambda e: e.tensor_sub(out=tyt[:], in0=tyt[:], in1=tkf[:]), [r_ty, r_tkf], [r_ty], partial=False)
                                P.op("dve", lambda e: e.tensor_single_scalar(out=tkf[:], in_=tyt[:], scalar=0.5, op=ALU.is_ge),
                                     [r_ty], [r_tkf], partial=False)
                                P.op("dve", lambda e: e.tensor_sub(out=tyt[:], in0=tyt[:], in1=tkf[:]), [r_ty, r_tkf], [r_ty], partial=False)
                                P.op("act", lambda e: e.activation(out=dst[:, sl], in_=tyt[:], func=AF.Sin, scale=TWO_PI),
                                     [r_ty], [r_dst], partial=(tb > 0))
                            src, r_src = dst, r_dst
                            kdim = 64
                P.barrier()
            trb = [sb(f"trb{v}", [128, S], F32, ph) for v in range(2)]
            dec = [sb(f"dec{v}", [128, S], F32, ph) for v in range(2)]
            ndl, r_ndl = sb("ndl", [128, 4], F32, ph)
            fT, r_fT = sb("fT", [128, S], BF16, ph)
            ftm = [sb(f"ftm{v}", [128, S], BF16, ph) for v in range(2)]
            CEf, r_CE = sb("CEf", [128, 8192], BF16, ph)
            Xb, r_X = sb("Xb", [128, 2, 32, 128], BF16, ph)
            tpf = [sb(f"tpf{i}", [128, 4, 128], F32, ph) for i in range(4)]
            P.dma("sp", ndl[:], C["ndelta"][:, :], [R_in], [r_ndl], partial=False)
            for v in range(2):
                P.dma("sp", trb[v][0][:], bc(C["trow"], v * S, S), [R_in], [trb[v][1]], partial=False)
            flist = [(c, l, o) for c in range(4) for l in range(DEPTH) for o in range(2)]
            if dbg and "filt_list" in dbg:
                flist = dbg["filt_list"]
            lastc = None
            for (c, l, o) in flist:
                if c != lastc:
                    for v in range(2):
                        P.op("act", lambda e: e.activation(out=dec[v][0][:], in_=trb[v][0][:], func=AF.Exp, scale=ndl[:, c:c + 1]),
                             [trb[v][1], r_ndl], [dec[v][1]], partial=False)
                    lastc = c
                for v in range(2):
                    col0 = (o * 2 + v) * 512 + c * 128
                    for tb in range(8):
                        sl = slice(tb * 512, (tb + 1) * 512)
                        pp, r_pp = next_psf()
                        P.op("pe", lambda e: e.matmul(pp[:, :], lhsT=w4b[l][0][:, col0:col0 + 128], rhs=h3[l][v][0][:, sl], start=True, stop=True),
                             [w4b[l][1], h3[l][v][1]], [r_pp], partial=False)
                        P.op("dve", lambda e: e.tensor_mul(out=fT[:, sl], in0=pp[:, :], in1=dec[v][0][:, sl]),
                             [r_pp, dec[v][1]], [r_fT], partial=(tb > 0))
                    if v == 1:
                        P.op("dve", lambda e: e.memset(fT[:, 0:1], 0.0), [], [r_fT], partial=True)
                    to_token_major(fT, r_fT, ftm[v][0], ftm[v][1])
                fft_fwd(ftm[0][0], ftm[0][1], ftm[1][0], ftm[1][1], CEf, r_CE, Xb, r_X, tpf)
                P.dma("sp", H_d[l, o, c], Xb[:], [r_X], [R_H[l][o][c]], partial=False, sem_of=r_X)
            P.barrier()
    if stop_after == "filt":
        layers = 0

    x_src = I["x"]
    R_xsrc = R_in
    for l in range(layers):
        x_dst, R_xdst = (xmid_d, R_xmid) if l < DEPTH - 1 else (out, R_out)
        lay = ExitStack()
        hT, r_hT = sb(f"hT", [128, 8, S], BF16, lay)
        with ExitStack() as ph:
            xt = [sb(f"xt{i}", [128, D], F32, ph) for i in range(2)]
            xn = [sb(f"xn{i}", [128, D], BF16, ph) for i in range(2)]
            st = [sb(f"st{i}", [128, 12], F32, ph) for i in range(2)]
            mv = [sb(f"mv{i}", [128, 2], F32, ph) for i in range(2)]
            for t in range(32):
                xtt, r_xt = xt[t % 2]
                xnn, r_xn = xn[t % 2]
                stt, r_st = st[t % 2]
                mvv, r_mv = mv[t % 2]
                P.dma(next_q(), xtt[:], x_src[t * 128:(t + 1) * 128, :], [R_xsrc], [r_xt], partial=False)
                P.op("dve", lambda e: e.bn_stats(out=stt[:, 0:6], in_=xtt[:, 0:512]), [r_xt], [r_st], partial=False)
                P.op("dve", lambda e: e.bn_stats(out=stt[:, 6:12], in_=xtt[:, 512:1024]), [r_xt], [r_st], partial=True)
                P.op("dve", lambda e: e.bn_aggr(out=mvv[:], in_=stt[:]), [r_st], [r_mv], partial=False)
                P.op("act", lambda e: e.activation(out=mvv[:, 1:2], in_=mvv[:, 1:2], func=AF.Sqrt, bias=epsT[:], scale=1.0),
                     [r_mv, r_eps], [r_mv], partial=False)
                P.op("dve", lambda e: e.reciprocal(out=mvv[:, 1:2], in_=mvv[:, 1:2]), [r_mv], [r_mv], partial=False)
                P.op("dve", lambda e: e.tensor_scalar(out=xnn[:], in0=xtt[:], scalar1=mvv[:, 0:1], scalar2=mvv[:, 1:2],
                                                      op0=ALU.subtract, op1=ALU.mult), [r_xt, r_mv], [r_xn], partial=False)
                pt, r_pt = next_psb()
                for kc in range(8):
                    P.op("pe", lambda e, kc=kc: e.transpose(out=pt[:, kc * 128:(kc + 1) * 128], in_=xnn[:, kc * 128:(kc + 1) * 128],
                                                            identity=ident[:]),
                         [r_xn, r_ident], [r_pt], sig=(kc == 7), partial=(kc > 0))
                for kc in range(8):
                    P.op("act", lambda e, kc=kc: e.activation(out=hT[:, kc, t * 128:(t + 1) * 128], in_=pt[:, kc * 128:(kc + 1) * 128],
                                                              func=AF.Identity, scale=sc1[:, l, kc:kc + 1], bias=modcol[:, l, kc:kc + 1]),
                         [r_pt, r_sc1, r_modcol], [r_hT], partial=True)
            P.barrier()
        if stop_after == "ln":
            lay.close()
            break

        with ExitStack() as ph:
            ws = [sb(f"ws{i}", [128, 8, 128], F32, ph) for i in range(3)]
            wb = [sb(f"wb{i}", [128, 8, 128], BF16, ph) for i in range(3)]
            ob = [sb(f"ob{i}", [128, S], BF16, ph) for i in range(2)]
            nfm = 0
            chunks = [j for j in range(72) if not (24 <= j < 36)]
            if dbg and "proj_chunks" in dbg:
                chunks = dbg["proj_chunks"]
            for j in chunks:
                wst, r_ws = ws[nfm % 3]
                wbt, r_wb = wb[nfm % 3]
                obt, r_ob = ob[nfm % 2]
                nfm += 1
                P.dma(next_q(), wst[:], I["w_in"][l, :, :, j * 128:(j + 1) * 128], [R_in], [r_ws], partial=False)
                P.op("pool", lambda e: e.tensor_copy(out=wbt[:], in_=wst[:]), [r_ws], [r_wb], partial=False)
                if 36 <= j < 40 or 52 <= j < 56:
                    fn = AF.Silu
                elif j >= 56:
                    fn = AF.Sigmoid
                else:
                    fn = AF.Identity
                for tb in range(8):
                    pp, r_pp = next_psf()
                    for kc in range(8):
                        P.op("pe", lambda e, kc=kc: e.matmul(pp[:, :], lhsT=wbt[:, kc, :], rhs=hT[:, kc, tb * 512:(tb + 1) * 512],
                                                             start=(kc == 0), stop=(kc == 7)),
                             [r_wb, r_hT], [r_pp], sig=(kc == 7), partial=(kc > 0))
                    P.op("act", lambda e: e.activation(out=obt[:, tb * 512:(tb + 1) * 512], in_=pp[:, :], func=fn,
                                                       bias=b_in_c[:, l, j:j + 1], scale=1.0),
                         [r_pp, r_binc], [r_ob], partial=(tb > 0))
                P.dma(next_q(), proj_d[j, :, :], obt[:], [r_ob], [R_proj[j]], partial=False, sem_of=r_ob)
            wvs = [sb(f"wvs{i}", [128, 8, 512], F32, ph) for i in range(1)]
            wvb = [sb(f"wvb{i}", [128, 8, 512], BF16, ph) for i in range(2)]
            vo = [sb(f"vo{i}", [128, 4, 512], BF16, ph) for i in range(2)]
            nv = 0
            groups = range(3) if not (dbg and "v_groups" in dbg) else dbg["v_groups"]
            for g in groups:
                d = (1, 4, 16)[g]
                Lg = S // d
                wst, r_ws = wvs[0]
                wbt, r_wb = wvb[g % 2]
                c0 = 3072 + g * 512
                P.dma("sp", wst[:, 0:4, :], I["w_in"][l, :, 0:4, c0:c0 + 512], [R_in], [r_ws], partial=False)
                P.dma("pool", wst[:, 4:8, :], I["w_in"][l, :, 4:8, c0:c0 + 512], [R_in], [r_ws])
                P.op("pool", lambda e: e.tensor_copy(out=wbt[:], in_=wst[:]), [r_ws], [r_wb], partial=False)
                for tq in range(8):
                    vot, r_vo = vo[nv % 2]
                    nv += 1
                    for t4 in range(4):
                        ti = tq * 4 + t4
                        r_, u = divmod(ti, Lg // 128)
                        pp, r_pp = next_psf()
                        for kc in range(8):
                            lt = hT[:, kc, :].rearrange("p (i d) -> p d i", d=d)[:, r_, u * 128:(u + 1) * 128]
                            P.op("pe", lambda e, kc=kc, lt=lt: e.matmul(pp[:, :], lhsT=lt, rhs=wbt[:, kc, :], start=(kc == 0), stop=False),
                                 [r_wb, r_hT], [r_pp], sig=False, partial=(kc > 0))
                        P.op("pe", lambda e: e.matmul(pp[:, :], lhsT=ones2[0:1, :], rhs=bin2[0:1, l, g * 512:(g + 1) * 512], start=False, stop=False),
                             [r_ones2, r_bin2], [r_pp], sig=False, partial=True)
                        P.op("pe", lambda e: e.matmul(pp[:, :], lhsT=ones2[0:1, :], rhs=binl[0:1, l, g * 512:(g + 1) * 512], start=False, stop=True),
                             [r_ones2, r_binl], [r_pp], sig=True, partial=True)
                        P.op("dve", lambda e, t4=t4: e.tensor_copy(out=vot[:, t4, :], in_=pp[:, :]), [r_pp], [r_vo], partial=(t4 > 0))
                    qn = next_q()
                    for j4 in range(4):
                        P.dma(qn, vtm_d[g, j4, :, tq * 4:(tq + 1) * 4, :], vot[:, :, j4 * 128:(j4 + 1) * 128], [r_vo], [R_vtm[g]], sem_of=r_vo)
            P.barrier()
        if stop_after == "proj":
            lay.close()
            break
        lay.close()
        with ExitStack() as ph:
            Oacc, r_O = sb(f"Oacc", [128, 2, S], F32, ph)
            qTs = [sb(f"qT{i}", [128, 2, S], BF16, ph) for i in range(2)]
            kTs = [sb(f"kT{i}", [128, S], BF16, ph) for i in range(2)]
            vts = [sb(f"vt{i}", [128, 32, 2, 128], BF16, ph) for i in range(2)]
            abs_ = [sb(f"ab{i}", [128, 3, 256], BF16, ph) for i in range(2)]
            pTs = [sb(f"pT{i}", [128, 256], BF16, ph) for i in range(8)]
            gs, r_gs = sb(f"gs", [128, S], BF16, ph)
            vs, r_vs = sb(f"vs", [128, 32, 128], BF16, ph)
            yb, r_yb = sb(f"yb", [128, S], BF16, ph)
            rzs = [sb(f"rz{i}", [128, 512], F32, ph) for i in range(2)]
            tms = [sb(f"tm{i}", [128, 512], F32, ph) for i in range(2)]
            for i in range(2):
                P.op("pool", lambda e, i=i: e.memset(vts[i][0][:], 0.0), [], [vts[i][1]], partial=False)
                P.op("pool", lambda e, i=i: e.memset(qTs[i][0][:], 0.0), [], [qTs[i][1]], partial=False)
            npT = 0
            nbuf = 0
            jlist = range(4) if not (dbg and "att_j" in dbg) else dbg["att_j"]
            for j in jlist:
                for g in (range(3) if not (dbg and "att_g" in dbg) else dbg["att_g"]):
                    d = (1, 4, 16)[g]
                    ntl = (S // d) // 128
                    qT, r_q = qTs[nbuf % 2]
                    kT, r_k = kTs[nbuf % 2]
                    vt, r_v = vts[nbuf % 2]
                    ab, r_ab = abs_[nbuf % 2]
                    nbuf += 1
                    P.dma("sp", qT[0:64, 0, :], proj_d[g * 4 + j, 0:64, :], [R_proj[g * 4 + j]], [r_q], partial=False)
                    P.dma("sp", qT[64:128, 1, :], proj_d[g * 4 + j, 64:128, :], [R_proj[g * 4 + j]], [r_q])
                    P.dma("sp", kT[:], proj_d[12 + g * 4 + j, :, :], [R_proj[12 + g * 4 + j]], [r_k], partial=False)
                    P.dma("sp", vs[:], vtm_d[g, j, :, :, :], [R_vtm[g]], [r_vs], partial=False)
                    P.op("pool", lambda e: e.tensor_copy(out=vt[:, :, 0, 0:64], in_=vs[:, :, 0:64]), [r_vs], [r_v], partial=False)
                    P.op("pool", lambda e: e.tensor_copy(out=vt[:, :, 1, 64:128], in_=vs[:, :, 64:128]), [r_vs], [r_v], partial=True)
                    P.dma("sp", ab[:], C["abias"][:, g * 4 + j, :, :, :].rearrange("p k h q -> p k (h q)"), [R_in], [r_ab], partial=False)
                    qv = [qT[:, hh, :].rearrange("p (i d) -> p d i", d=d) for hh in range(2)]
                    kv = [kT[:, :].rearrange("p (i d) -> p d i", d=d) for hh in range(2)]
                    accv = Oacc[:].rearrange("p c (i d) -> p c d i", d=d)
                    tiles = [(r_, u) for r_ in range(d) for u in range(ntl)]

                    def stageA(r_, u):
                        kts = [kt for kt in range(3) if 0 <= u + kt - 1 < ntl]
                        outl = []
                        for kt in kts:
                            ku = u + kt - 1
                            rr["sc"] = (rr.get("sc", 0) + 1) % 4
                            ps, r_ps = psf[rr["sc"]]
                            for hh in range(2):
                                P.op("pe", lambda e, hh=hh: e.matmul(ps[:, hh * 128:(hh + 1) * 128], lhsT=ident[:],
                                                                     rhs=ab[:, kt, hh * 128:(hh + 1) * 128], start=True, stop=False),
                                     [r_ident, r_ab], [r_ps], sig=False, partial=(hh > 0))
                                P.op("pe", lambda e, hh=hh: e.matmul(
                                    ps[:, hh * 128:(hh + 1) * 128], lhsT=kv[hh][:, r_, ku * 128:(ku + 1) * 128],
                                    rhs=qv[hh][:, r_, u * 128:(u + 1) * 128], start=False, stop=True),
                                    [r_k, r_q], [r_ps], sig=(hh == 1), partial=True)
                            rr["pt"] = (rr.get("pt", 0) + 1) % 8
                            pT, r_pT = pTs[rr["pt"]]
                            P.op("act", lambda e: e.activation(out=pT[:], in_=ps[:, 0:256], func=AF.Exp, scale=0.125),
                                 [r_ps], [r_pT], partial=False)
                            outl.append((r_ * ntl + ku, pT, r_pT))
                        return outl

                    def stageB(r_, u, pl):
                        rr["po"] = (rr.get("po", 0) + 1) % 2
                        po, r_po = psf[4 + rr["po"]]
                        n = len(pl) * 2
                        for part in range(2):
                            i = 0
                            for (ti, pT, r_pT) in pl:
                                for hh in range(2):
                                    lt = vt[:, ti, hh, :] if part == 0 else onesel[:, hh, :]
                                    P.op("pe", lambda e, hh=hh, lt=lt, i=i: e.matmul(
                                        po[:, part * 128:(part + 1) * 128], lhsT=lt, rhs=pT[:, hh * 128:(hh + 1) * 128],
                                        start=(i == 0), stop=(i == n - 1)),
                                        [r_v, r_onesel, r_pT], [r_po], sig=(part == 1 and i == n - 1), partial=not (part == 0 and i == 0))
                                    i += 1
                        av = accv[:, :, r_, u * 128:(u + 1) * 128]
                        pv = po[:, 0:256].rearrange("p (c q) -> p c q", c=2)
                        if g == 0:
                            P.op("dve", lambda e: e.tensor_copy(out=av, in_=pv), [r_po], [r_O], partial=True)
                        else:
                            P.op("dve", lambda e: e.tensor_add(out=av, in0=pv, in1=av), [r_po, r_O], [r_O], partial=True)

                    amode = dbg.get("att_mode", 3) if dbg else 3
                    prev = None
                    for (r_, u) in tiles:
                        cur = (r_, u, stageA(r_, u))
                        if prev is not None and amode >= 2:
                            stageB(*prev)
                        prev = cur
                    if amode >= 2:
                        stageB(*prev)
                P.dma("sp", gs[:], proj_d[36 + j, :, :], [R_proj[36 + j]], [r_gs], partial=False)
                for tb in range(8):
                    sl = slice(tb * 512, (tb + 1) * 512)
                    rz, r_rz = rzs[tb % 2]
                    tm, r_tm = tms[tb % 2]
                    P.op("dve", lambda e: e.reciprocal(out=rz[:], in_=Oacc[:, 1, sl]), [r_O], [r_rz], partial=False)
                    P.op("dve", lambda e: e.tensor_mul(out=tm[:], in0=Oacc[:, 0, sl], in1=rz[:]), [r_O, r_rz], [r_tm], partial=False)
                    P.op("pool", lambda e: e.tensor_mul(out=yb[:, sl], in0=tm[:], in1=gs[:, sl]), [r_tm, r_gs], [r_yb], partial=(tb > 0))
                P.dma("sp", yattn_d[j, :, :], yb[:], [r_yb], [R_yattn[j]], partial=False, sem_of=r_yb)
            P.barrier()
        if stop_after == "att":
            break
        with ExitStack() as ph:
            pb, r_pb = sb("pb", [128, S], BF16, ph)
            ufs = [sb(f"uf{i}", [128, 2048], F32, ph) for i in range(2)]
            ub, r_ub = sb("ub", [128, S], BF16, ph)
            zt, r_zt = sb("zt", [128, S], BF16, ph)
            xg, r_xg = sb("xg", [128, S], BF16, ph)
            CEf, r_CE = sb("CEf", [128, 8192], BF16, ph)
            Xb, r_X = sb("Xb", [128, 2, 32, 128], BF16, ph)
            Tt, r_T = sb("Tt", [128, 32, 2, 128], BF16, ph)
            Hb = [sb(f"Hb{i}", [128, 2, 8, 128], BF16, ph) for i in range(2)]
            tp = [sb(f"tp{i}", [128, 8, 128], F32, ph) for i in range(4)]
            fb1, r_fb1 = sb("fb1", [128, 2, 128], F32, ph)
            fb4, r_fb4 = sb("fb4", [128, 2, 32, 4, 4], F32, ph)
            e1, r_e1 = sb("e1", [128, 32, 4, 4], F32, ph)
            e2, r_e2 = sb("e2", [128, 32, 4, 4], F32, ph)
            gsT, r_gsT = sb("gsT", [128, S], BF16, ph)
            yT, r_yT = sb("yT", [128, S], BF16, ph)
            P.dma("sp", Tt[:], C["T"][:, :, :, :], [R_in], [r_T], partial=False)

            def conv3(chunk, widx, dst, r_dst, fftl):
                P.dma("sp", pb[:], proj_d[chunk, :, :], [R_proj[chunk]], [r_pb], partial=False)
                w0 = convw[:, l, 0, widx:widx + 1]
                w1 = convw[:, l, 1, widx:widx + 1]
                w2 = convw[:, l, 2, widx:widx + 1]
                cb = convb[:, l, widx:widx + 1]
                for hb_ in range(2):
                    uf, r_uf = ufs[hb_]
                    t0 = hb_ * 2048
                    P.op("act", lambda e: e.activation(out=uf[:, :], in_=pb[:, t0:t0 + 2048], func=AF.Identity, scale=w1, bias=cb),
                         [r_pb, r_convw, r_convb], [r_uf], partial=False)
                    a = 1 if hb_ == 0 else 0
                    P.op("dve", lambda e: e.scalar_tensor_tensor(out=uf[:, a:2048], in0=pb[:, t0 + a - 1:t0 + 2047], scalar=w0,
                                                                 in1=uf[:, a:2048], op0=ALU.mult, op1=ALU.add),
                         [r_pb, r_convw, r_uf], [r_uf], partial=False)
                    b_ = 2047 if hb_ == 1 else 2048
                    P.op("dve", lambda e: e.scalar_tensor_tensor(out=ub[:, t0:t0 + b_], in0=pb[:, t0 + 1:t0 + b_ + 1], scalar=w2,
                                                                 in1=uf[:, 0:b_], op0=ALU.mult, op1=ALU.add),
                         [r_pb, r_convw, r_uf], [r_ub], partial=(hb_ > 0))
                    if hb_ == 1:
                        P.op("dve", lambda e: e.tensor_copy(out=ub[:, S - 1:S], in_=uf[:, 2047:2048]), [r_uf], [r_ub], partial=True)
                to_token_major(ub, r_ub, dst, r_dst, fftl)

            def pointwise(o, c):
                for blk in range(4):
                    hb, r_hb = Hb[blk % 2]
                    P.dma("sp", hb[:], H_d[l, o, c, :, :, blk * 8:(blk + 1) * 8, :], [R_H[l][o][c]], [r_hb], partial=False)
                    xr, xi = Xb[:, 0, blk * 8:(blk + 1) * 8, :], Xb[:, 1, blk * 8:(blk + 1) * 8, :]
                    hr, hi = hb[:, 0, :, :], hb[:, 1, :, :]
                    (t1, r1), (t2, r2), (t3, r3), (t4, r4) = tp
                    P.op("dve", lambda e: e.tensor_mul(out=t1[:], in0=xr, in1=hr), [r_X, r_hb], [r1], partial=False)
                    P.op("dve", lambda e: e.tensor_mul(out=t2[:], in0=xi, in1=hi), [r_X, r_hb], [r2], partial=False)
                    P.op("dve", lambda e: e.tensor_mul(out=t3[:], in0=xr, in1=hi), [r_X, r_hb], [r3], partial=False)
                    P.op("dve", lambda e: e.tensor_mul(out=t4[:], in0=xi, in1=hr), [r_X, r_hb], [r4], partial=False)
                    P.op("dve", lambda e: e.tensor_sub(out=xr, in0=t1[:], in1=t2[:]), [r1, r2], [r_X], partial=True)
                    P.op("dve", lambda e: e.tensor_add(out=xi, in0=t3[:], in1=t4[:]), [r3, r4], [r_X], partial=True)

            clist = range(4) if not (dbg and "hy_c" in dbg) else dbg["hy_c"]
            for c in clist:
                for o in range(2):
                    P.dma("sp", fb1[:, o, :], bc(I["f_bias_r"], (l * 2 + o) * 512 + c * 128, 128), [R_in], [r_fb1], partial=(o > 0))
                for o in range(2):
                    for i4 in range(4):
                        P.op("dve", lambda e: e.tensor_copy(out=fb4[:, o, :, i4, :], in_=fb1[:, o, :].rearrange("p (g c) -> p g c", c=4)),
                             [r_fb1], [r_fb4], partial=not (o == 0 and i4 == 0))
                conv3(40 + c, c, zt, r_zt, True)
                conv3(44 + c, 4 + c, xg, r_xg, False)
                hmode = dbg.get("hy_mode", 9) if dbg else 9
                for o in range(2):
                    if hmode < 2:
                        break
                    fft_fwd(zt, r_zt, None, None, CEf, r_CE, Xb, r_X, tp)
                    if hmode < 3:
                        break
                    pointwise(o, c)
                    if hmode < 4:
                        break

                    def epi(nb, pyv, r_py, o=o):
                        zs = zt[:, :].rearrange("p (g n c) -> p g n c", g=32, n=32, c=4)[:, :, nb * 4:(nb + 1) * 4, :]
                        xs = xg[:, :].rearrange("p (n g c) -> p g n c", n=32, g=32, c=4)[:, :, nb * 4:(nb + 1) * 4, :]
                        pyf = pyv.rearrange("p n (g c) -> p g n c", c=4)
                        P.op("dve", lambda e: e.tensor_mul(out=e1[:], in0=zs, in1=fb4[:, o, :, :, :]), [r_zt, r_fb4], [r_e1], partial=False)
                        P.op("dve", lambda e: e.tensor_add(out=e2[:], in0=pyf, in1=e1[:]), [r_py, r_e1], [r_e2], partial=False)
                        if o == 0:
                            P.op("dve", lambda e: e.tensor_mul(out=zs, in0=e2[:], in1=xs), [r_e2, r_xg], [r_zt], partial=True)
                        else:
                            P.op("dve", lambda e: e.tensor_mul(out=xs, in0=e2[:], in1=xs), [r_e2, r_xg], [r_xg], partial=True)

                    fft_inv(Xb, r_X, CEf, r_CE, Tt, r_T, epi)
                    if o == 0:
                        conv3(48 + c, 8 + c, xg, r_xg, False)
                P.dma("sp", gsT[:], proj_d[52 + c, :, :], [R_proj[52 + c]], [r_gsT], partial=False)
                yv = yT[:, :].rearrange("p (a b) -> p b a", b=32)
                gv = gsT[:, :].rearrange("p (a b) -> p b a", b=32)
                for nb in range(4):
                    pt, r_pt = next_psb()
                    for ni in range(8):
                        n1 = nb * 8 + ni
                        P.op("pe", lambda e: e.transpose(out=pt[:, ni * 128:(ni + 1) * 128], in_=xg[:, n1 * 128:(n1 + 1) * 128], identity=ident[:]),
                             [r_xg, r_ident], [r_pt], sig=(ni == 7), partial=(ni > 0))
                    P.op("dve", lambda e: e.tensor_mul(out=yv[:, nb * 8:(nb + 1) * 8, :], in0=pt[:, :].rearrange("p (n c) -> p n c", n=8),
                                                       in1=gv[:, nb * 8:(nb + 1) * 8, :]), [r_pt, r_gsT], [r_yT], partial=(nb > 0))
                P.dma("sp", yhy_d[c, :, :], yT[:], [r_yT], [R_yhy[c]], partial=False, sem_of=r_yT)
            P.barrier()
        if stop_after == "hy":
            break
        with ExitStack() as ph:
            wpa, r_wpa = sb("wpa", [128, 4, D], BF16, ph)
            wph, r_wph = sb("wph", [128, 4, D], BF16, ph)
            wo, r_wo = sb("wo", [128, 8, D], BF16, ph)
            rowb, r_rowb = sb("rowb", [128, 3, D], F32, ph)
            with ExitStack() as ph2:
                wst, r_wst = sb("wstg", [128, 8, D], F32, ph2)
                P.dma("sp", wst[:, 0:4, :], I["w_pa"][l], [R_in], [r_wst], partial=False)
                P.op("pool", lambda e: e.tensor_copy(out=wpa[:], in_=wst[:, 0:4, :]), [r_wst], [r_wpa], partial=False)
                P.dma("sp", wst[:, 4:8, :], I["w_ph"][l], [R_in], [r_wst], partial=False)
                P.op("pool", lambda e: e.tensor_copy(out=wph[:], in_=wst[:, 4:8, :]), [r_wst], [r_wph], partial=False)
                P.dma("sp", wst[:, :, :], I["w_out"][l], [R_in], [r_wst], partial=False)
                for kc in range(8):
                    P.op("pool" if kc % 2 else "dve", lambda e: e.tensor_mul(out=wo[:, kc, :], in0=wst[:, kc, :], in1=gate_b[:, l, :]),
                         [r_wst, r_gate], [r_wo], partial=(kc > 0))
                P.barrier()
            P.dma("sp", rowb[:, 0, :], bc(I["b_out_r"], l * D, D), [R_in], [r_rowb], partial=False)
            P.dma("sp", rowb[:, 1, :], bc(I["ln_g_r"], l * D, D), [R_in], [r_rowb])
            P.dma("sp", rowb[:, 2, :], bc(I["ln_b_r"], l * D, D), [R_in], [r_rowb])
            gb, r_gb = sb("gb", [128, D], F32, ph)
            P.op("dve", lambda e: e.tensor_mul(out=gb[:], in0=rowb[:, 0, :], in1=gate_b[:, l, :]), [r_rowb, r_gate], [r_gb], partial=False)
            nb_, r_nb = sb("nbias", [128, 1], F32, ph)
            ya = [sb(f"ya{i}", [128, 4, 512], BF16, ph) for i in range(2)]
            yh = [sb(f"yh{i}", [128, 4, 512], BF16, ph) for i in range(2)]
            ga = [sb(f"ga{i}", [128, 16, 512], BF16, ph) for i in range(2)]
            mT, r_mT = sb("mT", [128, 8, 512], BF16, ph)
            m1s = [sb(f"m1_{i}", [128, 512], F32, ph) for i in range(2)]
            m2s = [sb(f"m2_{i}", [128, 512], F32, ph) for i in range(2)]
            xr_ = [sb(f"xr{i}", [128, D], F32, ph) for i in range(4)]
            rs_ = [sb(f"rs{i}", [128, D], F32, ph) for i in range(4)]
            sts = [sb(f"mst{i}", [128, 12], F32, ph) for i in range(4)]
            mvs = [sb(f"mmv{i}", [128, 2], F32, ph) for i in range(4)]
            nbs = [sb(f"nbias{i}", [128, 1], F32, ph) for i in range(4)]
            o1, r_o1 = sb("o1", [128, 512], F32, ph)
            stt, r_st = sb("mst", [128, 12], F32, ph)
            mvv, r_mv = sb("mmv", [128, 2], F32, ph)
            xo = [sb(f"xo{i}", [128, D], F32, ph) for i in range(4)]
            tbl = range(8) if not (dbg and "merge_tb" in dbg) else dbg["merge_tb"]
            nt = 0
            for tb in tbl:
                sl = slice(tb * 512, (tb + 1) * 512)
                yat, r_ya = ya[tb % 2]
                yht, r_yh = yh[tb % 2]
                gat, r_ga = ga[tb % 2]
                P.dma("sp", yat[:], yattn_d[:, :, sl].rearrange("c p t -> p c t"), R_yattn, [r_ya], partial=False)
                P.dma("sp", yht[:], yhy_d[:, :, sl].rearrange("c p t -> p c t"), R_yhy, [r_yh], partial=False)
                P.dma("sp", gat[:], proj_d[56:72, :, sl].rearrange("c p t -> p c t"), R_proj[56:72], [r_ga], partial=False)
                for fc in range(8):
                    m1, r_m1 = m1s[fc % 2]
                    m2, r_m2 = m2s[fc % 2]
                    pa, r_pa = next_psf()
                    pq, r_pq = next_psf()
                    for kc in range(4):
                        P.op("pe", lambda e: e.matmul(pa[:, :], lhsT=wpa[:, kc, fc * 128:(fc + 1) * 128], rhs=yat[:, kc, :],
                                                      start=(kc == 0), stop=(kc == 3)), [r_wpa, r_ya], [r_pa], sig=(kc == 3), partial=(kc > 0))
                    for kc in range(4):
                        P.op("pe", lambda e: e.matmul(pq[:, :], lhsT=wph[:, kc, fc * 128:(fc + 1) * 128], rhs=yht[:, kc, :],
                                                      start=(kc == 0), stop=(kc == 3)), [r_wph, r_yh], [r_pq], sig=(kc == 3), partial=(kc > 0))
                    P.op("dve", lambda e: e.tensor_mul(out=m1[:], in0=pa[:, :], in1=gat[:, fc, :]), [r_pa, r_ga], [r_m1], partial=False)
                    P.op("dve", lambda e: e.tensor_mul(out=m2[:], in0=pq[:, :], in1=gat[:, 8 + fc, :]), [r_pq, r_ga], [r_m2], partial=False)
                    P.op("pool", lambda e: e.tensor_add(out=mT[:, fc, :], in0=m1[:], in1=m2[:]), [r_m1, r_m2], [r_mT], partial=(fc > 0))
                tiles = []
                for tt in range(4):
                    row0 = tb * 512 + tt * 128
                    xrt, r_xr = xr_[tt]
                    rst, r_rs = rs_[tt]
                    xot, r_xo = xo[tt]
                    stt, r_st = sts[tt]
                    mvv, r_mv = mvs[tt]
                    nb_, r_nb = nbs[tt]
                    tiles.append((row0, xrt, r_xr, rst, r_rs, xot, r_xo, stt, r_st, mvv, r_mv, nb_, r_nb))
                    P.dma("sp", xrt[:], x_src[row0:row0 + 128, :], [R_xsrc], [r_xr], partial=False)
                for tt, (row0, xrt, r_xr, rst, r_rs, xot, r_xo, stt, r_st, mvv, r_mv, nb_, r_nb) in enumerate(tiles):
                    P.op("dve", lambda e: e.scalar_tensor_tensor(out=xrt[:], in0=xrt[:], scalar=ALPHA, in1=gb[:], op0=ALU.mult, op1=ALU.add),
                         [r_xr, r_gb], [r_xr], partial=False)
                    for hf_ in range(2):
                        hs = slice(hf_ * 512, (hf_ + 1) * 512)
                        po_, r_po = next_psf()
                        for kc in range(8):
                            P.op("pe", lambda e: e.matmul(po_[:, :], lhsT=mT[:, kc, tt * 128:(tt + 1) * 128], rhs=wo[:, kc, hs],
                                                          start=(kc == 0), stop=(kc == 7)), [r_mT, r_wo], [r_po], sig=(kc == 7), partial=(kc > 0))
                        P.op("dve", lambda e: e.tensor_add(out=rst[:, hs], in0=po_[:, :], in1=xrt[:, hs]), [r_po, r_xr], [r_rs], partial=(hf_ > 0))
                    P.op("dve", lambda e: e.bn_stats(out=stt[:, 0:6], in_=rst[:, 0:512]), [r_rs], [r_st], partial=False)
                    P.op("dve", lambda e: e.bn_stats(out=stt[:, 6:12], in_=rst[:, 512:1024]), [r_rs], [r_st], partial=True)
                    P.op("dve", lambda e: e.bn_aggr(out=mvv[:], in_=stt[:]), [r_st], [r_mv], partial=False)
                    P.op("act", lambda e: e.activation(out=mvv[:, 1:2], in_=mvv[:, 1:2], func=AF.Sqrt, bias=epsT[:], scale=1.0),
                         [r_mv, r_eps], [r_mv], partial=False)
                for (row0, xrt, r_xr, rst, r_rs, xot, r_xo, stt, r_st, mvv, r_mv, nb_, r_nb) in tiles:
                    P.op("dve", lambda e: e.reciprocal(out=mvv[:, 1:2], in_=mvv[:, 1:2]), [r_mv], [r_mv], partial=False)
                    P.op("dve", lambda e: e.tensor_scalar(out=nb_[:], in0=mvv[:, 0:1], scalar1=-1.0, scalar2=mvv[:, 1:2],
                                                          op0=ALU.mult, op1=ALU.mult), [r_mv], [r_nb], partial=False)
                    P.op("act", lambda e: e.activation(out=rst[:], in_=rst[:], func=AF.Identity, scale=mvv[:, 1:2], bias=nb_[:]),
                         [r_rs, r_mv, r_nb], [r_rs], partial=False)
                for (row0, xrt, r_xr, rst, r_rs, xot, r_xo, stt, r_st, mvv, r_mv, nb_, r_nb) in tiles:
                    P.op("dve", lambda e: e.tensor_mul(out=rst[:], in0=rst[:], in1=rowb[:, 1, :]), [r_rs, r_rowb], [r_rs], partial=False)
                    P.op("pool", lambda e: e.tensor_add(out=xot[:], in0=rst[:], in1=rowb[:, 2, :]), [r_rs, r_rowb], [r_xo], partial=False)
                    P.dma("sp", x_dst[row0:row0 + 128, :], xot[:], [r_xo], [R_xdst], sem_of=r_xo)
            P.barrier()
        x_src, R_xsrc = x_dst, R_xdst
    P.barrier()
    stack.close()
    return nc, dbg_out


_CACHE = {}


def kernel(**inputs):
    inp = {k: np.asarray(v, dtype=np.float32) for k, v in inputs.items()}
    consts = _const_tables()
    if "nc" not in _CACHE:
        _CACHE["nc"] = build()[0]
    nc = _CACHE["nc"]
    in_maps = []
    for b in range(8):
        m = _layout_inputs(inp, b)
        for k, v in consts.items():
            m["c_" + k] = v
        in_maps.append(m)
    res = run_bass_kernel_spmd(nc, in_maps, core_ids=list(range(8)))
    return np.stack([np.asarray(r["out"], dtype=np.float32) for r in res.results], axis=0)
```

```python
import math
from contextlib import ExitStack
import numpy as np
import ml_dtypes
import concourse.bass as bass
import concourse.mybir as mybir
from concourse.bass_utils import run_bass_kernel_spmd

F32 = mybir.dt.float32
BF16 = mybir.dt.bfloat16
AF = mybir.ActivationFunctionType
ALU = mybir.AluOpType
NPBF = ml_dtypes.bfloat16

S = 4096
D = 1024
NIN = 9216
DEPTH = 2
ALPHA = (2 * DEPTH) ** 0.25
EPS = 1e-5
NFFT = 8192
TWO_PI = 2.0 * math.pi


class Res:
    __slots__ = ("name", "writers", "readers", "prev", "sem", "dcount")

    def __init__(self, name, sem=None):
        self.name = name
        self.writers = []
        self.readers = []
        self.prev = []
        self.sem = sem if sem is not None else {}
        self.dcount = 0


class Prog:
    ENG = ("pe", "act", "dve", "pool", "sp")

    def __init__(self, nc, stack):
        self.nc = nc
        self.stack = stack
        self.e = {"pe": nc.tensor, "act": nc.scalar, "dve": nc.vector, "pool": nc.gpsimd, "sp": nc.sync}
        self.info = []
        self.pending = {k: [] for k in self.ENG}
        self.esem = {}
        self.ecount = {k: 0 for k in self.ENG}
        self.known = {k: {} for k in self.ENG}
        self.nsem = 0
        self.dma_since_barrier = []
        self.last_sig = {k: None for k in self.ENG}

    def new_sem(self, name):
        self.nsem += 1
        return self.stack.enter_context(self.nc.semaphore(f"{name}_{self.nsem}"))

    def _deps(self, reads, writes, partial):
        deps = set()
        for r in reads:
            deps.update(r.writers)
        for w in writes:
            if partial and not w.readers:
                deps.update(w.prev)
            else:
                w.prev = w.writers + w.readers
                deps.update(w.prev)
                w.writers = []
                w.readers = []
        return deps

    def _commit(self, oid, reads, writes):
        for r in reads:
            r.readers.append(oid)
        for w in writes:
            w.writers.append(oid)

    def _waits(self, eng, deps, is_dma):
        need = {}
        for d in deps:
            inf = self.info[d]
            assert inf is not None, "dependency on a non-signalling op"
            deng, sem, val = inf
            if deng == "pe" and eng == "pe" and not is_dma:
                continue
            k = id(sem)
            if k not in need or need[k][1] < val:
                need[k] = (sem, val)
        for k, (sem, val) in need.items():
            if self.known[eng].get(k, 0) >= val:
                continue
            self.e[eng].wait_ge(sem, val)
            self.known[eng][k] = val

    def op(self, eng, fn, reads=(), writes=(), sig=True, partial=False):
        deps = self._deps(reads, writes, partial)
        self._waits(eng, deps, False)
        ins = fn(self.e[eng])
        oid = len(self.info)
        if sig:
            if self.ecount[eng] % 30000 == 0:
                self.esem[eng] = self.new_sem("e" + eng)
                self.ecount[eng] = 0
            self.ecount[eng] += 1
            ins.then_inc(self.esem[eng], 1)
            inf = (eng, self.esem[eng], self.ecount[eng])
            self.info.append(inf)
            for p in self.pending[eng]:
                self.info[p] = inf
            self.pending[eng] = []
            self.last_sig[eng] = oid
        else:
            self.info.append(None)
            self.pending[eng].append(oid)
        self._commit(oid, reads, writes)
        return oid

    def dma(self, eng, out, in_, reads, writes, partial=True, sem_of=None, **kw):
        assert len(writes) == 1
        w = sem_of if sem_of is not None else writes[0]
        deps = self._deps(reads, writes, partial)
        self._waits(eng, deps, True)
        kind = "sw" if eng == "pool" else "hw"
        if kind not in w.sem:
            w.sem[kind] = [self.new_sem("d" + kind), 0]
        w.sem[kind][1] += 1
        self.e[eng].dma_start(out=out, in_=in_, **kw).then_inc(w.sem[kind][0], 16)
        oid = len(self.info)
        self.info.append(("dma", w.sem[kind][0], 16 * w.sem[kind][1]))
        self.dma_since_barrier.append(oid)
        self._commit(oid, reads, writes)
        return oid

    def barrier(self):
        deps = set(self.dma_since_barrier)
        for k in self.ENG:
            assert not self.pending[k], "pending non-signalled ops at barrier"
            if self.last_sig[k] is not None:
                deps.add(self.last_sig[k])
        for k in self.ENG:
            need = {}
            for d in deps:
                deng, sem, val = self.info[d]
                kk = id(sem)
                if kk not in need or need[kk][1] < val:
                    need[kk] = (sem, val)
            for kk, (sem, val) in need.items():
                if self.known[k].get(kk, 0) >= val:
                    continue
                self.e[k].wait_ge(sem, val)
                self.known[k][kk] = val
        self.dma_since_barrier = []


def _const_tables():
    c = {}
    c["ident"] = np.eye(128, dtype=np.float32).astype(NPBF)
    dil = (1, 4, 16)
    p = np.arange(128)[:, None]
    j = np.arange(128)[None, :]
    tab = np.zeros((128, 12, 3, 2, 128), np.float32)
    for g in range(3):
        for h in range(8):
            slope = 2.0 ** (-(h + 1))
            for kt in range(3):
                rel = p + (kt - 1) * 128 - j
                val = -8.0 * slope * dil[g] * np.abs(rel)
                val = np.where(np.abs(rel) <= 64, val, -32768.0)
                tab[:, g * 4 + h // 2, kt, h % 2, :] = val
    c["abias"] = tab.astype(NPBF)
    osel = np.zeros((128, 2, 128), np.float32)
    osel[:, 0, :64] = 1.0
    osel[:, 1, 64:] = 1.0
    c["onesel"] = osel.astype(NPBF)
    n2 = np.arange(128)[:, None].astype(np.float64)
    k2 = np.arange(128)[None, :].astype(np.float64)
    ang = -2.0 * np.pi * n2 * (k2 + 0.5) / 256.0
    Fr, Fi = np.cos(ang), np.sin(ang)
    c["F1"] = np.concatenate([Fr, Fi, -Fi], axis=1).astype(np.float32).astype(NPBF)
    ang2 = -2.0 * np.pi * (n2 + 128.0) * (k2 + 0.5) / 256.0
    Fr2, Fi2 = -np.cos(ang2), -np.sin(ang2)
    c["F1hi"] = np.concatenate([Fr2, Fi2, -Fi2], axis=1).astype(np.float32).astype(NPBF)
    n1 = np.arange(32).astype(np.float64)
    k1 = np.arange(32).astype(np.float64)
    eye4 = np.eye(4)
    a = -2.0 * np.pi * n1[:, None] * k1[None, :] / 32.0
    F32m = np.zeros((128, 3, 128), np.float32)
    F32m[:, 0, :] = np.kron(np.cos(a), eye4)
    F32m[:, 1, :] = np.kron(np.sin(a), eye4)
    F32m[:, 2, :] = -np.kron(np.sin(a), eye4)
    c["F32m"] = F32m.astype(NPBF)
    kk2 = np.arange(128).astype(np.float64)
    a = -2.0 * np.pi * np.repeat(n1, 4)[:, None] * (kk2[None, :] + 0.5) / NFFT
    tw = np.zeros((128, 2, 8, 128), np.float32)
    tw[:, 0, :, :] = np.cos(a)[:, None, :]
    tw[:, 1, :, :] = np.sin(a)[:, None, :]
    c["tw8"] = tw
    a = 2.0 * np.pi * k1[:, None] * n1[None, :] / 32.0
    Rr = np.kron(np.cos(a), eye4)
    Ri = np.kron(np.sin(a), eye4)
    R12 = np.zeros((128, 2, 256), np.float32)
    R12[:, 0, :128], R12[:, 0, 128:] = Rr, Ri
    R12[:, 1, :128], R12[:, 1, 128:] = -Ri, Rr
    c["R12"] = R12.astype(NPBF)
    T = np.zeros((128, 32, 2, 128), np.float32)
    kk = np.arange(128)[:, None].astype(np.float64)
    nn2 = np.arange(128)[None, :].astype(np.float64)
    for a1 in range(32):
        a = 2.0 * np.pi * (a1 + 32.0 * nn2) * (kk + 0.5) / NFFT
        T[:, a1, 0, :] = (2.0 / NFFT) * np.cos(a)
        T[:, a1, 1, :] = -(2.0 / NFFT) * np.sin(a)
    c["T"] = T.astype(NPBF)
    L = S
    t = np.linspace(0.0, 1.0, L, dtype=np.float32)[:, None]
    bands = 16
    w = (2.0 * np.pi * np.arange(L, dtype=np.float32)[:, None] / L).astype(np.float32)
    f = np.linspace(1e-4, bands - 1, bands, dtype=np.float32)[None, :]
    feat = np.concatenate([t, np.cos(f * w), -np.sin(f * w)], axis=-1).astype(np.float32)
    featT = np.ascontiguousarray(feat.T)
    rev = np.zeros_like(featT)
    rev[:, 1:] = featT[:, :0:-1]
    c["featT"] = np.stack([featT, rev], 0)
    trow = np.zeros((2, 1, L), np.float32)
    trow[0, 0] = t[:, 0]
    trow[1, 0, 1:] = t[:0:-1, 0]
    c["trow"] = trow
    deltas = np.linspace(math.log(1e-2) / 1.5, math.log(1e-2) / 0.3, 512, dtype=np.float32)
    c["ndelta"] = np.ascontiguousarray((-np.abs(deltas)).reshape(4, 128).T)
    return c


CONST_DT = {"ident": BF16, "abias": BF16, "onesel": BF16, "F1": BF16, "F1hi": BF16, "F32m": BF16, "tw8": F32,
            "R12": BF16, "T": BF16, "featT": F32, "trow": F32, "ndelta": F32}


def _layout_inputs(inp, b):
    m = {}
    m["x"] = np.ascontiguousarray(inp["x"][b])
    m["crep"] = np.ascontiguousarray(np.broadcast_to(
        inp["c"][b].reshape(8, 128).T[:, :, None], (128, 8, 128))).astype(np.float32)
    m["ccol"] = np.ascontiguousarray(inp["c"][b].reshape(8, 128).T)

    def kt(w, kc):
        Ld, K, N = w.shape
        return np.ascontiguousarray(w.reshape(Ld, kc, 128, N).transpose(0, 2, 1, 3))

    m["w_ada"] = kt(inp["w_ada"], 8)
    m["w_in"] = kt(inp["w_in"], 8)
    m["w_pa"] = kt(inp["w_proj_attn"], 4)
    m["w_ph"] = kt(inp["w_proj_hyena"], 4)
    m["w_out"] = kt(inp["w_out"], 8)

    def col(v):
        Ld, N = v.shape
        return np.ascontiguousarray(v.reshape(Ld, N // 128, 128).transpose(0, 2, 1))

    m["b_ada_c"] = col(inp["b_ada"])
    m["b_ada_r"] = np.ascontiguousarray(inp["b_ada"][:, None, :])
    m["b_in_c"] = col(inp["b_in"])
    m["b_in_r"] = np.ascontiguousarray(inp["b_in"][:, None, :])
    m["conv_w_c"] = np.ascontiguousarray(inp["conv_w"].reshape(DEPTH, 3, 12, 128).transpose(0, 3, 1, 2))
    m["conv_b_c"] = col(inp["conv_b"])
    m["f_w1"] = np.ascontiguousarray(inp["filt_w1"])
    m["f_w2"] = np.ascontiguousarray(inp["filt_w2"])
    m["f_w3"] = np.ascontiguousarray(inp["filt_w3"])
    m["f_w4"] = np.ascontiguousarray(inp["filt_w4"])
    m["f_b"] = np.ascontiguousarray(np.stack([inp["filt_b1"], inp["filt_b2"], inp["filt_b3"], inp["filt_freq"]], -1))
    m["f_bias_r"] = np.ascontiguousarray(inp["filt_bias"])
    m["b_out_r"] = np.ascontiguousarray(inp["b_out"][:, None, :])
    m["ln_g_r"] = np.ascontiguousarray(inp["ln_g"][:, None, :])
    m["ln_b_r"] = np.ascontiguousarray(inp["ln_b"][:, None, :])
    return m


IN_SHAPES = {
    "x": [S, D], "crep": [128, 8, 128], "ccol": [128, 8],
    "w_ada": [DEPTH, 128, 8, 3072], "w_in": [DEPTH, 128, 8, NIN], "w_pa": [DEPTH, 128, 4, D],
    "w_ph": [DEPTH, 128, 4, D], "w_out": [DEPTH, 128, 8, D],
    "b_ada_c": [DEPTH, 128, 24], "b_ada_r": [DEPTH, 1, 3072], "b_in_c": [DEPTH, 128, 72],
    "b_in_r": [DEPTH, 1, NIN], "conv_w_c": [DEPTH, 128, 3, 12], "conv_b_c": [DEPTH, 128, 12],
    "f_w1": [DEPTH, 33, 64], "f_w2": [DEPTH, 64, 64], "f_w3": [DEPTH, 64, 64], "f_w4": [DEPTH, 64, 2048],
    "f_b": [DEPTH, 64, 4], "f_bias_r": [DEPTH, 2, 512], "b_out_r": [DEPTH, 1, D],
    "ln_g_r": [DEPTH, 1, D], "ln_b_r": [DEPTH, 1, D],
}
CONST_SHAPES = {"ident": [128, 128], "abias": [128, 12, 3, 2, 128], "onesel": [128, 2, 128], "F1": [128, 384],
                "F1hi": [128, 384], "F32m": [128, 3, 128], "tw8": [128, 2, 8, 128], "R12": [128, 2, 256], "T": [128, 32, 2, 128],
                "featT": [2, 33, S], "trow": [2, 1, S], "ndelta": [128, 4]}


def bc(ap_t, offset, n):
    return bass.AP(ap_t.tensor, offset, [[0, 128], [1, n]])


class K:
    pass


def build(dbg=None, layers=DEPTH, stop_after=None):
    nc = bass.Bass("TRN2", target_bir_lowering=False)
    try:
        nc.allow_low_precision("bf16 matmul operands with fp32 accumulation")
    except Exception:
        pass
    stack = ExitStack()
    P = Prog(nc, stack)
    I = {k: nc.dram_tensor(k, s, F32, kind="ExternalInput").ap() for k, s in IN_SHAPES.items()}
    C = {k: nc.dram_tensor("c_" + k, s, CONST_DT[k], kind="ExternalInput").ap() for k, s in CONST_SHAPES.items()}
    out = nc.dram_tensor("out", [S, D], F32, kind="ExternalOutput").ap()
    dbg_out = {}

    def dram(name, shape, dt):
        kind = "ExternalOutput" if (dbg and name in dbg) else "Internal"
        t = nc.dram_tensor(name, shape, dt, kind=kind).ap()
        if kind == "ExternalOutput":
            dbg_out[name] = t
        return t

    proj_d = dram("proj_d", [72, 128, S], BF16)
    vtm_d = dram("vtm_d", [3, 4, 128, 32, 128], BF16)
    yattn_d = dram("yattn_d", [4, 128, S], BF16)
    yhy_d = dram("yhy_d", [4, 128, S], BF16)
    xmid_d = dram("xmid_d", [S, D], F32)
    H_d = dram("H_d", [DEPTH, 2, 4, 128, 2, 32, 128], BF16)
    fam = {}
    R_proj = [Res(f"proj{i}", fam) for i in range(72)]
    R_vtm = [Res(f"vtm{g}", fam) for g in range(3)]
    R_yattn = [Res(f"yattn{i}", fam) for i in range(4)]
    R_yhy = [Res(f"yhy{i}", fam) for i in range(4)]
    R_xmid = Res("xmid", fam)
    R_H = [[[Res(f"H{l}{o}{h}", fam) for h in range(4)] for o in range(2)] for l in range(DEPTH)]
    R_out = Res("out")
    R_in = Res("inputs")

    RES = {}
    cnt = {"n": 0}

    def sb(name, shape, dt, st=None):
        cnt["n"] += 1
        t = (st or stack).enter_context(nc.sbuf_tensor(f"s_{name}_{cnt['n']}", shape, dt))
        if name not in RES:
            RES[name] = Res(name)
        return t, RES[name]

    psf = []
    for i in range(6):
        t = stack.enter_context(nc.psum_tensor(f"psf{i}", [128, 512], F32))
        psf.append((t, Res(f"psf{i}")))
    psb = []
    for i in range(2):
        t = stack.enter_context(nc.psum_tensor(f"psb{i}", [128, 1024], BF16))
        psb.append((t, Res(f"psb{i}")))
    rr = {"f": 0, "b": 0, "q": 0}

    def next_psf():
        rr["f"] = (rr["f"] + 1) % 6
        return psf[rr["f"]]

    def next_psb():
        rr["b"] = (rr["b"] + 1) % 2
        return psb[rr["b"]]

    DQ = ("sp", "pool")

    def next_q():
        rr["q"] = (rr["q"] + 1) % len(DQ)
        return DQ[rr["q"]]

    ident, r_ident = sb("ident", [128, 128], BF16)
    onesel, r_onesel = sb("onesel", [128, 2, 128], BF16)
    F1, r_F1 = sb("F1", [128, 384], BF16)
    F1hi, r_F1hi = sb("F1hi", [128, 384], BF16)
    R12, r_R12 = sb("R12", [128, 2, 256], BF16)
    F32m, _ = sb("F32m", [128, 3, 128], BF16)
    tw8, _ = sb("tw8", [128, 2, 8, 128], F32)
    ones2, r_ones2 = sb("ones2", [2, 128], BF16)
    epsT, r_eps = sb("epsT", [128, 1], F32)
    npiT, r_npi = sb("npiT", [128, 1], F32)
    r_cst = Res("cst")
    r_ident = r_onesel = r_F1 = r_F1hi = r_R12 = r_cst
    P.dma("sp", ident[:], C["ident"][:, :], [R_in], [r_ident])
    P.dma("sp", onesel[:], C["onesel"][:, :, :], [R_in], [r_onesel])
    P.dma("sp", F1[:], C["F1"][:, :], [R_in], [r_F1])
    P.dma("sp", F1hi[:], C["F1hi"][:, :], [R_in], [r_F1hi])
    P.dma("sp", R12[:], C["R12"][:, :, :], [R_in], [r_R12])
    P.dma("sp", F32m[:], C["F32m"][:, :, :], [R_in], [r_cst])
    P.dma("sp", tw8[:], C["tw8"][:, :, :, :], [R_in], [r_cst])
    P.op("pool", lambda e: e.memset(ones2[:], 1.0), [], [r_ones2])
    P.op("pool", lambda e: e.memset(epsT[:], EPS), [], [r_eps])
    P.op("pool", lambda e: e.memset(npiT[:], -math.pi), [], [r_npi])

    modcol, r_modcol = sb("modcol", [128, DEPTH, 24], F32)
    sc1, r_sc1 = sb("sc1", [128, DEPTH, 8], F32)
    gate_b, r_gate = sb("gate_b", [128, DEPTH, D], F32)
    b_in_c, r_binc = sb("b_in_c", [128, DEPTH, 72], F32)
    convw, r_convw = sb("convw", [128, DEPTH, 3, 12], F32)
    convb, r_convb = sb("convb", [128, DEPTH, 12], F32)
    bin2, r_bin2 = sb("bin2", [1, DEPTH, 1536], BF16)
    binl, r_binl = sb("binl", [1, DEPTH, 1536], BF16)
    r_binc = r_convw = r_convb = r_cst
    for l in range(DEPTH):
        P.dma("sp", b_in_c[:, l, :], I["b_in_c"][l], [R_in], [r_binc])
        P.dma("sp", convw[:, l, :, :], I["conv_w_c"][l], [R_in], [r_convw])
        P.dma("sp", convb[:, l, :], I["conv_b_c"][l], [R_in], [r_convb])

    with ExitStack() as ph:
        ccol, r_ccol = sb("ccol", [128, 8], F32, ph)
        crep, r_crep = sb("crep", [128, 8, 128], F32, ph)
        badac, r_badac = sb("badac", [128, DEPTH, 24], F32, ph)
        badar, r_badar = sb("badar", [128, DEPTH, D], F32, ph)
        vb, r_vb = sb("vb", [1, DEPTH, 1536], F32, ph)
        vbh, r_vbh = sb("vbh", [1, DEPTH, 1536], F32, ph)
        r_ccol = r_crep = r_badac = r_badar = r_vb = r_cst
        P.dma("sp", ccol[:], I["ccol"][:, :], [R_in], [r_ccol])
        P.dma("sp", crep[:], I["crep"][:, :, :], [R_in], [r_crep])
        for l in range(DEPTH):
            P.dma("sp", badac[:, l, :], I["b_ada_c"][l], [R_in], [r_badac])
            P.dma("sp", badar[:, l, :], bc(I["b_ada_r"], l * 3072 + 2048, D), [R_in], [r_badar])
            P.dma("sp", vb[:, l, :], bass.AP(I["b_in_r"].tensor, l * NIN + 3072, [[0, 1], [1, 1536]]), [R_in], [r_vb])
        P.op("dve", lambda e: e.tensor_copy(out=bin2[:], in_=vb[:]), [r_vb], [r_bin2])
        P.op("dve", lambda e: e.tensor_copy(out=vbh[:], in_=bin2[:]), [r_bin2], [r_vbh])
        P.op("dve", lambda e: e.tensor_sub(out=vbh[:], in0=vb[:], in1=vbh[:]), [r_vb, r_vbh], [r_vbh])
        P.op("dve", lambda e: e.tensor_copy(out=binl[:], in_=vbh[:]), [r_vbh], [r_binl])
        wa = [sb(f"wa{i}", [128, 8, 512], F32, ph) for i in range(2)]
        for l in range(DEPTH):
            pc, r_pc = next_psf()
            for blk in range(6):
                wt, r_wt = wa[blk % 2]
                P.dma("sp", wt[:, 0:4, :], I["w_ada"][l, :, 0:4, blk * 512:(blk + 1) * 512], [R_in], [r_wt], partial=False)
                P.dma("pool", wt[:, 4:8, :], I["w_ada"][l, :, 4:8, blk * 512:(blk + 1) * 512], [R_in], [r_wt])
                for f in range(4):
                    fi = blk * 4 + f
                    for kc in range(8):
                        P.op("pe", lambda e, wt=wt, f=f, kc=kc, fi=fi: e.matmul(
                            pc[:, fi:fi + 1], lhsT=wt[:, kc, f * 128:(f + 1) * 128], rhs=ccol[:, kc:kc + 1],
                            start=(kc == 0), stop=(kc == 7)),
                            [r_wt, r_ccol], [r_pc], sig=(kc == 7), partial=True)
                if blk >= 4:
                    pg, r_pg = next_psf()
                    for kc in range(8):
                        P.op("pe", lambda e, wt=wt, kc=kc: e.matmul(
                            pg[:, :], lhsT=crep[:, kc, :], rhs=wt[:, kc, :], start=(kc == 0), stop=(kc == 7)),
                            [r_wt, r_crep], [r_pg], sig=(kc == 7), partial=True)
                    h0 = (blk - 4) * 512
                    P.op("dve", lambda e, l=l, h0=h0, pg=pg: e.tensor_add(
                        out=gate_b[:, l, h0:h0 + 512], in0=pg[:, :], in1=badar[:, l, h0:h0 + 512]),
                        [r_pg, r_badar], [r_gate], partial=True)
            P.op("dve", lambda e, l=l, pc=pc: e.tensor_add(out=modcol[:, l, :], in0=pc[:, 0:24], in1=badac[:, l, :]),
                 [r_pc, r_badac], [r_modcol], partial=True)
            P.op("dve", lambda e, l=l: e.tensor_scalar_add(out=sc1[:, l, :], in0=modcol[:, l, 8:16], scalar1=1.0),
                 [r_modcol], [r_sc1], partial=True)
        P.barrier()


    act_dve = {"n": 0}

    def evac(out_ap, in_ap, reads, writes, partial=True, simple=False):
        act_dve["n"] += 1
        if simple:
            P.op("act", lambda e: e.copy(out=out_ap, in_=in_ap), reads, writes, partial=partial)
        else:
            P.op("dve", lambda e: e.tensor_copy(out=out_ap, in_=in_ap), reads, writes, partial=partial)

    def fft_fwd(zl, r_zl, zh, r_zh, CEf, r_CE, X, r_X, tpb):
        Cv = CEf[:, 0:8192].rearrange("p (t c k) -> p t c k", t=2, c=32, k=128)
        for cp in range(16):
            pp, r_pp = next_psf()
            for gi in range(2):
                cg = cp * 2 + gi
                P.op("pe", lambda e: e.matmul(pp[:, gi * 256:(gi + 1) * 256], lhsT=zl[:, cg * 128:(cg + 1) * 128], rhs=F1[:, 0:256],
                                              start=True, stop=(zh is None)),
                     [r_zl, r_F1], [r_pp], sig=(zh is None and gi == 1), partial=(gi > 0))
                if zh is not None:
                    P.op("pe", lambda e: e.matmul(pp[:, gi * 256:(gi + 1) * 256], lhsT=zh[:, cg * 128:(cg + 1) * 128], rhs=F1hi[:, 0:256],
                                                  start=False, stop=True),
                         [r_zh, r_F1hi], [r_pp], sig=(gi == 1), partial=True)
            evac(Cv[:, :, cp * 2:cp * 2 + 2, :], pp[:, :].rearrange("p (g t k) -> p t g k", g=2, t=2, k=128), [r_pp], [r_CE],
                 partial=(cp > 0), simple=False)
        fmode = dbg.get("fft_mode", 9) if dbg else 9
        if fmode < 2:
            return
        (t1, r1), (t2, r2), (t3, r3), (t4, r4) = tpb
        bs = t1.shape[1]
        for blk in range(32 // bs):
            cs = slice(blk * bs, blk * bs + bs)
            cr, ci = Cv[:, 0, cs, :], Cv[:, 1, cs, :]
            P.op("dve", lambda e: e.tensor_mul(out=t1[:], in0=cr, in1=tw8[:, 0, 0:bs, :]), [r_CE, r_cst], [r1], partial=False)
            P.op("dve", lambda e: e.tensor_mul(out=t2[:], in0=ci, in1=tw8[:, 1, 0:bs, :]), [r_CE, r_cst], [r2], partial=False)
            P.op("dve", lambda e: e.tensor_mul(out=t3[:], in0=cr, in1=tw8[:, 1, 0:bs, :]), [r_CE, r_cst], [r3], partial=False)
            P.op("dve", lambda e: e.tensor_mul(out=t4[:], in0=ci, in1=tw8[:, 0, 0:bs, :]), [r_CE, r_cst], [r4], partial=False)
            P.op("dve", lambda e: e.tensor_sub(out=cr, in0=t1[:], in1=t2[:]), [r1, r2], [r_CE], partial=True)
            P.op("dve", lambda e: e.tensor_add(out=ci, in0=t3[:], in1=t4[:]), [r3, r4], [r_CE], partial=True)
            for hf_ in range(bs // 4 if fmode >= 3 else 0):
                c0 = (blk * bs + hf_ * 4) * 128
                rre = CEf[:, c0:c0 + 512]
                rim = CEf[:, 4096 + c0:4096 + c0 + 512]
                for t, (la, lb) in enumerate(((0, 2), (1, 0))):
                    px, r_px = next_psf()
                    P.op("pe", lambda e: e.matmul(px[:, :], lhsT=F32m[:, la, :], rhs=rre, start=True, stop=False),
                         [r_cst, r_CE], [r_px], sig=False, partial=False)
                    P.op("pe", lambda e: e.matmul(px[:, :], lhsT=F32m[:, lb, :], rhs=rim, start=False, stop=True),
                         [r_cst, r_CE], [r_px], sig=True, partial=True)
                    x0 = (t * 32 + blk * bs + hf_ * 4) * 128
                    evac(X[:].rearrange("p t c k -> p (t c k)")[:, x0:x0 + 512], px[:, :],
                         [r_px], [r_X], partial=not (blk == 0 and hf_ == 0 and t == 0), simple=True)

    def fft_inv(Y, r_Y, CEf, r_CE, Tt, r_T, epilogue):
        E5 = CEf[:, 0:32 * 2 * 128].rearrange("p (n t g c) -> p n t g c", n=32, t=2, g=32, c=4)
        E4 = CEf[:, 0:32 * 2 * 128].rearrange("p (n t c) -> p n t c", n=32, t=2, c=128)
        for cp in range(16):
            pe_, r_pe = next_psf()
            for gi in range(2):
                cg = cp * 2 + gi
                P.op("pe", lambda e: e.matmul(pe_[:, gi * 256:(gi + 1) * 256], lhsT=Y[:, 0, cg, :], rhs=R12[:, 0, :], start=True, stop=False),
                     [r_Y, r_R12], [r_pe], sig=False, partial=(gi > 0))
                P.op("pe", lambda e: e.matmul(pe_[:, gi * 256:(gi + 1) * 256], lhsT=Y[:, 1, cg, :], rhs=R12[:, 1, :], start=False, stop=True),
                     [r_Y, r_R12], [r_pe], sig=(gi == 1), partial=True)
            pv = pe_[:, :].rearrange("p (g t n c) -> p t n g c", g=2, t=2, n=32, c=4)
            for t in range(2):
                evac(E5[:, :, t, cp * 2:cp * 2 + 2, :], pv[:, t], [r_pe], [r_CE], partial=not (cp == 0 and t == 0))
        for nb in range(8):
            py, r_py = next_psf()
            for ni in range(4):
                n1 = nb * 4 + ni
                P.op("pe", lambda e: e.matmul(py[:, ni * 128:(ni + 1) * 128], lhsT=Tt[:, n1, 0, :], rhs=E4[:, n1, 0, :], start=True, stop=False),
                     [r_T, r_CE], [r_py], sig=False, partial=(ni > 0))
                P.op("pe", lambda e: e.matmul(py[:, ni * 128:(ni + 1) * 128], lhsT=Tt[:, n1, 1, :], rhs=E4[:, n1, 1, :], start=False, stop=True),
                     [r_T, r_CE], [r_py], sig=(ni == 3), partial=True)
            epilogue(nb, py[:, :].rearrange("p (n c) -> p n c", n=4), r_py)

    def to_token_major(srcT, r_src, dst, r_dst, fftl=True):
        sv = srcT[:, :].rearrange("p (a b) -> p b a", b=32)
        for nb in range(4):
            pt, r_pt = next_psb()
            for ni in range(8):
                n1 = nb * 8 + ni
                P.op("pe", lambda e: e.transpose(out=pt[:, ni * 128:(ni + 1) * 128], in_=sv[:, n1, :], identity=ident[:]),
                     [r_src, r_ident], [r_pt], sig=(ni == 7), partial=(ni > 0))
            if fftl:
                evac(dst[:, :].rearrange("p (g n c) -> p g n c", g=32, n=32, c=4)[:, :, nb * 8:(nb + 1) * 8, :],
                     pt[:, :].rearrange("p (n g c) -> p g n c", n=8, g=32, c=4), [r_pt], [r_dst], partial=(nb > 0))
            else:
                evac(dst[:, :].rearrange("p (n c) -> p n c", n=32)[:, nb * 8:(nb + 1) * 8, :],
                     pt[:, :].rearrange("p (n c) -> p n c", n=8), [r_pt], [r_dst], partial=(nb > 0))

    if not (dbg and dbg.get("skip_filters")):
        with ExitStack() as ph:
            h3 = [[sb(f"h3_{l}{v}", [64, S], BF16, ph) for v in range(2)] for l in range(DEPTH)]
            w4b = [sb(f"w4b{l}", [64, 2048], BF16, ph) for l in range(DEPTH)]
            with ExitStack() as ph2:
                feat = [sb(f"feat{v}", [33, S], F32, ph2) for v in range(2)]
                hA, r_hA = sb("hA", [64, S], F32, ph2)
                hB, r_hB = sb("hB", [64, S], F32, ph2)
                w4f, r_w4f = sb("w4f", [64, 2048], F32, ph2)
                fw = [sb(f"fw{i}", [64, 64], F32, ph2) for i in range(3)]
                fbt, r_fbt = sb("fbt", [64, 4], F32, ph2)
                fsc, r_fsc = sb("fsc", [64, 4], F32, ph2)
                ty = [sb(f"ty{i}", [64, 512], F32, ph2) for i in range(2)]
                tki, r_tki = sb("tki", [64, 512], mybir.dt.int32, ph2)
                tkf, r_tkf = sb("tkf", [64, 512], F32, ph2)
                for v in range(2):
                    P.dma("sp", feat[v][0][:], C["featT"][v], [R_in], [feat[v][1]], partial=False)
                for l in range(DEPTH):
                    P.dma("sp", fw[0][0][0:33, :], I["f_w1"][l], [R_in], [fw[0][1]], partial=False)
                    P.dma("sp", fw[1][0][:], I["f_w2"][l], [R_in], [fw[1][1]], partial=False)
                    P.dma("sp", fw[2][0][:], I["f_w3"][l], [R_in], [fw[2][1]], partial=False)
                    P.dma("sp", w4f[:], I["f_w4"][l], [R_in], [r_w4f], partial=False)
                    P.dma("sp", fbt[:], I["f_b"][l], [R_in], [r_fbt], partial=False)
                    P.op("pool", lambda e: e.tensor_copy(out=w4b[l][0][:], in_=w4f[:]), [r_w4f], [w4b[l][1]], partial=False)
                    P.op("dve", lambda e: e.tensor_scalar_mul(out=fsc[:, 3:4], in0=fbt[:, 3:4], scalar1=1.0 / TWO_PI), [r_fbt], [r_fsc], partial=False)
                    P.op("dve", lambda e: e.tensor_scalar(out=fsc[:, 0:3], in0=fbt[:, 0:3], scalar1=fsc[:, 3:4], scalar2=8.0,
                                                          op0=ALU.mult, op1=ALU.add), [r_fbt, r_fsc], [r_fsc], partial=False)
                    for v in range(2):
                        src, r_src = feat[v]
                        kdim = 33
                        for layer_i in range(3):
                            last = (layer_i == 2)
                            dst, r_dst = (h3[l][v] if last else ((hA, r_hA) if layer_i == 0 else (hB, r_hB)))
                            wt_, r_wt_ = fw[layer_i]
                            for tb in range(8):
                                sl = slice(tb * 512, (tb + 1) * 512)
                                pp, r_pp = next_psf()
                                P.op("pe", lambda e: e.matmul(pp[0:64, :], lhsT=wt_[0:kdim, :], rhs=src[0:kdim, sl], start=True, stop=True),
                                     [r_wt_, r_src], [r_pp], partial=False)
                                tyt, r_ty = ty[tb % 2]
                                P.op("dve", lambda e: e.tensor_scalar(out=tyt[:], in0=pp[0:64, :], scalar1=fsc[:, 3:4],
                                                                      scalar2=fsc[:, layer_i:layer_i + 1], op0=ALU.mult, op1=ALU.add),
                                     [r_pp, r_fsc], [r_ty], partial=False)
                                P.op("dve", lambda e: e.tensor_copy(out=tki[:], in_=tyt[:]), [r_ty], [r_tki], partial=False)
                                P.op("dve", lambda e: e.tensor_copy(out=tkf[:], in_=tki[:]), [r_tki], [r_tkf], partial=False)
                                P.op("dve", lambda e: e.tensor_sub(out=tyt[:], in0=tyt[:], in1=tkf[:]), [r_ty, r_tkf], [r_ty], partial=False)
                                P.op("dve", lambda e: e.tensor_single_scalar(out=tkf[:], in_=tyt[:], scalar=0.5, op=ALU.is_ge),
                                     [r_ty], [r_tkf], partial=False)
                                P.op("dve", lambda e: e.tensor_sub(out=tyt[:], in0=tyt[:], in1=tkf[:]), [r_ty, r_tkf], [r_ty], partial=False)
                                P.op("act", lambda e: e.activation(out=dst[:, sl], in_=tyt[:], func=AF.Sin, scale=TWO_PI),
                                     [r_ty], [r_dst], partial=(tb > 0))
                            src, r_src = dst, r_dst
                            kdim = 64
                P.barrier()
            trb = [sb(f"trb{v}", [128, S], F32, ph) for v in range(2)]
            dec = [sb(f"dec{v}", [128, S], F32, ph) for v in range(2)]
            ndl, r_ndl = sb("ndl", [128, 4], F32, ph)
            fT, r_fT = sb("fT", [128, S], BF16, ph)
            ftm = [sb(f"ftm{v}", [128, S], BF16, ph) for v in range(2)]
            CEf, r_CE = sb("CEf", [128, 8192], BF16, ph)
            Xb, r_X = sb("Xb", [128, 2, 32, 128], BF16, ph)
            tpf = [sb(f"tpf{i}", [128, 4, 128], F32, ph) for i in range(4)]
            P.dma("sp", ndl[:], C["ndelta"][:, :], [R_in], [r_ndl], partial=False)
            for v in range(2):
                P.dma("sp", trb[v][0][:], bc(C["trow"], v * S, S), [R_in], [trb[v][1]], partial=False)
            flist = [(c, l, o) for c in range(4) for l in range(DEPTH) for o in range(2)]
            if dbg and "filt_list" in dbg:
                flist = dbg["filt_list"]
            lastc = None
            for (c, l, o) in flist:
                if c != lastc:
                    for v in range(2):
                        P.op("act", lambda e: e.activation(out=dec[v][0][:], in_=trb[v][0][:], func=AF.Exp, scale=ndl[:, c:c + 1]),
                             [trb[v][1], r_ndl], [dec[v][1]], partial=False)
                    lastc = c
                for v in range(2):
                    col0 = (o * 2 + v) * 512 + c * 128
                    for tb in range(8):
                        sl = slice(tb * 512, (tb + 1) * 512)
                        pp, r_pp = next_psf()
                        P.op("pe", lambda e: e.matmul(pp[:, :], lhsT=w4b[l][0][:, col0:col0 + 128], rhs=h3[l][v][0][:, sl], start=True, stop=True),
                             [w4b[l][1], h3[l][v][1]], [r_pp], partial=False)
                        P.op("dve", lambda e: e.tensor_mul(out=fT[:, sl], in0=pp[:, :], in1=dec[v][0][:, sl]),
                             [r_pp, dec[v][1]], [r_fT], partial=(tb > 0))
                    if v == 1:
                        P.op("dve", lambda e: e.memset(fT[:, 0:1], 0.0), [], [r_fT], partial=True)
                    to_token_major(fT, r_fT, ftm[v][0], ftm[v][1])
                fft_fwd(ftm[0][0], ftm[0][1], ftm[1][0], ftm[1][1], CEf, r_CE, Xb, r_X, tpf)
                P.dma("sp", H_d[l, o, c], Xb[:], [r_X], [R_H[l][o][c]], partial=False, sem_of=r_X)
            P.barrier()
    if stop_after == "filt":
        layers = 0

    x_src = I["x"]
    R_xsrc = R_in
    for l in range(layers):
        x_dst, R_xdst = (xmid_d, R_xmid) if l < DEPTH - 1 else (out, R_out)
        lay = ExitStack()
        hT, r_hT = sb(f"hT", [128, 8, S], BF16, lay)
        with ExitStack() as ph:
            xt = [sb(f"xt{i}", [128, D], F32, ph) for i in range(4)]
            xn = [sb(f"xn{i}", [128, D], BF16, ph) for i in range(4)]
            st = [sb(f"st{i}", [128, 12], F32, ph) for i in range(4)]
            mv = [sb(f"mv{i}", [128, 2], F32, ph) for i in range(4)]
            for tg in range(8):
                grp = []
                for ti in range(4):
                    t = tg * 4 + ti
                    xtt, r_xt = xt[ti]
                    xnn, r_xn = xn[ti]
                    stt, r_st = st[ti]
                    mvv, r_mv = mv[ti]
                    grp.append((t, xtt, r_xt, xnn, r_xn, stt, r_st, mvv, r_mv))
                    P.dma(next_q(), xtt[:], x_src[t * 128:(t + 1) * 128, :], [R_xsrc], [r_xt], partial=False)
                for (t, xtt, r_xt, xnn, r_xn, stt, r_st, mvv, r_mv) in grp:
                    P.op("dve", lambda e: e.bn_stats(out=stt[:, 0:6], in_=xtt[:, 0:512]), [r_xt], [r_st], partial=False)
                    P.op("dve", lambda e: e.bn_stats(out=stt[:, 6:12], in_=xtt[:, 512:1024]), [r_xt], [r_st], partial=True)
                    P.op("dve", lambda e: e.bn_aggr(out=mvv[:], in_=stt[:]), [r_st], [r_mv], partial=False)
                    P.op("act", lambda e: e.activation(out=mvv[:, 1:2], in_=mvv[:, 1:2], func=AF.Sqrt, bias=epsT[:], scale=1.0),
                         [r_mv, r_eps], [r_mv], partial=False)
                for (t, xtt, r_xt, xnn, r_xn, stt, r_st, mvv, r_mv) in grp:
                    P.op("dve", lambda e: e.reciprocal(out=mvv[:, 1:2], in_=mvv[:, 1:2]), [r_mv], [r_mv], partial=False)
                    P.op("dve", lambda e: e.tensor_scalar(out=xnn[:], in0=xtt[:], scalar1=mvv[:, 0:1], scalar2=mvv[:, 1:2],
                                                          op0=ALU.subtract, op1=ALU.mult), [r_xt, r_mv], [r_xn], partial=False)
                for (t, xtt, r_xt, xnn, r_xn, stt, r_st, mvv, r_mv) in grp:
                    pt, r_pt = next_psb()
                    for kc in range(8):
                        P.op("pe", lambda e, kc=kc: e.transpose(out=pt[:, kc * 128:(kc + 1) * 128], in_=xnn[:, kc * 128:(kc + 1) * 128],
                                                                identity=ident[:]),
                             [r_xn, r_ident], [r_pt], sig=(kc == 7), partial=(kc > 0))
                    for kc in range(8):
                        P.op("act", lambda e, kc=kc: e.activation(out=hT[:, kc, t * 128:(t + 1) * 128], in_=pt[:, kc * 128:(kc + 1) * 128],
                                                                  func=AF.Identity, scale=sc1[:, l, kc:kc + 1], bias=modcol[:, l, kc:kc + 1]),
                             [r_pt, r_sc1, r_modcol], [r_hT], partial=True)
            P.barrier()
        if stop_after == "ln":
            lay.close()
            break

        with ExitStack() as ph:
            ws = [sb(f"ws{i}", [128, 8, 128], F32, ph) for i in range(3)]
            wb = [sb(f"wb{i}", [128, 8, 128], BF16, ph) for i in range(3)]
            ob = [sb(f"ob{i}", [128, S], BF16, ph) for i in range(2)]
            nfm = 0
            chunks = [j for j in range(72) if not (24 <= j < 36)]
            if dbg and "proj_chunks" in dbg:
                chunks = dbg["proj_chunks"]
            for j in chunks:
                wst, r_ws = ws[nfm % 3]
                wbt, r_wb = wb[nfm % 3]
                obt, r_ob = ob[nfm % 2]
                nfm += 1
                P.dma(next_q(), wst[:], I["w_in"][l, :, :, j * 128:(j + 1) * 128], [R_in], [r_ws], partial=False)
                P.op("pool", lambda e: e.tensor_copy(out=wbt[:], in_=wst[:]), [r_ws], [r_wb], partial=False)
                if 36 <= j < 40 or 52 <= j < 56:
                    fn = AF.Silu
                elif j >= 56:
                    fn = AF.Sigmoid
                else:
                    fn = AF.Identity
                for tb in range(8):
                    pp, r_pp = next_psf()
                    for kc in range(8):
                        P.op("pe", lambda e, kc=kc: e.matmul(pp[:, :], lhsT=wbt[:, kc, :], rhs=hT[:, kc, tb * 512:(tb + 1) * 512],
                                                             start=(kc == 0), stop=(kc == 7)),
                             [r_wb, r_hT], [r_pp], sig=(kc == 7), partial=(kc > 0))
                    P.op("act", lambda e: e.activation(out=obt[:, tb * 512:(tb + 1) * 512], in_=pp[:, :], func=fn,
                                                       bias=b_in_c[:, l, j:j + 1], scale=1.0),
                         [r_pp, r_binc], [r_ob], partial=(tb > 0))
                P.dma(next_q(), proj_d[j, :, :], obt[:], [r_ob], [R_proj[j]], partial=False, sem_of=r_ob)
            wvs = [sb(f"wvs{i}", [128, 8, 512], F32, ph) for i in range(1)]
            wvb = [sb(f"wvb{i}", [128, 8, 512], BF16, ph) for i in range(2)]
            vo = [sb(f"vo{i}", [128, 4, 512], BF16, ph) for i in range(2)]
            nv = 0
            groups = range(3) if not (dbg and "v_groups" in dbg) else dbg["v_groups"]
            for g in groups:
                d = (1, 4, 16)[g]
                Lg = S // d
                wst, r_ws = wvs[0]
                wbt, r_wb = wvb[g % 2]
                c0 = 3072 + g * 512
                P.dma("sp", wst[:, 0:4, :], I["w_in"][l, :, 0:4, c0:c0 + 512], [R_in], [r_ws], partial=False)
                P.dma("pool", wst[:, 4:8, :], I["w_in"][l, :, 4:8, c0:c0 + 512], [R_in], [r_ws])
                P.op("pool", lambda e: e.tensor_copy(out=wbt[:], in_=wst[:]), [r_ws], [r_wb], partial=False)
                for tq in range(8):
                    vot, r_vo = vo[nv % 2]
                    nv += 1
                    for t4 in range(4):
                        ti = tq * 4 + t4
                        r_, u = divmod(ti, Lg // 128)
                        pp, r_pp = next_psf()
                        for kc in range(8):
                            lt = hT[:, kc, :].rearrange("p (i d) -> p d i", d=d)[:, r_, u * 128:(u + 1) * 128]
                            P.op("pe", lambda e, kc=kc, lt=lt: e.matmul(pp[:, :], lhsT=lt, rhs=wbt[:, kc, :], start=(kc == 0), stop=False),
                                 [r_wb, r_hT], [r_pp], sig=False, partial=(kc > 0))
                        P.op("pe", lambda e: e.matmul(pp[:, :], lhsT=ones2[0:1, :], rhs=bin2[0:1, l, g * 512:(g + 1) * 512], start=False, stop=False),
                             [r_ones2, r_bin2], [r_pp], sig=False, partial=True)
                        P.op("pe", lambda e: e.matmul(pp[:, :], lhsT=ones2[0:1, :], rhs=binl[0:1, l, g * 512:(g + 1) * 512], start=False, stop=True),
                             [r_ones2, r_binl], [r_pp], sig=True, partial=True)
                        P.op("dve", lambda e, t4=t4: e.tensor_copy(out=vot[:, t4, :], in_=pp[:, :]), [r_pp], [r_vo], partial=(t4 > 0))
                    qn = next_q()
                    for j4 in range(4):
                        P.dma(qn, vtm_d[g, j4, :, tq * 4:(tq + 1) * 4, :], vot[:, :, j4 * 128:(j4 + 1) * 128], [r_vo], [R_vtm[g]], sem_of=r_vo)
            P.barrier()
        if stop_after == "proj":
            lay.close()
            break
        lay.close()
        with ExitStack() as ph:
            Oacc, r_O = sb(f"Oacc", [128, 2, S], F32, ph)
            qTs = [sb(f"qT{i}", [128, 2, S], BF16, ph) for i in range(2)]
            kTs = [sb(f"kT{i}", [128, S], BF16, ph) for i in range(2)]
            vts = [sb(f"vt{i}", [128, 32, 2, 128], BF16, ph) for i in range(2)]
            abs_ = [sb(f"ab{i}", [128, 3, 256], BF16, ph) for i in range(2)]
            pTs = [sb(f"pT{i}", [128, 256], BF16, ph) for i in range(8)]
            gs, r_gs = sb(f"gs", [128, S], BF16, ph)
            vs, r_vs = sb(f"vs", [128, 32, 128], BF16, ph)
            yb, r_yb = sb(f"yb", [128, S], BF16, ph)
            rzs = [sb(f"rz{i}", [128, 512], F32, ph) for i in range(2)]
            tms = [sb(f"tm{i}", [128, 512], F32, ph) for i in range(2)]
            for i in range(2):
                P.op("pool", lambda e, i=i: e.memset(vts[i][0][:], 0.0), [], [vts[i][1]], partial=False)
                P.op("pool", lambda e, i=i: e.memset(qTs[i][0][:], 0.0), [], [qTs[i][1]], partial=False)
            npT = 0
            nbuf = 0
            jlist = range(4) if not (dbg and "att_j" in dbg) else dbg["att_j"]
            for j in jlist:
                for g in (range(3) if not (dbg and "att_g" in dbg) else dbg["att_g"]):
                    d = (1, 4, 16)[g]
                    ntl = (S // d) // 128
                    qT, r_q = qTs[nbuf % 2]
                    kT, r_k = kTs[nbuf % 2]
                    vt, r_v = vts[nbuf % 2]
                    ab, r_ab = abs_[nbuf % 2]
                    nbuf += 1
                    P.dma("sp", qT[0:64, 0, :], proj_d[g * 4 + j, 0:64, :], [R_proj[g * 4 + j]], [r_q], partial=False)
                    P.dma("sp", qT[64:128, 1, :], proj_d[g * 4 + j, 64:128, :], [R_proj[g * 4 + j]], [r_q])
                    P.dma("sp", kT[:], proj_d[12 + g * 4 + j, :, :], [R_proj[12 + g * 4 + j]], [r_k], partial=False)
                    P.dma("sp", vs[:], vtm_d[g, j, :, :, :], [R_vtm[g]], [r_vs], partial=False)
                    P.op("pool", lambda e: e.tensor_copy(out=vt[:, :, 0, 0:64], in_=vs[:, :, 0:64]), [r_vs], [r_v], partial=False)
                    P.op("pool", lambda e: e.tensor_copy(out=vt[:, :, 1, 64:128], in_=vs[:, :, 64:128]), [r_vs], [r_v], partial=True)
                    P.dma("sp", ab[:], C["abias"][:, g * 4 + j, :, :, :].rearrange("p k h q -> p k (h q)"), [R_in], [r_ab], partial=False)
                    qv = [qT[:, hh, :].rearrange("p (i d) -> p d i", d=d) for hh in range(2)]
                    kv = [kT[:, :].rearrange("p (i d) -> p d i", d=d) for hh in range(2)]
                    accv = Oacc[:].rearrange("p c (i d) -> p c d i", d=d)
                    tiles = [(r_, u) for r_ in range(d) for u in range(ntl)]

                    def stageA(r_, u):
                        kts = [kt for kt in range(3) if 0 <= u + kt - 1 < ntl]
                        outl = []
                        for kt in kts:
                            ku = u + kt - 1
                            rr["sc"] = (rr.get("sc", 0) + 1) % 4
                            ps, r_ps = psf[rr["sc"]]
                            for hh in range(2):
                                P.op("pe", lambda e, hh=hh: e.matmul(ps[:, hh * 128:(hh + 1) * 128], lhsT=ident[:],
                                                                     rhs=ab[:, kt, hh * 128:(hh + 1) * 128], start=True, stop=False),
                                     [r_ident, r_ab], [r_ps], sig=False, partial=(hh > 0))
                                P.op("pe", lambda e, hh=hh: e.matmul(
                                    ps[:, hh * 128:(hh + 1) * 128], lhsT=kv[hh][:, r_, ku * 128:(ku + 1) * 128],
                                    rhs=qv[hh][:, r_, u * 128:(u + 1) * 128], start=False, stop=True),
                                    [r_k, r_q], [r_ps], sig=(hh == 1), partial=True)
                            rr["pt"] = (rr.get("pt", 0) + 1) % 8
                            pT, r_pT = pTs[rr["pt"]]
                            P.op("act", lambda e: e.activation(out=pT[:], in_=ps[:, 0:256], func=AF.Exp, scale=0.125),
                                 [r_ps], [r_pT], partial=False)
                            outl.append((r_ * ntl + ku, pT, r_pT))
                        return outl

                    def stageB(r_, u, pl):
                        rr["po"] = (rr.get("po", 0) + 1) % 2
                        po, r_po = psf[4 + rr["po"]]
                        n = len(pl) * 2
                        for part in range(2):
                            i = 0
                            for (ti, pT, r_pT) in pl:
                                for hh in range(2):
                                    lt = vt[:, ti, hh, :] if part == 0 else onesel[:, hh, :]
                                    P.op("pe", lambda e, hh=hh, lt=lt, i=i: e.matmul(
                                        po[:, part * 128:(part + 1) * 128], lhsT=lt, rhs=pT[:, hh * 128:(hh + 1) * 128],
                                        start=(i == 0), stop=(i == n - 1)),
                                        [r_v, r_onesel, r_pT], [r_po], sig=(part == 1 and i == n - 1), partial=not (part == 0 and i == 0))
                                    i += 1
                        av = accv[:, :, r_, u * 128:(u + 1) * 128]
                        pv = po[:, 0:256].rearrange("p (c q) -> p c q", c=2)
                        if g == 0:
                            P.op("dve", lambda e: e.tensor_copy(out=av, in_=pv), [r_po], [r_O], partial=True)
                        else:
                            P.op("dve", lambda e: e.tensor_add(out=av, in0=pv, in1=av), [r_po, r_O], [r_O], partial=True)

                    amode = dbg.get("att_mode", 3) if dbg else 3
                    prev = None
                    for (r_, u) in tiles:
                        cur = (r_, u, stageA(r_, u))
                        if prev is not None and amode >= 2:
                            stageB(*prev)
                        prev = cur
                    if amode >= 2:
                        stageB(*prev)
                P.dma("sp", gs[:], proj_d[36 + j, :, :], [R_proj[36 + j]], [r_gs], partial=False)
                for tb in range(8):
                    sl = slice(tb * 512, (tb + 1) * 512)
                    rz, r_rz = rzs[tb % 2]
                    tm, r_tm = tms[tb % 2]
                    P.op("dve", lambda e: e.reciprocal(out=rz[:], in_=Oacc[:, 1, sl]), [r_O], [r_rz], partial=False)
                    P.op("dve", lambda e: e.tensor_mul(out=tm[:], in0=Oacc[:, 0, sl], in1=rz[:]), [r_O, r_rz], [r_tm], partial=False)
                    P.op("pool", lambda e: e.tensor_mul(out=yb[:, sl], in0=tm[:], in1=gs[:, sl]), [r_tm, r_gs], [r_yb], partial=(tb > 0))
                P.dma("sp", yattn_d[j, :, :], yb[:], [r_yb], [R_yattn[j]], partial=False, sem_of=r_yb)
            P.barrier()
        if stop_after == "att":
            break
        with ExitStack() as ph:
            pb, r_pb = sb("pb", [128, S], BF16, ph)
            ufs = [sb(f"uf{i}", [128, 2048], F32, ph) for i in range(2)]
            ub, r_ub = sb("ub", [128, S], BF16, ph)
            zt, r_zt = sb("zt", [128, S], BF16, ph)
            xg, r_xg = sb("xg", [128, S], BF16, ph)
            CEf, r_CE = sb("CEf", [128, 8192], BF16, ph)
            Xb, r_X = sb("Xb", [128, 2, 32, 128], BF16, ph)
            Tt, r_T = sb("Tt", [128, 32, 2, 128], BF16, ph)
            Hb = [sb(f"Hb{i}", [128, 2, 8, 128], BF16, ph) for i in range(2)]
            tp = [sb(f"tp{i}", [128, 8, 128], F32, ph) for i in range(4)]
            fb1, r_fb1 = sb("fb1", [128, 2, 128], F32, ph)
            fb4, r_fb4 = sb("fb4", [128, 2, 32, 4, 4], F32, ph)
            e1, r_e1 = sb("e1", [128, 32, 4, 4], F32, ph)
            e2, r_e2 = sb("e2", [128, 32, 4, 4], F32, ph)
            gsT, r_gsT = sb("gsT", [128, S], BF16, ph)
            yT, r_yT = sb("yT", [128, S], BF16, ph)
            P.dma("sp", Tt[:], C["T"][:, :, :, :], [R_in], [r_T], partial=False)

            def conv3(chunk, widx, dst, r_dst, fftl):
                P.dma("sp", pb[:], proj_d[chunk, :, :], [R_proj[chunk]], [r_pb], partial=False)
                w0 = convw[:, l, 0, widx:widx + 1]
                w1 = convw[:, l, 1, widx:widx + 1]
                w2 = convw[:, l, 2, widx:widx + 1]
                cb = convb[:, l, widx:widx + 1]
                for hb_ in range(2):
                    uf, r_uf = ufs[hb_]
                    t0 = hb_ * 2048
                    P.op("act", lambda e: e.activation(out=uf[:, :], in_=pb[:, t0:t0 + 2048], func=AF.Identity, scale=w1, bias=cb),
                         [r_pb, r_convw, r_convb], [r_uf], partial=False)
                    a = 1 if hb_ == 0 else 0
                    P.op("dve", lambda e: e.scalar_tensor_tensor(out=uf[:, a:2048], in0=pb[:, t0 + a - 1:t0 + 2047], scalar=w0,
                                                                 in1=uf[:, a:2048], op0=ALU.mult, op1=ALU.add),
                         [r_pb, r_convw, r_uf], [r_uf], partial=False)
                    b_ = 2047 if hb_ == 1 else 2048
                    P.op("dve", lambda e: e.scalar_tensor_tensor(out=ub[:, t0:t0 + b_], in0=pb[:, t0 + 1:t0 + b_ + 1], scalar=w2,
                                                                 in1=uf[:, 0:b_], op0=ALU.mult, op1=ALU.add),
                         [r_pb, r_convw, r_uf], [r_ub], partial=(hb_ > 0))
                    if hb_ == 1:
                        P.op("dve", lambda e: e.tensor_copy(out=ub[:, S - 1:S], in_=uf[:, 2047:2048]), [r_uf], [r_ub], partial=True)
                to_token_major(ub, r_ub, dst, r_dst, fftl)

            def pointwise(o, c):
                for blk in range(4):
                    hb, r_hb = Hb[blk % 2]
                    P.dma("sp", hb[:], H_d[l, o, c, :, :, blk * 8:(blk + 1) * 8, :], [R_H[l][o][c]], [r_hb], partial=False)
                    xr, xi = Xb[:, 0, blk * 8:(blk + 1) * 8, :], Xb[:, 1, blk * 8:(blk + 1) * 8, :]
                    hr, hi = hb[:, 0, :, :], hb[:, 1, :, :]
                    (t1, r1), (t2, r2), (t3, r3), (t4, r4) = tp
                    P.op("dve", lambda e: e.tensor_mul(out=t1[:], in0=xr, in1=hr), [r_X, r_hb], [r1], partial=False)
                    P.op("dve", lambda e: e.tensor_mul(out=t2[:], in0=xi, in1=hi), [r_X, r_hb], [r2], partial=False)
                    P.op("dve", lambda e: e.tensor_mul(out=t3[:], in0=xr, in1=hi), [r_X, r_hb], [r3], partial=False)
                    P.op("dve", lambda e: e.tensor_mul(out=t4[:], in0=xi, in1=hr), [r_X, r_hb], [r4], partial=False)
                    P.op("dve", lambda e: e.tensor_sub(out=xr, in0=t1[:], in1=t2[:]), [r1, r2], [r_X], partial=True)
                    P.op("dve", lambda e: e.tensor_add(out=xi, in0=t3[:], in1=t4[:]), [r3, r4], [r_X], partial=True)

            clist = range(4) if not (dbg and "hy_c" in dbg) else dbg["hy_c"]
            for c in clist:
                for o in range(2):
                    P.dma("sp", fb1[:, o, :], bc(I["f_bias_r"], (l * 2 + o) * 512 + c * 128, 128), [R_in], [r_fb1], partial=(o > 0))
                for o in range(2):
                    for i4 in range(4):
                        P.op("dve", lambda e: e.tensor_copy(out=fb4[:, o, :, i4, :], in_=fb1[:, o, :].rearrange("p (g c) -> p g c", c=4)),
                             [r_fb1], [r_fb4], partial=not (o == 0 and i4 == 0))
                conv3(40 + c, c, zt, r_zt, True)
                conv3(44 + c, 4 + c, xg, r_xg, False)
                hmode = dbg.get("hy_mode", 9) if dbg else 9
                for o in range(2):
                    if hmode < 2:
                        break
                    fft_fwd(zt, r_zt, None, None, CEf, r_CE, Xb, r_X, tp)
                    if hmode < 3:
                        break
                    pointwise(o, c)
                    if hmode < 4:
                        break

                    def epi(nb, pyv, r_py, o=o):
                        zs = zt[:, :].rearrange("p (g n c) -> p g n c", g=32, n=32, c=4)[:, :, nb * 4:(nb + 1) * 4, :]
                        xs = xg[:, :].rearrange("p (n g c) -> p g n c", n=32, g=32, c=4)[:, :, nb * 4:(nb + 1) * 4, :]
                        pyf = pyv.rearrange("p n (g c) -> p g n c", c=4)
                        P.op("dve", lambda e: e.tensor_mul(out=e1[:], in0=zs, in1=fb4[:, o, :, :, :]), [r_zt, r_fb4], [r_e1], partial=False)
                        P.op("dve", lambda e: e.tensor_add(out=e2[:], in0=pyf, in1=e1[:]), [r_py, r_e1], [r_e2], partial=False)
                        if o == 0:
                            P.op("dve", lambda e: e.tensor_mul(out=zs, in0=e2[:], in1=xs), [r_e2, r_xg], [r_zt], partial=True)
                        else:
                            P.op("dve", lambda e: e.tensor_mul(out=xs, in0=e2[:], in1=xs), [r_e2, r_xg], [r_xg], partial=True)

                    fft_inv(Xb, r_X, CEf, r_CE, Tt, r_T, epi)
                    if o == 0:
                        conv3(48 + c, 8 + c, xg, r_xg, False)
                P.dma("sp", gsT[:], proj_d[52 + c, :, :], [R_proj[52 + c]], [r_gsT], partial=False)
                yv = yT[:, :].rearrange("p (a b) -> p b a", b=32)
                gv = gsT[:, :].rearrange("p (a b) -> p b a", b=32)
                for nb in range(4):
                    pt, r_pt = next_psb()
                    for ni in range(8):
                        n1 = nb * 8 + ni
                        P.op("pe", lambda e: e.transpose(out=pt[:, ni * 128:(ni + 1) * 128], in_=xg[:, n1 * 128:(n1 + 1) * 128], identity=ident[:]),
                             [r_xg, r_ident], [r_pt], sig=(ni == 7), partial=(ni > 0))
                    P.op("dve", lambda e: e.tensor_mul(out=yv[:, nb * 8:(nb + 1) * 8, :], in0=pt[:, :].rearrange("p (n c) -> p n c", n=8),
                                                       in1=gv[:, nb * 8:(nb + 1) * 8, :]), [r_pt, r_gsT], [r_yT], partial=(nb > 0))
                P.dma("sp", yhy_d[c, :, :], yT[:], [r_yT], [R_yhy[c]], partial=False, sem_of=r_yT)
            P.barrier()
        if stop_after == "hy":
            break
        with ExitStack() as ph:
            wpa, r_wpa = sb("wpa", [128, 4, D], BF16, ph)
            wph, r_wph = sb("wph", [128, 4, D], BF16, ph)
            wo, r_wo = sb("wo", [128, 8, D], BF16, ph)
            rowb, r_rowb = sb("rowb", [128, 3, D], F32, ph)
            with ExitStack() as ph2:
                wst, r_wst = sb("wstg", [128, 8, D], F32, ph2)
                P.dma("sp", wst[:, 0:4, :], I["w_pa"][l], [R_in], [r_wst], partial=False)
                P.op("pool", lambda e: e.tensor_copy(out=wpa[:], in_=wst[:, 0:4, :]), [r_wst], [r_wpa], partial=False)
                P.dma("sp", wst[:, 4:8, :], I["w_ph"][l], [R_in], [r_wst], partial=False)
                P.op("pool", lambda e: e.tensor_copy(out=wph[:], in_=wst[:, 4:8, :]), [r_wst], [r_wph], partial=False)
                P.dma("sp", wst[:, :, :], I["w_out"][l], [R_in], [r_wst], partial=False)
                for kc in range(8):
                    P.op("pool" if kc % 2 else "dve", lambda e: e.tensor_mul(out=wo[:, kc, :], in0=wst[:, kc, :], in1=gate_b[:, l, :]),
                         [r_wst, r_gate], [r_wo], partial=(kc > 0))
                P.barrier()
            P.dma("sp", rowb[:, 0, :], bc(I["b_out_r"], l * D, D), [R_in], [r_rowb], partial=False)
            P.dma("sp", rowb[:, 1, :], bc(I["ln_g_r"], l * D, D), [R_in], [r_rowb])
            P.dma("sp", rowb[:, 2, :], bc(I["ln_b_r"], l * D, D), [R_in], [r_rowb])
            gb, r_gb = sb("gb", [128, D], F32, ph)
            P.op("dve", lambda e: e.tensor_mul(out=gb[:], in0=rowb[:, 0, :], in1=gate_b[:, l, :]), [r_rowb, r_gate], [r_gb], partial=False)
            nb_, r_nb = sb("nbias", [128, 1], F32, ph)
            ya = [sb(f"ya{i}", [128, 4, 512], BF16, ph) for i in range(2)]
            yh = [sb(f"yh{i}", [128, 4, 512], BF16, ph) for i in range(2)]
            ga = [sb(f"ga{i}", [128, 16, 512], BF16, ph) for i in range(2)]
            mT, r_mT = sb("mT", [128, 8, 512], BF16, ph)
            m1s = [sb(f"m1_{i}", [128, 512], F32, ph) for i in range(2)]
            m2s = [sb(f"m2_{i}", [128, 512], F32, ph) for i in range(2)]
            xr_ = [sb(f"xr{i}", [128, D], F32, ph) for i in range(4)]
            rs_ = [sb(f"rs{i}", [128, D], F32, ph) for i in range(4)]
            sts = [sb(f"mst{i}", [128, 12], F32, ph) for i in range(4)]
            mvs = [sb(f"mmv{i}", [128, 2], F32, ph) for i in range(4)]
            nbs = [sb(f"nbias{i}", [128, 1], F32, ph) for i in range(4)]
            o1, r_o1 = sb("o1", [128, 512], F32, ph)
            stt, r_st = sb("mst", [128, 12], F32, ph)
            mvv, r_mv = sb("mmv", [128, 2], F32, ph)
            xo = [sb(f"xo{i}", [128, D], F32, ph) for i in range(4)]
            tbl = range(8) if not (dbg and "merge_tb" in dbg) else dbg["merge_tb"]
            nt = 0
            for tb in tbl:
                sl = slice(tb * 512, (tb + 1) * 512)
                yat, r_ya = ya[tb % 2]
                yht, r_yh = yh[tb % 2]
                gat, r_ga = ga[tb % 2]
                P.dma("sp", yat[:], yattn_d[:, :, sl].rearrange("c p t -> p c t"), R_yattn, [r_ya], partial=False)
                P.dma("sp", yht[:], yhy_d[:, :, sl].rearrange("c p t -> p c t"), R_yhy, [r_yh], partial=False)
                P.dma("sp", gat[:], proj_d[56:72, :, sl].rearrange("c p t -> p c t"), R_proj[56:72], [r_ga], partial=False)
                for fc in range(8):
                    m1, r_m1 = m1s[fc % 2]
                    m2, r_m2 = m2s[fc % 2]
                    pa, r_pa = next_psf()
                    pq, r_pq = next_psf()
                    for kc in range(4):
                        P.op("pe", lambda e: e.matmul(pa[:, :], lhsT=wpa[:, kc, fc * 128:(fc + 1) * 128], rhs=yat[:, kc, :],
                                                      start=(kc == 0), stop=(kc == 3)), [r_wpa, r_ya], [r_pa], sig=(kc == 3), partial=(kc > 0))
                    for kc in range(4):
                        P.op("pe", lambda e: e.matmul(pq[:, :], lhsT=wph[:, kc, fc * 128:(fc + 1) * 128], rhs=yht[:, kc, :],
                                                      start=(kc == 0), stop=(kc == 3)), [r_wph, r_yh], [r_pq], sig=(kc == 3), partial=(kc > 0))
                    P.op("dve", lambda e: e.tensor_mul(out=m1[:], in0=pa[:, :], in1=gat[:, fc, :]), [r_pa, r_ga], [r_m1], partial=False)
                    P.op("dve", lambda e: e.tensor_mul(out=m2[:], in0=pq[:, :], in1=gat[:, 8 + fc, :]), [r_pq, r_ga], [r_m2], partial=False)
                    P.op("pool", lambda e: e.tensor_add(out=mT[:, fc, :], in0=m1[:], in1=m2[:]), [r_m1, r_m2], [r_mT], partial=(fc > 0))
                tiles = []
                for tt in range(4):
                    row0 = tb * 512 + tt * 128
                    xrt, r_xr = xr_[tt]
                    rst, r_rs = rs_[tt]
                    xot, r_xo = xo[tt]
                    stt, r_st = sts[tt]
                    mvv, r_mv = mvs[tt]
                    nb_, r_nb = nbs[tt]
                    tiles.append((row0, xrt, r_xr, rst, r_rs, xot, r_xo, stt, r_st, mvv, r_mv, nb_, r_nb))
                    P.dma("sp", xrt[:], x_src[row0:row0 + 128, :], [R_xsrc], [r_xr], partial=False)
                for tt, (row0, xrt, r_xr, rst, r_rs, xot, r_xo, stt, r_st, mvv, r_mv, nb_, r_nb) in enumerate(tiles):
                    P.op("dve", lambda e: e.scalar_tensor_tensor(out=xrt[:], in0=xrt[:], scalar=ALPHA, in1=gb[:], op0=ALU.mult, op1=ALU.add),
                         [r_xr, r_gb], [r_xr], partial=False)
                    for hf_ in range(2):
                        hs = slice(hf_ * 512, (hf_ + 1) * 512)
                        po_, r_po = next_psf()
                        for kc in range(8):
                            P.op("pe", lambda e: e.matmul(po_[:, :], lhsT=mT[:, kc, tt * 128:(tt + 1) * 128], rhs=wo[:, kc, hs],
                                                          start=(kc == 0), stop=(kc == 7)), [r_mT, r_wo], [r_po], sig=(kc == 7), partial=(kc > 0))
                        P.op("dve", lambda e: e.tensor_add(out=rst[:, hs], in0=po_[:, :], in1=xrt[:, hs]), [r_po, r_xr], [r_rs], partial=(hf_ > 0))
                    P.op("dve", lambda e: e.bn_stats(out=stt[:, 0:6], in_=rst[:, 0:512]), [r_rs], [r_st], partial=False)
                    P.op("dve", lambda e: e.bn_stats(out=stt[:, 6:12], in_=rst[:, 512:1024]), [r_rs], [r_st], partial=True)
                    P.op("dve", lambda e: e.bn_aggr(out=mvv[:], in_=stt[:]), [r_st], [r_mv], partial=False)
                    P.op("act", lambda e: e.activation(out=mvv[:, 1:2], in_=mvv[:, 1:2], func=AF.Sqrt, bias=epsT[:], scale=1.0),
                         [r_mv, r_eps], [r_mv], partial=False)
                for (row0, xrt, r_xr, rst, r_rs, xot, r_xo, stt, r_st, mvv, r_mv, nb_, r_nb) in tiles:
                    P.op("dve", lambda e: e.reciprocal(out=mvv[:, 1:2], in_=mvv[:, 1:2]), [r_mv], [r_mv], partial=False)
                    P.op("dve", lambda e: e.tensor_scalar(out=nb_[:], in0=mvv[:, 0:1], scalar1=-1.0, scalar2=mvv[:, 1:2],
                                                          op0=ALU.mult, op1=ALU.mult), [r_mv], [r_nb], partial=False)
                    P.op("act", lambda e: e.activation(out=rst[:], in_=rst[:], func=AF.Identity, scale=mvv[:, 1:2], bias=nb_[:]),
                         [r_rs, r_mv, r_nb], [r_rs], partial=False)
                for (row0, xrt, r_xr, rst, r_rs, xot, r_xo, stt, r_st, mvv, r_mv, nb_, r_nb) in tiles:
                    P.op("dve", lambda e: e.tensor_mul(out=rst[:], in0=rst[:], in1=rowb[:, 1, :]), [r_rs, r_rowb], [r_rs], partial=False)
                    P.op("pool", lambda e: e.tensor_add(out=xot[:], in0=rst[:], in1=rowb[:, 2, :]), [r_rs, r_rowb], [r_xo], partial=False)
                    P.dma("sp", x_dst[row0:row0 + 128, :], xot[:], [r_xo], [R_xdst], sem_of=r_xo)
            P.barrier()
        x_src, R_xsrc = x_dst, R_xdst
    P.barrier()
    stack.close()
    return nc, dbg_out


_CACHE = {}


def kernel(**inputs):
    inp = {k: np.asarray(v, dtype=np.float32) for k, v in inputs.items()}
    consts = _const_tables()
    if "nc" not in _CACHE:
        _CACHE["nc"] = build()[0]
    nc = _CACHE["nc"]
    in_maps = []
    for b in range(8):
        m = _layout_inputs(inp, b)
        for k, v in consts.items():
            m["c_" + k] = v
        in_maps.append(m)
    res = run_bass_kernel_spmd(nc, in_maps, core_ids=list(range(8)))
    return np.stack([np.asarray(r["out"], dtype=np.float32) for r in res.results], axis=0)
```
